# Optimizing a Trainium2 kernel written in Bass

```python
import math
import jax
import jax.numpy as jnp
from jax import lax

D_MODEL = 1024
BATCH = 2
SEQ = 8192
DEPTH = 1

D_MIX = D_MODEL
HEAD_DIM = 64
N_Q_HEADS = 8
N_KV_HEADS = 2
Q_PER_KV = N_Q_HEADS // N_KV_HEADS
ATTN_WIDTH = N_Q_HEADS * HEAD_DIM
KV_WIDTH = N_KV_HEADS * HEAD_DIM
WINDOW = 128
ATTN_BLOCK = 128
ROPE_THETA = 10000.0
S5_WIDTH = D_MIX - ATTN_WIDTH
S5_GROUP_CH = 16
S5_GROUPS = S5_WIDTH // S5_GROUP_CH
S5_STATE = 64
IN_WIDTH = ATTN_WIDTH + 2 * KV_WIDTH + S5_WIDTH
N_MEM = 256
X_HEADS = 4
X_HEAD_DIM = D_MODEL // X_HEADS
N_EXPERTS = 32
TOP_K = 4
D_EXPERT = D_MODEL
SWIGLU_LIMIT = 7.0
SWIGLU_ALPHA = 1.702
MOE_BLOCK = 512
LN_EPS = 1e-5
DEEPNORM_ALPHA = (2.0 * DEPTH) ** 0.25
DEEPNORM_BETA = (8.0 * DEPTH) ** -0.25

kernel_name = 'hybrid_swa_s5_moe_deepnorm'

F32 = jnp.float32


def layer_norm(x, g, b):
    xf = x.astype(F32)
    mu = jnp.mean(xf, -1, keepdims=True)
    var = jnp.mean(jnp.square(xf - mu), -1, keepdims=True)
    return ((xf - mu) * lax.rsqrt(var + LN_EPS) * g.astype(F32) + b.astype(F32)).astype(x.dtype)


def rope_tables(seq_len):
    half = HEAD_DIM // 2
    inv_freq = ROPE_THETA ** (-jnp.arange(half, dtype=F32) * (2.0 / HEAD_DIM))
    ang = jnp.arange(seq_len, dtype=F32)[:, None] * inv_freq[None, :]
    return jnp.cos(ang), jnp.sin(ang)


def apply_rope(x, cos, sin):
    half = HEAD_DIM // 2
    xf = x.astype(F32)
    x1, x2 = xf[..., :half], xf[..., half:]
    c = cos[None, :, None, :]
    s = sin[None, :, None, :]
    return jnp.concatenate([x1 * c - x2 * s, x2 * c + x1 * s], -1).astype(x.dtype)


def sliding_window_attention(q, k, v, sinks):
    B, L = q.shape[:2]
    nb = L // ATTN_BLOCK
    qb = q.reshape(B, nb, ATTN_BLOCK, N_KV_HEADS, Q_PER_KV, HEAD_DIM)

    def band(t):
        pad = jnp.zeros((B, ATTN_BLOCK, N_KV_HEADS, HEAD_DIM), t.dtype)
        tb = jnp.concatenate([pad, t], 1).reshape(B, nb + 1, ATTN_BLOCK, N_KV_HEADS, HEAD_DIM)
        return jnp.concatenate([tb[:, :-1], tb[:, 1:]], 2)

    kb, vb = band(k), band(v)
    s = jnp.einsum('bnqkgd,bnskd->bnkgqs', qb, kb).astype(F32) * (HEAD_DIM ** -0.5)
    qi = jnp.arange(ATTN_BLOCK)[:, None]
    si = jnp.arange(2 * ATTN_BLOCK)[None, :]
    rel = qi + ATTN_BLOCK - si
    in_window = (rel >= 0) & (rel < WINDOW)
    key_pos = (jnp.arange(nb) - 1)[:, None] * ATTN_BLOCK + si
    valid = in_window[None] & (key_pos >= 0)[:, None, :]
    s = jnp.where(valid[None, :, None, None], s, -jnp.inf)
    sink = sinks.astype(F32).reshape(N_KV_HEADS, Q_PER_KV)[None, None, :, :, None, None]
    sink = jnp.broadcast_to(sink, s.shape[:-1] + (1,))
    p = jax.nn.softmax(jnp.concatenate([s, sink], -1), axis=-1)[..., :-1]
    o = jnp.einsum('bnkgqs,bnskd->bnqkgd', p.astype(v.dtype), vb)
    return o.reshape(B, L, ATTN_WIDTH)


def s5_mixer(u, lam_re, lam_im, log_dt, b_re, b_im, c_re, c_im, d_skip, w_glu, b_glu):
    B, L, _ = u.shape
    ug = u.reshape(B, L, S5_GROUPS, S5_GROUP_CH).astype(F32)
    lr = jnp.minimum(lam_re.astype(F32), -1e-4)
    li = lam_im.astype(F32)
    dt = jnp.exp(log_dt.astype(F32))[:, None]
    mag = jnp.exp(lr * dt)
    ar = mag * jnp.cos(li * dt)
    ai = mag * jnp.sin(li * dt)
    den = lr * lr + li * li
    zr = ((ar - 1.0) * lr + ai * li) / den
    zi = (ai * lr - (ar - 1.0) * li) / den
    br = b_re.astype(F32)
    bi = b_im.astype(F32)
    bbr = zr[..., None] * br - zi[..., None] * bi
    bbi = zr[..., None] * bi + zi[..., None] * br
    bu_r = jnp.einsum('blgh,gph->blgp', ug, bbr)
    bu_i = jnp.einsum('blgh,gph->blgp', ug, bbi)
    a_r = jnp.broadcast_to(ar, bu_r.shape)
    a_i = jnp.broadcast_to(ai, bu_i.shape)

    def combine(e1, e2):
        a1r, a1i, s1r, s1i = e1
        a2r, a2i, s2r, s2i = e2
        return (a2r * a1r - a2i * a1i,
                a2r * a1i + a2i * a1r,
                a2r * s1r - a2i * s1i + s2r,
                a2r * s1i + a2i * s1r + s2i)

    _, _, st_r, st_i = lax.associative_scan(combine, (a_r, a_i, bu_r, bu_i), axis=1)
    y = (jnp.einsum('blgp,ghp->blgh', st_r, c_re.astype(F32))
         - jnp.einsum('blgp,ghp->blgh', st_i, c_im.astype(F32)))
    y = y.reshape(B, L, S5_WIDTH) + d_skip.astype(F32) * u.astype(F32)
    y = jax.nn.gelu(y).astype(u.dtype)
    return y * jax.nn.sigmoid(y @ w_glu + b_glu)


def memory_cross_attention(x, mem, w_q, w_kv, w_o):
    B, L, _ = x.shape
    q = (x @ w_q).reshape(B, L, X_HEADS, X_HEAD_DIM)
    k, v = jnp.split(mem @ w_kv, 2, axis=-1)
    k = k.reshape(B, N_MEM, X_HEADS, X_HEAD_DIM)
    v = v.reshape(B, N_MEM, X_HEADS, X_HEAD_DIM)
    s = jnp.einsum('blhd,bmhd->bhlm', q, k).astype(F32) * (X_HEAD_DIM ** -0.5)
    p = jax.nn.softmax(s, axis=-1).astype(v.dtype)
    o = jnp.einsum('bhlm,bmhd->blhd', p, v).reshape(B, L, D_MODEL)
    return o @ w_o


def clamped_swiglu(h):
    g = jnp.minimum(h[..., ::2], SWIGLU_LIMIT)
    lin = jnp.clip(h[..., 1::2], -SWIGLU_LIMIT, SWIGLU_LIMIT)
    return g * jax.nn.sigmoid(SWIGLU_ALPHA * g) * (lin + 1.0)


def moe_ffn(x2d, w_router, b_router, w1, b1, w2, b2):
    N, D = x2d.shape
    logits = (x2d @ w_router + b_router).astype(F32)
    top_val, top_idx = lax.top_k(logits, TOP_K)
    gate = jax.nn.softmax(top_val, axis=-1).astype(x2d.dtype)
    M = N * TOP_K
    flat_e = top_idx.reshape(M)
    flat_tok = jnp.arange(M, dtype=jnp.int32) // TOP_K
    order = jnp.argsort(flat_e)
    sorted_e = flat_e[order]
    sizes = jnp.bincount(flat_e, length=N_EXPERTS)
    starts = jnp.cumsum(sizes) - sizes
    padded = (sizes + MOE_BLOCK - 1) // MOE_BLOCK * MOE_BLOCK
    pends = jnp.cumsum(padded)
    pstarts = pends - padded
    dest = pstarts[sorted_e] + (jnp.arange(M) - starts[sorted_e])
    n_blocks = -(-M // MOE_BLOCK) + N_EXPERTS
    buf = jnp.zeros((n_blocks * MOE_BLOCK, D), x2d.dtype).at[dest].set(x2d[flat_tok[order]])
    block_e = jnp.clip(jnp.searchsorted(pends, jnp.arange(n_blocks) * MOE_BLOCK, side='right'),
                       0, N_EXPERTS - 1)

    def expert_block(args):
        xb, e = args
        h = xb @ w1[e] + b1[e]
        return clamped_swiglu(h) @ w2[e] + b2[e]

    out_buf = lax.map(expert_block, (buf.reshape(n_blocks, MOE_BLOCK, D), block_e))
    y_sorted = out_buf.reshape(n_blocks * MOE_BLOCK, D)[dest]
    y_assign = jnp.zeros((M, D), x2d.dtype).at[order].set(y_sorted)
    return jnp.einsum('nkd,nk->nd', y_assign.reshape(N, TOP_K, D), gate)


def setup_inputs(seed: int = 0) -> dict:
    keys = iter(jax.random.split(jax.random.key(seed), 48))

    def nrm(shape, scale):
        return scale * jax.random.normal(next(keys), shape, F32)

    Ld = DEPTH
    n_idx = jnp.arange(S5_STATE, dtype=F32)
    return {
        'x': nrm((BATCH, SEQ, D_MODEL), 1.0),
        'mem': nrm((BATCH, N_MEM, D_MODEL), 1.0),
        'w_in': nrm((Ld, D_MODEL, IN_WIDTH), D_MODEL ** -0.5),
        'b_in': nrm((Ld, IN_WIDTH), 0.02),
        'attn_sinks': nrm((Ld, N_Q_HEADS), 0.5),
        's5_lambda_re': -0.5 + nrm((Ld, S5_GROUPS, S5_STATE), 0.01),
        's5_lambda_im': math.pi * n_idx + nrm((Ld, S5_GROUPS, S5_STATE), 0.01),
        's5_log_dt': jax.random.uniform(next(keys), (Ld, S5_GROUPS), F32,
                                        math.log(0.001), math.log(0.1)),
        's5_b_re': nrm((Ld, S5_GROUPS, S5_STATE, S5_GROUP_CH), (2.0 * S5_GROUP_CH) ** -0.5),
        's5_b_im': nrm((Ld, S5_GROUPS, S5_STATE, S5_GROUP_CH), (2.0 * S5_GROUP_CH) ** -0.5),
        's5_c_re': nrm((Ld, S5_GROUPS, S5_GROUP_CH, S5_STATE), S5_STATE ** -0.5),
        's5_c_im': nrm((Ld, S5_GROUPS, S5_GROUP_CH, S5_STATE), S5_STATE ** -0.5),
        's5_d': nrm((Ld, S5_WIDTH), 1.0),
        's5_w_glu': nrm((Ld, S5_WIDTH, S5_WIDTH), S5_WIDTH ** -0.5),
        's5_b_glu': nrm((Ld, S5_WIDTH), 0.02),
        'w_out': nrm((Ld, D_MIX, D_MODEL), D_MIX ** -0.5 * DEEPNORM_BETA),
        'b_out': nrm((Ld, D_MODEL), 0.02),
        'ln1_g': 1.0 + nrm((Ld, D_MODEL), 0.02),
        'ln1_b': nrm((Ld, D_MODEL), 0.02),
        'w_xq': nrm((Ld, D_MODEL, D_MODEL), D_MODEL ** -0.5),
        'w_xkv': nrm((Ld, D_MODEL, 2 * D_MODEL), D_MODEL ** -0.5),
        'w_xo': nrm((Ld, D_MODEL, D_MODEL), D_MODEL ** -0.5 * DEEPNORM_BETA),
        'ln2_g': 1.0 + nrm((Ld, D_MODEL), 0.02),
        'ln2_b': nrm((Ld, D_MODEL), 0.02),
        'w_router': nrm((Ld, D_MODEL, N_EXPERTS), D_MODEL ** -0.5),
        'b_router': nrm((Ld, N_EXPERTS), 0.01),
        'w_e1': nrm((Ld, N_EXPERTS, D_MODEL, 2 * D_EXPERT), D_MODEL ** -0.5),
        'b_e1': nrm((Ld, N_EXPERTS, 2 * D_EXPERT), 0.02),
        'w_e2': nrm((Ld, N_EXPERTS, D_EXPERT, D_MODEL), D_EXPERT ** -0.5 * DEEPNORM_BETA),
        'b_e2': nrm((Ld, N_EXPERTS, D_MODEL), 0.02),
        'ln3_g': 1.0 + nrm((Ld, D_MODEL), 0.02),
        'ln3_b': nrm((Ld, D_MODEL), 0.02),
    }


def reference(x, mem, w_in, b_in, attn_sinks, s5_lambda_re, s5_lambda_im, s5_log_dt,
              s5_b_re, s5_b_im, s5_c_re, s5_c_im, s5_d, s5_w_glu, s5_b_glu, w_out, b_out,
              ln1_g, ln1_b, w_xq, w_xkv, w_xo, ln2_g, ln2_b, w_router, b_router,
              w_e1, b_e1, w_e2, b_e2, ln3_g, ln3_b):
    B, L, D = x.shape
    cos, sin = rope_tables(L)
    for l in range(DEPTH):
        h = x @ w_in[l] + b_in[l]
        q, k, v, u = jnp.split(h, [ATTN_WIDTH, ATTN_WIDTH + KV_WIDTH, ATTN_WIDTH + 2 * KV_WIDTH], axis=-1)
        q = apply_rope(q.reshape(B, L, N_Q_HEADS, HEAD_DIM), cos, sin)
        k = apply_rope(k.reshape(B, L, N_KV_HEADS, HEAD_DIM), cos, sin)
        v = v.reshape(B, L, N_KV_HEADS, HEAD_DIM)
        attn = sliding_window_attention(q, k, v, attn_sinks[l])
        ssm = s5_mixer(u, s5_lambda_re[l], s5_lambda_im[l], s5_log_dt[l], s5_b_re[l], s5_b_im[l],
                       s5_c_re[l], s5_c_im[l], s5_d[l], s5_w_glu[l], s5_b_glu[l])
        mix = jnp.concatenate([attn, ssm], axis=-1) @ w_out[l] + b_out[l]
        x = layer_norm(DEEPNORM_ALPHA * x + mix, ln1_g[l], ln1_b[l])
        xa = memory_cross_attention(x, mem, w_xq[l], w_xkv[l], w_xo[l])
        x = layer_norm(DEEPNORM_ALPHA * x + xa, ln2_g[l], ln2_b[l])
        ff = moe_ffn(x.reshape(B * L, D), w_router[l], b_router[l], w_e1[l], b_e1[l],
                     w_e2[l], b_e2[l]).reshape(B, L, D)
        x = layer_norm(DEEPNORM_ALPHA * x + ff, ln3_g[l], ln3_b[l])
    return x
```

```python
from contextlib import ExitStack

import numpy as np
import concourse.bass as bass
import concourse.mybir as mybir
from concourse.bass_utils import run_bass_kernel_spmd

F32 = mybir.dt.float32
BF16 = mybir.dt.bfloat16
I32 = mybir.dt.int32
ALU = mybir.AluOpType
AF = mybir.ActivationFunctionType
AX = mybir.AxisListType

NCORES = 8
_DRAM = {}
TOK = 2048
NT = TOK // 128
HALO = 128
TH = TOK + HALO
D = 1024
KT = D // 128


class Buf:
    __slots__ = ("name", "lw", "rd")

    def __init__(self, name):
        self.name = name
        self.lw = None
        self.rd = {}


class Sched:
    ENG = ("pe", "act", "dve", "pool", "sp")

    def __init__(self, nc, es):
        self.nc = nc
        self.es = es
        self.sems = {e: es.enter_context(nc.semaphore("sem_" + e)) for e in self.ENG}
        self.cnt = {e: 0 for e in self.ENG}
        self.prog = {e: [] for e in self.ENG}
        self.seen = {e: {} for e in self.ENG}
        self.scnt = {}

    def _sem(self, key):
        if key not in self.sems:
            self.sems[key] = self.es.enter_context(self.nc.semaphore("sem_" + key))
            self.scnt[key] = 0
        return self.sems[key]

    def _waits(self, eng, reads, writes):
        deps = {}

        def add(tok):
            if tok is None:
                return
            key, val = tok
            if key == eng and eng == "pe":
                return
            if deps.get(key, 0) < val:
                deps[key] = val

        for b in reads:
            add(b.lw)
        for b in writes:
            add(b.lw)
            for t in b.rd.items():
                add(t)
        out = []
        for key, val in deps.items():
            if self.seen[eng].get(key, 0) >= val:
                continue
            self.seen[eng][key] = val
            out.append((self._sem(key), val))
        return out

    def _commit(self, tok, reads, writes):
        for b in writes:
            b.lw = tok
            b.rd = {}
        for b in reads:
            if b not in writes:
                if b.rd.get(tok[0], 0) < tok[1]:
                    b.rd[tok[0]] = tok[1]

    def op(self, eng, fns, reads=(), writes=()):
        if callable(fns):
            fns = [fns]
        waits = self._waits(eng, reads, writes)
        self.cnt[eng] += 1
        assert self.cnt[eng] < 60000, eng
        self.prog[eng].append((waits, fns, (self.sems[eng], 1)))
        self._commit((eng, self.cnt[eng]), reads, writes)

    def dma(self, eng, fn, stream=None, reads=(), writes=()):
        if stream is None:
            stream = "d_" + writes[0].name
        stream = stream + ("_sw" if eng == "pool" else "_hw")
        sem = self._sem(stream)
        waits = self._waits(eng, reads, writes)
        self.scnt[stream] += 16
        assert self.scnt[stream] < 60000, stream
        self.prog[eng].append((waits, [fn], (sem, 16)))
        self._commit((stream, self.scnt[stream]), reads, writes)

    def wait_all(self, eng, bufs):
        waits = self._waits(eng, bufs, ())
        self.prog[eng].append((waits, [], None))

    def barrier(self):
        toks = [(e, self.cnt[e]) for e in self.ENG if self.cnt[e]] + [(k, v) for k, v in self.scnt.items() if v]
        for eng in self.ENG:
            waits = []
            for key, val in toks:
                if key == eng or self.seen[eng].get(key, 0) >= val:
                    continue
                self.seen[eng][key] = val
                waits.append((self._sem(key), val))
            self.prog[eng].append((waits, [], None))

    def finish(self, eng="sp", prefix="out"):
        fins = []
        for key, val in self.scnt.items():
            if key.startswith(prefix) and val:
                b = Buf("fin_" + key)
                b.lw = (key, val)
                fins.append(b)
        self.wait_all(eng, fins)

    def emit(self):
        nc = self.nc
        with nc.Block() as block:
            table = (("pe", block.tensor), ("act", block.scalar), ("dve", block.vector),
                     ("pool", block.gpsimd), ("sp", block.sync))
            for eng, deco in table:
                def body(e, eng=eng):
                    for waits, fns, inc in self.prog[eng]:
                        for s, v in waits:
                            e.wait_ge(s, v)
                        for f in fns[:-1]:
                            f(e)
                        if fns:
                            fns[-1](e).then_inc(inc[0], inc[1])
                deco(body)
        self.prog = {e: [] for e in self.ENG}


def build_nc(stage="full"):
    nc = bass.Bass("TRN2", target_bir_lowering=False)
    es = ExitStack()
    S = Sched(nc, es)

    _DRAM.clear()

    def dram_in(name, shape, dt=F32):
        _DRAM[name] = nc.dram_tensor(name, list(shape), dt, kind="ExternalInput").ap()
        return _DRAM[name]

    def dram_out(name, shape, dt=F32):
        return nc.dram_tensor(name, list(shape), dt, kind="ExternalOutput").ap()

    scope = [es]

    def sb(name, shape, dt):
        return scope[-1].enter_context(nc.sbuf_tensor(name, list(shape), dt))

    def open_phase():
        ph = ExitStack()
        scope.append(ph)
        return ph

    def close_phase():
        S.barrier()
        S.emit()
        scope.pop().close()

    xloc = dram_in("xloc", [TOK, D])
    xhalo = dram_in("xhalo", [HALO, D])
    cos_d = dram_in("ropecos", [128, TH])
    sin_d = dram_in("ropesin", [128, TH])
    masks_d = dram_in("masks", [128, 3, 128])
    ident_d = dram_in("ident", [128, 128])
    w_in = dram_in("w_in", [D, 1280])
    b_in = dram_in("b_in", [1280])
    sinks_d = dram_in("attn_sinks", [8])

    PS = [es.enter_context(nc.psum_tensor(f"ps{i}", [128, 512], F32)) for i in range(6)]
    PS += [es.enter_context(nc.psum_tensor(f"ps{i}", [128, 1024], BF16)) for i in (6, 7)]
    PSB = [Buf(f"ps{i}") for i in range(8)]

    identb = sb("identb", [128, 128], BF16)
    identf = sb("identf", [128, 128], F32)
    catT = sb("catT", [128, 8, TOK], BF16)
    open_phase()
    cosT = sb("cosT", [128, TH], F32)
    sinT = sb("sinT", [128, TH], F32)
    maskb = sb("maskb", [128, 3, 128], BF16)
    B_ident, B_mask, B_cos, B_sin = Buf("ident"), Buf("mask"), Buf("cos"), Buf("sin")
    S.dma("pool", lambda e: e.dma_start(out=identb[:, :], in_=ident_d[:, :]), writes=[B_ident])
    B_identf = Buf("identf")
    S.dma("sp", lambda e: e.dma_start(out=identf[:, :], in_=ident_d[:, :]), writes=[B_identf])
    S.dma("pool", lambda e: e.dma_start(out=maskb[:, :, :], in_=masks_d[:, :, :]), writes=[B_mask])
    S.dma("sp", lambda e: e.dma_start(out=cosT[:, :], in_=cos_d[:, :]), writes=[B_cos])
    S.dma("sp", lambda e: e.dma_start(out=sinT[:, :], in_=sin_d[:, :]), writes=[B_sin])

    win = sb("win", [128, KT, 1280], BF16)
    B_win = Buf("win")
    w_in_v = w_in.rearrange("(kt p) f -> p kt f", p=128)
    for h in range(2):
        S.dma("pool", lambda e, h=h: e.dma_start(out=win[:, 4 * h:4 * h + 4, :], in_=w_in_v[:, 4 * h:4 * h + 4, :]),
              writes=[B_win])
    wkb = sb("wkb", [128, KT, 128], BF16)
    wrot = sb("wrot", [128, KT, 6, 128], BF16)
    B_wrot = Buf("wrot")

    def src_col(m, half):
        if m < 4:
            return 128 * m + 64 * half
        if m == 4:
            return 512 + 64 * half
        return 512 + 64 * (1 - half)

    for half in range(2):
        c0 = src_col(5, half)
        S.op("dve", lambda e, half=half, c0=c0: e.tensor_copy(out=wkb[:, :, 64 * half:64 * half + 64], in_=win[:, :, c0:c0 + 64]),
             reads=[B_win], writes=[B_wrot])
    for m in range(6):
        for half in range(2):
            c0 = src_col(m, half)
            d0 = 64 * half
            S.op("dve", lambda e, m=m, c0=c0, d0=d0: e.tensor_scalar(
                out=wrot[:, :, m, d0:d0 + 32], in0=win[:, :, c0 + 32:c0 + 64], scalar1=-1.0, scalar2=None, op0=ALU.mult),
                reads=[B_win], writes=[B_wrot])
            S.op("dve", lambda e, m=m, c0=c0, d0=d0: e.tensor_copy(out=wrot[:, :, m, d0 + 32:d0 + 64], in_=win[:, :, c0:c0 + 32]),
                 reads=[B_win], writes=[B_wrot])

    bfm = sb("bfm", [128, 10], F32)
    bqk = sb("bqk", [128, 6], F32)
    bqkr = sb("bqkr", [128, 6], F32)
    bvbc = sb("bvbc", [128, 128], F32)
    B_bias, B_bfm, B_bvbc = Buf("bias"), Buf("bfm"), Buf("bvbc")
    brow = sb("brow", [10, 128], F32)
    B_brow = Buf("brow")
    S.dma("sp", lambda e: e.dma_start(out=brow[:, :], in_=b_in.rearrange("(t p) -> t p", p=128)), writes=[B_brow])
    S.op("pe", lambda e: e.transpose(out=PS[5][:, 0:10], in_=brow[:, :], identity=identf[0:10, 0:10]),
         reads=[B_brow, B_identf], writes=[PSB[5]])
    S.op("dve", lambda e: e.tensor_copy(out=bfm[:, :], in_=PS[5][:, 0:10]), reads=[PSB[5]], writes=[B_bfm])
    S.dma("sp", lambda e: e.dma_start(out=bvbc[:, :], in_=b_in[640:768].partition_broadcast(128)), writes=[B_bvbc])
    S.op("dve", lambda e: e.tensor_copy(out=bqk[:, 0:5], in_=bfm[:, 0:5]), reads=[B_bfm], writes=[B_bias])
    S.op("dve", lambda e: e.tensor_copy(out=bqk[0:64, 5:6], in_=bfm[64:128, 4:5]), reads=[B_bfm, B_bias], writes=[B_bias])
    S.op("dve", lambda e: e.tensor_copy(out=bqk[64:128, 5:6], in_=bfm[0:64, 4:5]), reads=[B_bfm, B_bias], writes=[B_bias])
    for hb in (0, 64):
        S.op("dve", lambda e, hb=hb: e.tensor_scalar(out=bqkr[hb:hb + 32, :], in0=bqk[hb + 32:hb + 64, :], scalar1=-1.0,
                                                     scalar2=None, op0=ALU.mult), reads=[B_bias], writes=[B_bias])
        S.op("dve", lambda e, hb=hb: e.tensor_copy(out=bqkr[hb + 32:hb + 64, :], in_=bqk[hb:hb + 32, :]),
             reads=[B_bias], writes=[B_bias])

    es8 = sb("es8", [128, 8], F32)
    esfm = sb("esfm", [128, 4], F32)
    B_sink = Buf("sink")
    S.dma("sp", lambda e: e.dma_start(out=es8[:, :], in_=sinks_d.partition_broadcast(128)), writes=[B_sink])
    S.op("act", lambda e: e.activation(out=es8[:, :], in_=es8[:, :], func=AF.Exp), reads=[B_sink], writes=[B_sink])
    S.op("dve", lambda e: e.tensor_copy(out=esfm[0:64, :], in_=es8[0:64, 0:8:2]), reads=[B_sink], writes=[B_sink])
    S.op("dve", lambda e: e.tensor_copy(out=esfm[64:128, :], in_=es8[64:128, 1:8:2]), reads=[B_sink], writes=[B_sink])

    xT = sb("xT", [128, KT, TH], BF16)
    B_xT = [Buf(f"xT{t}") for t in range(NT + 1)]
    xtok = [sb(f"xtok{i}", [128, D], BF16) for i in range(2)]
    B_xtok = [Buf(f"xtok{i}") for i in range(2)]
    for t in range(NT + 1):
        sl = t % 2
        src = xhalo[:, :] if t == 0 else xloc[(t - 1) * 128:t * 128, :]
        S.dma("pool", lambda e, sl=sl, src=src: e.dma_start(out=xtok[sl][:, :], in_=src), writes=[B_xtok[sl]])
        pb = 6 + sl
        pv = PS[pb][:, :]
        S.op("pe", [lambda e, k=k, sl=sl, pv=pv: e.transpose(out=pv[:, k * 128:(k + 1) * 128], in_=xtok[sl][:, k * 128:(k + 1) * 128],
                                                             identity=identb[:, :]) for k in range(KT)],
             reads=[B_xtok[sl], B_ident], writes=[PSB[pb]])
        ev = "dve" if t % 2 == 0 else "act"
        if ev == "dve":
            S.op("dve", lambda e, t=t, pv=pv: e.tensor_copy(out=xT[:, :, t * 128:(t + 1) * 128],
                                                           in_=pv.rearrange("p (k n) -> p k n", k=KT)),
                 reads=[PSB[pb]], writes=[B_xT[t]])
        else:
            S.op("act", lambda e, t=t, pv=pv: e.copy(out=xT[:, :, t * 128:(t + 1) * 128],
                                                    in_=pv.rearrange("p (k n) -> p k n", k=KT)),
                 reads=[PSB[pb]], writes=[B_xT[t]])

    if stage == "xt":
        dx = dram_out("dbg_xT", [D, TH])
        S.dma("pool", lambda e: e.dma_start(out=dx.rearrange("(k p) t -> p k t", p=128)[:, :, :], in_=xT[:, :, :]), stream="out",
              reads=B_xT, writes=[Buf("o")])
        db = dram_out("dbg_bfm", [128, 10])
        S.dma("sp", lambda e: e.dma_start(out=db[:, :], in_=bfm[:, :]), stream="out", reads=[B_bfm], writes=[Buf("o1")])
        S.finish()
        close_phase(); es.close()
        return nc, ["dbg_xT", "dbg_bfm"]

    qT = sb("qT", [128, 4, TOK], BF16)
    kT = sb("kT", [128, 2, TH], BF16)
    vpad = sb("vpad", [128, NT + 1, 2, 192], BF16)
    opad = sb("opad", [128, 192], BF16)
    B_pad = Buf("pad")
    S.op("pool", lambda e: e.memset(vpad[:, :, :, :], 0.0), writes=[B_pad])
    S.op("pool", lambda e: e.memset(opad[:, :], 0.0), writes=[B_pad])
    S.op("pool", lambda e: e.memset(opad[:, 64:128], 1.0), reads=[B_pad], writes=[B_pad])
    B_q = [Buf(f"q{b}") for b in range(4)]
    B_k = [Buf(f"k{b}") for b in range(5)]
    B_v = [Buf(f"v{t}") for t in range(NT + 1)]
    rtmp = [sb(f"rtmp{i}", [128, 2, 512], F32) for i in range(2)]
    B_rtmp = [Buf(f"rtmp{i}") for i in range(2)]
    it = 0

    def wcols(m):
        if m < 4:
            return lambda k: win[:, k, 128 * m:128 * m + 128]
        if m == 4:
            return lambda k: win[:, k, 512:640]
        return lambda k: wkb[:, k, :]

    blocks = [(0, 128, 0)] + [(128 + 512 * b, 512, b + 1) for b in range(4)]
    for (c0, n, bi) in blocks:
        tiles = range((c0 // 128), (c0 + n) // 128)
        xdeps = [B_xT[t] for t in tiles]
        for m in (range(6) if bi > 0 else (4, 5)):
            pa, pb = 2 * (it % 2), 2 * (it % 2) + 1
            wc = wcols(m)
            S.op("pe", [lambda e, k=k, wc=wc, pa=pa, c0=c0, n=n: e.matmul(PS[pa][:, 0:n], lhsT=wc(k), rhs=xT[:, k, c0:c0 + n],
                                                                         start=(k == 0), stop=(k == KT - 1)) for k in range(KT)],
                 reads=xdeps + [B_win, B_wrot], writes=[PSB[pa]])
            S.op("pe", [lambda e, k=k, m=m, pb=pb, c0=c0, n=n: e.matmul(PS[pb][:, 0:n], lhsT=wrot[:, k, m, :], rhs=xT[:, k, c0:c0 + n],
                                                                       start=(k == 0), stop=(k == KT - 1)) for k in range(KT)],
                 reads=xdeps + [B_wrot], writes=[PSB[pb]])
            rt = it % 2
            S.op("dve", lambda e, pa=pa, m=m, c0=c0, n=n, rt=rt: e.scalar_tensor_tensor(
                out=rtmp[rt][:, 0, 0:n], in0=PS[pa][:, 0:n], scalar=bqk[:, m:m + 1], in1=cosT[:, c0:c0 + n], op0=ALU.add, op1=ALU.mult),
                reads=[PSB[pa], B_bias, B_cos], writes=[B_rtmp[rt]])
            S.op("dve", lambda e, pb=pb, m=m, c0=c0, n=n, rt=rt: e.scalar_tensor_tensor(
                out=rtmp[rt][:, 1, 0:n], in0=PS[pb][:, 0:n], scalar=bqkr[:, m:m + 1], in1=sinT[:, c0:c0 + n], op0=ALU.add, op1=ALU.mult),
                reads=[PSB[pb], B_bias, B_sin], writes=[B_rtmp[rt]])
            if m < 4:
                dst, dbuf = qT[:, m, c0 - 128:c0 - 128 + n], B_q[bi - 1]
            else:
                dst, dbuf = kT[:, m - 4, c0:c0 + n], B_k[bi]
            S.op("dve", lambda e, dst=dst, rt=rt, n=n: e.tensor_tensor(out=dst, in0=rtmp[rt][:, 0, 0:n], in1=rtmp[rt][:, 1, 0:n], op=ALU.add),
                 reads=[B_rtmp[rt]], writes=[dbuf])
            it += 1
    for t in range(NT + 1):
        pv_ = 4 + (t % 2)
        S.op("pe", [lambda e, k=k, t=t, pv_=pv_: e.matmul(PS[pv_][:, 0:128], lhsT=xT[:, k, t * 128:(t + 1) * 128], rhs=win[:, k, 640:768],
                                                          start=(k == 0), stop=(k == KT - 1)) for k in range(KT)],
             reads=[B_xT[t], B_win], writes=[PSB[pv_]])
        S.op("dve", lambda e, t=t, pv_=pv_: e.tensor_tensor(out=vpad[:, t, :, 64:128], in0=PS[pv_][:, 0:128].rearrange("p (g d) -> p g d", g=2),
                                                            in1=bvbc[:, :].rearrange("p (g d) -> p g d", g=2), op=ALU.add),
             reads=[PSB[pv_], B_bvbc, B_pad], writes=[B_v[t]])

    if stage == "qk":
        dq = dram_out("dbg_q", [512, TOK])
        S.dma("pool", lambda e: e.dma_start(out=dq.rearrange("(c p) t -> p c t", p=128)[:, :, :], in_=qT[:, :, :]), stream="out", reads=B_q, writes=[Buf("o2")])
        dk = dram_out("dbg_k", [256, TH])
        S.dma("pool", lambda e: e.dma_start(out=dk.rearrange("(c p) t -> p c t", p=128)[:, :, :], in_=kT[:, :, :]), stream="out", reads=B_k, writes=[Buf("o3")])
        dvv = dram_out("dbg_v", [TH, 128])
        for g in range(2):
            S.dma("pool", lambda e, g=g: e.dma_start(out=dvv[:, 64 * g:64 * g + 64].rearrange("(t p) d -> p t d", p=128)[:, :, :],
                                                     in_=vpad[:, :, g, 64:128]),
                  stream="out", reads=B_v, writes=[Buf(f"o4{g}")])
        S.finish()
        close_phase(); es.close()
        return nc, ["dbg_q", "dbg_k", "dbg_v"]

    kTz = sb("kTz", [128, 4, TH], BF16)
    B_kz0 = Buf("kz0")
    S.op("pool", lambda e: e.memset(kTz[:, :, :], 0.0), writes=[B_kz0])
    B_kz = [Buf(f"kz{b}") for b in range(5)]
    kz_map = ((0, 0, 0), (1, 1, 1), (2, 0, 1), (3, 1, 0))
    for (c0, n, bi) in blocks:
        for (vi, half, var) in kz_map:
            p0 = 64 * half
            S.op("dve", lambda e, vi=vi, p0=p0, var=var, c0=c0, n=n: e.tensor_copy(out=kTz[p0:p0 + 64, vi, c0:c0 + n], in_=kT[p0:p0 + 64, var, c0:c0 + n]),
                 reads=[B_k[bi], B_kz0], writes=[B_kz[bi]])

    B_cat = [Buf(f"cat{t}") for t in range(NT)]
    pT = [sb(f"pT{i}", [128, 2, 512], BF16) for i in range(2)]
    B_pT = [Buf(f"pT{i}") for i in range(2)]
    ltmp = [sb(f"ltmp{i}", [128, 128], F32) for i in range(2)]
    B_ltmp = [Buf(f"ltmp{i}") for i in range(2)]
    kblk = lambda t: B_k[0] if t == 0 else B_k[1 + (t - 1) // 4]
    kzblk = lambda t: B_kz[0] if t == 0 else B_kz[1 + (t - 1) // 4]
    n_qblk = 1 if stage == "attn1" else NT

    def swa_F(i, g, sl):
        for kk, ktile in enumerate((i, i + 1)):
            pbk = 2 * sl + kk
            fns = []
            for hh in range(4):
                h = 4 * g + hh
                half = h % 2
                fns.append(lambda e, pbk=pbk, hh=hh, h=h, half=half, ktile=ktile: e.matmul(
                    PS[pbk][:, hh * 128:(hh + 1) * 128],
                    lhsT=kTz[:, 2 * g + half, ktile * 128:(ktile + 1) * 128],
                    rhs=qT[:, h // 2, i * 128:(i + 1) * 128], start=True, stop=True))
            S.op("pe", fns, reads=[kzblk(ktile), B_q[i // 4]], writes=[PSB[pbk]])
            S.op("act", lambda e, pbk=pbk, kk=kk: e.activation(out=pT[sl][:, kk, :], in_=PS[pbk][:, :], func=AF.Exp, scale=0.125),
                 reads=[PSB[pbk]], writes=[B_pT[sl]])
            mi = 1 if kk == 1 else (2 if i == 0 else 0)
            S.op("dve", lambda e, kk=kk, mi=mi: e.tensor_tensor(
                out=pT[sl][:, kk, :].rearrange("p (h q) -> p h q", h=4), in0=pT[sl][:, kk, :].rearrange("p (h q) -> p h q", h=4),
                in1=maskb[:, mi, :].unsqueeze(1).to_broadcast([128, 4, 128]), op=ALU.mult),
                reads=[B_pT[sl], B_mask], writes=[B_pT[sl]])

    def swa_G(i, g, sl):
        for tt in range(2):
            ct = 2 * g + tt
            po = 4 + tt
            fo, fl = [], []
            n_mm = 0
            for hl in range(2):
                hh = 2 * tt + hl
                for kk, ktile in enumerate((i, i + 1)):
                    first, last = (n_mm == 0), (n_mm == 3)
                    vs = (64, 192) if hl == 0 else (0, 128)
                    fo.append(lambda e, po=po, vs=vs, ktile=ktile, kk=kk, hh=hh, first=first, last=last: e.matmul(
                        PS[po][:, 0:128], lhsT=vpad[:, ktile, g, vs[0]:vs[1]], rhs=pT[sl][:, kk, hh * 128:(hh + 1) * 128],
                        start=first, stop=last))
                    fl.append(lambda e, po=po, vs=vs, kk=kk, hh=hh, first=first, last=last: e.matmul(
                        PS[po][:, 128:256], lhsT=opad[:, vs[0]:vs[1]], rhs=pT[sl][:, kk, hh * 128:(hh + 1) * 128],
                        start=first, stop=last))
                    n_mm += 1
            S.op("pe", fo + fl, reads=[B_pT[sl], B_v[i], B_v[i + 1], B_pad], writes=[PSB[po]])
            ls = tt
            S.op("dve", lambda e, po=po, ct=ct, ls=ls: e.tensor_scalar(out=ltmp[ls][:, :], in0=PS[po][:, 128:256], scalar1=esfm[:, ct:ct + 1],
                                                                        scalar2=None, op0=ALU.add), reads=[PSB[po], B_sink], writes=[B_ltmp[ls]])
            S.op("dve", lambda e, ls=ls: e.reciprocal(out=ltmp[ls][:, :], in_=ltmp[ls][:, :]), reads=[B_ltmp[ls]], writes=[B_ltmp[ls]])
            S.op("dve", lambda e, po=po, ct=ct, ls=ls: e.tensor_tensor(out=catT[:, ct, i * 128:(i + 1) * 128], in0=PS[po][:, 0:128],
                                                                       in1=ltmp[ls][:, :], op=ALU.mult),
                 reads=[PSB[po], B_ltmp[ls]], writes=[B_cat[i]])

    its = [(i, g) for i in range(n_qblk) for g in range(2)]
    for n, (i, g) in enumerate(its):
        swa_F(i, g, n % 2)
        if n > 0:
            swa_G(its[n - 1][0], its[n - 1][1], (n - 1) % 2)
    swa_G(its[-1][0], its[-1][1], (len(its) - 1) % 2)

    outs = []
    if stage in ("attn", "attn1"):
        dbg = dram_out("dbg_attn", [512, TOK])
        dv = dbg.rearrange("(c p) t -> p c t", p=128)
        nqo = 128 * n_qblk
        S.dma("pool", lambda e: e.dma_start(out=dv[:, :, 0:nqo], in_=catT[:, 0:4, 0:nqo]), stream="out", reads=B_cat[:n_qblk], writes=[Buf("o")])
        dq = dram_out("dbg_q", [512, TOK])
        S.dma("pool", lambda e: e.dma_start(out=dq.rearrange("(c p) t -> p c t", p=128)[:, :, :], in_=qT[:, :, :]), stream="out", reads=B_q, writes=[Buf("o2")])
        dk = dram_out("dbg_k", [256, TH])
        S.dma("pool", lambda e: e.dma_start(out=dk.rearrange("(c p) t -> p c t", p=128)[:, :, :], in_=kT[:, :, :]), stream="out", reads=B_k, writes=[Buf("o3")])
        outs = ["dbg_attn", "dbg_q", "dbg_k"]
        S.finish()
        close_phase(); es.close()
        return nc, outs
    close_phase()
    print("[build] sbuf free after phase 1:", nc.sbuf_bytes_remaining)
    return build_rest(nc, es, S, sb, open_phase, close_phase, dram_in, dram_out, PS, PSB, identb, identf, catT, B_cat,
                      B_ident, B_identf, stage)


def build_rest(nc, es, S, sb, open_phase, close_phase, dram_in, dram_out, PS, PSB, identb, identf, catT, B_cat,
               B_ident, B_identf, stage):
    xloc = nc_ap(nc, "xloc")
    xprev = dram_in("xprev", [3 * TOK, D])
    segv_d = dram_in("segvalid", [128, 4])
    w_in = nc_ap(nc, "w_in")
    b_in = nc_ap(nc, "b_in")
    lam_re_d = dram_in("s5_lambda_re", [32, 64]); lam_im_d = dram_in("s5_lambda_im", [32, 64])
    logdt_d = dram_in("s5_log_dt", [32])
    bre_d = dram_in("s5_b_re", [32, 64, 16]); bim_d = dram_in("s5_b_im", [32, 64, 16])
    cre_d = dram_in("s5_c_re", [32, 16, 64]); cim_d = dram_in("s5_c_im", [32, 16, 64])
    d_d = dram_in("s5_d", [512])
    wglu_d = dram_in("s5_w_glu", [512, 512]); bglu_d = dram_in("s5_b_glu", [512])
    cmask_d = dram_in("cmask", [128, 2, 256])

    open_phase()
    B_tb = Buf("tb")
    B_t4 = Buf("t4")

    def tt(out, a, b, op, eng="dve"):
        S.op(eng, lambda e: e.tensor_tensor(out=out, in0=a, in1=b, op=op), reads=[B_tb, B_t4], writes=[B_tb, B_t4])

    def ts(out, a, s1, op0, s2=None, op1=None):
        if op1 is None:
            S.op("dve", lambda e: e.tensor_scalar(out=out, in0=a, scalar1=s1, scalar2=None, op0=op0), reads=[B_tb, B_t4], writes=[B_tb, B_t4])
        else:
            S.op("dve", lambda e: e.tensor_scalar(out=out, in0=a, scalar1=s1, scalar2=s2, op0=op0, op1=op1), reads=[B_tb, B_t4], writes=[B_tb, B_t4])

    def TT(out, a, b, op, reads, writes, eng="dve"):
        S.op(eng, lambda e: e.tensor_tensor(out=out, in0=a, in1=b, op=op), reads=reads, writes=writes)

    def cp(out, a):
        S.op("dve", lambda e: e.tensor_copy(out=out, in_=a), reads=[B_tb, B_t4], writes=[B_tb, B_t4])

    def act(out, a, func, scale=1.0):
        S.op("act", lambda e: e.activation(out=out, in_=a, func=func, scale=scale), reads=[B_tb, B_t4], writes=[B_tb, B_t4])

    def cmul(outr, outi, ar, ai, br, bi, t1, t2, neg_i=False):
        tt(t1, ar, br, ALU.mult); tt(t2, ai, bi, ALU.mult); tt(outr, t1, t2, ALU.subtract)
        tt(t1, ar, bi, ALU.mult); tt(t2, ai, br, ALU.mult)
        if neg_i:
            tt(t1, t1, t2, ALU.add); ts(outi, t1, -1.0, ALU.mult)
        else:
            tt(outi, t1, t2, ALU.add)

    U = sb("s5U", [128, 32, 16, 16], BF16)
    UT = sb("s5UT", [128, 32, 2, 128], BF16)
    Hst = sb("s5H", [128, 32, 128], BF16)
    A_ = sb("s5A", [64, 2, 32, 17], F32)
    AI = sb("s5AI", [64, 2, 32, 16], F32)
    B15 = sb("s5B15", [64, 2, 32, 16], F32)
    CT = sb("s5CT", [64, 2, 32, 16], F32)
    C15 = sb("s5C15", [64, 2, 32, 16], F32)
    t4 = sb("s5t4", [128, 2, 2048], F32)
    t4a = t4[0:64, 0, :].rearrange("p (g s h) -> p g s h", g=8, s=16)
    t4b = t4[0:64, 1, :].rearrange("p (g s h) -> p g s h", g=8, s=16)
    ztmp = t4[:, 0, :].rearrange("p (g c) -> p g c", g=16)
    PSt = sb("s5PSt", [128, 8, 256], BF16)
    B_U, B_UT, B_H = Buf("s5U"), Buf("s5UT"), Buf("s5H")

    def build_PSt(g0):
        bc_s = lambda t, ri: t[:, ri, g0:g0 + 8, :].unsqueeze(3).to_broadcast([64, 8, 16, 16])
        bc_h = lambda t, ri: t[:, ri, g0:g0 + 8, :].unsqueeze(2).to_broadcast([64, 8, 16, 16])
        o4 = lambda lo: PSt[lo:lo + 64, :, :].rearrange("p g (s h) -> p g s h", s=16)
        tt(t4a, bc_s(AI, 0), bc_h(B15, 0), ALU.mult); tt(t4b, bc_s(AI, 1), bc_h(B15, 1), ALU.mult)
        tt(o4(0), t4a, t4b, ALU.subtract)
        tt(t4a, bc_s(AI, 0), bc_h(B15, 1), ALU.mult); tt(t4b, bc_s(AI, 1), bc_h(B15, 0), ALU.mult)
        tt(o4(64), t4a, t4b, ALU.add)

    open_phase()
    LA = sb("s5LA", [128, 32, 2, 128], BF16)
    R = sb("s5R", [128, 2, 16, 129], F32)
    rho_p = sb("s5rho", [128, 16], F32)
    segv = sb("s5segv", [128, 4], F32)
    open_phase()
    sm = sb("s5sm", [64, 56, 32], F32)
    smi = iter(range(56))
    T = lambda: sm[:, next(smi), :]
    lamn = sb("s5lamn", [32, 2, 64], F32)
    Bnat = sb("s5Bnat", [128, 2, 16, 16], F32)
    Cnat = sb("s5Cnat", [128, 2, 4, 64], F32)
    Bq = sb("s5Bq", [64, 2, 32, 16], F32)
    Bbar = sb("s5Bbar", [64, 2, 32, 16], F32)
    t3a = sb("s5t3a", [64, 32, 16], F32)
    t3b = sb("s5t3b", [64, 32, 16], F32)
    S.dma("sp", lambda e: e.dma_start(out=lamn[:, 0, :], in_=lam_re_d[:, :]), stream="tbl", writes=[B_tb])
    S.dma("sp", lambda e: e.dma_start(out=lamn[:, 1, :], in_=lam_im_d[:, :]), stream="tbl", writes=[B_tb])
    dtb = T()
    S.dma("sp", lambda e: e.dma_start(out=dtb, in_=logdt_d.partition_broadcast(64)), stream="tbl", writes=[B_tb])
    for ri, src in enumerate((bre_d, bim_d)):
        S.dma("sp", lambda e, ri=ri, src=src: e.dma_start(out=Bnat[:, ri, :, :], in_=src.rearrange("g p h -> (g p) h").rearrange("(gg r) h -> r gg h", r=128)),
              stream="tbl", writes=[B_tb])
    for ri, src in enumerate((cre_d, cim_d)):
        S.dma("sp", lambda e, ri=ri, src=src: e.dma_start(out=Cnat[:, ri, :, :], in_=src.rearrange("g h p -> (g h) p").rearrange("(t r) p -> r t p", r=128)),
              stream="tbl", writes=[B_tb])
    S.dma("sp", lambda e: e.dma_start(out=segv[:, :], in_=segv_d[:, :]), stream="tbl", writes=[B_tb])
    S.op("pe", [lambda e, ri=ri: e.transpose(out=PS[5][0:64, 32 * ri:32 * ri + 32], in_=lamn[:, ri, :], identity=identf[0:32, 0:32]) for ri in range(2)],
         reads=[B_tb, B_identf], writes=[PSB[5]])
    lam_r, lam_i = T(), T()
    S.op("dve", lambda e: e.tensor_copy(out=lam_r, in_=PS[5][0:64, 0:32]), reads=[PSB[5], B_tb], writes=[B_tb])
    S.op("dve", lambda e: e.tensor_copy(out=lam_i, in_=PS[5][0:64, 32:64]), reads=[PSB[5], B_tb], writes=[B_tb])
    for ri in range(2):
        S.op("pe", [lambda e, ri=ri, t=t: e.transpose(out=PS[4][0:64, 128 * t:128 * t + 128], in_=Cnat[:, ri, t, :], identity=identf[:, :]) for t in range(4)],
             reads=[B_tb, B_identf], writes=[PSB[4]])
        S.op("dve", lambda e, ri=ri: e.tensor_copy(out=CT[:, ri, :, :].rearrange("p g h -> p (g h)"), in_=PS[4][0:64, :]), reads=[PSB[4], B_tb], writes=[B_tb])
    for ri in range(2):
        cp(Bq[:, ri, 0:32:2, :], Bnat[0:64, ri, :, :])
        cp(Bq[:, ri, 1:32:2, :], Bnat[64:128, ri, :, :])
    act(dtb, dtb, AF.Exp)
    lr = T(); ts(lr, lam_r, -1e-4, ALU.min)
    mu, th = T(), T(); tt(mu, lr, dtb, ALU.mult); tt(th, lam_i, dtb, ALU.mult)
    sn, sh, cs = T(), T(), T()
    act(sn, th, AF.Sin, scale=0.125); act(sh, th, AF.Sin, scale=0.0625)
    tt(sh, sh, sh, ALU.mult); ts(cs, sh, -2.0, ALU.mult, 1.0, ALU.add)
    u1, u2 = T(), T()
    for _ in range(3):
        cn, s2 = T(), T()
        tt(u1, cs, cs, ALU.mult); tt(u2, sn, sn, ALU.mult); tt(cn, u1, u2, ALU.subtract)
        tt(u1, cs, sn, ALU.mult); ts(s2, u1, 2.0, ALU.mult)
        cs, sn = cn, s2
    mag, ar, ai = T(), T(), T()
    act(mag, mu, AF.Exp); tt(ar, mag, cs, ALU.mult); tt(ai, mag, sn, ALU.mult)
    den, am1, zr, zi = T(), T(), T(), T()
    tt(u1, lr, lr, ALU.mult); tt(u2, lam_i, lam_i, ALU.mult); tt(den, u1, u2, ALU.add)
    S.op("dve", lambda e: e.reciprocal(out=den, in_=den), reads=[B_tb, B_t4], writes=[B_tb, B_t4])
    ts(am1, ar, -1.0, ALU.add)
    tt(u1, am1, lr, ALU.mult); tt(u2, ai, lam_i, ALU.mult); tt(u1, u1, u2, ALU.add); tt(zr, u1, den, ALU.mult)
    tt(u1, ai, lr, ALU.mult); tt(u2, am1, lam_i, ALU.mult); tt(u1, u1, u2, ALU.subtract); tt(zi, u1, den, ALU.mult)
    bz = lambda t: t.unsqueeze(2).to_broadcast([64, 32, 16])
    cmul(Bbar[:, 0, :, :], Bbar[:, 1, :, :], bz(zr), bz(zi), Bq[:, 0, :, :], Bq[:, 1, :, :], t3a[:, :, :], t3b[:, :, :])
    air, aii = T(), T()
    tt(u1, ar, ar, ALU.mult); tt(u2, ai, ai, ALU.mult); tt(u1, u1, u2, ALU.add)
    S.op("dve", lambda e: e.reciprocal(out=u1, in_=u1), reads=[B_tb, B_t4], writes=[B_tb, B_t4])
    tt(air, ar, u1, ALU.mult); tt(aii, ai, u1, ALU.mult); ts(aii, aii, -1.0, ALU.mult)

    def cpow(tab, n, br, bi, npart, shp, tmpa, tmpb, bm_bufs):
        nd = len(shp)
        S.op("dve", lambda e: e.memset(tab_slice(tab, 0, nd, 0, 1), 1.0), reads=[B_tb, B_t4], writes=[B_tb, B_t4])
        S.op("dve", lambda e: e.memset(tab_slice(tab, 1, nd, 0, 1), 0.0), reads=[B_tb, B_t4], writes=[B_tb, B_t4])
        filled, bmr, bmi, bi_ = 1, br, bi, 0
        while filled < n:
            cnt = min(filled, n - filled)
            bb = lambda t: t.unsqueeze(nd + 1).to_broadcast([npart] + shp + [cnt])
            cmul(tab_slice(tab, 0, nd, filled, filled + cnt), tab_slice(tab, 1, nd, filled, filled + cnt),
                 tab_slice(tab, 0, nd, 0, cnt), tab_slice(tab, 1, nd, 0, cnt), bb(bmr), bb(bmi),
                 tab_slice(tmpa, None, nd, 0, cnt), tab_slice(tmpb, None, nd, 0, cnt))
            filled += cnt
            if filled < n:
                nr, ni = bm_bufs[bi_]; bi_ += 1
                q1, q2 = bm_bufs[-1]
                tt(q1, bmr, bmr, ALU.mult); tt(q2, bmi, bmi, ALU.mult); tt(nr, q1, q2, ALU.subtract)
                tt(q1, bmr, bmi, ALU.mult); ts(ni, q1, 2.0, ALU.mult)
                bmr, bmi = nr, ni

    def tab_slice(tab, ri, nd, k0, k1):
        if ri is None:
            return tab[:, :, k0:k1] if nd == 1 else tab[:, :, :, k0:k1]
        return tab[:, ri, :, k0:k1] if nd == 1 else tab[:, ri, :, :, k0:k1]

    t17a = sb("s5t17a", [64, 32, 8], F32); t17b = sb("s5t17b", [64, 32, 8], F32)
    bmA = [(T(), T()) for _ in range(5)]
    cpow(A_, 17, ar, ai, 64, [32], t17a, t17b, bmA)
    bmI = [(T(), T()) for _ in range(5)]
    cpow(AI, 16, air, aii, 64, [32], t17a, t17b, bmI)
    b15 = lambda t, ri, k: t[:, ri, :, k:k + 1].to_broadcast([64, 32, 16])
    cmul(B15[:, 0, :, :], B15[:, 1, :, :], b15(A_, 0, 15), b15(A_, 1, 15), Bbar[:, 0, :, :], Bbar[:, 1, :, :], t3a[:, :, :], t3b[:, :, :])
    cmul(C15[:, 0, :, :], C15[:, 1, :, :], b15(AI, 0, 15), b15(AI, 1, 15), CT[:, 0, :, :], CT[:, 1, :, :], t3a[:, :, :], t3b[:, :, :])
    rho = T(); act(rho, mu, AF.Exp, scale=16.0)
    rr = T()
    S.op("dve", lambda e: e.reciprocal(out=rr, in_=rho), reads=[B_tb, B_t4], writes=[B_tb, B_t4])
    rot_r, rot_i = T(), T()
    tt(rot_r, A_[:, 0, :, 16], rr, ALU.mult); tt(rot_i, A_[:, 1, :, 16], rr, ALU.mult)
    rotp = sb("s5rotp", [128, 2, 16], F32)
    for src, dst in ((rot_r, rotp[:, 0, :]), (rot_i, rotp[:, 1, :]), (rho, rho_p[:, :])):
        cp(dst[0:64], src[:, 0:32:2]); cp(dst[64:128], src[:, 1:32:2])
    tRa = sb("s5tRa", [128, 16, 64], F32); tRb = sb("s5tRb", [128, 16, 64], F32)
    smp = sb("s5smp", [128, 16, 16], F32)
    bmR = [(smp[:, 2 * i, :], smp[:, 2 * i + 1, :]) for i in range(8)]
    cpow(R, 129, rotp[:, 0, :], rotp[:, 1, :], 128, [16], tRa, tRb, bmR)
    for gb in range(4):
        build_PSt(8 * gb)
        for hb in range(2):
            bank = 6 + hb
            S.op("pe", [lambda e, j=j, hb=hb, bank=bank: e.transpose(out=PS[bank][:, 128 * j:128 * j + 128],
                                                                     in_=PSt[:, 4 * hb + j // 2, 128 * (j % 2):128 * (j % 2) + 128], identity=identb[:, :])
                        for j in range(8)], reads=[B_tb, B_ident], writes=[PSB[bank]])
            S.op("act", lambda e, gb=gb, hb=hb, bank=bank: e.copy(out=LA[:, 8 * gb + 4 * hb:8 * gb + 4 * hb + 4, :, :].rearrange("p g k c -> p (g k c)"),
                                                                  in_=PS[bank][:, :]), reads=[PSB[bank], B_tb], writes=[B_tb])

    close_phase()
    print("[build] sbuf free before S5 segment buffers:", nc.sbuf_bytes_remaining)

    wu = sb("s5wu", [128, KT, 512], BF16)
    bubc = sb("s5bubc", [128, 512], F32)
    B_wu, B_bu = Buf("s5wu"), Buf("s5bu")
    S.dma("pool", lambda e: e.dma_start(out=wu[:, :, :], in_=w_in.rearrange("(kt p) f -> p kt f", p=128)[:, :, 768:1280]), writes=[B_wu])
    S.dma("sp", lambda e: e.dma_start(out=bubc[:, :], in_=b_in[768:1280].partition_broadcast(128)), writes=[B_bu])
    xs = [sb(f"s5xs{i}", [128, D], BF16) for i in range(2)]
    B_xs = [Buf(f"s5xs{i}") for i in range(2)]
    xsT = [sb(f"s5xsT{i}", [128, KT, 128], BF16) for i in range(2)]
    B_xsT = [Buf(f"s5xsT{i}") for i in range(2)]
    Z = sb("s5Z", [128, 16, 2, 128], F32)
    G2 = sb("s5G2", [128, 16, 2, 129], F32)
    B_Z, B_G = Buf("s5Z"), Buf("s5G")
    ctmp = sb("s5ctmp", [128, 4, 16], F32)
    S.op("dve", lambda e: e.memset(G2[:, :, :, 0:1], 0.0), reads=[B_G], writes=[B_G])
    ev = 0
    Ud, UTd, BUd, BUTd = U, UT, B_U, B_UT
    for seg in range(4):
        seg_v = (xprev[seg * TOK:(seg + 1) * TOK, :] if seg < 3 else xloc[:, :]).rearrange("(c s) d -> s c d", s=16)
        for s_ in range(16):
            sl = s_ % 2
            S.dma("pool", lambda e, sl=sl, s_=s_, seg_v=seg_v: e.dma_start(out=xs[sl][:, :], in_=seg_v[s_]), writes=[B_xs[sl]])
            bank = 6 + sl
            S.op("pe", [lambda e, k=k, sl=sl, bank=bank: e.transpose(out=PS[bank][:, k * 128:(k + 1) * 128], in_=xs[sl][:, k * 128:(k + 1) * 128],
                                                                     identity=identb[:, :]) for k in range(KT)],
                 reads=[B_xs[sl], B_ident], writes=[PSB[bank]])
            eng = "dve" if s_ % 2 == 0 else "act"
            fn = (lambda e, sl=sl, bank=bank: e.tensor_copy(out=xsT[sl][:, :, :], in_=PS[bank][:, :].rearrange("p (k n) -> p k n", k=KT))) if eng == "dve" else \
                 (lambda e, sl=sl, bank=bank: e.copy(out=xsT[sl][:, :, :], in_=PS[bank][:, :].rearrange("p (k n) -> p k n", k=KT)))
            S.op(eng, fn, reads=[PSB[bank]], writes=[B_xsT[sl]])
            pb = s_ % 4
            S.op("pe", [lambda e, k=k, sl=sl, pb=pb: e.matmul(PS[pb][:, :], lhsT=xsT[sl][:, k, :], rhs=wu[:, k, :], start=(k == 0), stop=(k == KT - 1))
                        for k in range(KT)], reads=[B_xsT[sl], B_wu], writes=[PSB[pb]])
            S.op("dve", lambda e, s_=s_, pb=pb: e.tensor_tensor(out=Ud[:, :, s_, :], in0=PS[pb][:, :].rearrange("p (g h) -> p g h", g=32),
                                                                 in1=bubc[:, :].rearrange("p (g h) -> p g h", g=32), op=ALU.add),
                 reads=[PSB[pb], B_bu], writes=[BUd])
        for gq in range(8):
            bank = 6 + gq % 2
            S.op("pe", [lambda e, j=j, gq=gq, bank=bank, Ud=Ud: e.transpose(out=PS[bank][:, 128 * j:128 * j + 128],
                                                                          in_=Ud[:, 4 * gq + j // 2, 8 * (j % 2):8 * (j % 2) + 8, :].rearrange("p s h -> p (s h)"),
                                                                          identity=identb[:, :]) for j in range(8)],
                 reads=[BUd, B_ident], writes=[PSB[bank]])
            eng = "dve" if gq % 2 == 0 else "act"
            dst = UTd[:, 4 * gq:4 * gq + 4, :, :].rearrange("p g k c -> p (g k c)")
            fn = (lambda e, dst=dst, bank=bank: e.tensor_copy(out=dst, in_=PS[bank][:, :])) if eng == "dve" else (lambda e, dst=dst, bank=bank: e.copy(out=dst, in_=PS[bank][:, :]))
            S.op(eng, fn, reads=[PSB[bank]], writes=[BUTd])
        for gq in range(8):
            pb = gq % 4
            fns = []
            for j in range(4):
                g = 4 * gq + j
                for kh in range(2):
                    fns.append(lambda e, g=g, kh=kh, j=j, pb=pb, UTd=UTd: e.matmul(PS[pb][:, 128 * j:128 * j + 128], lhsT=LA[:, g, kh, :], rhs=UTd[:, g, kh, :],
                                                                                 start=(kh == 0), stop=(kh == 1)))
            S.op("pe", fns, reads=[BUTd, B_tb], writes=[PSB[pb]])
            pv = PS[pb][:, :].rearrange("p (j c) -> p j c", j=4)
            for q in range(2):
                for ri in range(2):
                    eng = "dve" if ev % 2 == 0 else "act"; ev += 1
                    dst = Z[64 * q:64 * q + 64, 2 * gq:2 * gq + 2, ri, :]
                    srcv = pv[64 * ri:64 * ri + 64, q:4:2, :]
                    fn = (lambda e, dst=dst, srcv=srcv: e.tensor_copy(out=dst, in_=srcv)) if eng == "dve" else (lambda e, dst=dst, srcv=srcv: e.copy(out=dst, in_=srcv))
                    S.op(eng, fn, reads=[PSB[pb], B_Z], writes=[B_Z])
        if stage == "s5z" and seg == 3:
            dz = dram_out("dbg_Z", [128, 16, 2, 128])
            S.dma("sp", lambda e: e.dma_start(out=dz.rearrange("p a b c -> p (a b c)"), in_=Z[:, :, :, :].rearrange("p a b c -> p (a b c)")), stream="out", reads=[B_Z], writes=[Buf("o")])
            S.finish(); close_phase(); close_phase(); es.close()
            return nc, ["dbg_Z"]
        Rr, Ri = R[:, 0, :, 1:129], R[:, 1, :, 1:129]
        zr_, zi_ = Z[:, :, 0, :], Z[:, :, 1, :]
        dz = dict(reads=[B_Z, B_tb, B_G, B_t4], writes=[B_G, B_t4])
        gi_r, gi_i = G2[:, :, 0, 1:129], G2[:, :, 1, 1:129]
        TT(gi_r, Rr, zr_, ALU.mult, **dz)
        TT(ztmp, Ri, zi_, ALU.mult, **dz)
        TT(gi_r, gi_r, ztmp, ALU.add, **dz)
        TT(gi_i, Rr, zi_, ALU.mult, **dz)
        TT(ztmp, Ri, zr_, ALU.mult, **dz)
        TT(gi_i, gi_i, ztmp, ALU.subtract, **dz)
        S.op("dve", [lambda e, gg=gg, ri=ri: e.tensor_tensor_scan(out=G2[:, gg, ri, 1:129], data0=rho_p[:, gg:gg + 1].to_broadcast([128, 128]),
                                                                    data1=G2[:, gg, ri, 1:129], initial=G2[:, gg, ri, 0:1], op0=ALU.mult, op1=ALU.add)
                     for gg in range(16) for ri in range(2)], reads=[B_G, B_tb], writes=[B_G])
        if seg < 3:
            c_ = lambda i: ctmp[:, i, :]
            TT(c_(0), R[:, 0, :, 128], G2[:, :, 0, 128], ALU.mult, **dz)
            TT(c_(1), R[:, 1, :, 128], G2[:, :, 1, 128], ALU.mult, **dz)
            TT(c_(0), c_(0), c_(1), ALU.subtract, **dz)
            TT(c_(2), R[:, 0, :, 128], G2[:, :, 1, 128], ALU.mult, **dz)
            TT(c_(3), R[:, 1, :, 128], G2[:, :, 0, 128], ALU.mult, **dz)
            TT(c_(2), c_(2), c_(3), ALU.add, **dz)
            S.op("dve", lambda e, seg=seg: e.tensor_scalar(out=G2[:, :, 0, 0], in0=c_(0), scalar1=segv[:, seg:seg + 1], scalar2=None, op0=ALU.mult), **dz)
            S.op("dve", lambda e, seg=seg: e.tensor_scalar(out=G2[:, :, 1, 0], in0=c_(2), scalar1=segv[:, seg:seg + 1], scalar2=None, op0=ALU.mult), **dz)
    Rr, Ri = R[:, 0, :, 0:128], R[:, 1, :, 0:128]
    gr, gi = G2[:, :, 0, 0:128], G2[:, :, 1, 0:128]
    dh = dict(reads=[B_G, B_tb, B_H], writes=[B_H])
    z0, z1 = Z[:, :, 0, :], Z[:, :, 1, :]
    dzz = dict(reads=[B_G, B_tb, B_Z, B_t4, B_H], writes=[B_Z, B_t4, B_H])
    TT(z0, Rr, gr, ALU.mult, **dzz)
    TT(ztmp, Ri, gi, ALU.mult, **dzz)
    for q in range(2):
        TT(Hst[0:64, q:32:2, :], z0[64 * q:64 * q + 64], ztmp[64 * q:64 * q + 64], ALU.subtract, **dzz)
    TT(z0, Rr, gi, ALU.mult, **dzz)
    TT(z1, Ri, gr, ALU.mult, **dzz)
    for q in range(2):
        TT(Hst[64:128, q:32:2, :], z0[64 * q:64 * q + 64], z1[64 * q:64 * q + 64], ALU.add, **dzz)
    if stage == "s5h":
        dhh = dram_out("dbg_H", [128, 32, 128])
        S.dma("pool", lambda e: e.dma_start(out=dhh.rearrange("p g c -> p (g c)"), in_=Hst[:, :, :].rearrange("p g c -> p (g c)")), stream="out", reads=[B_H], writes=[Buf("o")])
        S.finish(); close_phase(); close_phase(); es.close()
        return nc, ["dbg_H"]
    close_phase()

    open_phase()
    cmask = sb("s5cmask", [128, 2, 256], BF16)
    B_cm = Buf("s5cm")
    S.dma("pool", lambda e: e.dma_start(out=cmask[:, :, :], in_=cmask_d[:, :, :]), writes=[B_cm])
    QS2 = sb("s5QS2", [128, 8, 256], BF16)
    RB = sb("s5RB", [128, 8, 256], BF16)
    Mj = sb("s5Mj", [128, 8, 2, 256], BF16)
    dbc = sb("s5dbc", [128, 512], F32)
    wglu = sb("s5wglu", [128, 4, 512], BF16)
    bgr = sb("s5bgr", [4, 128], F32); bgfm = sb("s5bgfm", [128, 4], F32)
    yT = sb("s5yT", [128, 4, TOK], BF16)
    ygs = sb("s5ygs", [128, 16, 128], BF16)
    yf = [sb(f"s5yf{i}", [128, 2, 16, 16], F32) for i in range(2)]
    yw = [sb(f"s5yw{i}", [128, 2, 16, 16], F32) for i in range(2)]
    B_d, B_wg, B_bg, B_yT, B_ygs = Buf("s5d"), Buf("s5wg"), Buf("s5bg"), [Buf(f"s5yT{j}") for j in range(4)], Buf("s5ygs")
    B_yf = [Buf(f"s5yf{i}") for i in range(2)]
    S.dma("sp", lambda e: e.dma_start(out=dbc[:, :], in_=d_d.partition_broadcast(128)), writes=[B_d])
    S.dma("pool", lambda e: e.dma_start(out=wglu[:, :, :], in_=wglu_d.rearrange("(kt p) f -> p kt f", p=128)), writes=[B_wg])
    S.dma("sp", lambda e: e.dma_start(out=bgr[:, :], in_=bglu_d.rearrange("(t p) -> t p", p=128)), writes=[B_bg])
    S.op("pe", lambda e: e.transpose(out=PS[5][:, 0:4], in_=bgr[:, :], identity=identf[0:4, 0:4]), reads=[B_bg, B_identf], writes=[PSB[5]])
    S.op("dve", lambda e: e.tensor_copy(out=bgfm[:, :], in_=PS[5][:, 0:4]), reads=[PSB[5]], writes=[B_bg])

    def build_Q(dst, g0, Ctab, k0):
        bc_s = lambda ri: A_[:, ri, g0:g0 + 8, k0:k0 + 16].unsqueeze(3).to_broadcast([64, 8, 16, 16])
        bc_h = lambda ri: Ctab[:, ri, g0:g0 + 8, :].unsqueeze(2).to_broadcast([64, 8, 16, 16])
        o4 = lambda lo: dst[lo:lo + 64, :, :].rearrange("p g (s h) -> p g s h", s=16)
        tt(t4a, bc_s(0), bc_h(0), ALU.mult); tt(t4b, bc_s(1), bc_h(1), ALU.mult)
        tt(o4(0), t4a, t4b, ALU.subtract)
        tt(t4a, bc_s(0), bc_h(1), ALU.mult); tt(t4b, bc_s(1), bc_h(0), ALU.mult)
        tt(t4a, t4a, t4b, ALU.add)
        ts(o4(64), t4a, -1.0, ALU.mult)

    for j in range(4):
        g0 = 8 * j
        build_PSt(g0); build_Q(QS2, g0, C15, 0); build_Q(RB, g0, CT, 1)
        for g8 in range(8):
            pb = 4 + g8 % 2
            S.op("pe", [lambda e, g8=g8, kh=kh, pb=pb: e.matmul(PS[pb][:, 256 * kh:256 * kh + 256], lhsT=PSt[:, g8, 128 * kh:128 * kh + 128], rhs=QS2[:, g8, :],
                                                                start=True, stop=True) for kh in range(2)], reads=[B_tb], writes=[PSB[pb]])
            S.op("dve", lambda e, g8=g8, pb=pb: e.tensor_tensor(out=Mj[:, g8, :, :], in0=PS[pb][:, :].rearrange("p (k n) -> p k n", k=2), in1=cmask[:, :, :], op=ALU.mult),
                 reads=[PSB[pb], B_cm, B_tb], writes=[B_tb])
        for gp in range(4):
            pb = gp
            fns = []
            for q in range(2):
                g8 = 2 * gp + q; g = g0 + g8
                o = PS[pb][:, 256 * q:256 * q + 256]
                fns.append(lambda e, o=o, g=g, g8=g8: e.matmul(o, lhsT=Hst[:, g, :], rhs=RB[:, g8, :], start=True, stop=False))
                fns.append(lambda e, o=o, g=g, g8=g8: e.matmul(o, lhsT=UT[:, g, 0, :], rhs=Mj[:, g8, 0, :], start=False, stop=False))
                fns.append(lambda e, o=o, g=g, g8=g8: e.matmul(o, lhsT=UT[:, g, 1, :], rhs=Mj[:, g8, 1, :], start=False, stop=True))
            S.op("pe", fns, reads=[B_H, B_UT, B_tb], writes=[PSB[pb]])
            sl = gp % 2
            ga = g0 + 2 * gp
            pv4 = PS[pb][:, :].rearrange("p (g s h) -> p g s h", g=2, s=16)
            dv4 = dbc[:, 16 * ga:16 * ga + 32].rearrange("p (g h) -> p g h", g=2).unsqueeze(2).to_broadcast([128, 2, 16, 16])
            y_, w_ = yf[sl][:, :, :, :], yw[sl][:, :, :, :]
            S.op("dve", lambda e, y_=y_, ga=ga, dv4=dv4: e.tensor_tensor(out=y_, in0=U[:, ga:ga + 2, :, :], in1=dv4, op=ALU.mult), reads=[B_U, B_d, B_yf[sl]], writes=[B_yf[sl]])
            S.op("dve", lambda e, y_=y_, pv4=pv4: e.tensor_tensor(out=y_, in0=y_, in1=pv4, op=ALU.add), reads=[PSB[pb], B_yf[sl]], writes=[B_yf[sl]])
            if stage == "s5y" and j == 0 and gp == 0:
                dy = dram_out("dbg_y", [128, 2, 16, 16])
                S.dma("sp", lambda e: e.dma_start(out=dy.rearrange("p a b c -> p (a b c)"), in_=yf[0][:, :, :, :].rearrange("p a b c -> p (a b c)")), stream="out", reads=[B_yf[0]], writes=[Buf("o")])
                S.finish(); close_phase(); close_phase(); es.close()
                return nc, ["dbg_y"]
            S.op("dve", lambda e, y_=y_, w_=w_: e.tensor_tensor(out=w_, in0=y_, in1=y_, op=ALU.mult), reads=[B_yf[sl]], writes=[B_yf[sl]])
            S.op("dve", lambda e, w_=w_: e.tensor_scalar(out=w_, in0=w_, scalar1=0.044715, scalar2=1.0, op0=ALU.mult, op1=ALU.add), reads=[B_yf[sl]], writes=[B_yf[sl]])
            S.op("dve", lambda e, y_=y_, w_=w_: e.tensor_tensor(out=w_, in0=w_, in1=y_, op=ALU.mult), reads=[B_yf[sl]], writes=[B_yf[sl]])
            S.op("act", lambda e, w_=w_: e.activation(out=w_, in_=w_, func=AF.Sigmoid, scale=1.5957691216057308), reads=[B_yf[sl]], writes=[B_yf[sl]])
            dst = ygs[:, :, 32 * gp:32 * gp + 32].rearrange("p s (g h) -> p g s h", g=2)
            S.op("dve", lambda e, y_=y_, w_=w_, dst=dst: e.tensor_tensor(out=dst, in0=y_, in1=w_, op=ALU.mult), reads=[B_yf[sl], B_ygs], writes=[B_ygs])
        for hb in range(2):
            bank = 6 + hb
            S.op("pe", [lambda e, i=i, hb=hb, bank=bank: e.transpose(out=PS[bank][:, 128 * i:128 * i + 128], in_=ygs[:, 8 * hb + i, :], identity=identb[:, :]) for i in range(8)],
                 reads=[B_ygs, B_ident], writes=[PSB[bank]])
            dst = yT[:, j, :].rearrange("p (c s) -> p s c", s=16)[:, 8 * hb:8 * hb + 8, :]
            S.op("dve", lambda e, dst=dst, bank=bank: e.tensor_copy(out=dst, in_=PS[bank][:, :].rearrange("p (s c) -> p s c", s=8)), reads=[PSB[bank]], writes=[B_yT[j]])
    gt = [sb(f"s5gt{i}", [128, 512], BF16) for i in range(2)]
    B_gt = [Buf(f"s5gt{i}") for i in range(2)]
    ig = 0
    for co in range(4):
        for blk in range(4):
            pb = ig % 4; sl = ig % 2; ig += 1
            S.op("pe", [lambda e, k=k, co=co, blk=blk, pb=pb: e.matmul(PS[pb][:, :], lhsT=wglu[:, k, 128 * co:128 * co + 128], rhs=yT[:, k, 512 * blk:512 * blk + 512],
                                                                       start=(k == 0), stop=(k == 3)) for k in range(4)], reads=B_yT + [B_wg], writes=[PSB[pb]])
            S.op("act", lambda e, co=co, pb=pb, sl=sl: e.activation(out=gt[sl][:, :], in_=PS[pb][:, :], func=AF.Sigmoid, bias=bgfm[:, co:co + 1]), reads=[PSB[pb], B_bg], writes=[B_gt[sl]])
            S.op("dve", lambda e, co=co, blk=blk, sl=sl: e.tensor_tensor(out=catT[:, 4 + co, 512 * blk:512 * blk + 512], in0=yT[:, co, 512 * blk:512 * blk + 512], in1=gt[sl][:, :], op=ALU.mult),
                 reads=[B_gt[sl], B_yT[co]], writes=[B_cat[4 * blk + i] for i in range(4)])
    if stage == "s5":
        dss = dram_out("dbg_ssm", [512, TOK])
        S.dma("pool", lambda e: e.dma_start(out=dss.rearrange("(c p) t -> p c t", p=128), in_=catT[:, 4:8, :]), stream="out", reads=B_cat, writes=[Buf("o")])
        S.finish(); close_phase(); close_phase(); es.close()
        return nc, ["dbg_ssm"]
    close_phase()
    close_phase()
    ALPHA = 2.0 ** 0.25
    CAP = 384
    xloc = nc_ap(nc, "xloc")
    mem_d = dram_in("mem", [256, D])
    wout_d = dram_in("w_out", [D, D]); bout_d = dram_in("b_out", [D])
    ln_d = {k: dram_in(k, [D]) for k in ("ln1_g", "ln1_b", "ln2_g", "ln2_b", "ln3_g", "ln3_b")}
    wxq_d = dram_in("w_xq", [D, D]); wxkv_d = dram_in("w_xkv", [D, 2 * D]); wxo_d = dram_in("w_xo", [D, D])
    wr_d = dram_in("w_router", [D, 32]); br_d = dram_in("b_router", [32])
    tri_d = dram_in("tri", [128, 128]); ecap_d = dram_in("ecap", [128, 32])
    x2f = nc.dram_tensor("x2f", [TOK, D], F32, kind="Internal").ap()
    xg = nc.dram_tensor("xg", [32 * CAP, D], BF16, kind="Internal").ap()
    yg = nc.dram_tensor("yg", [32 * CAP, D], F32, kind="Internal").ap()

    gates = sb("gates", [128, NT, 4], F32)
    dest = sb("dest", [128, NT, 4], I32)
    B_route = [Buf(f"route{t}") for t in range(NT)]
    B_xg, B_x2f = Buf("xg"), [Buf(f"x2f{t}") for t in range(NT)]

    open_phase()
    onesb = sb("onesb", [128, 128], BF16)
    trib = sb("tri_sb", [128, 128], BF16)
    ecap = sb("ecap_sb", [128, 32], F32)
    B_c3 = Buf("c3")
    S.op("pool", lambda e: e.memset(onesb[:, :], 1.0), writes=[B_c3])
    B_tri, B_ecap = Buf("tri"), Buf("ecap")
    S.dma("pool", lambda e: e.dma_start(out=trib[:, :], in_=tri_d[:, :]), writes=[B_tri])
    S.dma("sp", lambda e: e.dma_start(out=ecap[:, :], in_=ecap_d[:, :]), writes=[B_ecap])
    bc = {}
    B_bc = {}
    for k, src in [("b_out", bout_d)] + [(k, ln_d[k]) for k in ("ln1_g", "ln1_b", "ln2_g", "ln2_b")]:
        bc[k] = sb("bc_" + k, [128, D], F32)
        B_bc[k] = Buf("bc_" + k)
        S.dma("sp", lambda e, k=k, src=src: e.dma_start(out=bc[k][:, :], in_=src.partition_broadcast(128)), writes=[B_bc[k]])
    brbc = sb("brbc", [128, 32], F32); wr = sb("wr", [128, KT, 32], F32)
    B_br, B_wr = Buf("brbc"), Buf("wr")
    S.dma("sp", lambda e: e.dma_start(out=brbc[:, :], in_=br_d.partition_broadcast(128)), writes=[B_br])
    S.dma("sp", lambda e: e.dma_start(out=wr[:, :, :], in_=wr_d.rearrange("(kt p) f -> p kt f", p=128)), writes=[B_wr])
    wout = sb("wout", [128, KT, D], BF16); wxq = sb("wxq", [128, KT, D], BF16); wxo = sb("wxo", [128, KT, D], BF16)
    B_wout, B_wxq, B_wxo = Buf("wout"), Buf("wxq"), Buf("wxo")
    for t_, src, bf in ((wout, wout_d, B_wout), (wxq, wxq_d, B_wxq), (wxo, wxo_d, B_wxo)):
        S.dma("pool", lambda e, t_=t_, src=src: e.dma_start(out=t_[:, :, :], in_=src.rearrange("(kt p) f -> p kt f", p=128)), writes=[bf])
    kmemT = sb("kmemT", [128, KT, 256], BF16)
    vmem = sb("vmem", [128, 2, D], BF16)
    B_kv = Buf("kv")
    open_phase()
    wxkv = sb("wxkv", [128, KT, 2 * D], BF16)
    memb = sb("memb", [128, 2, D], BF16)
    memT = sb("memT", [128, KT, 256], BF16)
    B_wxkv, B_memb, B_memT = Buf("wxkv"), Buf("memb"), Buf("memT")
    for h in range(2):
        S.dma("pool", lambda e, h=h: e.dma_start(out=wxkv[:, 4 * h:4 * h + 4, :], in_=wxkv_d.rearrange("(kt p) f -> p kt f", p=128)[:, 4 * h:4 * h + 4, :]), writes=[B_wxkv])
    S.dma("pool", lambda e: e.dma_start(out=memb[:, :, :], in_=mem_d.rearrange("(t p) d -> p t d", p=128)), writes=[B_memb])
    for mt in range(2):
        bank = 6 + mt
        S.op("pe", [lambda e, k=k, mt=mt, bank=bank: e.transpose(out=PS[bank][:, k * 128:(k + 1) * 128], in_=memb[:, mt, k * 128:(k + 1) * 128], identity=identb[:, :])
                    for k in range(KT)], reads=[B_memb, B_ident], writes=[PSB[bank]])
        S.op("dve", lambda e, mt=mt, bank=bank: e.tensor_copy(out=memT[:, :, mt * 128:(mt + 1) * 128], in_=PS[bank][:, :].rearrange("p (k n) -> p k n", k=KT)),
             reads=[PSB[bank]], writes=[B_memT])
    for ct in range(KT):
        pb = ct % 4
        S.op("pe", [lambda e, k=k, ct=ct, pb=pb: e.matmul(PS[pb][:, 0:256], lhsT=wxkv[:, k, ct * 128:(ct + 1) * 128], rhs=memT[:, k, :], start=(k == 0), stop=(k == KT - 1))
                    for k in range(KT)], reads=[B_wxkv, B_memT], writes=[PSB[pb]])
        S.op("dve", lambda e, ct=ct, pb=pb: e.tensor_copy(out=kmemT[:, ct, :], in_=PS[pb][:, 0:256]), reads=[PSB[pb]], writes=[B_kv])
    for mt in range(2):
        for hf in range(2):
            pb = 2 * mt + hf
            S.op("pe", [lambda e, k=k, mt=mt, hf=hf, pb=pb: e.matmul(PS[pb][:, :], lhsT=memT[:, k, mt * 128:(mt + 1) * 128], rhs=wxkv[:, k, D + 512 * hf:D + 512 * hf + 512],
                                                                    start=(k == 0), stop=(k == KT - 1)) for k in range(KT)], reads=[B_wxkv, B_memT], writes=[PSB[pb]])
            S.op("act", lambda e, mt=mt, hf=hf, pb=pb: e.copy(out=vmem[:, mt, 512 * hf:512 * hf + 512], in_=PS[pb][:, :]), reads=[PSB[pb]], writes=[B_kv])
    close_phase()

    x1 = sb("x1", [128, 4, D], F32)
    x1T = sb("x1T", [128, KT, 512], BF16)
    qxT = sb("qxT", [128, KT, 512], BF16)
    oT = sb("oT", [128, KT, 512], BF16)
    pX = [sb(f"pX{i}", [128, 2, 512], BF16) for i in range(2)]
    rl = sb("rl", [128, 512], F32)
    xin = [sb(f"xin{i}", [128, D], F32) for i in range(2)]
    hh = [sb(f"hh{i}", [128, D], F32) for i in range(2)]
    hb = [sb(f"hb{i}", [128, D], BF16) for i in range(2)]
    x2t = [sb(f"x2t{i}", [128, D], F32) for i in range(2)]
    x2T = sb("x2T", [128, KT, 128], F32)
    st6_ = [sb(f"st6_{i}", [128, 2, 6], F32) for i in range(2)]; mv_ = [sb(f"mv_{i}", [128, 2], F32) for i in range(2)]; rstd_ = [sb(f"rstd_{i}", [128, 1], F32) for i in range(2)]
    B_ln = [Buf(f"lnscr{i}") for i in range(2)]; ln_i = [0]
    lg = sb("lg", [128, NT, 32], F32); v8 = sb("v8", [128, NT, 8], F32); e4 = sb("e4", [128, NT, 4], F32); ssum = sb("ssum", [128, NT], F32)
    oh = sb("oh", [128, NT, 4, 32], F32); maskall = sb("maskall", [128, NT, 32], BF16); slt = sb("slt", [128, NT, 32], F32)
    destf = sb("destf", [128, NT, 4], F32)
    B_lg = [Buf(f"lg{t}") for t in range(NT)]
    B_x1 = [Buf(f"x1_{i}") for i in range(4)]; B_x1T, B_qxT, B_oT = Buf("x1T"), Buf("qxT"), Buf("oT")
    B_pX = [Buf(f"pX{i}") for i in range(2)]; B_rl = Buf("rl")
    B_xin = [Buf(f"xin{i}") for i in range(2)]; B_hh = [Buf(f"hh{i}") for i in range(2)]; B_hb = [Buf(f"hb{i}") for i in range(2)]
    B_x2t = [Buf(f"x2t{i}") for i in range(2)]; B_x2T = Buf("x2T"); B_sm = Buf("sm3"); B_mask = Buf("maskall")

    def layer_norm(src, dst, gk, bk, bsrc, bdst):
        i = ln_i[0] % 2; ln_i[0] += 1
        st6, mv, rstd, bl = st6_[i], mv_[i], rstd_[i], B_ln[i]
        S.op("dve", [lambda e, c=c: e.bn_stats(out=st6[:, c, :], in_=src[:, 512 * c:512 * c + 512]) for c in range(2)], reads=[bsrc, bl], writes=[bl])
        S.op("dve", lambda e: e.bn_aggr(out=mv[:, :], in_=st6[:, :, :].rearrange("p a b -> p (a b)")), reads=[bl], writes=[bl])
        S.op("dve", lambda e: e.tensor_scalar(out=rstd[:, :], in0=mv[:, 1:2], scalar1=1e-5, scalar2=None, op0=ALU.add), reads=[bl], writes=[bl])
        S.op("act", lambda e: e.activation(out=rstd[:, :], in_=rstd[:, :], func=AF.Sqrt), reads=[bl], writes=[bl])
        S.op("dve", lambda e: e.reciprocal(out=rstd[:, :], in_=rstd[:, :]), reads=[bl], writes=[bl])
        S.op("dve", lambda e: e.tensor_scalar(out=src, in0=src, scalar1=mv[:, 0:1], scalar2=rstd[:, 0:1], op0=ALU.subtract, op1=ALU.mult), reads=[bsrc, bl], writes=[bsrc])
        S.op("dve", lambda e: e.tensor_tensor(out=src, in0=src, in1=bc[gk][:, :], op=ALU.mult), reads=[bsrc, B_bc[gk]], writes=[bsrc])
        S.op("dve", lambda e: e.tensor_tensor(out=dst, in0=src, in1=bc[bk][:, :], op=ALU.add), reads=[bsrc, B_bc[bk]], writes=[bdst])

    for st in range(4):
        def A1(ti):
            t = 4 * st + ti; sl = t % 2
            S.dma("sp", lambda e, t=t, sl=sl: e.dma_start(out=xin[sl][:, :], in_=xloc[t * 128:(t + 1) * 128, :]), writes=[B_xin[sl]])
            for hf in range(2):
                pb = 2 * sl + hf
                S.op("pe", [lambda e, k=k, t=t, hf=hf, pb=pb: e.matmul(PS[pb][:, :], lhsT=catT[:, k, t * 128:(t + 1) * 128], rhs=wout[:, k, 512 * hf:512 * hf + 512],
                                                                      start=(k == 0), stop=(k == KT - 1)) for k in range(KT)], reads=[B_cat[t], B_wout], writes=[PSB[pb]])
                S.op("dve", lambda e, sl=sl, hf=hf, pb=pb: e.scalar_tensor_tensor(out=hh[sl][:, 512 * hf:512 * hf + 512], in0=xin[sl][:, 512 * hf:512 * hf + 512], scalar=ALPHA,
                                                                                  in1=PS[pb][:, :], op0=ALU.mult, op1=ALU.add), reads=[PSB[pb], B_xin[sl], B_hh[sl]], writes=[B_hh[sl]])
            S.op("dve", lambda e, sl=sl: e.tensor_tensor(out=hh[sl][:, :], in0=hh[sl][:, :], in1=bc["b_out"][:, :], op=ALU.add), reads=[B_hh[sl], B_bc["b_out"]], writes=[B_hh[sl]])
            layer_norm(hh[sl][:, :], x1[:, ti, :], "ln1_g", "ln1_b", B_hh[sl], B_x1[ti])
            S.op("act", lambda e, ti=ti, sl=sl: e.copy(out=hb[sl][:, :], in_=x1[:, ti, :]), reads=[B_x1[ti]], writes=[B_hb[sl]])
        def B1(ti):
            t = 4 * st + ti; sl = t % 2
            bank = 6 + sl
            S.op("pe", [lambda e, k=k, sl=sl, bank=bank: e.transpose(out=PS[bank][:, k * 128:(k + 1) * 128], in_=hb[sl][:, k * 128:(k + 1) * 128], identity=identb[:, :])
                        for k in range(KT)], reads=[B_hb[sl], B_ident], writes=[PSB[bank]])
            S.op("dve", lambda e, ti=ti, bank=bank: e.tensor_copy(out=x1T[:, :, ti * 128:(ti + 1) * 128], in_=PS[bank][:, :].rearrange("p (k n) -> p k n", k=KT)),
                 reads=[PSB[bank]], writes=[B_x1T])
        for ti in range(4):
            A1(ti)
            if ti > 0:
                B1(ti - 1)
        B1(3)
        if stage == "x1" and st == 0:
            dx1 = dram_out("dbg_x1", [512, D])
            S.dma("sp", lambda e: e.dma_start(out=dx1.rearrange("(t p) d -> p t d", p=128), in_=x1[:, :, :]), stream="out", reads=B_x1, writes=[Buf("o")])
            S.finish(); close_phase(); es.close()
            return nc, ["dbg_x1"]
        for ct in range(KT):
            pb = ct % 4
            S.op("pe", [lambda e, k=k, ct=ct, pb=pb: e.matmul(PS[pb][:, :], lhsT=wxq[:, k, ct * 128:(ct + 1) * 128], rhs=x1T[:, k, :], start=(k == 0), stop=(k == KT - 1))
                        for k in range(KT)], reads=[B_x1T, B_wxq], writes=[PSB[pb]])
            eng = "act" if ct % 2 else "dve"
            fn = (lambda e, ct=ct, pb=pb: e.copy(out=qxT[:, ct, :], in_=PS[pb][:, :])) if eng == "act" else (lambda e, ct=ct, pb=pb: e.tensor_copy(out=qxT[:, ct, :], in_=PS[pb][:, :]))
            S.op(eng, fn, reads=[PSB[pb]], writes=[B_qxT])
        for h in range(4):
            sl = h % 2
            for mt in range(2):
                pb = mt
                S.op("pe", [lambda e, cc=cc, h=h, mt=mt, pb=pb: e.matmul(PS[pb][:, :], lhsT=kmemT[:, 2 * h + cc, mt * 128:(mt + 1) * 128], rhs=qxT[:, 2 * h + cc, :],
                                                                        start=(cc == 0), stop=(cc == 1)) for cc in range(2)], reads=[B_kv, B_qxT], writes=[PSB[pb]])
                S.op("act", lambda e, sl=sl, mt=mt, pb=pb: e.activation(out=pX[sl][:, mt, :], in_=PS[pb][:, :], func=AF.Exp, scale=0.0625), reads=[PSB[pb]], writes=[B_pX[sl]])
            S.op("pe", [lambda e, mt=mt, sl=sl: e.matmul(PS[4][:, :], lhsT=onesb[:, :], rhs=pX[sl][:, mt, :], start=(mt == 0), stop=(mt == 1)) for mt in range(2)],
                 reads=[B_pX[sl], B_c3], writes=[PSB[4]])
            S.op("dve", lambda e: e.tensor_copy(out=rl[:, :], in_=PS[4][:, :]), reads=[PSB[4], B_rl], writes=[B_rl])
            S.op("dve", lambda e: e.reciprocal(out=rl[:, :], in_=rl[:, :]), reads=[B_rl], writes=[B_rl])
            for cc in range(2):
                pb = 2 + cc
                S.op("pe", [lambda e, mt=mt, cc=cc, h=h, sl=sl, pb=pb: e.matmul(PS[pb][:, :], lhsT=vmem[:, mt, (2 * h + cc) * 128:(2 * h + cc + 1) * 128], rhs=pX[sl][:, mt, :],
                                                                              start=(mt == 0), stop=(mt == 1)) for mt in range(2)], reads=[B_pX[sl], B_kv], writes=[PSB[pb]])
                S.op("dve", lambda e, cc=cc, h=h, pb=pb: e.tensor_tensor(out=oT[:, 2 * h + cc, :], in0=PS[pb][:, :], in1=rl[:, :], op=ALU.mult), reads=[PSB[pb], B_rl], writes=[B_oT])
        def A2(ti):
            t = 4 * st + ti; sl = t % 2
            for hf in range(2):
                pb = 2 * sl + hf
                S.op("pe", [lambda e, k=k, ti=ti, hf=hf, pb=pb: e.matmul(PS[pb][:, :], lhsT=oT[:, k, ti * 128:(ti + 1) * 128], rhs=wxo[:, k, 512 * hf:512 * hf + 512],
                                                                       start=(k == 0), stop=(k == KT - 1)) for k in range(KT)], reads=[B_oT, B_wxo], writes=[PSB[pb]])
                S.op("dve", lambda e, ti=ti, sl=sl, hf=hf, pb=pb: e.scalar_tensor_tensor(out=hh[sl][:, 512 * hf:512 * hf + 512], in0=x1[:, ti, 512 * hf:512 * hf + 512], scalar=ALPHA,
                                                                                         in1=PS[pb][:, :], op0=ALU.mult, op1=ALU.add), reads=[PSB[pb], B_x1[ti], B_hh[sl]], writes=[B_hh[sl]])
            layer_norm(hh[sl][:, :], x2t[sl][:, :], "ln2_g", "ln2_b", B_hh[sl], B_x2t[sl])
            if stage == "x2a":
                dxa = dram_out("dbg_x2a", [128, D])
                S.dma("sp", lambda e, sl=sl: e.dma_start(out=dxa[:, :], in_=x2t[sl][:, :]), stream="out", reads=[B_x2t[sl]], writes=[Buf("o")])
                S.finish(); close_phase(); es.close()
                return nc, ["dbg_x2a"]
            S.dma("sp", lambda e, t=t, sl=sl: e.dma_start(out=x2f[t * 128:(t + 1) * 128, :], in_=x2t[sl][:, :]), stream=f"x2f{sl}", reads=[B_x2t[sl]], writes=[B_x2f[t]])
        def B2(ti):
            t = 4 * st + ti; sl = t % 2
            for hf in range(2):
                S.op("pe", [lambda e, k=k, sl=sl, hf=hf: e.transpose(out=PS[4 + hf][:, k * 128:(k + 1) * 128], in_=x2t[sl][:, (4 * hf + k) * 128:(4 * hf + k + 1) * 128], identity=identf[:, :])
                            for k in range(4)], reads=[B_x2t[sl], B_identf], writes=[PSB[4 + hf]])
                eng = "act" if hf else "dve"
                fn = (lambda e, hf=hf: e.copy(out=x2T[:, 4 * hf:4 * hf + 4, :], in_=PS[4 + hf][:, :].rearrange("p (k n) -> p k n", k=4))) if eng == "act" else \
                     (lambda e, hf=hf: e.tensor_copy(out=x2T[:, 4 * hf:4 * hf + 4, :], in_=PS[4 + hf][:, :].rearrange("p (k n) -> p k n", k=4)))
                S.op(eng, fn, reads=[PSB[4 + hf]], writes=[B_x2T])
            S.op("pe", [lambda e, k=k: e.matmul(PS[4][:, 0:32], lhsT=x2T[:, k, :], rhs=wr[:, k, :], start=(k == 0), stop=(k == KT - 1)) for k in range(KT)],
                 reads=[B_x2T, B_wr], writes=[PSB[4]])
            S.op("dve", lambda e, t=t: e.tensor_tensor(out=lg[:, t, :], in0=PS[4][:, 0:32], in1=brbc[:, :], op=ALU.add), reads=[PSB[4], B_br], writes=[B_lg[t]])
            S.op("dve", lambda e, t=t: e.max(out=v8[:, t, :], in_=lg[:, t, :]), reads=[B_lg[t]], writes=[B_lg[t]])
        for ti in range(4):
            A2(ti)
            if ti > 0:
                B2(ti - 1)
        B2(3)
    dsm = dict(reads=[B_sm], writes=[B_sm])
    S.op("dve", lambda e: e.tensor_tensor(out=e4[:, :, :], in0=v8[:, :, 0:4], in1=v8[:, :, 0:1].to_broadcast([128, NT, 4]), op=ALU.subtract), reads=B_lg + [B_sm], writes=[B_sm])
    S.op("act", lambda e: e.activation(out=e4[:, :, :], in_=e4[:, :, :], func=AF.Exp), **dsm)
    S.op("dve", lambda e: e.reduce_sum(out=ssum[:, :], in_=e4[:, :, :], axis=AX.X), **dsm)
    S.op("dve", lambda e: e.reciprocal(out=ssum[:, :], in_=ssum[:, :]), **dsm)
    S.op("dve", lambda e: e.tensor_tensor(out=gates[:, :, :], in0=e4[:, :, :], in1=ssum[:, :].unsqueeze(2).to_broadcast([128, NT, 4]), op=ALU.mult), reads=[B_sm], writes=[B_sm] + B_route)
    S.op("dve", lambda e: e.tensor_tensor(out=oh[:, :, :, :], in0=lg[:, :, :].unsqueeze(2).to_broadcast([128, NT, 4, 32]),
                                          in1=v8[:, :, 0:4].unsqueeze(3).to_broadcast([128, NT, 4, 32]), op=ALU.is_equal), reads=B_lg + [B_sm], writes=[B_sm])
    S.op("dve", lambda e: e.tensor_tensor(out=slt[:, :, :], in0=oh[:, :, 0, :], in1=oh[:, :, 1, :], op=ALU.add), **dsm)
    S.op("dve", lambda e: e.tensor_tensor(out=slt[:, :, :], in0=slt[:, :, :], in1=oh[:, :, 2, :], op=ALU.add), **dsm)
    S.op("dve", lambda e: e.tensor_tensor(out=maskall[:, :, :], in0=slt[:, :, :], in1=oh[:, :, 3, :], op=ALU.add), reads=[B_sm, B_mask], writes=[B_mask])
    fns = []
    for t in range(NT):
        fns.append(lambda e, t=t: e.matmul(PS[5][:, 32 * t:32 * t + 32], lhsT=trib[:, :], rhs=maskall[:, t, :], start=True, stop=(t == 0)))
        for tp in range(t):
            fns.append(lambda e, t=t, tp=tp: e.matmul(PS[5][:, 32 * t:32 * t + 32], lhsT=onesb[:, :], rhs=maskall[:, tp, :], start=False, stop=(tp == t - 1)))
    S.op("pe", fns, reads=[B_mask, B_tri, B_c3], writes=[PSB[5]])
    S.op("dve", lambda e: e.tensor_scalar(out=slt[:, :, :], in0=PS[5][:, :].rearrange("p (t e) -> p t e", t=NT), scalar1=float(CAP - 1), scalar2=None, op0=ALU.min),
         reads=[PSB[5], B_sm], writes=[B_sm])
    S.op("dve", lambda e: e.tensor_tensor(out=slt[:, :, :], in0=slt[:, :, :], in1=ecap[:, :].unsqueeze(1).to_broadcast([128, NT, 32]), op=ALU.add), reads=[B_ecap, B_sm], writes=[B_sm])
    S.op("dve", lambda e: e.tensor_tensor(out=oh[:, :, :, :], in0=oh[:, :, :, :], in1=slt[:, :, :].unsqueeze(2).to_broadcast([128, NT, 4, 32]), op=ALU.mult), **dsm)
    S.op("dve", lambda e: e.reduce_sum(out=destf[:, :, :], in_=oh[:, :, :, :], axis=AX.X), **dsm)
    S.op("dve", lambda e: e.tensor_copy(out=dest[:, :, :], in_=destf[:, :, :]), reads=[B_sm], writes=[B_sm] + B_route)
    for t in range(NT):
        sl = t % 2
        S.dma("sp", lambda e, t=t, sl=sl: e.dma_start(out=xin[sl][:, :], in_=x2f[t * 128:(t + 1) * 128, :]), reads=[B_x2f[t]], writes=[B_xin[sl]])
        S.op("act", lambda e, sl=sl: e.copy(out=hb[sl][:, :], in_=xin[sl][:, :]), reads=[B_xin[sl]], writes=[B_hb[sl]])
        for k in range(4):
            S.dma("pool", lambda e, t=t, k=k, sl=sl: e.indirect_dma_start(out=xg[:, :], out_offset=bass.IndirectOffsetOnAxis(dest[:, t, k:k + 1], 0),
                                                                          in_=hb[sl][:, :], in_offset=None), stream=f"xgsc{sl}", reads=[B_hb[sl], B_route[t]], writes=[B_xg])
    if stage == "x2":
        dx2 = dram_out("dbg_x2", [TOK, D]); dg = dram_out("dbg_gates", [128, NT, 4]); dd = dram_out("dbg_dest", [128, NT, 4], I32)
        S.dma("sp", lambda e: e.dma_start(out=dx2[:, :], in_=x2f[:, :]), stream="out", reads=B_x2f, writes=[Buf("o")])
        S.dma("sp", lambda e: e.dma_start(out=dg.rearrange("p a b -> p (a b)"), in_=gates[:, :, :].rearrange("p a b -> p (a b)")), stream="out", reads=B_route, writes=[Buf("o1")])
        S.dma("sp", lambda e: e.dma_start(out=dd.rearrange("p a b -> p (a b)"), in_=dest[:, :, :].rearrange("p a b -> p (a b)")), stream="out", reads=B_route, writes=[Buf("o2")])
        dxg = dram_out("dbg_xg", [32 * CAP, D])
        S.dma("pool", lambda e: e.dma_start(out=dxg[:, :], in_=xg[:, :]), stream="outsw", reads=[B_xg], writes=[Buf("o3")])
        S.finish(); close_phase(); es.close()
        return nc, ["dbg_x2", "dbg_gates", "dbg_dest", "dbg_xg"]
    close_phase()
    we1_d = dram_in("w_e1", [32, D, 2 * D]); be1_d = dram_in("b_e1", [32, 2 * D])
    we2_d = dram_in("w_e2", [32, D, D]); be2_d = dram_in("b_e2", [32, D])
    n_exp = 32 if stage not in ("moe2",) else 2
    B_yg = Buf("yg")
    open_phase()
    b1all = sb("b1all", [32, 2 * D], F32)
    b1fm = sb("b1fm", [128, 8, 2, 32], F32)
    B_b1a, B_b1 = Buf("b1all"), Buf("b1fm")
    S.dma("sp", lambda e: e.dma_start(out=b1all[:, :], in_=be1_d[:, :]), writes=[B_b1a])
    S.op("pe", [lambda e, ft=ft, two=two: e.transpose(out=PS[5][:, 32 * (2 * ft + two):32 * (2 * ft + two) + 32], in_=b1all[:, 256 * ft + two:256 * (ft + 1):2],
                                                     identity=identf[0:32, 0:32]) for ft in range(8) for two in range(2)], reads=[B_b1a, B_identf], writes=[PSB[5]])
    S.op("dve", lambda e: e.tensor_copy(out=b1fm[:, :, :, :].rearrange("p a b c -> p (a b c)"), in_=PS[5][:, :]), reads=[PSB[5]], writes=[B_b1])
    w1 = [sb(f"w1_{i}", [128, KT, 2 * D], BF16) for i in range(2)]
    w2 = [sb(f"w2_{i}", [128, KT, D], BF16) for i in range(2)]
    b2bc = [sb(f"b2bc{i}", [128, D], F32) for i in range(2)]
    B_w1 = [Buf(f"w1_{i}") for i in range(2)]; B_w2 = [Buf(f"w2_{i}") for i in range(2)]; B_b2 = [Buf(f"b2bc{i}") for i in range(2)]
    NS = CAP // 128
    xgl = sb("xgl", [128, NS, D], BF16)
    xgT2 = [sb("xgT", [128, KT, CAP], BF16)] * 2; actT2 = [sb("actT", [128, 8, CAP], BF16)] * 2
    B_xgl = Buf("xgl"); B_xgT2 = [Buf("xgT")] * 2; B_actT2 = [Buf("actT")] * 2
    NWST = 4
    wst = [sb(f"wst{i}", [128, D], F32) for i in range(NWST)]
    B_wst = [Buf(f"wst{i}") for i in range(NWST)]
    gg_ = [sb(f"sg_g{i}", [128, CAP], F32) for i in range(2)]; ll_ = [sb(f"sg_l{i}", [128, CAP], F32) for i in range(2)]; ss_ = [sb(f"sg_s{i}", [128, CAP], F32) for i in range(2)]
    B_sg = [Buf(f"sg{i}") for i in range(2)]
    ysb = [sb(f"ysb{i}", [128, D], F32) for i in range(2)]
    B_ysb = [Buf(f"ysb{i}") for i in range(2)]

    wstep = [0]

    def load_step(e_, j):
        sl = e_ % 2
        i = wstep[0] % NWST; wstep[0] += 1
        if j < 16:
            k, hf = j // 2, j % 2
            S.dma("sp", lambda e, i=i, e_=e_, k=k, hf=hf: e.dma_start(out=wst[i][:, :], in_=we1_d[e_][k * 128:(k + 1) * 128, hf * D:(hf + 1) * D]), writes=[B_wst[i]])
            S.op("act", lambda e, i=i, sl=sl, k=k, hf=hf: e.copy(out=w1[sl][:, k, hf * D:(hf + 1) * D], in_=wst[i][:, :]), reads=[B_wst[i], B_w1[sl]], writes=[B_w1[sl]])
        else:
            k = j - 16
            S.dma("sp", lambda e, i=i, e_=e_, k=k: e.dma_start(out=wst[i][:, :], in_=we2_d[e_][k * 128:(k + 1) * 128, :]), writes=[B_wst[i]])
            S.op("act", lambda e, i=i, sl=sl, k=k: e.copy(out=w2[sl][:, k, :], in_=wst[i][:, :]), reads=[B_wst[i], B_w2[sl]], writes=[B_w2[sl]])

    def load_b2(e_):
        sl = e_ % 2
        S.dma("pool", lambda e, sl=sl, e_=e_: e.dma_start(out=b2bc[sl][:, :], in_=be2_d[e_].partition_broadcast(128)), writes=[B_b2[sl]])

    def load_expert(e_):
        for j in range(24):
            load_step(e_, j)
        load_b2(e_)

    load_expert(0)
    iy = 0
    for e_ in range(n_exp):
        sl = e_ % 2
        nxt = e_ + 1 if e_ + 1 < n_exp else None
        if nxt is not None:
            load_b2(nxt)
        xgT, actT, B_xgT, B_actT = xgT2[sl], actT2[sl], B_xgT2[sl], B_actT2[sl]
        S.dma("sp", lambda e, e_=e_: e.dma_start(out=xgl[:, :, :], in_=xg[e_ * CAP:(e_ + 1) * CAP, :].rearrange("(s p) d -> p s d", p=128)), reads=[B_xg], writes=[B_xgl])
        for st_ in range(NS):
            bank = 6 + st_ % 2
            S.op("pe", [lambda e, k=k, st_=st_, bank=bank: e.transpose(out=PS[bank][:, k * 128:(k + 1) * 128], in_=xgl[:, st_, k * 128:(k + 1) * 128], identity=identb[:, :])
                        for k in range(KT)], reads=[B_xgl, B_ident], writes=[PSB[bank]])
            eng = "act" if st_ % 2 else "dve"
            fn = (lambda e, st_=st_, bank=bank, xgT=xgT: e.copy(out=xgT[:, :, st_ * 128:(st_ + 1) * 128], in_=PS[bank][:, :].rearrange("p (k n) -> p k n", k=KT))) if eng == "act" else \
                 (lambda e, st_=st_, bank=bank, xgT=xgT: e.tensor_copy(out=xgT[:, :, st_ * 128:(st_ + 1) * 128], in_=PS[bank][:, :].rearrange("p (k n) -> p k n", k=KT)))
            S.op(eng, fn, reads=[PSB[bank]], writes=[B_xgT])
        for ft in range(8):
            if nxt is not None:
                for j in range(3 * ft, 3 * ft + 3):
                    load_step(nxt, j)
            s2 = ft % 2
            pg, pl = 2 * s2, 2 * s2 + 1
            for two, pb in ((0, pg), (1, pl)):
                S.op("pe", [lambda e, k=k, ft=ft, two=two, pb=pb, sl=sl, xgT=xgT: e.matmul(PS[pb][:, 0:CAP], lhsT=w1[sl][:, k, 256 * ft + two:256 * (ft + 1):2], rhs=xgT[:, k, :],
                                                                                 start=(k == 0), stop=(k == KT - 1)) for k in range(KT)], reads=[B_w1[sl], B_xgT], writes=[PSB[pb]])
            g_, l_, s_ = gg_[s2][:, :], ll_[s2][:, :], ss_[s2][:, :]
            S.op("dve", lambda e, g_=g_, pg=pg, ft=ft, e_=e_: e.tensor_scalar(out=g_, in0=PS[pg][:, 0:CAP], scalar1=b1fm[:, ft, 0, e_:e_ + 1], scalar2=7.0, op0=ALU.add, op1=ALU.min),
                 reads=[PSB[pg], B_b1, B_sg[s2]], writes=[B_sg[s2]])
            S.op("act", lambda e, g_=g_, s_=s_: e.activation(out=s_, in_=g_, func=AF.Sigmoid, scale=1.702), reads=[B_sg[s2]], writes=[B_sg[s2]])
            S.op("dve", lambda e, l_=l_, pl=pl, ft=ft, e_=e_: e.tensor_scalar(out=l_, in0=PS[pl][:, 0:CAP], scalar1=b1fm[:, ft, 1, e_:e_ + 1], scalar2=7.0, op0=ALU.add, op1=ALU.min),
                 reads=[PSB[pl], B_b1, B_sg[s2]], writes=[B_sg[s2]])
            S.op("dve", lambda e, l_=l_: e.tensor_scalar(out=l_, in0=l_, scalar1=-7.0, scalar2=1.0, op0=ALU.max, op1=ALU.add), reads=[B_sg[s2]], writes=[B_sg[s2]])
            S.op("dve", lambda e, g_=g_, s_=s_: e.tensor_tensor(out=g_, in0=g_, in1=s_, op=ALU.mult), reads=[B_sg[s2]], writes=[B_sg[s2]])
            S.op("dve", lambda e, g_=g_, l_=l_, ft=ft, actT=actT: e.tensor_tensor(out=actT[:, ft, :], in0=g_, in1=l_, op=ALU.mult), reads=[B_sg[s2], B_actT], writes=[B_actT])
        for st_ in range(NS):
            ys = iy % 2; iy += 1
            for hf in range(2):
                pb = 4 + hf
                S.op("pe", [lambda e, ft=ft, st_=st_, hf=hf, pb=pb, sl=sl, actT=actT: e.matmul(PS[pb][:, :], lhsT=actT[:, ft, st_ * 128:(st_ + 1) * 128], rhs=w2[sl][:, ft, 512 * hf:512 * hf + 512],
                                                                                   start=(ft == 0), stop=(ft == 7)) for ft in range(8)], reads=[B_actT, B_w2[sl]], writes=[PSB[pb]])
                S.op("dve", lambda e, ys=ys, hf=hf, pb=pb, sl=sl: e.tensor_tensor(out=ysb[ys][:, 512 * hf:512 * hf + 512], in0=PS[pb][:, :], in1=b2bc[sl][:, 512 * hf:512 * hf + 512], op=ALU.add),
                     reads=[PSB[pb], B_b2[sl], B_ysb[ys]], writes=[B_ysb[ys]])
            r0 = e_ * CAP + st_ * 128
            S.dma("pool", lambda e, ys=ys, r0=r0: e.dma_start(out=yg[r0:r0 + 128, :], in_=ysb[ys][:, :]), stream=f"ygst{ys}", reads=[B_ysb[ys]], writes=[B_yg])
    close_phase()

    out_d = dram_out("out", [TOK, D])
    open_phase()
    g3 = sb("ln3g", [128, D], F32); b3 = sb("ln3b", [128, D], F32)
    B_g3, B_b3 = Buf("ln3g"), Buf("ln3b")
    S.dma("sp", lambda e: e.dma_start(out=g3[:, :], in_=ln_d["ln3_g"].partition_broadcast(128)), writes=[B_g3])
    S.dma("sp", lambda e: e.dma_start(out=b3[:, :], in_=ln_d["ln3_b"].partition_broadcast(128)), writes=[B_b3])
    ygat = [[sb(f"ygat{i}_{k}", [128, D], F32) for k in range(4)] for i in range(2)]
    B_ygat = [[Buf(f"ygat{i}_{k}") for k in range(4)] for i in range(2)]
    x2r = [sb(f"x2r{i}", [128, D], F32) for i in range(2)]; B_x2r = [Buf(f"x2r{i}") for i in range(2)]
    acc = [sb(f"acc{i}", [128, D], F32) for i in range(2)]; B_acc = [Buf(f"acc{i}") for i in range(2)]
    oo = [sb(f"oo{i}", [128, D], F32) for i in range(2)]; B_oo = [Buf(f"oo{i}") for i in range(2)]
    st6b = sb("st6b", [128, 2, 6], F32); mvb = sb("mvb", [128, 2], F32); rstdb = sb("rstdb", [128, 1], F32)
    B_sm5 = Buf("sm5")
    for t in range(NT):
        sl = t % 2
        S.dma("sp", lambda e, t=t, sl=sl: e.dma_start(out=x2r[sl][:, :], in_=x2f[t * 128:(t + 1) * 128, :]), reads=[B_x2f[t]], writes=[B_x2r[sl]])
        for k in range(4):
            S.dma("pool", lambda e, t=t, k=k, sl=sl: e.indirect_dma_start(out=ygat[sl][k][:, :], out_offset=None, in_=yg[:, :],
                                                                          in_offset=bass.IndirectOffsetOnAxis(dest[:, t, k:k + 1], 0)), reads=[B_yg, B_route[t]], writes=[B_ygat[sl][k]])
        a_ = acc[sl][:, :]
        S.op("dve", lambda e, a_=a_, t=t, sl=sl: e.tensor_scalar(out=a_, in0=ygat[sl][0][:, :], scalar1=gates[:, t, 0:1], scalar2=None, op0=ALU.mult),
             reads=[B_ygat[sl][0], B_route[t], B_acc[sl]], writes=[B_acc[sl]])
        for k in range(1, 4):
            S.op("dve", lambda e, a_=a_, t=t, k=k, sl=sl: e.scalar_tensor_tensor(out=a_, in0=ygat[sl][k][:, :], scalar=gates[:, t, k:k + 1], in1=a_, op0=ALU.mult, op1=ALU.add),
                 reads=[B_ygat[sl][k], B_route[t], B_acc[sl]], writes=[B_acc[sl]])
        S.op("dve", lambda e, a_=a_, sl=sl: e.scalar_tensor_tensor(out=a_, in0=x2r[sl][:, :], scalar=ALPHA, in1=a_, op0=ALU.mult, op1=ALU.add),
             reads=[B_x2r[sl], B_acc[sl]], writes=[B_acc[sl]])
        S.op("dve", [lambda e, c=c, a_=a_: e.bn_stats(out=st6b[:, c, :], in_=a_[:, 512 * c:512 * c + 512]) for c in range(2)], reads=[B_acc[sl], B_sm5], writes=[B_sm5])
        S.op("dve", lambda e: e.bn_aggr(out=mvb[:, :], in_=st6b[:, :, :].rearrange("p a b -> p (a b)")), reads=[B_sm5], writes=[B_sm5])
        S.op("dve", lambda e: e.tensor_scalar(out=rstdb[:, :], in0=mvb[:, 1:2], scalar1=1e-5, scalar2=None, op0=ALU.add), reads=[B_sm5], writes=[B_sm5])
        S.op("act", lambda e: e.activation(out=rstdb[:, :], in_=rstdb[:, :], func=AF.Sqrt), reads=[B_sm5], writes=[B_sm5])
        S.op("dve", lambda e: e.reciprocal(out=rstdb[:, :], in_=rstdb[:, :]), reads=[B_sm5], writes=[B_sm5])
        S.op("dve", lambda e, a_=a_: e.tensor_scalar(out=a_, in0=a_, scalar1=mvb[:, 0:1], scalar2=rstdb[:, 0:1], op0=ALU.subtract, op1=ALU.mult), reads=[B_acc[sl], B_sm5], writes=[B_acc[sl]])
        S.op("dve", lambda e, a_=a_: e.tensor_tensor(out=a_, in0=a_, in1=g3[:, :], op=ALU.mult), reads=[B_acc[sl], B_g3], writes=[B_acc[sl]])
        S.op("dve", lambda e, a_=a_, sl=sl: e.tensor_tensor(out=oo[sl][:, :], in0=a_, in1=b3[:, :], op=ALU.add), reads=[B_acc[sl], B_b3, B_oo[sl]], writes=[B_oo[sl]])
        S.dma("sp", lambda e, t=t, sl=sl: e.dma_start(out=out_d[t * 128:(t + 1) * 128, :], in_=oo[sl][:, :]), stream=f"out{sl}", reads=[B_oo[sl]], writes=[Buf(f"o_{t}")])
    S.finish()
    close_phase()
    es.close()
    return nc, ["out"]


def nc_ap(nc, name):
    return _DRAM[name]


def _rope_tables(pos):
    half = 32
    inv = (10000.0 ** (-np.arange(half, dtype=np.float32) * np.float32(2.0 / 64))).astype(np.float32)
    ang = (pos.astype(np.float32)[:, None] * inv[None, :]).astype(np.float32)
    cos = np.cos(ang.astype(np.float64)).astype(np.float32).T
    sin = np.sin(ang.astype(np.float64)).astype(np.float32).T
    return np.ascontiguousarray(np.tile(cos, (4, 1))), np.ascontiguousarray(np.tile(sin, (4, 1)))


def prep_core_inputs(inputs, c, stage_needs_s5=True):
    b, j = c // 4, c % 4
    t0 = j * TOK
    x = inputs["x"]
    m = {}
    m["xloc"] = np.ascontiguousarray(x[b, t0:t0 + TOK])
    m["xhalo"] = np.ascontiguousarray(x[b, t0 - HALO:t0]) if j > 0 else np.zeros((HALO, D), np.float32)
    pos = np.arange(t0 - HALO, t0 + TOK)
    m["ropecos"], m["ropesin"] = _rope_tables(pos)
    kj = np.arange(128)[:, None]
    qi = np.arange(128)[None, :]
    prev = (kj > qi).astype(np.float32)
    own = (kj <= qi).astype(np.float32)
    first = prev if j > 0 else np.zeros_like(prev)
    m["masks"] = np.ascontiguousarray(np.stack([prev, own, first], axis=1))
    m["ident"] = np.eye(128, dtype=np.float32)
    m["w_in"] = np.ascontiguousarray(inputs["w_in"][0])
    m["b_in"] = np.ascontiguousarray(inputs["b_in"][0])
    m["attn_sinks"] = np.ascontiguousarray(inputs["attn_sinks"][0])
    if stage_needs_s5:
        xp = np.zeros((3 * TOK, D), np.float32)
        sv = np.zeros((128, 4), np.float32)
        for k in range(3):
            sj = j - 3 + k
            if sj >= 0:
                xp[k * TOK:(k + 1) * TOK] = x[b, sj * TOK:(sj + 1) * TOK]
                sv[:, k] = 1.0
        m["xprev"], m["segvalid"] = xp, sv
        for k in ("s5_lambda_re", "s5_lambda_im", "s5_log_dt", "s5_b_re", "s5_b_im", "s5_c_re", "s5_c_im", "s5_d", "s5_w_glu", "s5_b_glu"):
            m[k] = np.ascontiguousarray(inputs[k][0])
        sp = np.arange(128) // 16
        so = np.arange(256) // 16
        for k in ("w_out", "b_out", "ln1_g", "ln1_b", "ln2_g", "ln2_b", "ln3_g", "ln3_b", "w_xq", "w_xkv", "w_xo", "w_router", "b_router", "w_e1", "b_e1", "w_e2", "b_e2"):
            m[k] = np.ascontiguousarray(inputs[k][0])
        m["mem"] = np.ascontiguousarray(inputs["mem"][b])
        m["tri"] = np.triu(np.ones((128, 128), np.float32), 1)
        m["ecap"] = np.ascontiguousarray(np.tile((np.arange(32, dtype=np.float32) * 384.0)[None, :], (128, 1)))
        m["cmask"] = np.ascontiguousarray(np.stack([(so[None, :] >= (8 * kh + sp)[:, None]) for kh in range(2)], axis=1).astype(np.float32))
    return m


def kernel(**inputs):
    inputs = {k: np.asarray(v) for k, v in inputs.items()}
    nc, _ = build_nc("full")
    in_maps = [prep_core_inputs(inputs, c) for c in range(NCORES)]
    res = run_bass_kernel_spmd(nc, in_maps, core_ids=list(range(NCORES)))
    out = np.zeros((2, 8192, D), np.float32)
    for c in range(NCORES):
        out[c // 4, (c % 4) * TOK:(c % 4 + 1) * TOK] = res.results[c]["out"]
    return out
```

```python
from contextlib import ExitStack

import numpy as np
import concourse.bass as bass
import concourse.mybir as mybir
from concourse.bass_utils import run_bass_kernel_spmd

F32 = mybir.dt.float32
BF16 = mybir.dt.bfloat16
I32 = mybir.dt.int32
ALU = mybir.AluOpType
AF = mybir.ActivationFunctionType
AX = mybir.AxisListType

NCORES = 8
_DRAM = {}
TOK = 2048
NT = TOK // 128
HALO = 128
TH = TOK + HALO
D = 1024
KT = D // 128


class Buf:
    __slots__ = ("name", "lw", "rd")

    def __init__(self, name):
        self.name = name
        self.lw = None
        self.rd = {}


class Sched:
    ENG = ("pe", "act", "dve", "pool", "sp")

    def __init__(self, nc, es):
        self.nc = nc
        self.es = es
        self.sems = {e: es.enter_context(nc.semaphore("sem_" + e)) for e in self.ENG}
        self.cnt = {e: 0 for e in self.ENG}
        self.prog = {e: [] for e in self.ENG}
        self.seen = {e: {} for e in self.ENG}
        self.scnt = {}

    def _sem(self, key):
        if key not in self.sems:
            self.sems[key] = self.es.enter_context(self.nc.semaphore("sem_" + key))
            self.scnt[key] = 0
        return self.sems[key]

    def _waits(self, eng, reads, writes):
        deps = {}

        def add(tok):
            if tok is None:
                return
            key, val = tok
            if key == eng and eng == "pe":
                return
            if deps.get(key, 0) < val:
                deps[key] = val

        for b in reads:
            add(b.lw)
        for b in writes:
            add(b.lw)
            for t in b.rd.items():
                add(t)
        out = []
        for key, val in deps.items():
            if self.seen[eng].get(key, 0) >= val:
                continue
            self.seen[eng][key] = val
            out.append((self._sem(key), val))
        return out

    def _commit(self, tok, reads, writes):
        for b in writes:
            b.lw = tok
            b.rd = {}
        for b in reads:
            if b not in writes:
                if b.rd.get(tok[0], 0) < tok[1]:
                    b.rd[tok[0]] = tok[1]

    def op(self, eng, fns, reads=(), writes=()):
        if callable(fns):
            fns = [fns]
        waits = self._waits(eng, reads, writes)
        self.cnt[eng] += 1
        assert self.cnt[eng] < 60000, eng
        self.prog[eng].append((waits, fns, (self.sems[eng], 1)))
        self._commit((eng, self.cnt[eng]), reads, writes)

    def dma(self, eng, fn, stream=None, reads=(), writes=()):
        if stream is None:
            stream = "d_" + writes[0].name
        stream = stream + ("_sw" if eng == "pool" else "_hw")
        sem = self._sem(stream)
        waits = self._waits(eng, reads, writes)
        self.scnt[stream] += 16
        assert self.scnt[stream] < 60000, stream
        self.prog[eng].append((waits, [fn], (sem, 16)))
        self._commit((stream, self.scnt[stream]), reads, writes)

    def wait_all(self, eng, bufs):
        waits = self._waits(eng, bufs, ())
        self.prog[eng].append((waits, [], None))

    def barrier(self):
        toks = [(e, self.cnt[e]) for e in self.ENG if self.cnt[e]] + [(k, v) for k, v in self.scnt.items() if v]
        for eng in self.ENG:
            waits = []
            for key, val in toks:
                if key == eng or self.seen[eng].get(key, 0) >= val:
                    continue
                self.seen[eng][key] = val
                waits.append((self._sem(key), val))
            self.prog[eng].append((waits, [], None))

    def finish(self, eng="sp", prefix="out"):
        fins = []
        for key, val in self.scnt.items():
            if key.startswith(prefix) and val:
                b = Buf("fin_" + key)
                b.lw = (key, val)
                fins.append(b)
        self.wait_all(eng, fins)

    def emit(self):
        nc = self.nc
        with nc.Block() as block:
            table = (("pe", block.tensor), ("act", block.scalar), ("dve", block.vector),
                     ("pool", block.gpsimd), ("sp", block.sync))
            for eng, deco in table:
                def body(e, eng=eng):
                    for waits, fns, inc in self.prog[eng]:
                        for s, v in waits:
                            e.wait_ge(s, v)
                        for f in fns[:-1]:
                            f(e)
                        if fns:
                            fns[-1](e).then_inc(inc[0], inc[1])
                deco(body)
        self.prog = {e: [] for e in self.ENG}


def build_nc(stage="full"):
    nc = bass.Bass("TRN2", target_bir_lowering=False)
    es = ExitStack()
    S = Sched(nc, es)

    _DRAM.clear()

    def dram_in(name, shape, dt=F32):
        _DRAM[name] = nc.dram_tensor(name, list(shape), dt, kind="ExternalInput").ap()
        return _DRAM[name]

    def dram_out(name, shape, dt=F32):
        return nc.dram_tensor(name, list(shape), dt, kind="ExternalOutput").ap()

    scope = [es]

    def sb(name, shape, dt):
        return scope[-1].enter_context(nc.sbuf_tensor(name, list(shape), dt))

    def open_phase():
        ph = ExitStack()
        scope.append(ph)
        return ph

    def close_phase():
        S.barrier()
        S.emit()
        scope.pop().close()

    xloc = dram_in("xloc", [TOK, D])
    xhalo = dram_in("xhalo", [HALO, D])
    cos_d = dram_in("ropecos", [128, TH])
    sin_d = dram_in("ropesin", [128, TH])
    masks_d = dram_in("masks", [128, 3, 128])
    ident_d = dram_in("ident", [128, 128])
    w_in = dram_in("w_in", [D, 1280])
    b_in = dram_in("b_in", [1280])
    sinks_d = dram_in("attn_sinks", [8])

    PS = [es.enter_context(nc.psum_tensor(f"ps{i}", [128, 512], F32)) for i in range(6)]
    PS += [es.enter_context(nc.psum_tensor(f"ps{i}", [128, 1024], BF16)) for i in (6, 7)]
    PSB = [Buf(f"ps{i}") for i in range(8)]

    identb = sb("identb", [128, 128], BF16)
    identf = sb("identf", [128, 128], F32)
    catT = sb("catT", [128, 8, TOK], BF16)
    open_phase()
    cosT = sb("cosT", [128, TH], F32)
    sinT = sb("sinT", [128, TH], F32)
    maskb = sb("maskb", [128, 3, 128], BF16)
    B_ident, B_mask, B_cos, B_sin = Buf("ident"), Buf("mask"), Buf("cos"), Buf("sin")
    S.dma("pool", lambda e: e.dma_start(out=identb[:, :], in_=ident_d[:, :]), writes=[B_ident])
    B_identf = Buf("identf")
    S.dma("sp", lambda e: e.dma_start(out=identf[:, :], in_=ident_d[:, :]), writes=[B_identf])
    S.dma("pool", lambda e: e.dma_start(out=maskb[:, :, :], in_=masks_d[:, :, :]), writes=[B_mask])
    S.dma("sp", lambda e: e.dma_start(out=cosT[:, :], in_=cos_d[:, :]), writes=[B_cos])
    S.dma("sp", lambda e: e.dma_start(out=sinT[:, :], in_=sin_d[:, :]), writes=[B_sin])

    win = sb("win", [128, KT, 1280], BF16)
    B_win = Buf("win")
    w_in_v = w_in.rearrange("(kt p) f -> p kt f", p=128)
    for h in range(2):
        S.dma("pool", lambda e, h=h: e.dma_start(out=win[:, 4 * h:4 * h + 4, :], in_=w_in_v[:, 4 * h:4 * h + 4, :]),
              writes=[B_win])
    wkb = sb("wkb", [128, KT, 128], BF16)
    wrot = sb("wrot", [128, KT, 6, 128], BF16)
    B_wrot = Buf("wrot")

    def src_col(m, half):
        if m < 4:
            return 128 * m + 64 * half
        if m == 4:
            return 512 + 64 * half
        return 512 + 64 * (1 - half)

    for half in range(2):
        c0 = src_col(5, half)
        S.op("dve", lambda e, half=half, c0=c0: e.tensor_copy(out=wkb[:, :, 64 * half:64 * half + 64], in_=win[:, :, c0:c0 + 64]),
             reads=[B_win], writes=[B_wrot])
    for m in range(6):
        for half in range(2):
            c0 = src_col(m, half)
            d0 = 64 * half
            S.op("dve", lambda e, m=m, c0=c0, d0=d0: e.tensor_scalar(
                out=wrot[:, :, m, d0:d0 + 32], in0=win[:, :, c0 + 32:c0 + 64], scalar1=-1.0, scalar2=None, op0=ALU.mult),
                reads=[B_win], writes=[B_wrot])
            S.op("dve", lambda e, m=m, c0=c0, d0=d0: e.tensor_copy(out=wrot[:, :, m, d0 + 32:d0 + 64], in_=win[:, :, c0:c0 + 32]),
                 reads=[B_win], writes=[B_wrot])

    bfm = sb("bfm", [128, 10], F32)
    bqk = sb("bqk", [128, 6], F32)
    bqkr = sb("bqkr", [128, 6], F32)
    bvbc = sb("bvbc", [128, 128], F32)
    B_bias, B_bfm, B_bvbc = Buf("bias"), Buf("bfm"), Buf("bvbc")
    brow = sb("brow", [10, 128], F32)
    B_brow = Buf("brow")
    S.dma("sp", lambda e: e.dma_start(out=brow[:, :], in_=b_in.rearrange("(t p) -> t p", p=128)), writes=[B_brow])
    S.op("pe", lambda e: e.transpose(out=PS[5][:, 0:10], in_=brow[:, :], identity=identf[0:10, 0:10]),
         reads=[B_brow, B_identf], writes=[PSB[5]])
    S.op("dve", lambda e: e.tensor_copy(out=bfm[:, :], in_=PS[5][:, 0:10]), reads=[PSB[5]], writes=[B_bfm])
    S.dma("sp", lambda e: e.dma_start(out=bvbc[:, :], in_=b_in[640:768].partition_broadcast(128)), writes=[B_bvbc])
    S.op("dve", lambda e: e.tensor_copy(out=bqk[:, 0:5], in_=bfm[:, 0:5]), reads=[B_bfm], writes=[B_bias])
    S.op("dve", lambda e: e.tensor_copy(out=bqk[0:64, 5:6], in_=bfm[64:128, 4:5]), reads=[B_bfm, B_bias], writes=[B_bias])
    S.op("dve", lambda e: e.tensor_copy(out=bqk[64:128, 5:6], in_=bfm[0:64, 4:5]), reads=[B_bfm, B_bias], writes=[B_bias])
    for hb in (0, 64):
        S.op("dve", lambda e, hb=hb: e.tensor_scalar(out=bqkr[hb:hb + 32, :], in0=bqk[hb + 32:hb + 64, :], scalar1=-1.0,
                                                     scalar2=None, op0=ALU.mult), reads=[B_bias], writes=[B_bias])
        S.op("dve", lambda e, hb=hb: e.tensor_copy(out=bqkr[hb + 32:hb + 64, :], in_=bqk[hb:hb + 32, :]),
             reads=[B_bias], writes=[B_bias])

    es8 = sb("es8", [128, 8], F32)
    esfm = sb("esfm", [128, 4], F32)
    B_sink = Buf("sink")
    S.dma("sp", lambda e: e.dma_start(out=es8[:, :], in_=sinks_d.partition_broadcast(128)), writes=[B_sink])
    S.op("act", lambda e: e.activation(out=es8[:, :], in_=es8[:, :], func=AF.Exp), reads=[B_sink], writes=[B_sink])
    S.op("dve", lambda e: e.tensor_copy(out=esfm[0:64, :], in_=es8[0:64, 0:8:2]), reads=[B_sink], writes=[B_sink])
    S.op("dve", lambda e: e.tensor_copy(out=esfm[64:128, :], in_=es8[64:128, 1:8:2]), reads=[B_sink], writes=[B_sink])

    xT = sb("xT", [128, KT, TH], BF16)
    B_xT = [Buf(f"xT{t}") for t in range(NT + 1)]
    xtok = [sb(f"xtok{i}", [128, D], BF16) for i in range(2)]
    B_xtok = [Buf(f"xtok{i}") for i in range(2)]
    for t in range(NT + 1):
        sl = t % 2
        src = xhalo[:, :] if t == 0 else xloc[(t - 1) * 128:t * 128, :]
        S.dma("pool", lambda e, sl=sl, src=src: e.dma_start(out=xtok[sl][:, :], in_=src), writes=[B_xtok[sl]])
        pb = 6 + sl
        pv = PS[pb][:, :]
        S.op("pe", [lambda e, k=k, sl=sl, pv=pv: e.transpose(out=pv[:, k * 128:(k + 1) * 128], in_=xtok[sl][:, k * 128:(k + 1) * 128],
                                                             identity=identb[:, :]) for k in range(KT)],
             reads=[B_xtok[sl], B_ident], writes=[PSB[pb]])
        ev = "dve" if t % 2 == 0 else "act"
        if ev == "dve":
            S.op("dve", lambda e, t=t, pv=pv: e.tensor_copy(out=xT[:, :, t * 128:(t + 1) * 128],
                                                           in_=pv.rearrange("p (k n) -> p k n", k=KT)),
                 reads=[PSB[pb]], writes=[B_xT[t]])
        else:
            S.op("act", lambda e, t=t, pv=pv: e.copy(out=xT[:, :, t * 128:(t + 1) * 128],
                                                    in_=pv.rearrange("p (k n) -> p k n", k=KT)),
                 reads=[PSB[pb]], writes=[B_xT[t]])

    if stage == "xt":
        dx = dram_out("dbg_xT", [D, TH])
        S.dma("pool", lambda e: e.dma_start(out=dx.rearrange("(k p) t -> p k t", p=128)[:, :, :], in_=xT[:, :, :]), stream="out",
              reads=B_xT, writes=[Buf("o")])
        db = dram_out("dbg_bfm", [128, 10])
        S.dma("sp", lambda e: e.dma_start(out=db[:, :], in_=bfm[:, :]), stream="out", reads=[B_bfm], writes=[Buf("o1")])
        S.finish()
        close_phase(); es.close()
        return nc, ["dbg_xT", "dbg_bfm"]

    qT = sb("qT", [128, 4, TOK], BF16)
    kT = sb("kT", [128, 2, TH], BF16)
    vpad = sb("vpad", [128, NT + 1, 2, 192], BF16)
    opad = sb("opad", [128, 192], BF16)
    B_pad = Buf("pad")
    S.op("pool", lambda e: e.memset(vpad[:, :, :, :], 0.0), writes=[B_pad])
    S.op("pool", lambda e: e.memset(opad[:, :], 0.0), writes=[B_pad])
    S.op("pool", lambda e: e.memset(opad[:, 64:128], 1.0), reads=[B_pad], writes=[B_pad])
    B_q = [Buf(f"q{b}") for b in range(4)]
    B_k = [Buf(f"k{b}") for b in range(5)]
    B_v = [Buf(f"v{t}") for t in range(NT + 1)]
    rtmp = [sb(f"rtmp{i}", [128, 2, 512], F32) for i in range(2)]
    B_rtmp = [Buf(f"rtmp{i}") for i in range(2)]
    it = 0

    def wcols(m):
        if m < 4:
            return lambda k: win[:, k, 128 * m:128 * m + 128]
        if m == 4:
            return lambda k: win[:, k, 512:640]
        return lambda k: wkb[:, k, :]

    blocks = [(0, 128, 0)] + [(128 + 512 * b, 512, b + 1) for b in range(4)]
    for (c0, n, bi) in blocks:
        tiles = range((c0 // 128), (c0 + n) // 128)
        xdeps = [B_xT[t] for t in tiles]
        for m in (range(6) if bi > 0 else (4, 5)):
            pa, pb = 2 * (it % 2), 2 * (it % 2) + 1
            wc = wcols(m)
            S.op("pe", [lambda e, k=k, wc=wc, pa=pa, c0=c0, n=n: e.matmul(PS[pa][:, 0:n], lhsT=wc(k), rhs=xT[:, k, c0:c0 + n],
                                                                         start=(k == 0), stop=(k == KT - 1)) for k in range(KT)],
                 reads=xdeps + [B_win, B_wrot], writes=[PSB[pa]])
            S.op("pe", [lambda e, k=k, m=m, pb=pb, c0=c0, n=n: e.matmul(PS[pb][:, 0:n], lhsT=wrot[:, k, m, :], rhs=xT[:, k, c0:c0 + n],
                                                                       start=(k == 0), stop=(k == KT - 1)) for k in range(KT)],
                 reads=xdeps + [B_wrot], writes=[PSB[pb]])
            rt = it % 2
            S.op("dve", lambda e, pa=pa, m=m, c0=c0, n=n, rt=rt: e.scalar_tensor_tensor(
                out=rtmp[rt][:, 0, 0:n], in0=PS[pa][:, 0:n], scalar=bqk[:, m:m + 1], in1=cosT[:, c0:c0 + n], op0=ALU.add, op1=ALU.mult),
                reads=[PSB[pa], B_bias, B_cos], writes=[B_rtmp[rt]])
            S.op("dve", lambda e, pb=pb, m=m, c0=c0, n=n, rt=rt: e.scalar_tensor_tensor(
                out=rtmp[rt][:, 1, 0:n], in0=PS[pb][:, 0:n], scalar=bqkr[:, m:m + 1], in1=sinT[:, c0:c0 + n], op0=ALU.add, op1=ALU.mult),
                reads=[PSB[pb], B_bias, B_sin], writes=[B_rtmp[rt]])
            if m < 4:
                dst, dbuf = qT[:, m, c0 - 128:c0 - 128 + n], B_q[bi - 1]
            else:
                dst, dbuf = kT[:, m - 4, c0:c0 + n], B_k[bi]
            S.op("dve", lambda e, dst=dst, rt=rt, n=n: e.tensor_tensor(out=dst, in0=rtmp[rt][:, 0, 0:n], in1=rtmp[rt][:, 1, 0:n], op=ALU.add),
                 reads=[B_rtmp[rt]], writes=[dbuf])
            it += 1
    for t in range(NT + 1):
        pv_ = 4 + (t % 2)
        S.op("pe", [lambda e, k=k, t=t, pv_=pv_: e.matmul(PS[pv_][:, 0:128], lhsT=xT[:, k, t * 128:(t + 1) * 128], rhs=win[:, k, 640:768],
                                                          start=(k == 0), stop=(k == KT - 1)) for k in range(KT)],
             reads=[B_xT[t], B_win], writes=[PSB[pv_]])
        S.op("dve", lambda e, t=t, pv_=pv_: e.tensor_tensor(out=vpad[:, t, :, 64:128], in0=PS[pv_][:, 0:128].rearrange("p (g d) -> p g d", g=2),
                                                            in1=bvbc[:, :].rearrange("p (g d) -> p g d", g=2), op=ALU.add),
             reads=[PSB[pv_], B_bvbc, B_pad], writes=[B_v[t]])

    if stage == "qk":
        dq = dram_out("dbg_q", [512, TOK])
        S.dma("pool", lambda e: e.dma_start(out=dq.rearrange("(c p) t -> p c t", p=128)[:, :, :], in_=qT[:, :, :]), stream="out", reads=B_q, writes=[Buf("o2")])
        dk = dram_out("dbg_k", [256, TH])
        S.dma("pool", lambda e: e.dma_start(out=dk.rearrange("(c p) t -> p c t", p=128)[:, :, :], in_=kT[:, :, :]), stream="out", reads=B_k, writes=[Buf("o3")])
        dvv = dram_out("dbg_v", [TH, 128])
        for g in range(2):
            S.dma("pool", lambda e, g=g: e.dma_start(out=dvv[:, 64 * g:64 * g + 64].rearrange("(t p) d -> p t d", p=128)[:, :, :],
                                                     in_=vpad[:, :, g, 64:128]),
                  stream="out", reads=B_v, writes=[Buf(f"o4{g}")])
        S.finish()
        close_phase(); es.close()
        return nc, ["dbg_q", "dbg_k", "dbg_v"]

    kTz = sb("kTz", [128, 4, TH], BF16)
    B_kz0 = Buf("kz0")
    S.op("pool", lambda e: e.memset(kTz[:, :, :], 0.0), writes=[B_kz0])
    B_kz = [Buf(f"kz{b}") for b in range(5)]
    kz_map = ((0, 0, 0), (1, 1, 1), (2, 0, 1), (3, 1, 0))
    for (c0, n, bi) in blocks:
        for (vi, half, var) in kz_map:
            p0 = 64 * half
            S.op("dve", lambda e, vi=vi, p0=p0, var=var, c0=c0, n=n: e.tensor_copy(out=kTz[p0:p0 + 64, vi, c0:c0 + n], in_=kT[p0:p0 + 64, var, c0:c0 + n]),
                 reads=[B_k[bi], B_kz0], writes=[B_kz[bi]])

    B_cat = [Buf(f"cat{t}") for t in range(NT)]
    pT = [sb(f"pT{i}", [128, 2, 512], BF16) for i in range(2)]
    B_pT = [Buf(f"pT{i}") for i in range(2)]
    ltmp = [sb(f"ltmp{i}", [128, 128], F32) for i in range(2)]
    B_ltmp = [Buf(f"ltmp{i}") for i in range(2)]
    kblk = lambda t: B_k[0] if t == 0 else B_k[1 + (t - 1) // 4]
    kzblk = lambda t: B_kz[0] if t == 0 else B_kz[1 + (t - 1) // 4]
    n_qblk = 1 if stage == "attn1" else NT

    def swa_F(i, g, sl):
        for kk, ktile in enumerate((i, i + 1)):
            pbk = 2 * sl + kk
            fns = []
            for hh in range(4):
                h = 4 * g + hh
                half = h % 2
                fns.append(lambda e, pbk=pbk, hh=hh, h=h, half=half, ktile=ktile: e.matmul(
                    PS[pbk][:, hh * 128:(hh + 1) * 128],
                    lhsT=kTz[:, 2 * g + half, ktile * 128:(ktile + 1) * 128],
                    rhs=qT[:, h // 2, i * 128:(i + 1) * 128], start=True, stop=True))
            S.op("pe", fns, reads=[kzblk(ktile), B_q[i // 4]], writes=[PSB[pbk]])
            S.op("act", lambda e, pbk=pbk, kk=kk: e.activation(out=pT[sl][:, kk, :], in_=PS[pbk][:, :], func=AF.Exp, scale=0.125),
                 reads=[PSB[pbk]], writes=[B_pT[sl]])
            mi = 1 if kk == 1 else (2 if i == 0 else 0)
            S.op("dve", lambda e, kk=kk, mi=mi: e.tensor_tensor(
                out=pT[sl][:, kk, :].rearrange("p (h q) -> p h q", h=4), in0=pT[sl][:, kk, :].rearrange("p (h q) -> p h q", h=4),
                in1=maskb[:, mi, :].unsqueeze(1).to_broadcast([128, 4, 128]), op=ALU.mult),
                reads=[B_pT[sl], B_mask], writes=[B_pT[sl]])

    def swa_G(i, g, sl):
        for tt in range(2):
            ct = 2 * g + tt
            po = 4 + tt
            fo, fl = [], []
            n_mm = 0
            for hl in range(2):
                hh = 2 * tt + hl
                for kk, ktile in enumerate((i, i + 1)):
                    first, last = (n_mm == 0), (n_mm == 3)
                    vs = (64, 192) if hl == 0 else (0, 128)
                    fo.append(lambda e, po=po, vs=vs, ktile=ktile, kk=kk, hh=hh, first=first, last=last: e.matmul(
                        PS[po][:, 0:128], lhsT=vpad[:, ktile, g, vs[0]:vs[1]], rhs=pT[sl][:, kk, hh * 128:(hh + 1) * 128],
                        start=first, stop=last))
                    fl.append(lambda e, po=po, vs=vs, kk=kk, hh=hh, first=first, last=last: e.matmul(
                        PS[po][:, 128:256], lhsT=opad[:, vs[0]:vs[1]], rhs=pT[sl][:, kk, hh * 128:(hh + 1) * 128],
                        start=first, stop=last))
                    n_mm += 1
            S.op("pe", fo + fl, reads=[B_pT[sl], B_v[i], B_v[i + 1], B_pad], writes=[PSB[po]])
            ls = tt
            S.op("dve", lambda e, po=po, ct=ct, ls=ls: e.tensor_scalar(out=ltmp[ls][:, :], in0=PS[po][:, 128:256], scalar1=esfm[:, ct:ct + 1],
                                                                        scalar2=None, op0=ALU.add), reads=[PSB[po], B_sink], writes=[B_ltmp[ls]])
            S.op("dve", lambda e, ls=ls: e.reciprocal(out=ltmp[ls][:, :], in_=ltmp[ls][:, :]), reads=[B_ltmp[ls]], writes=[B_ltmp[ls]])
            S.op("dve", lambda e, po=po, ct=ct, ls=ls: e.tensor_tensor(out=catT[:, ct, i * 128:(i + 1) * 128], in0=PS[po][:, 0:128],
                                                                       in1=ltmp[ls][:, :], op=ALU.mult),
                 reads=[PSB[po], B_ltmp[ls]], writes=[B_cat[i]])

    its = [(i, g) for i in range(n_qblk) for g in range(2)]
    for n, (i, g) in enumerate(its):
        swa_F(i, g, n % 2)
        if n > 0:
            swa_G(its[n - 1][0], its[n - 1][1], (n - 1) % 2)
    swa_G(its[-1][0], its[-1][1], (len(its) - 1) % 2)

    outs = []
    if stage in ("attn", "attn1"):
        dbg = dram_out("dbg_attn", [512, TOK])
        dv = dbg.rearrange("(c p) t -> p c t", p=128)
        nqo = 128 * n_qblk
        S.dma("pool", lambda e: e.dma_start(out=dv[:, :, 0:nqo], in_=catT[:, 0:4, 0:nqo]), stream="out", reads=B_cat[:n_qblk], writes=[Buf("o")])
        dq = dram_out("dbg_q", [512, TOK])
        S.dma("pool", lambda e: e.dma_start(out=dq.rearrange("(c p) t -> p c t", p=128)[:, :, :], in_=qT[:, :, :]), stream="out", reads=B_q, writes=[Buf("o2")])
        dk = dram_out("dbg_k", [256, TH])
        S.dma("pool", lambda e: e.dma_start(out=dk.rearrange("(c p) t -> p c t", p=128)[:, :, :], in_=kT[:, :, :]), stream="out", reads=B_k, writes=[Buf("o3")])
        outs = ["dbg_attn", "dbg_q", "dbg_k"]
        S.finish()
        close_phase(); es.close()
        return nc, outs
    close_phase()
    print("[build] sbuf free after phase 1:", nc.sbuf_bytes_remaining)
    return build_rest(nc, es, S, sb, open_phase, close_phase, dram_in, dram_out, PS, PSB, identb, identf, catT, B_cat,
                      B_ident, B_identf, stage)


def build_rest(nc, es, S, sb, open_phase, close_phase, dram_in, dram_out, PS, PSB, identb, identf, catT, B_cat,
               B_ident, B_identf, stage):
    xloc = nc_ap(nc, "xloc")
    xprev = dram_in("xprev", [3 * TOK, D])
    segv_d = dram_in("segvalid", [128, 4])
    w_in = nc_ap(nc, "w_in")
    b_in = nc_ap(nc, "b_in")
    lam_re_d = dram_in("s5_lambda_re", [32, 64]); lam_im_d = dram_in("s5_lambda_im", [32, 64])
    logdt_d = dram_in("s5_log_dt", [32])
    bre_d = dram_in("s5_b_re", [32, 64, 16]); bim_d = dram_in("s5_b_im", [32, 64, 16])
    cre_d = dram_in("s5_c_re", [32, 16, 64]); cim_d = dram_in("s5_c_im", [32, 16, 64])
    d_d = dram_in("s5_d", [512])
    wglu_d = dram_in("s5_w_glu", [512, 512]); bglu_d = dram_in("s5_b_glu", [512])
    cmask_d = dram_in("cmask", [128, 2, 256])

    open_phase()
    B_tb = Buf("tb")
    B_t4 = Buf("t4")

    def tt(out, a, b, op, eng="dve"):
        S.op(eng, lambda e: e.tensor_tensor(out=out, in0=a, in1=b, op=op), reads=[B_tb, B_t4], writes=[B_tb, B_t4])

    def ts(out, a, s1, op0, s2=None, op1=None):
        if op1 is None:
            S.op("dve", lambda e: e.tensor_scalar(out=out, in0=a, scalar1=s1, scalar2=None, op0=op0), reads=[B_tb, B_t4], writes=[B_tb, B_t4])
        else:
            S.op("dve", lambda e: e.tensor_scalar(out=out, in0=a, scalar1=s1, scalar2=s2, op0=op0, op1=op1), reads=[B_tb, B_t4], writes=[B_tb, B_t4])

    def TT(out, a, b, op, reads, writes, eng="dve"):
        S.op(eng, lambda e: e.tensor_tensor(out=out, in0=a, in1=b, op=op), reads=reads, writes=writes)

    def cp(out, a):
        S.op("dve", lambda e: e.tensor_copy(out=out, in_=a), reads=[B_tb, B_t4], writes=[B_tb, B_t4])

    def act(out, a, func, scale=1.0):
        S.op("act", lambda e: e.activation(out=out, in_=a, func=func, scale=scale), reads=[B_tb, B_t4], writes=[B_tb, B_t4])

    def cmul(outr, outi, ar, ai, br, bi, t1, t2, neg_i=False):
        tt(t1, ar, br, ALU.mult); tt(t2, ai, bi, ALU.mult); tt(outr, t1, t2, ALU.subtract)
        tt(t1, ar, bi, ALU.mult); tt(t2, ai, br, ALU.mult)
        if neg_i:
            tt(t1, t1, t2, ALU.add); ts(outi, t1, -1.0, ALU.mult)
        else:
            tt(outi, t1, t2, ALU.add)

    U = sb("s5U", [128, 32, 16, 16], BF16)
    UT = sb("s5UT", [128, 32, 2, 128], BF16)
    Hst = sb("s5H", [128, 32, 128], BF16)
    A_ = sb("s5A", [64, 2, 32, 17], F32)
    AI = sb("s5AI", [64, 2, 32, 16], F32)
    B15 = sb("s5B15", [64, 2, 32, 16], F32)
    CT = sb("s5CT", [64, 2, 32, 16], F32)
    C15 = sb("s5C15", [64, 2, 32, 16], F32)
    t4 = sb("s5t4", [128, 2, 2048], F32)
    t4a = t4[0:64, 0, :].rearrange("p (g s h) -> p g s h", g=8, s=16)
    t4b = t4[0:64, 1, :].rearrange("p (g s h) -> p g s h", g=8, s=16)
    ztmp = t4[:, 0, :].rearrange("p (g c) -> p g c", g=16)
    PSt = sb("s5PSt", [128, 8, 256], BF16)
    B_U, B_UT, B_H = Buf("s5U"), Buf("s5UT"), Buf("s5H")

    def build_PSt(g0):
        bc_s = lambda t, ri: t[:, ri, g0:g0 + 8, :].unsqueeze(3).to_broadcast([64, 8, 16, 16])
        bc_h = lambda t, ri: t[:, ri, g0:g0 + 8, :].unsqueeze(2).to_broadcast([64, 8, 16, 16])
        o4 = lambda lo: PSt[lo:lo + 64, :, :].rearrange("p g (s h) -> p g s h", s=16)
        tt(t4a, bc_s(AI, 0), bc_h(B15, 0), ALU.mult); tt(t4b, bc_s(AI, 1), bc_h(B15, 1), ALU.mult)
        tt(o4(0), t4a, t4b, ALU.subtract)
        tt(t4a, bc_s(AI, 0), bc_h(B15, 1), ALU.mult); tt(t4b, bc_s(AI, 1), bc_h(B15, 0), ALU.mult)
        tt(o4(64), t4a, t4b, ALU.add)

    open_phase()
    LA = sb("s5LA", [128, 32, 2, 128], BF16)
    R = sb("s5R", [128, 2, 16, 129], F32)
    rho_p = sb("s5rho", [128, 16], F32)
    segv = sb("s5segv", [128, 4], F32)
    open_phase()
    sm = sb("s5sm", [64, 56, 32], F32)
    smi = iter(range(56))
    T = lambda: sm[:, next(smi), :]
    lamn = sb("s5lamn", [32, 2, 64], F32)
    Bnat = sb("s5Bnat", [128, 2, 16, 16], F32)
    Cnat = sb("s5Cnat", [128, 2, 4, 64], F32)
    Bq = sb("s5Bq", [64, 2, 32, 16], F32)
    Bbar = sb("s5Bbar", [64, 2, 32, 16], F32)
    t3a = sb("s5t3a", [64, 32, 16], F32)
    t3b = sb("s5t3b", [64, 32, 16], F32)
    S.dma("sp", lambda e: e.dma_start(out=lamn[:, 0, :], in_=lam_re_d[:, :]), stream="tbl", writes=[B_tb])
    S.dma("sp", lambda e: e.dma_start(out=lamn[:, 1, :], in_=lam_im_d[:, :]), stream="tbl", writes=[B_tb])
    dtb = T()
    S.dma("sp", lambda e: e.dma_start(out=dtb, in_=logdt_d.partition_broadcast(64)), stream="tbl", writes=[B_tb])
    for ri, src in enumerate((bre_d, bim_d)):
        S.dma("sp", lambda e, ri=ri, src=src: e.dma_start(out=Bnat[:, ri, :, :], in_=src.rearrange("g p h -> (g p) h").rearrange("(gg r) h -> r gg h", r=128)),
              stream="tbl", writes=[B_tb])
    for ri, src in enumerate((cre_d, cim_d)):
        S.dma("sp", lambda e, ri=ri, src=src: e.dma_start(out=Cnat[:, ri, :, :], in_=src.rearrange("g h p -> (g h) p").rearrange("(t r) p -> r t p", r=128)),
              stream="tbl", writes=[B_tb])
    S.dma("sp", lambda e: e.dma_start(out=segv[:, :], in_=segv_d[:, :]), stream="tbl", writes=[B_tb])
    S.op("pe", [lambda e, ri=ri: e.transpose(out=PS[5][0:64, 32 * ri:32 * ri + 32], in_=lamn[:, ri, :], identity=identf[0:32, 0:32]) for ri in range(2)],
         reads=[B_tb, B_identf], writes=[PSB[5]])
    lam_r, lam_i = T(), T()
    S.op("dve", lambda e: e.tensor_copy(out=lam_r, in_=PS[5][0:64, 0:32]), reads=[PSB[5], B_tb], writes=[B_tb])
    S.op("dve", lambda e: e.tensor_copy(out=lam_i, in_=PS[5][0:64, 32:64]), reads=[PSB[5], B_tb], writes=[B_tb])
    for ri in range(2):
        S.op("pe", [lambda e, ri=ri, t=t: e.transpose(out=PS[4][0:64, 128 * t:128 * t + 128], in_=Cnat[:, ri, t, :], identity=identf[:, :]) for t in range(4)],
             reads=[B_tb, B_identf], writes=[PSB[4]])
        S.op("dve", lambda e, ri=ri: e.tensor_copy(out=CT[:, ri, :, :].rearrange("p g h -> p (g h)"), in_=PS[4][0:64, :]), reads=[PSB[4], B_tb], writes=[B_tb])
    for ri in range(2):
        cp(Bq[:, ri, 0:32:2, :], Bnat[0:64, ri, :, :])
        cp(Bq[:, ri, 1:32:2, :], Bnat[64:128, ri, :, :])
    act(dtb, dtb, AF.Exp)
    lr = T(); ts(lr, lam_r, -1e-4, ALU.min)
    mu, th = T(), T(); tt(mu, lr, dtb, ALU.mult); tt(th, lam_i, dtb, ALU.mult)
    sn, sh, cs = T(), T(), T()
    act(sn, th, AF.Sin, scale=0.125); act(sh, th, AF.Sin, scale=0.0625)
    tt(sh, sh, sh, ALU.mult); ts(cs, sh, -2.0, ALU.mult, 1.0, ALU.add)
    u1, u2 = T(), T()
    for _ in range(3):
        cn, s2 = T(), T()
        tt(u1, cs, cs, ALU.mult); tt(u2, sn, sn, ALU.mult); tt(cn, u1, u2, ALU.subtract)
        tt(u1, cs, sn, ALU.mult); ts(s2, u1, 2.0, ALU.mult)
        cs, sn = cn, s2
    mag, ar, ai = T(), T(), T()
    act(mag, mu, AF.Exp); tt(ar, mag, cs, ALU.mult); tt(ai, mag, sn, ALU.mult)
    den, am1, zr, zi = T(), T(), T(), T()
    tt(u1, lr, lr, ALU.mult); tt(u2, lam_i, lam_i, ALU.mult); tt(den, u1, u2, ALU.add)
    S.op("dve", lambda e: e.reciprocal(out=den, in_=den), reads=[B_tb, B_t4], writes=[B_tb, B_t4])
    ts(am1, ar, -1.0, ALU.add)
    tt(u1, am1, lr, ALU.mult); tt(u2, ai, lam_i, ALU.mult); tt(u1, u1, u2, ALU.add); tt(zr, u1, den, ALU.mult)
    tt(u1, ai, lr, ALU.mult); tt(u2, am1, lam_i, ALU.mult); tt(u1, u1, u2, ALU.subtract); tt(zi, u1, den, ALU.mult)
    bz = lambda t: t.unsqueeze(2).to_broadcast([64, 32, 16])
    cmul(Bbar[:, 0, :, :], Bbar[:, 1, :, :], bz(zr), bz(zi), Bq[:, 0, :, :], Bq[:, 1, :, :], t3a[:, :, :], t3b[:, :, :])
    air, aii = T(), T()
    tt(u1, ar, ar, ALU.mult); tt(u2, ai, ai, ALU.mult); tt(u1, u1, u2, ALU.add)
    S.op("dve", lambda e: e.reciprocal(out=u1, in_=u1), reads=[B_tb, B_t4], writes=[B_tb, B_t4])
    tt(air, ar, u1, ALU.mult); tt(aii, ai, u1, ALU.mult); ts(aii, aii, -1.0, ALU.mult)

    def cpow(tab, n, br, bi, npart, shp, tmpa, tmpb, bm_bufs):
        nd = len(shp)
        S.op("dve", lambda e: e.memset(tab_slice(tab, 0, nd, 0, 1), 1.0), reads=[B_tb, B_t4], writes=[B_tb, B_t4])
        S.op("dve", lambda e: e.memset(tab_slice(tab, 1, nd, 0, 1), 0.0), reads=[B_tb, B_t4], writes=[B_tb, B_t4])
        filled, bmr, bmi, bi_ = 1, br, bi, 0
        while filled < n:
            cnt = min(filled, n - filled)
            bb = lambda t: t.unsqueeze(nd + 1).to_broadcast([npart] + shp + [cnt])
            cmul(tab_slice(tab, 0, nd, filled, filled + cnt), tab_slice(tab, 1, nd, filled, filled + cnt),
                 tab_slice(tab, 0, nd, 0, cnt), tab_slice(tab, 1, nd, 0, cnt), bb(bmr), bb(bmi),
                 tab_slice(tmpa, None, nd, 0, cnt), tab_slice(tmpb, None, nd, 0, cnt))
            filled += cnt
            if filled < n:
                nr, ni = bm_bufs[bi_]; bi_ += 1
                q1, q2 = bm_bufs[-1]
                tt(q1, bmr, bmr, ALU.mult); tt(q2, bmi, bmi, ALU.mult); tt(nr, q1, q2, ALU.subtract)
                tt(q1, bmr, bmi, ALU.mult); ts(ni, q1, 2.0, ALU.mult)
                bmr, bmi = nr, ni

    def tab_slice(tab, ri, nd, k0, k1):
        if ri is None:
            return tab[:, :, k0:k1] if nd == 1 else tab[:, :, :, k0:k1]
        return tab[:, ri, :, k0:k1] if nd == 1 else tab[:, ri, :, :, k0:k1]

    t17a = sb("s5t17a", [64, 32, 8], F32); t17b = sb("s5t17b", [64, 32, 8], F32)
    bmA = [(T(), T()) for _ in range(5)]
    cpow(A_, 17, ar, ai, 64, [32], t17a, t17b, bmA)
    bmI = [(T(), T()) for _ in range(5)]
    cpow(AI, 16, air, aii, 64, [32], t17a, t17b, bmI)
    b15 = lambda t, ri, k: t[:, ri, :, k:k + 1].to_broadcast([64, 32, 16])
    cmul(B15[:, 0, :, :], B15[:, 1, :, :], b15(A_, 0, 15), b15(A_, 1, 15), Bbar[:, 0, :, :], Bbar[:, 1, :, :], t3a[:, :, :], t3b[:, :, :])
    cmul(C15[:, 0, :, :], C15[:, 1, :, :], b15(AI, 0, 15), b15(AI, 1, 15), CT[:, 0, :, :], CT[:, 1, :, :], t3a[:, :, :], t3b[:, :, :])
    rho = T(); act(rho, mu, AF.Exp, scale=16.0)
    rr = T()
    S.op("dve", lambda e: e.reciprocal(out=rr, in_=rho), reads=[B_tb, B_t4], writes=[B_tb, B_t4])
    rot_r, rot_i = T(), T()
    tt(rot_r, A_[:, 0, :, 16], rr, ALU.mult); tt(rot_i, A_[:, 1, :, 16], rr, ALU.mult)
    rotp = sb("s5rotp", [128, 2, 16], F32)
    for src, dst in ((rot_r, rotp[:, 0, :]), (rot_i, rotp[:, 1, :]), (rho, rho_p[:, :])):
        cp(dst[0:64], src[:, 0:32:2]); cp(dst[64:128], src[:, 1:32:2])
    tRa = sb("s5tRa", [128, 16, 64], F32); tRb = sb("s5tRb", [128, 16, 64], F32)
    smp = sb("s5smp", [128, 16, 16], F32)
    bmR = [(smp[:, 2 * i, :], smp[:, 2 * i + 1, :]) for i in range(8)]
    cpow(R, 129, rotp[:, 0, :], rotp[:, 1, :], 128, [16], tRa, tRb, bmR)
    for gb in range(4):
        build_PSt(8 * gb)
        for hb in range(2):
            bank = 6 + hb
            S.op("pe", [lambda e, j=j, hb=hb, bank=bank: e.transpose(out=PS[bank][:, 128 * j:128 * j + 128],
                                                                     in_=PSt[:, 4 * hb + j // 2, 128 * (j % 2):128 * (j % 2) + 128], identity=identb[:, :])
                        for j in range(8)], reads=[B_tb, B_ident], writes=[PSB[bank]])
            S.op("act", lambda e, gb=gb, hb=hb, bank=bank: e.copy(out=LA[:, 8 * gb + 4 * hb:8 * gb + 4 * hb + 4, :, :].rearrange("p g k c -> p (g k c)"),
                                                                  in_=PS[bank][:, :]), reads=[PSB[bank], B_tb], writes=[B_tb])

    close_phase()
    print("[build] sbuf free before S5 segment buffers:", nc.sbuf_bytes_remaining)

    wu = sb("s5wu", [128, KT, 512], BF16)
    bubc = sb("s5bubc", [128, 512], F32)
    B_wu, B_bu = Buf("s5wu"), Buf("s5bu")
    S.dma("pool", lambda e: e.dma_start(out=wu[:, :, :], in_=w_in.rearrange("(kt p) f -> p kt f", p=128)[:, :, 768:1280]), writes=[B_wu])
    S.dma("sp", lambda e: e.dma_start(out=bubc[:, :], in_=b_in[768:1280].partition_broadcast(128)), writes=[B_bu])
    xs = [sb(f"s5xs{i}", [128, D], BF16) for i in range(2)]
    B_xs = [Buf(f"s5xs{i}") for i in range(2)]
    xsf = [sb(f"s5xsf{i}", [128, D], F32) for i in range(2)]
    B_xsf = [Buf(f"s5xsf{i}") for i in range(2)]
    xsT = [sb(f"s5xsT{i}", [128, KT, 128], BF16) for i in range(2)]
    B_xsT = [Buf(f"s5xsT{i}") for i in range(2)]
    Z = sb("s5Z", [128, 16, 2, 128], F32)
    G2 = sb("s5G2", [128, 16, 2, 129], F32)
    B_Z, B_G = Buf("s5Z"), Buf("s5G")
    ctmp = sb("s5ctmp", [128, 4, 16], F32)
    S.op("dve", lambda e: e.memset(G2[:, :, :, 0:1], 0.0), reads=[B_G], writes=[B_G])
    ev = 0
    Ud, UTd, BUd, BUTd = U, UT, B_U, B_UT
    for seg in range(4):
        seg_v = (xprev[seg * TOK:(seg + 1) * TOK, :] if seg < 3 else xloc[:, :]).rearrange("(c s) d -> s c d", s=16)
        for s_ in range(16):
            sl = s_ % 2
            S.dma("sp", lambda e, sl=sl, s_=s_, seg_v=seg_v: e.dma_start(out=xsf[sl][:, :], in_=seg_v[s_]), writes=[B_xsf[sl]])
            S.op("act", lambda e, sl=sl: e.copy(out=xs[sl][:, :], in_=xsf[sl][:, :]), reads=[B_xsf[sl]], writes=[B_xs[sl]])
            bank = 6 + sl
            S.op("pe", [lambda e, k=k, sl=sl, bank=bank: e.transpose(out=PS[bank][:, k * 128:(k + 1) * 128], in_=xs[sl][:, k * 128:(k + 1) * 128],
                                                                     identity=identb[:, :]) for k in range(KT)],
                 reads=[B_xs[sl], B_ident], writes=[PSB[bank]])
            eng = "dve" if s_ % 2 == 0 else "act"
            fn = (lambda e, sl=sl, bank=bank: e.tensor_copy(out=xsT[sl][:, :, :], in_=PS[bank][:, :].rearrange("p (k n) -> p k n", k=KT))) if eng == "dve" else \
                 (lambda e, sl=sl, bank=bank: e.copy(out=xsT[sl][:, :, :], in_=PS[bank][:, :].rearrange("p (k n) -> p k n", k=KT)))
            S.op(eng, fn, reads=[PSB[bank]], writes=[B_xsT[sl]])
            pb = s_ % 4
            S.op("pe", [lambda e, k=k, sl=sl, pb=pb: e.matmul(PS[pb][:, :], lhsT=xsT[sl][:, k, :], rhs=wu[:, k, :], start=(k == 0), stop=(k == KT - 1))
                        for k in range(KT)], reads=[B_xsT[sl], B_wu], writes=[PSB[pb]])
            S.op("dve", lambda e, s_=s_, pb=pb: e.tensor_tensor(out=Ud[:, :, s_, :], in0=PS[pb][:, :].rearrange("p (g h) -> p g h", g=32),
                                                                 in1=bubc[:, :].rearrange("p (g h) -> p g h", g=32), op=ALU.add),
                 reads=[PSB[pb], B_bu], writes=[BUd])
        for gq in range(8):
            bank = 6 + gq % 2
            S.op("pe", [lambda e, j=j, gq=gq, bank=bank, Ud=Ud: e.transpose(out=PS[bank][:, 128 * j:128 * j + 128],
                                                                          in_=Ud[:, 4 * gq + j // 2, 8 * (j % 2):8 * (j % 2) + 8, :].rearrange("p s h -> p (s h)"),
                                                                          identity=identb[:, :]) for j in range(8)],
                 reads=[BUd, B_ident], writes=[PSB[bank]])
            eng = "dve" if gq % 2 == 0 else "act"
            dst = UTd[:, 4 * gq:4 * gq + 4, :, :].rearrange("p g k c -> p (g k c)")
            fn = (lambda e, dst=dst, bank=bank: e.tensor_copy(out=dst, in_=PS[bank][:, :])) if eng == "dve" else (lambda e, dst=dst, bank=bank: e.copy(out=dst, in_=PS[bank][:, :]))
            S.op(eng, fn, reads=[PSB[bank]], writes=[BUTd])
        for gq in range(8):
            pb = gq % 4
            fns = []
            for j in range(4):
                g = 4 * gq + j
                for kh in range(2):
                    fns.append(lambda e, g=g, kh=kh, j=j, pb=pb, UTd=UTd: e.matmul(PS[pb][:, 128 * j:128 * j + 128], lhsT=LA[:, g, kh, :], rhs=UTd[:, g, kh, :],
                                                                                 start=(kh == 0), stop=(kh == 1)))
            S.op("pe", fns, reads=[BUTd, B_tb], writes=[PSB[pb]])
            pv = PS[pb][:, :].rearrange("p (j c) -> p j c", j=4)
            for q in range(2):
                for ri in range(2):
                    eng = "dve" if ev % 2 == 0 else "act"; ev += 1
                    dst = Z[64 * q:64 * q + 64, 2 * gq:2 * gq + 2, ri, :]
                    srcv = pv[64 * ri:64 * ri + 64, q:4:2, :]
                    fn = (lambda e, dst=dst, srcv=srcv: e.tensor_copy(out=dst, in_=srcv)) if eng == "dve" else (lambda e, dst=dst, srcv=srcv: e.copy(out=dst, in_=srcv))
                    S.op(eng, fn, reads=[PSB[pb], B_Z], writes=[B_Z])
        if stage == "s5z" and seg == 3:
            dz = dram_out("dbg_Z", [128, 16, 2, 128])
            S.dma("sp", lambda e: e.dma_start(out=dz.rearrange("p a b c -> p (a b c)"), in_=Z[:, :, :, :].rearrange("p a b c -> p (a b c)")), stream="out", reads=[B_Z], writes=[Buf("o")])
            S.finish(); close_phase(); close_phase(); es.close()
            return nc, ["dbg_Z"]
        Rr, Ri = R[:, 0, :, 1:129], R[:, 1, :, 1:129]
        zr_, zi_ = Z[:, :, 0, :], Z[:, :, 1, :]
        dz = dict(reads=[B_Z, B_tb, B_G, B_t4], writes=[B_G, B_t4])
        gi_r, gi_i = G2[:, :, 0, 1:129], G2[:, :, 1, 1:129]
        TT(gi_r, Rr, zr_, ALU.mult, **dz)
        TT(ztmp, Ri, zi_, ALU.mult, **dz)
        TT(gi_r, gi_r, ztmp, ALU.add, **dz)
        TT(gi_i, Rr, zi_, ALU.mult, **dz)
        TT(ztmp, Ri, zr_, ALU.mult, **dz)
        TT(gi_i, gi_i, ztmp, ALU.subtract, **dz)
        S.op("dve", [lambda e, gg=gg, ri=ri: e.tensor_tensor_scan(out=G2[:, gg, ri, 1:129], data0=rho_p[:, gg:gg + 1].to_broadcast([128, 128]),
                                                                    data1=G2[:, gg, ri, 1:129], initial=G2[:, gg, ri, 0:1], op0=ALU.mult, op1=ALU.add)
                     for gg in range(16) for ri in range(2)], reads=[B_G, B_tb], writes=[B_G])
        if seg < 3:
            c_ = lambda i: ctmp[:, i, :]
            TT(c_(0), R[:, 0, :, 128], G2[:, :, 0, 128], ALU.mult, **dz)
            TT(c_(1), R[:, 1, :, 128], G2[:, :, 1, 128], ALU.mult, **dz)
            TT(c_(0), c_(0), c_(1), ALU.subtract, **dz)
            TT(c_(2), R[:, 0, :, 128], G2[:, :, 1, 128], ALU.mult, **dz)
            TT(c_(3), R[:, 1, :, 128], G2[:, :, 0, 128], ALU.mult, **dz)
            TT(c_(2), c_(2), c_(3), ALU.add, **dz)
            S.op("dve", lambda e, seg=seg: e.tensor_scalar(out=G2[:, :, 0, 0], in0=c_(0), scalar1=segv[:, seg:seg + 1], scalar2=None, op0=ALU.mult), **dz)
            S.op("dve", lambda e, seg=seg: e.tensor_scalar(out=G2[:, :, 1, 0], in0=c_(2), scalar1=segv[:, seg:seg + 1], scalar2=None, op0=ALU.mult), **dz)
    Rr, Ri = R[:, 0, :, 0:128], R[:, 1, :, 0:128]
    gr, gi = G2[:, :, 0, 0:128], G2[:, :, 1, 0:128]
    dh = dict(reads=[B_G, B_tb, B_H], writes=[B_H])
    z0, z1 = Z[:, :, 0, :], Z[:, :, 1, :]
    dzz = dict(reads=[B_G, B_tb, B_Z, B_t4, B_H], writes=[B_Z, B_t4, B_H])
    TT(z0, Rr, gr, ALU.mult, **dzz)
    TT(ztmp, Ri, gi, ALU.mult, **dzz)
    for q in range(2):
        TT(Hst[0:64, q:32:2, :], z0[64 * q:64 * q + 64], ztmp[64 * q:64 * q + 64], ALU.subtract, **dzz)
    TT(z0, Rr, gi, ALU.mult, **dzz)
    TT(z1, Ri, gr, ALU.mult, **dzz)
    for q in range(2):
        TT(Hst[64:128, q:32:2, :], z0[64 * q:64 * q + 64], z1[64 * q:64 * q + 64], ALU.add, **dzz)
    if stage == "s5h":
        dhh = dram_out("dbg_H", [128, 32, 128])
        S.dma("pool", lambda e: e.dma_start(out=dhh.rearrange("p g c -> p (g c)"), in_=Hst[:, :, :].rearrange("p g c -> p (g c)")), stream="out", reads=[B_H], writes=[Buf("o")])
        S.finish(); close_phase(); close_phase(); es.close()
        return nc, ["dbg_H"]
    close_phase()

    open_phase()
    cmask = sb("s5cmask", [128, 2, 256], BF16)
    B_cm = Buf("s5cm")
    S.dma("pool", lambda e: e.dma_start(out=cmask[:, :, :], in_=cmask_d[:, :, :]), writes=[B_cm])
    QS2 = sb("s5QS2", [128, 8, 256], BF16)
    RB = sb("s5RB", [128, 8, 256], BF16)
    Mj = sb("s5Mj", [128, 8, 2, 256], BF16)
    dbc = sb("s5dbc", [128, 512], F32)
    wglu = sb("s5wglu", [128, 4, 512], BF16)
    bgr = sb("s5bgr", [4, 128], F32); bgfm = sb("s5bgfm", [128, 4], F32)
    yT = sb("s5yT", [128, 4, TOK], BF16)
    ygs = sb("s5ygs", [128, 16, 128], BF16)
    yf = [sb(f"s5yf{i}", [128, 2, 16, 16], F32) for i in range(2)]
    yw = [sb(f"s5yw{i}", [128, 2, 16, 16], F32) for i in range(2)]
    B_d, B_wg, B_bg, B_yT, B_ygs = Buf("s5d"), Buf("s5wg"), Buf("s5bg"), [Buf(f"s5yT{j}") for j in range(4)], Buf("s5ygs")
    B_yf = [Buf(f"s5yf{i}") for i in range(2)]
    S.dma("sp", lambda e: e.dma_start(out=dbc[:, :], in_=d_d.partition_broadcast(128)), writes=[B_d])
    S.dma("pool", lambda e: e.dma_start(out=wglu[:, :, :], in_=wglu_d.rearrange("(kt p) f -> p kt f", p=128)), writes=[B_wg])
    S.dma("sp", lambda e: e.dma_start(out=bgr[:, :], in_=bglu_d.rearrange("(t p) -> t p", p=128)), writes=[B_bg])
    S.op("pe", lambda e: e.transpose(out=PS[5][:, 0:4], in_=bgr[:, :], identity=identf[0:4, 0:4]), reads=[B_bg, B_identf], writes=[PSB[5]])
    S.op("dve", lambda e: e.tensor_copy(out=bgfm[:, :], in_=PS[5][:, 0:4]), reads=[PSB[5]], writes=[B_bg])

    def build_Q(dst, g0, Ctab, k0):
        bc_s = lambda ri: A_[:, ri, g0:g0 + 8, k0:k0 + 16].unsqueeze(3).to_broadcast([64, 8, 16, 16])
        bc_h = lambda ri: Ctab[:, ri, g0:g0 + 8, :].unsqueeze(2).to_broadcast([64, 8, 16, 16])
        o4 = lambda lo: dst[lo:lo + 64, :, :].rearrange("p g (s h) -> p g s h", s=16)
        tt(t4a, bc_s(0), bc_h(0), ALU.mult); tt(t4b, bc_s(1), bc_h(1), ALU.mult)
        tt(o4(0), t4a, t4b, ALU.subtract)
        tt(t4a, bc_s(0), bc_h(1), ALU.mult); tt(t4b, bc_s(1), bc_h(0), ALU.mult)
        tt(t4a, t4a, t4b, ALU.add)
        ts(o4(64), t4a, -1.0, ALU.mult)

    for j in range(4):
        g0 = 8 * j
        build_PSt(g0); build_Q(QS2, g0, C15, 0); build_Q(RB, g0, CT, 1)
        for g8 in range(8):
            pb = 4 + g8 % 2
            S.op("pe", [lambda e, g8=g8, kh=kh, pb=pb: e.matmul(PS[pb][:, 256 * kh:256 * kh + 256], lhsT=PSt[:, g8, 128 * kh:128 * kh + 128], rhs=QS2[:, g8, :],
                                                                start=True, stop=True) for kh in range(2)], reads=[B_tb], writes=[PSB[pb]])
            S.op("dve", lambda e, g8=g8, pb=pb: e.tensor_tensor(out=Mj[:, g8, :, :], in0=PS[pb][:, :].rearrange("p (k n) -> p k n", k=2), in1=cmask[:, :, :], op=ALU.mult),
                 reads=[PSB[pb], B_cm, B_tb], writes=[B_tb])
        for gp in range(4):
            pb = gp
            fns = []
            for q in range(2):
                g8 = 2 * gp + q; g = g0 + g8
                o = PS[pb][:, 256 * q:256 * q + 256]
                fns.append(lambda e, o=o, g=g, g8=g8: e.matmul(o, lhsT=Hst[:, g, :], rhs=RB[:, g8, :], start=True, stop=False))
                fns.append(lambda e, o=o, g=g, g8=g8: e.matmul(o, lhsT=UT[:, g, 0, :], rhs=Mj[:, g8, 0, :], start=False, stop=False))
                fns.append(lambda e, o=o, g=g, g8=g8: e.matmul(o, lhsT=UT[:, g, 1, :], rhs=Mj[:, g8, 1, :], start=False, stop=True))
            S.op("pe", fns, reads=[B_H, B_UT, B_tb], writes=[PSB[pb]])
            sl = gp % 2
            ga = g0 + 2 * gp
            pv4 = PS[pb][:, :].rearrange("p (g s h) -> p g s h", g=2, s=16)
            dv4 = dbc[:, 16 * ga:16 * ga + 32].rearrange("p (g h) -> p g h", g=2).unsqueeze(2).to_broadcast([128, 2, 16, 16])
            y_, w_ = yf[sl][:, :, :, :], yw[sl][:, :, :, :]
            S.op("dve", lambda e, y_=y_, ga=ga, dv4=dv4: e.tensor_tensor(out=y_, in0=U[:, ga:ga + 2, :, :], in1=dv4, op=ALU.mult), reads=[B_U, B_d, B_yf[sl]], writes=[B_yf[sl]])
            S.op("dve", lambda e, y_=y_, pv4=pv4: e.tensor_tensor(out=y_, in0=y_, in1=pv4, op=ALU.add), reads=[PSB[pb], B_yf[sl]], writes=[B_yf[sl]])
            if stage == "s5y" and j == 0 and gp == 0:
                dy = dram_out("dbg_y", [128, 2, 16, 16])
                S.dma("sp", lambda e: e.dma_start(out=dy.rearrange("p a b c -> p (a b c)"), in_=yf[0][:, :, :, :].rearrange("p a b c -> p (a b c)")), stream="out", reads=[B_yf[0]], writes=[Buf("o")])
                S.finish(); close_phase(); close_phase(); es.close()
                return nc, ["dbg_y"]
            S.op("dve", lambda e, y_=y_, w_=w_: e.tensor_tensor(out=w_, in0=y_, in1=y_, op=ALU.mult), reads=[B_yf[sl]], writes=[B_yf[sl]])
            S.op("dve", lambda e, w_=w_: e.tensor_scalar(out=w_, in0=w_, scalar1=0.044715, scalar2=1.0, op0=ALU.mult, op1=ALU.add), reads=[B_yf[sl]], writes=[B_yf[sl]])
            S.op("dve", lambda e, y_=y_, w_=w_: e.tensor_tensor(out=w_, in0=w_, in1=y_, op=ALU.mult), reads=[B_yf[sl]], writes=[B_yf[sl]])
            S.op("act", lambda e, w_=w_: e.activation(out=w_, in_=w_, func=AF.Sigmoid, scale=1.5957691216057308), reads=[B_yf[sl]], writes=[B_yf[sl]])
            dst = ygs[:, :, 32 * gp:32 * gp + 32].rearrange("p s (g h) -> p g s h", g=2)
            S.op("dve", lambda e, y_=y_, w_=w_, dst=dst: e.tensor_tensor(out=dst, in0=y_, in1=w_, op=ALU.mult), reads=[B_yf[sl], B_ygs], writes=[B_ygs])
        for hb in range(2):
            bank = 6 + hb
            S.op("pe", [lambda e, i=i, hb=hb, bank=bank: e.transpose(out=PS[bank][:, 128 * i:128 * i + 128], in_=ygs[:, 8 * hb + i, :], identity=identb[:, :]) for i in range(8)],
                 reads=[B_ygs, B_ident], writes=[PSB[bank]])
            dst = yT[:, j, :].rearrange("p (c s) -> p s c", s=16)[:, 8 * hb:8 * hb + 8, :]
            S.op("dve", lambda e, dst=dst, bank=bank: e.tensor_copy(out=dst, in_=PS[bank][:, :].rearrange("p (s c) -> p s c", s=8)), reads=[PSB[bank]], writes=[B_yT[j]])
    gt = [sb(f"s5gt{i}", [128, 512], BF16) for i in range(2)]
    B_gt = [Buf(f"s5gt{i}") for i in range(2)]
    ig = 0
    for co in range(4):
        for blk in range(4):
            pb = ig % 4; sl = ig % 2; ig += 1
            S.op("pe", [lambda e, k=k, co=co, blk=blk, pb=pb: e.matmul(PS[pb][:, :], lhsT=wglu[:, k, 128 * co:128 * co + 128], rhs=yT[:, k, 512 * blk:512 * blk + 512],
                                                                       start=(k == 0), stop=(k == 3)) for k in range(4)], reads=B_yT + [B_wg], writes=[PSB[pb]])
            S.op("act", lambda e, co=co, pb=pb, sl=sl: e.activation(out=gt[sl][:, :], in_=PS[pb][:, :], func=AF.Sigmoid, bias=bgfm[:, co:co + 1]), reads=[PSB[pb], B_bg], writes=[B_gt[sl]])
            S.op("dve", lambda e, co=co, blk=blk, sl=sl: e.tensor_tensor(out=catT[:, 4 + co, 512 * blk:512 * blk + 512], in0=yT[:, co, 512 * blk:512 * blk + 512], in1=gt[sl][:, :], op=ALU.mult),
                 reads=[B_gt[sl], B_yT[co]], writes=[B_cat[4 * blk + i] for i in range(4)])
    if stage == "s5":
        dss = dram_out("dbg_ssm", [512, TOK])
        S.dma("pool", lambda e: e.dma_start(out=dss.rearrange("(c p) t -> p c t", p=128), in_=catT[:, 4:8, :]), stream="out", reads=B_cat, writes=[Buf("o")])
        S.finish(); close_phase(); close_phase(); es.close()
        return nc, ["dbg_ssm"]
    close_phase()
    close_phase()
    ALPHA = 2.0 ** 0.25
    CAP = 384
    xloc = nc_ap(nc, "xloc")
    mem_d = dram_in("mem", [256, D])
    wout_d = dram_in("w_out", [D, D]); bout_d = dram_in("b_out", [D])
    ln_d = {k: dram_in(k, [D]) for k in ("ln1_g", "ln1_b", "ln2_g", "ln2_b", "ln3_g", "ln3_b")}
    wxq_d = dram_in("w_xq", [D, D]); wxkv_d = dram_in("w_xkv", [D, 2 * D]); wxo_d = dram_in("w_xo", [D, D])
    wr_d = dram_in("w_router", [D, 32]); br_d = dram_in("b_router", [32])
    tri_d = dram_in("tri", [128, 128]); ecap_d = dram_in("ecap", [128, 32])
    x2f = nc.dram_tensor("x2f", [TOK, D], F32, kind="Internal").ap()
    xg = nc.dram_tensor("xg", [32 * CAP, D], BF16, kind="Internal").ap()
    yg = nc.dram_tensor("yg", [32 * CAP, D], F32, kind="Internal").ap()

    gates = sb("gates", [128, NT, 4], F32)
    dest = sb("dest", [128, NT, 4], I32)
    B_route = [Buf(f"route{t}") for t in range(NT)]
    B_xg, B_x2f = Buf("xg"), [Buf(f"x2f{t}") for t in range(NT)]

    open_phase()
    onesb = sb("onesb", [128, 128], BF16)
    trib = sb("tri_sb", [128, 128], BF16)
    ecap = sb("ecap_sb", [128, 32], F32)
    B_c3 = Buf("c3")
    S.op("pool", lambda e: e.memset(onesb[:, :], 1.0), writes=[B_c3])
    B_tri, B_ecap = Buf("tri"), Buf("ecap")
    S.dma("pool", lambda e: e.dma_start(out=trib[:, :], in_=tri_d[:, :]), writes=[B_tri])
    S.dma("sp", lambda e: e.dma_start(out=ecap[:, :], in_=ecap_d[:, :]), writes=[B_ecap])
    bc = {}
    B_bc = {}
    for k, src in [("b_out", bout_d)] + [(k, ln_d[k]) for k in ("ln1_g", "ln1_b", "ln2_g", "ln2_b")]:
        bc[k] = sb("bc_" + k, [128, D], F32)
        B_bc[k] = Buf("bc_" + k)
        S.dma("sp", lambda e, k=k, src=src: e.dma_start(out=bc[k][:, :], in_=src.partition_broadcast(128)), writes=[B_bc[k]])
    brbc = sb("brbc", [128, 32], F32); wr = sb("wr", [128, KT, 32], F32)
    B_br, B_wr = Buf("brbc"), Buf("wr")
    S.dma("sp", lambda e: e.dma_start(out=brbc[:, :], in_=br_d.partition_broadcast(128)), writes=[B_br])
    S.dma("sp", lambda e: e.dma_start(out=wr[:, :, :], in_=wr_d.rearrange("(kt p) f -> p kt f", p=128)), writes=[B_wr])
    wout = sb("wout", [128, KT, D], BF16); wxq = sb("wxq", [128, KT, D], BF16); wxo = sb("wxo", [128, KT, D], BF16)
    B_wout, B_wxq, B_wxo = Buf("wout"), Buf("wxq"), Buf("wxo")
    wst3 = [sb(f"wst3_{i}", [128, D], F32) for i in range(4)]
    B_wst3 = [Buf(f"wst3_{i}") for i in range(4)]
    w3step = [0]

    def load_w3(dst, src, ncol, bf):
        for k in range(KT):
            for c0 in range(0, ncol, D):
                i = w3step[0] % 4; w3step[0] += 1
                S.dma("sp", lambda e, i=i, k=k, c0=c0, src=src: e.dma_start(out=wst3[i][:, :], in_=src[k * 128:(k + 1) * 128, c0:c0 + D]), writes=[B_wst3[i]])
                S.op("act", lambda e, i=i, k=k, c0=c0, dst=dst: e.copy(out=dst[:, k, c0:c0 + D], in_=wst3[i][:, :]), reads=[B_wst3[i], bf], writes=[bf])
    kmemT = sb("kmemT", [128, KT, 256], BF16)
    vmem = sb("vmem", [128, 2, D], BF16)
    B_kv = Buf("kv")
    open_phase()
    wxkv = sb("wxkv", [128, KT, 2 * D], BF16)
    B_wxkv = Buf("wxkv")
    load_w3(wxkv, wxkv_d, 2 * D, B_wxkv)
    load_w3(wout, wout_d, D, B_wout); load_w3(wxq, wxq_d, D, B_wxq); load_w3(wxo, wxo_d, D, B_wxo)
    memb = sb("memb", [128, 2, D], BF16)
    memT = sb("memT", [128, KT, 256], BF16)
    B_memb, B_memT = Buf("memb"), Buf("memT")
    S.dma("pool", lambda e: e.dma_start(out=memb[:, :, :], in_=mem_d.rearrange("(t p) d -> p t d", p=128)), writes=[B_memb])
    for mt in range(2):
        bank = 6 + mt
        S.op("pe", [lambda e, k=k, mt=mt, bank=bank: e.transpose(out=PS[bank][:, k * 128:(k + 1) * 128], in_=memb[:, mt, k * 128:(k + 1) * 128], identity=identb[:, :])
                    for k in range(KT)], reads=[B_memb, B_ident], writes=[PSB[bank]])
        S.op("dve", lambda e, mt=mt, bank=bank: e.tensor_copy(out=memT[:, :, mt * 128:(mt + 1) * 128], in_=PS[bank][:, :].rearrange("p (k n) -> p k n", k=KT)),
             reads=[PSB[bank]], writes=[B_memT])
    for ct in range(KT):
        pb = ct % 4
        S.op("pe", [lambda e, k=k, ct=ct, pb=pb: e.matmul(PS[pb][:, 0:256], lhsT=wxkv[:, k, ct * 128:(ct + 1) * 128], rhs=memT[:, k, :], start=(k == 0), stop=(k == KT - 1))
                    for k in range(KT)], reads=[B_wxkv, B_memT], writes=[PSB[pb]])
        S.op("dve", lambda e, ct=ct, pb=pb: e.tensor_copy(out=kmemT[:, ct, :], in_=PS[pb][:, 0:256]), reads=[PSB[pb]], writes=[B_kv])
    for mt in range(2):
        for hf in range(2):
            pb = 2 * mt + hf
            S.op("pe", [lambda e, k=k, mt=mt, hf=hf, pb=pb: e.matmul(PS[pb][:, :], lhsT=memT[:, k, mt * 128:(mt + 1) * 128], rhs=wxkv[:, k, D + 512 * hf:D + 512 * hf + 512],
                                                                    start=(k == 0), stop=(k == KT - 1)) for k in range(KT)], reads=[B_wxkv, B_memT], writes=[PSB[pb]])
            S.op("act", lambda e, mt=mt, hf=hf, pb=pb: e.copy(out=vmem[:, mt, 512 * hf:512 * hf + 512], in_=PS[pb][:, :]), reads=[PSB[pb]], writes=[B_kv])
    close_phase()

    x1 = sb("x1", [128, 4, D], F32)
    x1T = sb("x1T", [128, KT, 512], BF16)
    qxT = sb("qxT", [128, KT, 512], BF16)
    oT = sb("oT", [128, KT, 512], BF16)
    pX = [sb(f"pX{i}", [128, 2, 512], BF16) for i in range(2)]
    rl = sb("rl", [128, 512], F32)
    xin = [sb(f"xin{i}", [128, D], F32) for i in range(2)]
    hh = [sb(f"hh{i}", [128, D], F32) for i in range(2)]
    hb = [sb(f"hb{i}", [128, D], BF16) for i in range(2)]
    x2t = [sb(f"x2t{i}", [128, D], F32) for i in range(2)]
    x2T = sb("x2T", [128, KT, 128], F32)
    st6_ = [sb(f"st6_{i}", [128, 2, 6], F32) for i in range(2)]; mv_ = [sb(f"mv_{i}", [128, 2], F32) for i in range(2)]; rstd_ = [sb(f"rstd_{i}", [128, 1], F32) for i in range(2)]
    B_ln = [Buf(f"lnscr{i}") for i in range(2)]; ln_i = [0]
    lg = sb("lg", [128, 32], F32); v8 = sb("v8", [128, 8], F32); e4 = sb("e4", [128, 4], F32); ssum = sb("ssum", [128, 1], F32)
    nv1 = sb("nv1", [128, 1], F32)
    oh = sb("oh", [128, 4, 32], F32); maskall = sb("maskall", [128, NT, 32], BF16); slt = sb("slt", [128, 32], F32)
    destf = sb("destf", [128, 4], F32)
    B_x1 = [Buf(f"x1_{i}") for i in range(4)]; B_x1T, B_qxT, B_oT = Buf("x1T"), Buf("qxT"), Buf("oT")
    B_pX = [Buf(f"pX{i}") for i in range(2)]; B_rl = Buf("rl")
    B_xin = [Buf(f"xin{i}") for i in range(2)]; B_hh = [Buf(f"hh{i}") for i in range(2)]; B_hb = [Buf(f"hb{i}") for i in range(2)]
    B_x2t = [Buf(f"x2t{i}") for i in range(2)]; B_x2T = Buf("x2T"); B_sm = Buf("sm3"); B_mask = Buf("maskall")

    def layer_norm(src, dst, gk, bk, bsrc, bdst):
        i = ln_i[0] % 2; ln_i[0] += 1
        st6, mv, rstd, bl = st6_[i], mv_[i], rstd_[i], B_ln[i]
        S.op("dve", [lambda e, c=c: e.bn_stats(out=st6[:, c, :], in_=src[:, 512 * c:512 * c + 512]) for c in range(2)], reads=[bsrc, bl], writes=[bl])
        S.op("dve", lambda e: e.bn_aggr(out=mv[:, :], in_=st6[:, :, :].rearrange("p a b -> p (a b)")), reads=[bl], writes=[bl])
        S.op("dve", lambda e: e.tensor_scalar(out=rstd[:, :], in0=mv[:, 1:2], scalar1=1e-5, scalar2=None, op0=ALU.add), reads=[bl], writes=[bl])
        S.op("act", lambda e: e.activation(out=rstd[:, :], in_=rstd[:, :], func=AF.Sqrt), reads=[bl], writes=[bl])
        S.op("dve", lambda e: e.reciprocal(out=rstd[:, :], in_=rstd[:, :]), reads=[bl], writes=[bl])
        S.op("dve", lambda e: e.tensor_scalar(out=src, in0=src, scalar1=mv[:, 0:1], scalar2=rstd[:, 0:1], op0=ALU.subtract, op1=ALU.mult), reads=[bsrc, bl], writes=[bsrc])
        S.op("dve", lambda e: e.tensor_tensor(out=src, in0=src, in1=bc[gk][:, :], op=ALU.mult), reads=[bsrc, B_bc[gk]], writes=[bsrc])
        S.op("dve", lambda e: e.tensor_tensor(out=dst, in0=src, in1=bc[bk][:, :], op=ALU.add), reads=[bsrc, B_bc[bk]], writes=[bdst])

    for st in range(4):
        def A1(ti):
            t = 4 * st + ti; sl = t % 2
            S.dma("sp", lambda e, t=t, sl=sl: e.dma_start(out=xin[sl][:, :], in_=xloc[t * 128:(t + 1) * 128, :]), writes=[B_xin[sl]])
            for hf in range(2):
                pb = 2 * sl + hf
                S.op("pe", [lambda e, k=k, t=t, hf=hf, pb=pb: e.matmul(PS[pb][:, :], lhsT=catT[:, k, t * 128:(t + 1) * 128], rhs=wout[:, k, 512 * hf:512 * hf + 512],
                                                                      start=(k == 0), stop=(k == KT - 1)) for k in range(KT)], reads=[B_cat[t], B_wout], writes=[PSB[pb]])
                S.op("dve", lambda e, sl=sl, hf=hf, pb=pb: e.scalar_tensor_tensor(out=hh[sl][:, 512 * hf:512 * hf + 512], in0=xin[sl][:, 512 * hf:512 * hf + 512], scalar=ALPHA,
                                                                                  in1=PS[pb][:, :], op0=ALU.mult, op1=ALU.add), reads=[PSB[pb], B_xin[sl], B_hh[sl]], writes=[B_hh[sl]])
            S.op("dve", lambda e, sl=sl: e.tensor_tensor(out=hh[sl][:, :], in0=hh[sl][:, :], in1=bc["b_out"][:, :], op=ALU.add), reads=[B_hh[sl], B_bc["b_out"]], writes=[B_hh[sl]])
            layer_norm(hh[sl][:, :], x1[:, ti, :], "ln1_g", "ln1_b", B_hh[sl], B_x1[ti])
            S.op("act", lambda e, ti=ti, sl=sl: e.copy(out=hb[sl][:, :], in_=x1[:, ti, :]), reads=[B_x1[ti]], writes=[B_hb[sl]])
        def B1(ti):
            t = 4 * st + ti; sl = t % 2
            bank = 6 + sl
            S.op("pe", [lambda e, k=k, sl=sl, bank=bank: e.transpose(out=PS[bank][:, k * 128:(k + 1) * 128], in_=hb[sl][:, k * 128:(k + 1) * 128], identity=identb[:, :])
                        for k in range(KT)], reads=[B_hb[sl], B_ident], writes=[PSB[bank]])
            S.op("dve", lambda e, ti=ti, bank=bank: e.tensor_copy(out=x1T[:, :, ti * 128:(ti + 1) * 128], in_=PS[bank][:, :].rearrange("p (k n) -> p k n", k=KT)),
                 reads=[PSB[bank]], writes=[B_x1T])
        for ti in range(4):
            A1(ti)
            if ti > 0:
                B1(ti - 1)
        B1(3)
        if stage == "x1" and st == 0:
            dx1 = dram_out("dbg_x1", [512, D])
            S.dma("sp", lambda e: e.dma_start(out=dx1.rearrange("(t p) d -> p t d", p=128), in_=x1[:, :, :]), stream="out", reads=B_x1, writes=[Buf("o")])
            S.finish(); close_phase(); es.close()
            return nc, ["dbg_x1"]
        for ct in range(KT):
            pb = ct % 4
            S.op("pe", [lambda e, k=k, ct=ct, pb=pb: e.matmul(PS[pb][:, :], lhsT=wxq[:, k, ct * 128:(ct + 1) * 128], rhs=x1T[:, k, :], start=(k == 0), stop=(k == KT - 1))
                        for k in range(KT)], reads=[B_x1T, B_wxq], writes=[PSB[pb]])
            eng = "act" if ct % 2 else "dve"
            fn = (lambda e, ct=ct, pb=pb: e.copy(out=qxT[:, ct, :], in_=PS[pb][:, :])) if eng == "act" else (lambda e, ct=ct, pb=pb: e.tensor_copy(out=qxT[:, ct, :], in_=PS[pb][:, :]))
            S.op(eng, fn, reads=[PSB[pb]], writes=[B_qxT])
        for h in range(4):
            sl = h % 2
            for mt in range(2):
                pb = mt
                S.op("pe", [lambda e, cc=cc, h=h, mt=mt, pb=pb: e.matmul(PS[pb][:, :], lhsT=kmemT[:, 2 * h + cc, mt * 128:(mt + 1) * 128], rhs=qxT[:, 2 * h + cc, :],
                                                                        start=(cc == 0), stop=(cc == 1)) for cc in range(2)], reads=[B_kv, B_qxT], writes=[PSB[pb]])
                S.op("act", lambda e, sl=sl, mt=mt, pb=pb: e.activation(out=pX[sl][:, mt, :], in_=PS[pb][:, :], func=AF.Exp, scale=0.0625), reads=[PSB[pb]], writes=[B_pX[sl]])
            S.op("pe", [lambda e, mt=mt, sl=sl: e.matmul(PS[4][:, :], lhsT=onesb[:, :], rhs=pX[sl][:, mt, :], start=(mt == 0), stop=(mt == 1)) for mt in range(2)],
                 reads=[B_pX[sl], B_c3], writes=[PSB[4]])
            S.op("dve", lambda e: e.tensor_copy(out=rl[:, :], in_=PS[4][:, :]), reads=[PSB[4], B_rl], writes=[B_rl])
            S.op("dve", lambda e: e.reciprocal(out=rl[:, :], in_=rl[:, :]), reads=[B_rl], writes=[B_rl])
            for cc in range(2):
                pb = 2 + cc
                S.op("pe", [lambda e, mt=mt, cc=cc, h=h, sl=sl, pb=pb: e.matmul(PS[pb][:, :], lhsT=vmem[:, mt, (2 * h + cc) * 128:(2 * h + cc + 1) * 128], rhs=pX[sl][:, mt, :],
                                                                              start=(mt == 0), stop=(mt == 1)) for mt in range(2)], reads=[B_pX[sl], B_kv], writes=[PSB[pb]])
                S.op("dve", lambda e, cc=cc, h=h, pb=pb: e.tensor_tensor(out=oT[:, 2 * h + cc, :], in0=PS[pb][:, :], in1=rl[:, :], op=ALU.mult), reads=[PSB[pb], B_rl], writes=[B_oT])
        def A2(ti):
            t = 4 * st + ti; sl = t % 2
            for hf in range(2):
                pb = 2 * sl + hf
                S.op("pe", [lambda e, k=k, ti=ti, hf=hf, pb=pb: e.matmul(PS[pb][:, :], lhsT=oT[:, k, ti * 128:(ti + 1) * 128], rhs=wxo[:, k, 512 * hf:512 * hf + 512],
                                                                       start=(k == 0), stop=(k == KT - 1)) for k in range(KT)], reads=[B_oT, B_wxo], writes=[PSB[pb]])
                S.op("dve", lambda e, ti=ti, sl=sl, hf=hf, pb=pb: e.scalar_tensor_tensor(out=hh[sl][:, 512 * hf:512 * hf + 512], in0=x1[:, ti, 512 * hf:512 * hf + 512], scalar=ALPHA,
                                                                                         in1=PS[pb][:, :], op0=ALU.mult, op1=ALU.add), reads=[PSB[pb], B_x1[ti], B_hh[sl]], writes=[B_hh[sl]])
            layer_norm(hh[sl][:, :], x2t[sl][:, :], "ln2_g", "ln2_b", B_hh[sl], B_x2t[sl])
            if stage == "x2a":
                dxa = dram_out("dbg_x2a", [128, D])
                S.dma("sp", lambda e, sl=sl: e.dma_start(out=dxa[:, :], in_=x2t[sl][:, :]), stream="out", reads=[B_x2t[sl]], writes=[Buf("o")])
                S.finish(); close_phase(); es.close()
                return nc, ["dbg_x2a"]
            S.dma("sp", lambda e, t=t, sl=sl: e.dma_start(out=x2f[t * 128:(t + 1) * 128, :], in_=x2t[sl][:, :]), stream=f"x2f{sl}", reads=[B_x2t[sl]], writes=[B_x2f[t]])
            S.op("act", lambda e, sl=sl: e.copy(out=hb[sl][:, :], in_=x2t[sl][:, :]), reads=[B_x2t[sl]], writes=[B_hb[sl]])
        def B2(ti):
            t = 4 * st + ti; sl = t % 2
            for hf in range(2):
                S.op("pe", [lambda e, k=k, sl=sl, hf=hf: e.transpose(out=PS[4 + hf][:, k * 128:(k + 1) * 128], in_=x2t[sl][:, (4 * hf + k) * 128:(4 * hf + k + 1) * 128], identity=identf[:, :])
                            for k in range(4)], reads=[B_x2t[sl], B_identf], writes=[PSB[4 + hf]])
                eng = "act" if hf else "dve"
                fn = (lambda e, hf=hf: e.copy(out=x2T[:, 4 * hf:4 * hf + 4, :], in_=PS[4 + hf][:, :].rearrange("p (k n) -> p k n", k=4))) if eng == "act" else \
                     (lambda e, hf=hf: e.tensor_copy(out=x2T[:, 4 * hf:4 * hf + 4, :], in_=PS[4 + hf][:, :].rearrange("p (k n) -> p k n", k=4)))
                S.op(eng, fn, reads=[PSB[4 + hf]], writes=[B_x2T])
            S.op("pe", [lambda e, k=k: e.matmul(PS[4][:, 0:32], lhsT=x2T[:, k, :], rhs=wr[:, k, :], start=(k == 0), stop=(k == KT - 1)) for k in range(KT)],
                 reads=[B_x2T, B_wr], writes=[PSB[4]])
            dsm = dict(reads=[B_sm], writes=[B_sm])
            S.op("dve", lambda e: e.tensor_tensor(out=lg[:, :], in0=PS[4][:, 0:32], in1=brbc[:, :], op=ALU.add), reads=[PSB[4], B_br, B_sm], writes=[B_sm])
            S.op("dve", lambda e: e.max(out=v8[:, :], in_=lg[:, :]), **dsm)
            S.op("dve", lambda e: e.tensor_scalar(out=nv1[:, :], in0=v8[:, 0:1], scalar1=-1.0, scalar2=None, op0=ALU.mult), **dsm)
            S.op("act", lambda e: e.activation(out=e4[:, :], in_=v8[:, 0:4], func=AF.Exp, bias=nv1[:, 0:1]), **dsm)
            S.op("dve", lambda e: e.reduce_sum(out=ssum[:, :], in_=e4[:, :], axis=AX.X), **dsm)
            S.op("dve", lambda e: e.reciprocal(out=ssum[:, :], in_=ssum[:, :]), **dsm)
            S.op("dve", lambda e, t=t: e.tensor_scalar(out=gates[:, t, :], in0=e4[:, :], scalar1=ssum[:, 0:1], scalar2=None, op0=ALU.mult), reads=[B_sm], writes=[B_sm, B_route[t]])
            for k in range(4):
                S.op("dve", lambda e, k=k: e.tensor_scalar(out=oh[:, k, :], in0=lg[:, :], scalar1=v8[:, k:k + 1], scalar2=None, op0=ALU.is_equal), **dsm)
            S.op("dve", lambda e: e.tensor_tensor(out=slt[:, :], in0=oh[:, 0, :], in1=oh[:, 1, :], op=ALU.add), **dsm)
            S.op("dve", lambda e: e.tensor_tensor(out=slt[:, :], in0=slt[:, :], in1=oh[:, 2, :], op=ALU.add), **dsm)
            S.op("dve", lambda e, t=t: e.tensor_tensor(out=maskall[:, t, :], in0=slt[:, :], in1=oh[:, 3, :], op=ALU.add), reads=[B_sm, B_mask], writes=[B_mask])
            S.op("pe", [lambda e, t=t: e.matmul(PS[5][:, 0:32], lhsT=trib[:, :], rhs=maskall[:, t, :], start=True, stop=(t == 0))] +
                       [lambda e, t=t, tp=tp: e.matmul(PS[5][:, 0:32], lhsT=onesb[:, :], rhs=maskall[:, tp, :], start=False, stop=(tp == t - 1)) for tp in range(t)],
                 reads=[B_mask, B_tri, B_c3], writes=[PSB[5]])
            S.op("dve", lambda e: e.tensor_scalar(out=slt[:, :], in0=PS[5][:, 0:32], scalar1=float(CAP - 1), scalar2=None, op0=ALU.min), reads=[PSB[5], B_sm], writes=[B_sm])
            S.op("dve", lambda e: e.tensor_tensor(out=slt[:, :], in0=slt[:, :], in1=ecap[:, :], op=ALU.add), reads=[B_ecap, B_sm], writes=[B_sm])
            S.op("dve", lambda e: e.tensor_tensor(out=oh[:, :, :], in0=oh[:, :, :], in1=slt[:, :].unsqueeze(1).to_broadcast([128, 4, 32]), op=ALU.mult), **dsm)
            S.op("dve", lambda e: e.reduce_sum(out=destf[:, :], in_=oh[:, :, :], axis=AX.X), **dsm)
            S.op("dve", lambda e, t=t: e.tensor_copy(out=dest[:, t, :], in_=destf[:, :]), reads=[B_sm], writes=[B_sm, B_route[t]])
            for k in range(4):
                S.dma("pool", lambda e, t=t, k=k, sl=sl: e.indirect_dma_start(out=xg[:, :], out_offset=bass.IndirectOffsetOnAxis(dest[:, t, k:k + 1], 0),
                                                                              in_=hb[sl][:, :], in_offset=None), stream=f"xgsc{sl}", reads=[B_hb[sl], B_route[t]], writes=[B_xg])
        for ti in range(4):
            A2(ti)
            if ti > 0:
                B2(ti - 1)
        B2(3)
    if stage == "x2":
        dx2 = dram_out("dbg_x2", [TOK, D]); dg = dram_out("dbg_gates", [128, NT, 4]); dd = dram_out("dbg_dest", [128, NT, 4], I32)
        S.dma("sp", lambda e: e.dma_start(out=dx2[:, :], in_=x2f[:, :]), stream="out", reads=B_x2f, writes=[Buf("o")])
        S.dma("sp", lambda e: e.dma_start(out=dg.rearrange("p a b -> p (a b)"), in_=gates[:, :, :].rearrange("p a b -> p (a b)")), stream="out", reads=B_route, writes=[Buf("o1")])
        S.dma("sp", lambda e: e.dma_start(out=dd.rearrange("p a b -> p (a b)"), in_=dest[:, :, :].rearrange("p a b -> p (a b)")), stream="out", reads=B_route, writes=[Buf("o2")])
        dxg = dram_out("dbg_xg", [32 * CAP, D])
        S.dma("pool", lambda e: e.dma_start(out=dxg[:, :], in_=xg[:, :]), stream="outsw", reads=[B_xg], writes=[Buf("o3")])
        S.finish(); close_phase(); es.close()
        return nc, ["dbg_x2", "dbg_gates", "dbg_dest", "dbg_xg"]
    close_phase()
    we1_d = dram_in("w_e1", [32, D, 2 * D]); be1_d = dram_in("b_e1", [32, 2 * D])
    we2_d = dram_in("w_e2", [32, D, D]); be2_d = dram_in("b_e2", [32, D])
    n_exp = 32 if stage not in ("moe2",) else 2
    B_yg = Buf("yg")
    open_phase()
    b1all = sb("b1all", [32, 2 * D], F32)
    b1fm = sb("b1fm", [128, 8, 2, 32], F32)
    B_b1a, B_b1 = Buf("b1all"), Buf("b1fm")
    S.dma("sp", lambda e: e.dma_start(out=b1all[:, :], in_=be1_d[:, :]), writes=[B_b1a])
    S.op("pe", [lambda e, ft=ft, two=two: e.transpose(out=PS[5][:, 32 * (2 * ft + two):32 * (2 * ft + two) + 32], in_=b1all[:, 256 * ft + two:256 * (ft + 1):2],
                                                     identity=identf[0:32, 0:32]) for ft in range(8) for two in range(2)], reads=[B_b1a, B_identf], writes=[PSB[5]])
    S.op("dve", lambda e: e.tensor_copy(out=b1fm[:, :, :, :].rearrange("p a b c -> p (a b c)"), in_=PS[5][:, :]), reads=[PSB[5]], writes=[B_b1])
    w1 = [sb(f"w1_{i}", [128, KT, 2 * D], BF16) for i in range(2)]
    w2 = [sb(f"w2_{i}", [128, KT, D], BF16) for i in range(2)]
    b2bc = [sb(f"b2bc{i}", [128, D], F32) for i in range(2)]
    B_w1 = [Buf(f"w1_{i}") for i in range(2)]; B_w2 = [Buf(f"w2_{i}") for i in range(2)]; B_b2 = [Buf(f"b2bc{i}") for i in range(2)]
    NS = CAP // 128
    xgl = sb("xgl", [128, NS, D], BF16)
    xgT2 = [sb("xgT", [128, KT, CAP], BF16)] * 2; actT2 = [sb("actT", [128, 8, CAP], BF16)] * 2
    B_xgl = Buf("xgl"); B_xgT2 = [Buf("xgT")] * 2; B_actT2 = [Buf("actT")] * 2
    NWST = 4
    wst = [sb(f"wst{i}", [128, D], F32) for i in range(NWST)]
    B_wst = [Buf(f"wst{i}") for i in range(NWST)]
    gg_ = [sb(f"sg_g{i}", [128, CAP], F32) for i in range(2)]; ll_ = [sb(f"sg_l{i}", [128, CAP], F32) for i in range(2)]; ss_ = [sb(f"sg_s{i}", [128, CAP], F32) for i in range(2)]
    B_sg = [Buf(f"sg{i}") for i in range(2)]
    ysb = [sb(f"ysb{i}", [128, D], F32) for i in range(2)]
    B_ysb = [Buf(f"ysb{i}") for i in range(2)]

    wstep = [0]

    def load_step(e_, j):
        sl = e_ % 2
        i = wstep[0] % NWST; wstep[0] += 1
        if j < 16:
            k, hf = j // 2, j % 2
            S.dma("sp", lambda e, i=i, e_=e_, k=k, hf=hf: e.dma_start(out=wst[i][:, :], in_=we1_d[e_][k * 128:(k + 1) * 128, hf * D:(hf + 1) * D]), writes=[B_wst[i]])
            S.op("act", lambda e, i=i, sl=sl, k=k, hf=hf: e.copy(out=w1[sl][:, k, hf * D:(hf + 1) * D], in_=wst[i][:, :]), reads=[B_wst[i], B_w1[sl]], writes=[B_w1[sl]])
        else:
            k = j - 16
            S.dma("sp", lambda e, i=i, e_=e_, k=k: e.dma_start(out=wst[i][:, :], in_=we2_d[e_][k * 128:(k + 1) * 128, :]), writes=[B_wst[i]])
            S.op("act", lambda e, i=i, sl=sl, k=k: e.copy(out=w2[sl][:, k, :], in_=wst[i][:, :]), reads=[B_wst[i], B_w2[sl]], writes=[B_w2[sl]])

    def load_b2(e_):
        sl = e_ % 2
        S.dma("pool", lambda e, sl=sl, e_=e_: e.dma_start(out=b2bc[sl][:, :], in_=be2_d[e_].partition_broadcast(128)), writes=[B_b2[sl]])

    def load_expert(e_):
        for j in range(24):
            load_step(e_, j)
        load_b2(e_)

    load_expert(0)
    iy = 0
    for e_ in range(n_exp):
        sl = e_ % 2
        nxt = e_ + 1 if e_ + 1 < n_exp else None
        if nxt is not None:
            load_b2(nxt)
        xgT, actT, B_xgT, B_actT = xgT2[sl], actT2[sl], B_xgT2[sl], B_actT2[sl]
        S.dma("sp", lambda e, e_=e_: e.dma_start(out=xgl[:, :, :], in_=xg[e_ * CAP:(e_ + 1) * CAP, :].rearrange("(s p) d -> p s d", p=128)), reads=[B_xg], writes=[B_xgl])
        for st_ in range(NS):
            bank = 6 + st_ % 2
            S.op("pe", [lambda e, k=k, st_=st_, bank=bank: e.transpose(out=PS[bank][:, k * 128:(k + 1) * 128], in_=xgl[:, st_, k * 128:(k + 1) * 128], identity=identb[:, :])
                        for k in range(KT)], reads=[B_xgl, B_ident], writes=[PSB[bank]])
            eng = "act" if st_ % 2 else "dve"
            fn = (lambda e, st_=st_, bank=bank, xgT=xgT: e.copy(out=xgT[:, :, st_ * 128:(st_ + 1) * 128], in_=PS[bank][:, :].rearrange("p (k n) -> p k n", k=KT))) if eng == "act" else \
                 (lambda e, st_=st_, bank=bank, xgT=xgT: e.tensor_copy(out=xgT[:, :, st_ * 128:(st_ + 1) * 128], in_=PS[bank][:, :].rearrange("p (k n) -> p k n", k=KT)))
            S.op(eng, fn, reads=[PSB[bank]], writes=[B_xgT])
        for ft in range(8):
            if nxt is not None:
                for j in range(3 * ft, 3 * ft + 3):
                    load_step(nxt, j)
            s2 = ft % 2
            pg, pl = 2 * s2, 2 * s2 + 1
            for two, pb in ((0, pg), (1, pl)):
                S.op("pe", [lambda e, k=k, ft=ft, two=two, pb=pb, sl=sl, xgT=xgT: e.matmul(PS[pb][:, 0:CAP], lhsT=w1[sl][:, k, 256 * ft + two:256 * (ft + 1):2], rhs=xgT[:, k, :],
                                                                                 start=(k == 0), stop=(k == KT - 1)) for k in range(KT)], reads=[B_w1[sl], B_xgT], writes=[PSB[pb]])
            g_, l_, s_ = gg_[s2][:, :], ll_[s2][:, :], ss_[s2][:, :]
            S.op("dve", lambda e, g_=g_, pg=pg, ft=ft, e_=e_: e.tensor_scalar(out=g_, in0=PS[pg][:, 0:CAP], scalar1=b1fm[:, ft, 0, e_:e_ + 1], scalar2=7.0, op0=ALU.add, op1=ALU.min),
                 reads=[PSB[pg], B_b1, B_sg[s2]], writes=[B_sg[s2]])
            S.op("act", lambda e, g_=g_, s_=s_: e.activation(out=s_, in_=g_, func=AF.Sigmoid, scale=1.702), reads=[B_sg[s2]], writes=[B_sg[s2]])
            S.op("dve", lambda e, l_=l_, pl=pl, ft=ft, e_=e_: e.tensor_scalar(out=l_, in0=PS[pl][:, 0:CAP], scalar1=b1fm[:, ft, 1, e_:e_ + 1], scalar2=7.0, op0=ALU.add, op1=ALU.min),
                 reads=[PSB[pl], B_b1, B_sg[s2]], writes=[B_sg[s2]])
            S.op("dve", lambda e, l_=l_: e.tensor_scalar(out=l_, in0=l_, scalar1=-7.0, scalar2=1.0, op0=ALU.max, op1=ALU.add), reads=[B_sg[s2]], writes=[B_sg[s2]])
            S.op("dve", lambda e, g_=g_, s_=s_: e.tensor_tensor(out=g_, in0=g_, in1=s_, op=ALU.mult), reads=[B_sg[s2]], writes=[B_sg[s2]])
            S.op("dve", lambda e, g_=g_, l_=l_, ft=ft, actT=actT: e.tensor_tensor(out=actT[:, ft, :], in0=g_, in1=l_, op=ALU.mult), reads=[B_sg[s2], B_actT], writes=[B_actT])
        for st_ in range(NS):
            ys = iy % 2; iy += 1
            for hf in range(2):
                pb = 4 + hf
                S.op("pe", [lambda e, ft=ft, st_=st_, hf=hf, pb=pb, sl=sl, actT=actT: e.matmul(PS[pb][:, :], lhsT=actT[:, ft, st_ * 128:(st_ + 1) * 128], rhs=w2[sl][:, ft, 512 * hf:512 * hf + 512],
                                                                                   start=(ft == 0), stop=(ft == 7)) for ft in range(8)], reads=[B_actT, B_w2[sl]], writes=[PSB[pb]])
                S.op("dve", lambda e, ys=ys, hf=hf, pb=pb, sl=sl: e.tensor_tensor(out=ysb[ys][:, 512 * hf:512 * hf + 512], in0=PS[pb][:, :], in1=b2bc[sl][:, 512 * hf:512 * hf + 512], op=ALU.add),
                     reads=[PSB[pb], B_b2[sl], B_ysb[ys]], writes=[B_ysb[ys]])
            r0 = e_ * CAP + st_ * 128
            S.dma("pool", lambda e, ys=ys, r0=r0: e.dma_start(out=yg[r0:r0 + 128, :], in_=ysb[ys][:, :]), stream=f"ygst{ys}", reads=[B_ysb[ys]], writes=[B_yg])
    close_phase()

    out_d = dram_out("out", [TOK, D])
    open_phase()
    g3 = sb("ln3g", [128, D], F32); b3 = sb("ln3b", [128, D], F32)
    B_g3, B_b3 = Buf("ln3g"), Buf("ln3b")
    S.dma("sp", lambda e: e.dma_start(out=g3[:, :], in_=ln_d["ln3_g"].partition_broadcast(128)), writes=[B_g3])
    S.dma("sp", lambda e: e.dma_start(out=b3[:, :], in_=ln_d["ln3_b"].partition_broadcast(128)), writes=[B_b3])
    ygat = [[sb(f"ygat{i}_{k}", [128, D], F32) for k in range(4)] for i in range(2)]
    B_ygat = [[Buf(f"ygat{i}_{k}") for k in range(4)] for i in range(2)]
    x2r = [sb(f"x2r{i}", [128, D], F32) for i in range(2)]; B_x2r = [Buf(f"x2r{i}") for i in range(2)]
    acc = [sb(f"acc{i}", [128, D], F32) for i in range(2)]; B_acc = [Buf(f"acc{i}") for i in range(2)]
    oo = [sb(f"oo{i}", [128, D], F32) for i in range(2)]; B_oo = [Buf(f"oo{i}") for i in range(2)]
    st6b = sb("st6b", [128, 2, 6], F32); mvb = sb("mvb", [128, 2], F32); rstdb = sb("rstdb", [128, 1], F32)
    B_sm5 = Buf("sm5")
    for t in range(NT):
        sl = t % 2
        S.dma("sp", lambda e, t=t, sl=sl: e.dma_start(out=x2r[sl][:, :], in_=x2f[t * 128:(t + 1) * 128, :]), reads=[B_x2f[t]], writes=[B_x2r[sl]])
        for k in range(4):
            S.dma("pool", lambda e, t=t, k=k, sl=sl: e.indirect_dma_start(out=ygat[sl][k][:, :], out_offset=None, in_=yg[:, :],
                                                                          in_offset=bass.IndirectOffsetOnAxis(dest[:, t, k:k + 1], 0)), reads=[B_yg, B_route[t]], writes=[B_ygat[sl][k]])
        a_ = acc[sl][:, :]
        S.op("dve", lambda e, a_=a_, t=t, sl=sl: e.tensor_scalar(out=a_, in0=ygat[sl][0][:, :], scalar1=gates[:, t, 0:1], scalar2=None, op0=ALU.mult),
             reads=[B_ygat[sl][0], B_route[t], B_acc[sl]], writes=[B_acc[sl]])
        for k in range(1, 4):
            S.op("dve", lambda e, a_=a_, t=t, k=k, sl=sl: e.scalar_tensor_tensor(out=a_, in0=ygat[sl][k][:, :], scalar=gates[:, t, k:k + 1], in1=a_, op0=ALU.mult, op1=ALU.add),
                 reads=[B_ygat[sl][k], B_route[t], B_acc[sl]], writes=[B_acc[sl]])
        S.op("dve", lambda e, a_=a_, sl=sl: e.scalar_tensor_tensor(out=a_, in0=x2r[sl][:, :], scalar=ALPHA, in1=a_, op0=ALU.mult, op1=ALU.add),
             reads=[B_x2r[sl], B_acc[sl]], writes=[B_acc[sl]])
        S.op("dve", [lambda e, c=c, a_=a_: e.bn_stats(out=st6b[:, c, :], in_=a_[:, 512 * c:512 * c + 512]) for c in range(2)], reads=[B_acc[sl], B_sm5], writes=[B_sm5])
        S.op("dve", lambda e: e.bn_aggr(out=mvb[:, :], in_=st6b[:, :, :].rearrange("p a b -> p (a b)")), reads=[B_sm5], writes=[B_sm5])
        S.op("dve", lambda e: e.tensor_scalar(out=rstdb[:, :], in0=mvb[:, 1:2], scalar1=1e-5, scalar2=None, op0=ALU.add), reads=[B_sm5], writes=[B_sm5])
        S.op("act", lambda e: e.activation(out=rstdb[:, :], in_=rstdb[:, :], func=AF.Sqrt), reads=[B_sm5], writes=[B_sm5])
        S.op("dve", lambda e: e.reciprocal(out=rstdb[:, :], in_=rstdb[:, :]), reads=[B_sm5], writes=[B_sm5])
        S.op("dve", lambda e, a_=a_: e.tensor_scalar(out=a_, in0=a_, scalar1=mvb[:, 0:1], scalar2=rstdb[:, 0:1], op0=ALU.subtract, op1=ALU.mult), reads=[B_acc[sl], B_sm5], writes=[B_acc[sl]])
        S.op("dve", lambda e, a_=a_: e.tensor_tensor(out=a_, in0=a_, in1=g3[:, :], op=ALU.mult), reads=[B_acc[sl], B_g3], writes=[B_acc[sl]])
        S.op("dve", lambda e, a_=a_, sl=sl: e.tensor_tensor(out=oo[sl][:, :], in0=a_, in1=b3[:, :], op=ALU.add), reads=[B_acc[sl], B_b3, B_oo[sl]], writes=[B_oo[sl]])
        S.dma("sp", lambda e, t=t, sl=sl: e.dma_start(out=out_d[t * 128:(t + 1) * 128, :], in_=oo[sl][:, :]), stream=f"out{sl}", reads=[B_oo[sl]], writes=[Buf(f"o_{t}")])
    S.finish()
    close_phase()
    es.close()
    return nc, ["out"]


def nc_ap(nc, name):
    return _DRAM[name]


def _rope_tables(pos):
    half = 32
    inv = (10000.0 ** (-np.arange(half, dtype=np.float32) * np.float32(2.0 / 64))).astype(np.float32)
    ang = (pos.astype(np.float32)[:, None] * inv[None, :]).astype(np.float32)
    cos = np.cos(ang.astype(np.float64)).astype(np.float32).T
    sin = np.sin(ang.astype(np.float64)).astype(np.float32).T
    return np.ascontiguousarray(np.tile(cos, (4, 1))), np.ascontiguousarray(np.tile(sin, (4, 1)))


def prep_core_inputs(inputs, c, stage_needs_s5=True):
    b, j = c // 4, c % 4
    t0 = j * TOK
    x = inputs["x"]
    m = {}
    m["xloc"] = np.ascontiguousarray(x[b, t0:t0 + TOK])
    m["xhalo"] = np.ascontiguousarray(x[b, t0 - HALO:t0]) if j > 0 else np.zeros((HALO, D), np.float32)
    pos = np.arange(t0 - HALO, t0 + TOK)
    m["ropecos"], m["ropesin"] = _rope_tables(pos)
    kj = np.arange(128)[:, None]
    qi = np.arange(128)[None, :]
    prev = (kj > qi).astype(np.float32)
    own = (kj <= qi).astype(np.float32)
    first = prev if j > 0 else np.zeros_like(prev)
    m["masks"] = np.ascontiguousarray(np.stack([prev, own, first], axis=1))
    m["ident"] = np.eye(128, dtype=np.float32)
    m["w_in"] = np.ascontiguousarray(inputs["w_in"][0])
    m["b_in"] = np.ascontiguousarray(inputs["b_in"][0])
    m["attn_sinks"] = np.ascontiguousarray(inputs["attn_sinks"][0])
    if stage_needs_s5:
        xp = np.zeros((3 * TOK, D), np.float32)
        sv = np.zeros((128, 4), np.float32)
        for k in range(3):
            sj = j - 3 + k
            if sj >= 0:
                xp[k * TOK:(k + 1) * TOK] = x[b, sj * TOK:(sj + 1) * TOK]
                sv[:, k] = 1.0
        m["xprev"], m["segvalid"] = xp, sv
        for k in ("s5_lambda_re", "s5_lambda_im", "s5_log_dt", "s5_b_re", "s5_b_im", "s5_c_re", "s5_c_im", "s5_d", "s5_w_glu", "s5_b_glu"):
            m[k] = np.ascontiguousarray(inputs[k][0])
        sp = np.arange(128) // 16
        so = np.arange(256) // 16
        for k in ("w_out", "b_out", "ln1_g", "ln1_b", "ln2_g", "ln2_b", "ln3_g", "ln3_b", "w_xq", "w_xkv", "w_xo", "w_router", "b_router", "w_e1", "b_e1", "w_e2", "b_e2"):
            m[k] = np.ascontiguousarray(inputs[k][0])
        m["mem"] = np.ascontiguousarray(inputs["mem"][b])
        m["tri"] = np.triu(np.ones((128, 128), np.float32), 1)
        m["ecap"] = np.ascontiguousarray(np.tile((np.arange(32, dtype=np.float32) * 384.0)[None, :], (128, 1)))
        m["cmask"] = np.ascontiguousarray(np.stack([(so[None, :] >= (8 * kh + sp)[:, None]) for kh in range(2)], axis=1).astype(np.float32))
    return m


def kernel(**inputs):
    inputs = {k: np.asarray(v) for k, v in inputs.items()}
    nc, _ = build_nc("full")
    in_maps = [prep_core_inputs(inputs, c) for c in range(NCORES)]
    res = run_bass_kernel_spmd(nc, in_maps, core_ids=list(range(NCORES)))
    out = np.zeros((2, 8192, D), np.float32)
    for c in range(NCORES):
        out[c // 4, (c % 4) * TOK:(c % 4 + 1) * TOK] = res.results[c]["out"]
    return out
```

```python
from contextlib import ExitStack

import numpy as np
import concourse.bass as bass
import concourse.mybir as mybir
from concourse.bass_utils import run_bass_kernel_spmd

F32 = mybir.dt.float32
BF16 = mybir.dt.bfloat16
I32 = mybir.dt.int32
ALU = mybir.AluOpType
AF = mybir.ActivationFunctionType
AX = mybir.AxisListType

NCORES = 8
_DRAM = {}
TOK = 2048
NT = TOK // 128
HALO = 128
TH = TOK + HALO
D = 1024
KT = D // 128


class Buf:
    __slots__ = ("name", "lw", "rd")

    def __init__(self, name):
        self.name = name
        self.lw = None
        self.rd = {}


class Sched:
    ENG = ("pe", "act", "dve", "pool", "sp")

    def __init__(self, nc, es):
        self.nc = nc
        self.es = es
        self.sems = {e: es.enter_context(nc.semaphore("sem_" + e)) for e in self.ENG}
        self.cnt = {e: 0 for e in self.ENG}
        self.prog = {e: [] for e in self.ENG}
        self.seen = {e: {} for e in self.ENG}
        self.scnt = {}

    def _sem(self, key):
        if key not in self.sems:
            self.sems[key] = self.es.enter_context(self.nc.semaphore("sem_" + key))
            self.scnt[key] = 0
        return self.sems[key]

    def _waits(self, eng, reads, writes):
        deps = {}

        def add(tok):
            if tok is None:
                return
            key, val = tok
            if key == eng and eng == "pe":
                return
            if deps.get(key, 0) < val:
                deps[key] = val

        for b in reads:
            add(b.lw)
        for b in writes:
            add(b.lw)
            for t in b.rd.items():
                add(t)
        out = []
        for key, val in deps.items():
            if self.seen[eng].get(key, 0) >= val:
                continue
            self.seen[eng][key] = val
            out.append((self._sem(key), val))
        return out

    def _commit(self, tok, reads, writes):
        for b in writes:
            b.lw = tok
            b.rd = {}
        for b in reads:
            if b not in writes:
                if b.rd.get(tok[0], 0) < tok[1]:
                    b.rd[tok[0]] = tok[1]

    def op(self, eng, fns, reads=(), writes=()):
        if callable(fns):
            fns = [fns]
        waits = self._waits(eng, reads, writes)
        self.cnt[eng] += 1
        assert self.cnt[eng] < 60000, eng
        self.prog[eng].append((waits, fns, (self.sems[eng], 1)))
        self._commit((eng, self.cnt[eng]), reads, writes)

    def dma(self, eng, fn, stream=None, reads=(), writes=()):
        if stream is None:
            stream = "d_" + writes[0].name
        stream = stream + ("_sw" if eng == "pool" else "_hw")
        sem = self._sem(stream)
        waits = self._waits(eng, reads, writes)
        self.scnt[stream] += 16
        assert self.scnt[stream] < 60000, stream
        self.prog[eng].append((waits, [fn], (sem, 16)))
        self._commit((stream, self.scnt[stream]), reads, writes)

    def wait_all(self, eng, bufs):
        waits = self._waits(eng, bufs, ())
        self.prog[eng].append((waits, [], None))

    def barrier(self):
        toks = [(e, self.cnt[e]) for e in self.ENG if self.cnt[e]] + [(k, v) for k, v in self.scnt.items() if v]
        for eng in self.ENG:
            waits = []
            for key, val in toks:
                if key == eng or self.seen[eng].get(key, 0) >= val:
                    continue
                self.seen[eng][key] = val
                waits.append((self._sem(key), val))
            self.prog[eng].append((waits, [], None))

    def finish(self, eng="sp", prefix="out"):
        fins = []
        for key, val in self.scnt.items():
            if key.startswith(prefix) and val:
                b = Buf("fin_" + key)
                b.lw = (key, val)
                fins.append(b)
        self.wait_all(eng, fins)

    def emit(self):
        nc = self.nc
        with nc.Block() as block:
            table = (("pe", block.tensor), ("act", block.scalar), ("dve", block.vector),
                     ("pool", block.gpsimd), ("sp", block.sync))
            for eng, deco in table:
                def body(e, eng=eng):
                    for waits, fns, inc in self.prog[eng]:
                        for s, v in waits:
                            e.wait_ge(s, v)
                        for f in fns[:-1]:
                            f(e)
                        if fns:
                            fns[-1](e).then_inc(inc[0], inc[1])
                deco(body)
        self.prog = {e: [] for e in self.ENG}


def build_nc(stage="full"):
    nc = bass.Bass("TRN2", target_bir_lowering=False)
    es = ExitStack()
    S = Sched(nc, es)

    _DRAM.clear()

    def dram_in(name, shape, dt=F32):
        _DRAM[name] = nc.dram_tensor(name, list(shape), dt, kind="ExternalInput").ap()
        return _DRAM[name]

    def dram_out(name, shape, dt=F32):
        return nc.dram_tensor(name, list(shape), dt, kind="ExternalOutput").ap()

    scope = [es]

    def sb(name, shape, dt):
        return scope[-1].enter_context(nc.sbuf_tensor(name, list(shape), dt))

    def open_phase():
        ph = ExitStack()
        scope.append(ph)
        return ph

    def close_phase():
        S.barrier()
        S.emit()
        scope.pop().close()

    xloc = dram_in("xloc", [TOK, D])
    xhalo = dram_in("xhalo", [HALO, D])
    cos_d = dram_in("ropecos", [128, TH])
    sin_d = dram_in("ropesin", [128, TH])
    masks_d = dram_in("masks", [128, 3, 128])
    ident_d = dram_in("ident", [128, 128])
    w_in = dram_in("w_in", [D, 1280])
    b_in = dram_in("b_in", [1280])
    sinks_d = dram_in("attn_sinks", [8])

    PS = [es.enter_context(nc.psum_tensor(f"ps{i}", [128, 512], F32)) for i in range(6)]
    PS += [es.enter_context(nc.psum_tensor(f"ps{i}", [128, 1024], BF16)) for i in (6, 7)]
    PSB = [Buf(f"ps{i}") for i in range(8)]

    identb = sb("identb", [128, 128], BF16)
    identf = sb("identf", [128, 128], F32)
    catT = sb("catT", [128, 8, TOK], BF16)
    open_phase()
    cosT = sb("cosT", [128, TH], F32)
    sinT = sb("sinT", [128, TH], F32)
    maskb = sb("maskb", [128, 3, 128], BF16)
    B_ident, B_mask, B_cos, B_sin = Buf("ident"), Buf("mask"), Buf("cos"), Buf("sin")
    S.dma("pool", lambda e: e.dma_start(out=identb[:, :], in_=ident_d[:, :]), writes=[B_ident])
    B_identf = Buf("identf")
    S.dma("sp", lambda e: e.dma_start(out=identf[:, :], in_=ident_d[:, :]), writes=[B_identf])
    S.dma("pool", lambda e: e.dma_start(out=maskb[:, :, :], in_=masks_d[:, :, :]), writes=[B_mask])
    S.dma("sp", lambda e: e.dma_start(out=cosT[:, :], in_=cos_d[:, :]), writes=[B_cos])
    S.dma("sp", lambda e: e.dma_start(out=sinT[:, :], in_=sin_d[:, :]), writes=[B_sin])

    win = sb("win", [128, KT, 1280], BF16)
    B_win = Buf("win")
    w_in_v = w_in.rearrange("(kt p) f -> p kt f", p=128)
    for h in range(2):
        S.dma("pool", lambda e, h=h: e.dma_start(out=win[:, 4 * h:4 * h + 4, :], in_=w_in_v[:, 4 * h:4 * h + 4, :]),
              writes=[B_win])
    wkb = sb("wkb", [128, KT, 128], BF16)
    wrot = sb("wrot", [128, KT, 6, 128], BF16)
    B_wrot = Buf("wrot")

    def src_col(m, half):
        if m < 4:
            return 128 * m + 64 * half
        if m == 4:
            return 512 + 64 * half
        return 512 + 64 * (1 - half)

    for half in range(2):
        c0 = src_col(5, half)
        S.op("dve", lambda e, half=half, c0=c0: e.tensor_copy(out=wkb[:, :, 64 * half:64 * half + 64], in_=win[:, :, c0:c0 + 64]),
             reads=[B_win], writes=[B_wrot])
    for m in range(6):
        for half in range(2):
            c0 = src_col(m, half)
            d0 = 64 * half
            S.op("dve", lambda e, m=m, c0=c0, d0=d0: e.tensor_scalar(
                out=wrot[:, :, m, d0:d0 + 32], in0=win[:, :, c0 + 32:c0 + 64], scalar1=-1.0, scalar2=None, op0=ALU.mult),
                reads=[B_win], writes=[B_wrot])
            S.op("dve", lambda e, m=m, c0=c0, d0=d0: e.tensor_copy(out=wrot[:, :, m, d0 + 32:d0 + 64], in_=win[:, :, c0:c0 + 32]),
                 reads=[B_win], writes=[B_wrot])

    bfm = sb("bfm", [128, 10], F32)
    bqk = sb("bqk", [128, 6], F32)
    bqkr = sb("bqkr", [128, 6], F32)
    bvbc = sb("bvbc", [128, 128], F32)
    B_bias, B_bfm, B_bvbc = Buf("bias"), Buf("bfm"), Buf("bvbc")
    brow = sb("brow", [10, 128], F32)
    B_brow = Buf("brow")
    S.dma("sp", lambda e: e.dma_start(out=brow[:, :], in_=b_in.rearrange("(t p) -> t p", p=128)), writes=[B_brow])
    S.op("pe", lambda e: e.transpose(out=PS[5][:, 0:10], in_=brow[:, :], identity=identf[0:10, 0:10]),
         reads=[B_brow, B_identf], writes=[PSB[5]])
    S.op("dve", lambda e: e.tensor_copy(out=bfm[:, :], in_=PS[5][:, 0:10]), reads=[PSB[5]], writes=[B_bfm])
    S.dma("sp", lambda e: e.dma_start(out=bvbc[:, :], in_=b_in[640:768].partition_broadcast(128)), writes=[B_bvbc])
    S.op("dve", lambda e: e.tensor_copy(out=bqk[:, 0:5], in_=bfm[:, 0:5]), reads=[B_bfm], writes=[B_bias])
    S.op("dve", lambda e: e.tensor_copy(out=bqk[0:64, 5:6], in_=bfm[64:128, 4:5]), reads=[B_bfm, B_bias], writes=[B_bias])
    S.op("dve", lambda e: e.tensor_copy(out=bqk[64:128, 5:6], in_=bfm[0:64, 4:5]), reads=[B_bfm, B_bias], writes=[B_bias])
    for hb in (0, 64):
        S.op("dve", lambda e, hb=hb: e.tensor_scalar(out=bqkr[hb:hb + 32, :], in0=bqk[hb + 32:hb + 64, :], scalar1=-1.0,
                                                     scalar2=None, op0=ALU.mult), reads=[B_bias], writes=[B_bias])
        S.op("dve", lambda e, hb=hb: e.tensor_copy(out=bqkr[hb + 32:hb + 64, :], in_=bqk[hb:hb + 32, :]),
             reads=[B_bias], writes=[B_bias])

    es8 = sb("es8", [128, 8], F32)
    esfm = sb("esfm", [128, 4], F32)
    B_sink = Buf("sink")
    S.dma("sp", lambda e: e.dma_start(out=es8[:, :], in_=sinks_d.partition_broadcast(128)), writes=[B_sink])
    S.op("act", lambda e: e.activation(out=es8[:, :], in_=es8[:, :], func=AF.Exp), reads=[B_sink], writes=[B_sink])
    S.op("dve", lambda e: e.tensor_copy(out=esfm[0:64, :], in_=es8[0:64, 0:8:2]), reads=[B_sink], writes=[B_sink])
    S.op("dve", lambda e: e.tensor_copy(out=esfm[64:128, :], in_=es8[64:128, 1:8:2]), reads=[B_sink], writes=[B_sink])

    xT = sb("xT", [128, KT, TH], BF16)
    B_xT = [Buf(f"xT{t}") for t in range(NT + 1)]
    xtok = [sb(f"xtok{i}", [128, D], BF16) for i in range(2)]
    B_xtok = [Buf(f"xtok{i}") for i in range(2)]
    for t in range(NT + 1):
        sl = t % 2
        src = xhalo[:, :] if t == 0 else xloc[(t - 1) * 128:t * 128, :]
        S.dma("pool", lambda e, sl=sl, src=src: e.dma_start(out=xtok[sl][:, :], in_=src), writes=[B_xtok[sl]])
        pb = 6 + sl
        pv = PS[pb][:, :]
        S.op("pe", [lambda e, k=k, sl=sl, pv=pv: e.transpose(out=pv[:, k * 128:(k + 1) * 128], in_=xtok[sl][:, k * 128:(k + 1) * 128],
                                                             identity=identb[:, :]) for k in range(KT)],
             reads=[B_xtok[sl], B_ident], writes=[PSB[pb]])
        ev = "dve" if t % 2 == 0 else "act"
        if ev == "dve":
            S.op("dve", lambda e, t=t, pv=pv: e.tensor_copy(out=xT[:, :, t * 128:(t + 1) * 128],
                                                           in_=pv.rearrange("p (k n) -> p k n", k=KT)),
                 reads=[PSB[pb]], writes=[B_xT[t]])
        else:
            S.op("act", lambda e, t=t, pv=pv: e.copy(out=xT[:, :, t * 128:(t + 1) * 128],
                                                    in_=pv.rearrange("p (k n) -> p k n", k=KT)),
                 reads=[PSB[pb]], writes=[B_xT[t]])

    if stage == "xt":
        dx = dram_out("dbg_xT", [D, TH])
        S.dma("pool", lambda e: e.dma_start(out=dx.rearrange("(k p) t -> p k t", p=128)[:, :, :], in_=xT[:, :, :]), stream="out",
              reads=B_xT, writes=[Buf("o")])
        db = dram_out("dbg_bfm", [128, 10])
        S.dma("sp", lambda e: e.dma_start(out=db[:, :], in_=bfm[:, :]), stream="out", reads=[B_bfm], writes=[Buf("o1")])
        S.finish()
        close_phase(); es.close()
        return nc, ["dbg_xT", "dbg_bfm"]

    qT = sb("qT", [128, 4, TOK], BF16)
    kT = sb("kT", [128, 2, TH], BF16)
    vpad = sb("vpad", [128, NT + 1, 2, 192], BF16)
    opad = sb("opad", [128, 192], BF16)
    B_pad = Buf("pad")
    S.op("pool", lambda e: e.memset(vpad[:, :, :, :], 0.0), writes=[B_pad])
    S.op("pool", lambda e: e.memset(opad[:, :], 0.0), writes=[B_pad])
    S.op("pool", lambda e: e.memset(opad[:, 64:128], 1.0), reads=[B_pad], writes=[B_pad])
    B_q = [Buf(f"q{b}") for b in range(4)]
    B_k = [Buf(f"k{b}") for b in range(5)]
    B_v = [Buf(f"v{t}") for t in range(NT + 1)]
    rtmp = [sb(f"rtmp{i}", [128, 2, 512], F32) for i in range(2)]
    B_rtmp = [Buf(f"rtmp{i}") for i in range(2)]
    it = 0

    def wcols(m):
        if m < 4:
            return lambda k: win[:, k, 128 * m:128 * m + 128]
        if m == 4:
            return lambda k: win[:, k, 512:640]
        return lambda k: wkb[:, k, :]

    blocks = [(0, 128, 0)] + [(128 + 512 * b, 512, b + 1) for b in range(4)]
    for (c0, n, bi) in blocks:
        tiles = range((c0 // 128), (c0 + n) // 128)
        xdeps = [B_xT[t] for t in tiles]
        for m in (range(6) if bi > 0 else (4, 5)):
            pa, pb = 2 * (it % 2), 2 * (it % 2) + 1
            wc = wcols(m)
            S.op("pe", [lambda e, k=k, wc=wc, pa=pa, c0=c0, n=n: e.matmul(PS[pa][:, 0:n], lhsT=wc(k), rhs=xT[:, k, c0:c0 + n],
                                                                         start=(k == 0), stop=(k == KT - 1)) for k in range(KT)],
                 reads=xdeps + [B_win, B_wrot], writes=[PSB[pa]])
            S.op("pe", [lambda e, k=k, m=m, pb=pb, c0=c0, n=n: e.matmul(PS[pb][:, 0:n], lhsT=wrot[:, k, m, :], rhs=xT[:, k, c0:c0 + n],
                                                                       start=(k == 0), stop=(k == KT - 1)) for k in range(KT)],
                 reads=xdeps + [B_wrot], writes=[PSB[pb]])
            rt = it % 2
            S.op("dve", lambda e, pa=pa, m=m, c0=c0, n=n, rt=rt: e.scalar_tensor_tensor(
                out=rtmp[rt][:, 0, 0:n], in0=PS[pa][:, 0:n], scalar=bqk[:, m:m + 1], in1=cosT[:, c0:c0 + n], op0=ALU.add, op1=ALU.mult),
                reads=[PSB[pa], B_bias, B_cos], writes=[B_rtmp[rt]])
            S.op("dve", lambda e, pb=pb, m=m, c0=c0, n=n, rt=rt: e.scalar_tensor_tensor(
                out=rtmp[rt][:, 1, 0:n], in0=PS[pb][:, 0:n], scalar=bqkr[:, m:m + 1], in1=sinT[:, c0:c0 + n], op0=ALU.add, op1=ALU.mult),
                reads=[PSB[pb], B_bias, B_sin], writes=[B_rtmp[rt]])
            if m < 4:
                dst, dbuf = qT[:, m, c0 - 128:c0 - 128 + n], B_q[bi - 1]
            else:
                dst, dbuf = kT[:, m - 4, c0:c0 + n], B_k[bi]
            S.op("dve", lambda e, dst=dst, rt=rt, n=n: e.tensor_tensor(out=dst, in0=rtmp[rt][:, 0, 0:n], in1=rtmp[rt][:, 1, 0:n], op=ALU.add),
                 reads=[B_rtmp[rt]], writes=[dbuf])
            it += 1
    for t in range(NT + 1):
        pv_ = 4 + (t % 2)
        S.op("pe", [lambda e, k=k, t=t, pv_=pv_: e.matmul(PS[pv_][:, 0:128], lhsT=xT[:, k, t * 128:(t + 1) * 128], rhs=win[:, k, 640:768],
                                                          start=(k == 0), stop=(k == KT - 1)) for k in range(KT)],
             reads=[B_xT[t], B_win], writes=[PSB[pv_]])
        S.op("dve", lambda e, t=t, pv_=pv_: e.tensor_tensor(out=vpad[:, t, :, 64:128], in0=PS[pv_][:, 0:128].rearrange("p (g d) -> p g d", g=2),
                                                            in1=bvbc[:, :].rearrange("p (g d) -> p g d", g=2), op=ALU.add),
             reads=[PSB[pv_], B_bvbc, B_pad], writes=[B_v[t]])

    if stage == "qk":
        dq = dram_out("dbg_q", [512, TOK])
        S.dma("pool", lambda e: e.dma_start(out=dq.rearrange("(c p) t -> p c t", p=128)[:, :, :], in_=qT[:, :, :]), stream="out", reads=B_q, writes=[Buf("o2")])
        dk = dram_out("dbg_k", [256, TH])
        S.dma("pool", lambda e: e.dma_start(out=dk.rearrange("(c p) t -> p c t", p=128)[:, :, :], in_=kT[:, :, :]), stream="out", reads=B_k, writes=[Buf("o3")])
        dvv = dram_out("dbg_v", [TH, 128])
        for g in range(2):
            S.dma("pool", lambda e, g=g: e.dma_start(out=dvv[:, 64 * g:64 * g + 64].rearrange("(t p) d -> p t d", p=128)[:, :, :],
                                                     in_=vpad[:, :, g, 64:128]),
                  stream="out", reads=B_v, writes=[Buf(f"o4{g}")])
        S.finish()
        close_phase(); es.close()
        return nc, ["dbg_q", "dbg_k", "dbg_v"]

    kTz = sb("kTz", [128, 4, TH], BF16)
    B_kz0 = Buf("kz0")
    S.op("pool", lambda e: e.memset(kTz[:, :, :], 0.0), writes=[B_kz0])
    B_kz = [Buf(f"kz{b}") for b in range(5)]
    kz_map = ((0, 0, 0), (1, 1, 1), (2, 0, 1), (3, 1, 0))
    for (c0, n, bi) in blocks:
        for (vi, half, var) in kz_map:
            p0 = 64 * half
            S.op("dve", lambda e, vi=vi, p0=p0, var=var, c0=c0, n=n: e.tensor_copy(out=kTz[p0:p0 + 64, vi, c0:c0 + n], in_=kT[p0:p0 + 64, var, c0:c0 + n]),
                 reads=[B_k[bi], B_kz0], writes=[B_kz[bi]])

    B_cat = [Buf(f"cat{t}") for t in range(NT)]
    pT = [sb(f"pT{i}", [128, 2, 512], BF16) for i in range(2)]
    B_pT = [Buf(f"pT{i}") for i in range(2)]
    ltmp = [sb(f"ltmp{i}", [128, 128], F32) for i in range(2)]
    B_ltmp = [Buf(f"ltmp{i}") for i in range(2)]
    kblk = lambda t: B_k[0] if t == 0 else B_k[1 + (t - 1) // 4]
    kzblk = lambda t: B_kz[0] if t == 0 else B_kz[1 + (t - 1) // 4]
    n_qblk = 1 if stage == "attn1" else NT

    def swa_F(i, g, sl):
        for kk, ktile in enumerate((i, i + 1)):
            pbk = 2 * sl + kk
            fns = []
            for hh in range(4):
                h = 4 * g + hh
                half = h % 2
                fns.append(lambda e, pbk=pbk, hh=hh, h=h, half=half, ktile=ktile: e.matmul(
                    PS[pbk][:, hh * 128:(hh + 1) * 128],
                    lhsT=kTz[:, 2 * g + half, ktile * 128:(ktile + 1) * 128],
                    rhs=qT[:, h // 2, i * 128:(i + 1) * 128], start=True, stop=True))
            S.op("pe", fns, reads=[kzblk(ktile), B_q[i // 4]], writes=[PSB[pbk]])
            S.op("act", lambda e, pbk=pbk, kk=kk: e.activation(out=pT[sl][:, kk, :], in_=PS[pbk][:, :], func=AF.Exp, scale=0.125),
                 reads=[PSB[pbk]], writes=[B_pT[sl]])
            mi = 1 if kk == 1 else (2 if i == 0 else 0)
            S.op("dve", lambda e, kk=kk, mi=mi: e.tensor_tensor(
                out=pT[sl][:, kk, :].rearrange("p (h q) -> p h q", h=4), in0=pT[sl][:, kk, :].rearrange("p (h q) -> p h q", h=4),
                in1=maskb[:, mi, :].unsqueeze(1).to_broadcast([128, 4, 128]), op=ALU.mult),
                reads=[B_pT[sl], B_mask], writes=[B_pT[sl]])

    def swa_G(i, g, sl):
        for tt in range(2):
            ct = 2 * g + tt
            po = 4 + tt
            fo, fl = [], []
            n_mm = 0
            for hl in range(2):
                hh = 2 * tt + hl
                for kk, ktile in enumerate((i, i + 1)):
                    first, last = (n_mm == 0), (n_mm == 3)
                    vs = (64, 192) if hl == 0 else (0, 128)
                    fo.append(lambda e, po=po, vs=vs, ktile=ktile, kk=kk, hh=hh, first=first, last=last: e.matmul(
                        PS[po][:, 0:128], lhsT=vpad[:, ktile, g, vs[0]:vs[1]], rhs=pT[sl][:, kk, hh * 128:(hh + 1) * 128],
                        start=first, stop=last))
                    fl.append(lambda e, po=po, vs=vs, kk=kk, hh=hh, first=first, last=last: e.matmul(
                        PS[po][:, 128:256], lhsT=opad[:, vs[0]:vs[1]], rhs=pT[sl][:, kk, hh * 128:(hh + 1) * 128],
                        start=first, stop=last))
                    n_mm += 1
            S.op("pe", fo + fl, reads=[B_pT[sl], B_v[i], B_v[i + 1], B_pad], writes=[PSB[po]])
            ls = tt
            S.op("dve", lambda e, po=po, ct=ct, ls=ls: e.tensor_scalar(out=ltmp[ls][:, :], in0=PS[po][:, 128:256], scalar1=esfm[:, ct:ct + 1],
                                                                        scalar2=None, op0=ALU.add), reads=[PSB[po], B_sink], writes=[B_ltmp[ls]])
            S.op("dve", lambda e, ls=ls: e.reciprocal(out=ltmp[ls][:, :], in_=ltmp[ls][:, :]), reads=[B_ltmp[ls]], writes=[B_ltmp[ls]])
            S.op("dve", lambda e, po=po, ct=ct, ls=ls: e.tensor_tensor(out=catT[:, ct, i * 128:(i + 1) * 128], in0=PS[po][:, 0:128],
                                                                       in1=ltmp[ls][:, :], op=ALU.mult),
                 reads=[PSB[po], B_ltmp[ls]], writes=[B_cat[i]])

    its = [(i, g) for i in range(n_qblk) for g in range(2)]
    for n, (i, g) in enumerate(its):
        swa_F(i, g, n % 2)
        if n > 0:
            swa_G(its[n - 1][0], its[n - 1][1], (n - 1) % 2)
    swa_G(its[-1][0], its[-1][1], (len(its) - 1) % 2)

    outs = []
    if stage in ("attn", "attn1"):
        dbg = dram_out("dbg_attn", [512, TOK])
        dv = dbg.rearrange("(c p) t -> p c t", p=128)
        nqo = 128 * n_qblk
        S.dma("pool", lambda e: e.dma_start(out=dv[:, :, 0:nqo], in_=catT[:, 0:4, 0:nqo]), stream="out", reads=B_cat[:n_qblk], writes=[Buf("o")])
        dq = dram_out("dbg_q", [512, TOK])
        S.dma("pool", lambda e: e.dma_start(out=dq.rearrange("(c p) t -> p c t", p=128)[:, :, :], in_=qT[:, :, :]), stream="out", reads=B_q, writes=[Buf("o2")])
        dk = dram_out("dbg_k", [256, TH])
        S.dma("pool", lambda e: e.dma_start(out=dk.rearrange("(c p) t -> p c t", p=128)[:, :, :], in_=kT[:, :, :]), stream="out", reads=B_k, writes=[Buf("o3")])
        outs = ["dbg_attn", "dbg_q", "dbg_k"]
        S.finish()
        close_phase(); es.close()
        return nc, outs
    close_phase()
    print("[build] sbuf free after phase 1:", nc.sbuf_bytes_remaining)
    return build_rest(nc, es, S, sb, open_phase, close_phase, dram_in, dram_out, PS, PSB, identb, identf, catT, B_cat,
                      B_ident, B_identf, stage)


def build_rest(nc, es, S, sb, open_phase, close_phase, dram_in, dram_out, PS, PSB, identb, identf, catT, B_cat,
               B_ident, B_identf, stage):
    xloc = nc_ap(nc, "xloc")
    xprev = dram_in("xprev", [3 * TOK, D])
    segv_d = dram_in("segvalid", [128, 4])
    w_in = nc_ap(nc, "w_in")
    b_in = nc_ap(nc, "b_in")
    lam_re_d = dram_in("s5_lambda_re", [32, 64]); lam_im_d = dram_in("s5_lambda_im", [32, 64])
    logdt_d = dram_in("s5_log_dt", [32])
    bre_d = dram_in("s5_b_re", [32, 64, 16]); bim_d = dram_in("s5_b_im", [32, 64, 16])
    cre_d = dram_in("s5_c_re", [32, 16, 64]); cim_d = dram_in("s5_c_im", [32, 16, 64])
    d_d = dram_in("s5_d", [512])
    wglu_d = dram_in("s5_w_glu", [512, 512]); bglu_d = dram_in("s5_b_glu", [512])
    cmask_d = dram_in("cmask", [128, 2, 256])

    open_phase()
    B_tb = Buf("tb")
    B_t4 = Buf("t4")

    def tt(out, a, b, op, eng="dve"):
        S.op(eng, lambda e: e.tensor_tensor(out=out, in0=a, in1=b, op=op), reads=[B_tb, B_t4], writes=[B_tb, B_t4])

    def ts(out, a, s1, op0, s2=None, op1=None):
        if op1 is None:
            S.op("dve", lambda e: e.tensor_scalar(out=out, in0=a, scalar1=s1, scalar2=None, op0=op0), reads=[B_tb, B_t4], writes=[B_tb, B_t4])
        else:
            S.op("dve", lambda e: e.tensor_scalar(out=out, in0=a, scalar1=s1, scalar2=s2, op0=op0, op1=op1), reads=[B_tb, B_t4], writes=[B_tb, B_t4])

    def TT(out, a, b, op, reads, writes, eng="dve"):
        S.op(eng, lambda e: e.tensor_tensor(out=out, in0=a, in1=b, op=op), reads=reads, writes=writes)

    def cp(out, a):
        S.op("dve", lambda e: e.tensor_copy(out=out, in_=a), reads=[B_tb, B_t4], writes=[B_tb, B_t4])

    def act(out, a, func, scale=1.0):
        S.op("act", lambda e: e.activation(out=out, in_=a, func=func, scale=scale), reads=[B_tb, B_t4], writes=[B_tb, B_t4])

    def cmul(outr, outi, ar, ai, br, bi, t1, t2, neg_i=False):
        tt(t1, ar, br, ALU.mult); tt(t2, ai, bi, ALU.mult); tt(outr, t1, t2, ALU.subtract)
        tt(t1, ar, bi, ALU.mult); tt(t2, ai, br, ALU.mult)
        if neg_i:
            tt(t1, t1, t2, ALU.add); ts(outi, t1, -1.0, ALU.mult)
        else:
            tt(outi, t1, t2, ALU.add)

    U = sb("s5U", [128, 32, 16, 16], BF16)
    UT = sb("s5UT", [128, 32, 2, 128], BF16)
    Hst = sb("s5H", [128, 32, 128], BF16)
    A_ = sb("s5A", [64, 2, 32, 17], F32)
    AI = sb("s5AI", [64, 2, 32, 16], F32)
    B15 = sb("s5B15", [64, 2, 32, 16], F32)
    CT = sb("s5CT", [64, 2, 32, 16], F32)
    C15 = sb("s5C15", [64, 2, 32, 16], F32)
    t4 = sb("s5t4", [128, 2, 2048], F32)
    t4a = t4[0:64, 0, :].rearrange("p (g s h) -> p g s h", g=8, s=16)
    t4b = t4[0:64, 1, :].rearrange("p (g s h) -> p g s h", g=8, s=16)
    ztmp = t4[:, 0, :].rearrange("p (g c) -> p g c", g=16)
    PSt = sb("s5PSt", [128, 8, 256], BF16)
    B_U, B_UT, B_H = Buf("s5U"), Buf("s5UT"), Buf("s5H")

    def build_PSt(g0):
        bc_s = lambda t, ri: t[:, ri, g0:g0 + 8, :].unsqueeze(3).to_broadcast([64, 8, 16, 16])
        bc_h = lambda t, ri: t[:, ri, g0:g0 + 8, :].unsqueeze(2).to_broadcast([64, 8, 16, 16])
        o4 = lambda lo: PSt[lo:lo + 64, :, :].rearrange("p g (s h) -> p g s h", s=16)
        tt(t4a, bc_s(AI, 0), bc_h(B15, 0), ALU.mult); tt(t4b, bc_s(AI, 1), bc_h(B15, 1), ALU.mult)
        tt(o4(0), t4a, t4b, ALU.subtract)
        tt(t4a, bc_s(AI, 0), bc_h(B15, 1), ALU.mult); tt(t4b, bc_s(AI, 1), bc_h(B15, 0), ALU.mult)
        tt(o4(64), t4a, t4b, ALU.add)

    open_phase()
    LA = sb("s5LA", [128, 32, 2, 128], BF16)
    R = sb("s5R", [128, 2, 16, 129], F32)
    rho_p = sb("s5rho", [128, 16], F32)
    segv = sb("s5segv", [128, 4], F32)
    open_phase()
    sm = sb("s5sm", [64, 56, 32], F32)
    smi = iter(range(56))
    T = lambda: sm[:, next(smi), :]
    lamn = sb("s5lamn", [32, 2, 64], F32)
    Bnat = sb("s5Bnat", [128, 2, 16, 16], F32)
    Cnat = sb("s5Cnat", [128, 2, 4, 64], F32)
    Bq = sb("s5Bq", [64, 2, 32, 16], F32)
    Bbar = sb("s5Bbar", [64, 2, 32, 16], F32)
    t3a = sb("s5t3a", [64, 32, 16], F32)
    t3b = sb("s5t3b", [64, 32, 16], F32)
    S.dma("sp", lambda e: e.dma_start(out=lamn[:, 0, :], in_=lam_re_d[:, :]), stream="tbl", writes=[B_tb])
    S.dma("sp", lambda e: e.dma_start(out=lamn[:, 1, :], in_=lam_im_d[:, :]), stream="tbl", writes=[B_tb])
    dtb = T()
    S.dma("sp", lambda e: e.dma_start(out=dtb, in_=logdt_d.partition_broadcast(64)), stream="tbl", writes=[B_tb])
    for ri, src in enumerate((bre_d, bim_d)):
        S.dma("sp", lambda e, ri=ri, src=src: e.dma_start(out=Bnat[:, ri, :, :], in_=src.rearrange("g p h -> (g p) h").rearrange("(gg r) h -> r gg h", r=128)),
              stream="tbl", writes=[B_tb])
    for ri, src in enumerate((cre_d, cim_d)):
        S.dma("sp", lambda e, ri=ri, src=src: e.dma_start(out=Cnat[:, ri, :, :], in_=src.rearrange("g h p -> (g h) p").rearrange("(t r) p -> r t p", r=128)),
              stream="tbl", writes=[B_tb])
    S.dma("sp", lambda e: e.dma_start(out=segv[:, :], in_=segv_d[:, :]), stream="tbl", writes=[B_tb])
    S.op("pe", [lambda e, ri=ri: e.transpose(out=PS[5][0:64, 32 * ri:32 * ri + 32], in_=lamn[:, ri, :], identity=identf[0:32, 0:32]) for ri in range(2)],
         reads=[B_tb, B_identf], writes=[PSB[5]])
    lam_r, lam_i = T(), T()
    S.op("dve", lambda e: e.tensor_copy(out=lam_r, in_=PS[5][0:64, 0:32]), reads=[PSB[5], B_tb], writes=[B_tb])
    S.op("dve", lambda e: e.tensor_copy(out=lam_i, in_=PS[5][0:64, 32:64]), reads=[PSB[5], B_tb], writes=[B_tb])
    for ri in range(2):
        S.op("pe", [lambda e, ri=ri, t=t: e.transpose(out=PS[4][0:64, 128 * t:128 * t + 128], in_=Cnat[:, ri, t, :], identity=identf[:, :]) for t in range(4)],
             reads=[B_tb, B_identf], writes=[PSB[4]])
        S.op("dve", lambda e, ri=ri: e.tensor_copy(out=CT[:, ri, :, :].rearrange("p g h -> p (g h)"), in_=PS[4][0:64, :]), reads=[PSB[4], B_tb], writes=[B_tb])
    for ri in range(2):
        cp(Bq[:, ri, 0:32:2, :], Bnat[0:64, ri, :, :])
        cp(Bq[:, ri, 1:32:2, :], Bnat[64:128, ri, :, :])
    act(dtb, dtb, AF.Exp)
    lr = T(); ts(lr, lam_r, -1e-4, ALU.min)
    mu, th = T(), T(); tt(mu, lr, dtb, ALU.mult); tt(th, lam_i, dtb, ALU.mult)
    sn, sh, cs = T(), T(), T()
    act(sn, th, AF.Sin, scale=0.125); act(sh, th, AF.Sin, scale=0.0625)
    tt(sh, sh, sh, ALU.mult); ts(cs, sh, -2.0, ALU.mult, 1.0, ALU.add)
    u1, u2 = T(), T()
    for _ in range(3):
        cn, s2 = T(), T()
        tt(u1, cs, cs, ALU.mult); tt(u2, sn, sn, ALU.mult); tt(cn, u1, u2, ALU.subtract)
        tt(u1, cs, sn, ALU.mult); ts(s2, u1, 2.0, ALU.mult)
        cs, sn = cn, s2
    mag, ar, ai = T(), T(), T()
    act(mag, mu, AF.Exp); tt(ar, mag, cs, ALU.mult); tt(ai, mag, sn, ALU.mult)
    den, am1, zr, zi = T(), T(), T(), T()
    tt(u1, lr, lr, ALU.mult); tt(u2, lam_i, lam_i, ALU.mult); tt(den, u1, u2, ALU.add)
    S.op("dve", lambda e: e.reciprocal(out=den, in_=den), reads=[B_tb, B_t4], writes=[B_tb, B_t4])
    ts(am1, ar, -1.0, ALU.add)
    tt(u1, am1, lr, ALU.mult); tt(u2, ai, lam_i, ALU.mult); tt(u1, u1, u2, ALU.add); tt(zr, u1, den, ALU.mult)
    tt(u1, ai, lr, ALU.mult); tt(u2, am1, lam_i, ALU.mult); tt(u1, u1, u2, ALU.subtract); tt(zi, u1, den, ALU.mult)
    bz = lambda t: t.unsqueeze(2).to_broadcast([64, 32, 16])
    cmul(Bbar[:, 0, :, :], Bbar[:, 1, :, :], bz(zr), bz(zi), Bq[:, 0, :, :], Bq[:, 1, :, :], t3a[:, :, :], t3b[:, :, :])
    air, aii = T(), T()
    tt(u1, ar, ar, ALU.mult); tt(u2, ai, ai, ALU.mult); tt(u1, u1, u2, ALU.add)
    S.op("dve", lambda e: e.reciprocal(out=u1, in_=u1), reads=[B_tb, B_t4], writes=[B_tb, B_t4])
    tt(air, ar, u1, ALU.mult); tt(aii, ai, u1, ALU.mult); ts(aii, aii, -1.0, ALU.mult)

    def cpow(tab, n, br, bi, npart, shp, tmpa, tmpb, bm_bufs):
        nd = len(shp)
        S.op("dve", lambda e: e.memset(tab_slice(tab, 0, nd, 0, 1), 1.0), reads=[B_tb, B_t4], writes=[B_tb, B_t4])
        S.op("dve", lambda e: e.memset(tab_slice(tab, 1, nd, 0, 1), 0.0), reads=[B_tb, B_t4], writes=[B_tb, B_t4])
        filled, bmr, bmi, bi_ = 1, br, bi, 0
        while filled < n:
            cnt = min(filled, n - filled)
            bb = lambda t: t.unsqueeze(nd + 1).to_broadcast([npart] + shp + [cnt])
            cmul(tab_slice(tab, 0, nd, filled, filled + cnt), tab_slice(tab, 1, nd, filled, filled + cnt),
                 tab_slice(tab, 0, nd, 0, cnt), tab_slice(tab, 1, nd, 0, cnt), bb(bmr), bb(bmi),
                 tab_slice(tmpa, None, nd, 0, cnt), tab_slice(tmpb, None, nd, 0, cnt))
            filled += cnt
            if filled < n:
                nr, ni = bm_bufs[bi_]; bi_ += 1
                q1, q2 = bm_bufs[-1]
                tt(q1, bmr, bmr, ALU.mult); tt(q2, bmi, bmi, ALU.mult); tt(nr, q1, q2, ALU.subtract)
                tt(q1, bmr, bmi, ALU.mult); ts(ni, q1, 2.0, ALU.mult)
                bmr, bmi = nr, ni

    def tab_slice(tab, ri, nd, k0, k1):
        if ri is None:
            return tab[:, :, k0:k1] if nd == 1 else tab[:, :, :, k0:k1]
        return tab[:, ri, :, k0:k1] if nd == 1 else tab[:, ri, :, :, k0:k1]

    t17a = sb("s5t17a", [64, 32, 8], F32); t17b = sb("s5t17b", [64, 32, 8], F32)
    bmA = [(T(), T()) for _ in range(5)]
    cpow(A_, 17, ar, ai, 64, [32], t17a, t17b, bmA)
    bmI = [(T(), T()) for _ in range(5)]
    cpow(AI, 16, air, aii, 64, [32], t17a, t17b, bmI)
    b15 = lambda t, ri, k: t[:, ri, :, k:k + 1].to_broadcast([64, 32, 16])
    cmul(B15[:, 0, :, :], B15[:, 1, :, :], b15(A_, 0, 15), b15(A_, 1, 15), Bbar[:, 0, :, :], Bbar[:, 1, :, :], t3a[:, :, :], t3b[:, :, :])
    cmul(C15[:, 0, :, :], C15[:, 1, :, :], b15(AI, 0, 15), b15(AI, 1, 15), CT[:, 0, :, :], CT[:, 1, :, :], t3a[:, :, :], t3b[:, :, :])
    rho = T(); act(rho, mu, AF.Exp, scale=16.0)
    rr = T()
    S.op("dve", lambda e: e.reciprocal(out=rr, in_=rho), reads=[B_tb, B_t4], writes=[B_tb, B_t4])
    rot_r, rot_i = T(), T()
    tt(rot_r, A_[:, 0, :, 16], rr, ALU.mult); tt(rot_i, A_[:, 1, :, 16], rr, ALU.mult)
    rotp = sb("s5rotp", [128, 2, 16], F32)
    for src, dst in ((rot_r, rotp[:, 0, :]), (rot_i, rotp[:, 1, :]), (rho, rho_p[:, :])):
        cp(dst[0:64], src[:, 0:32:2]); cp(dst[64:128], src[:, 1:32:2])
    tRa = sb("s5tRa", [128, 16, 64], F32); tRb = sb("s5tRb", [128, 16, 64], F32)
    smp = sb("s5smp", [128, 16, 16], F32)
    bmR = [(smp[:, 2 * i, :], smp[:, 2 * i + 1, :]) for i in range(8)]
    cpow(R, 129, rotp[:, 0, :], rotp[:, 1, :], 128, [16], tRa, tRb, bmR)
    for gb in range(4):
        build_PSt(8 * gb)
        for hb in range(2):
            bank = 6 + hb
            S.op("pe", [lambda e, j=j, hb=hb, bank=bank: e.transpose(out=PS[bank][:, 128 * j:128 * j + 128],
                                                                     in_=PSt[:, 4 * hb + j // 2, 128 * (j % 2):128 * (j % 2) + 128], identity=identb[:, :])
                        for j in range(8)], reads=[B_tb, B_ident], writes=[PSB[bank]])
            S.op("act", lambda e, gb=gb, hb=hb, bank=bank: e.copy(out=LA[:, 8 * gb + 4 * hb:8 * gb + 4 * hb + 4, :, :].rearrange("p g k c -> p (g k c)"),
                                                                  in_=PS[bank][:, :]), reads=[PSB[bank], B_tb], writes=[B_tb])

    close_phase()
    print("[build] sbuf free before S5 segment buffers:", nc.sbuf_bytes_remaining)

    wu = sb("s5wu", [128, KT, 512], BF16)
    bubc = sb("s5bubc", [128, 512], F32)
    B_wu, B_bu = Buf("s5wu"), Buf("s5bu")
    S.dma("pool", lambda e: e.dma_start(out=wu[:, :, :], in_=w_in.rearrange("(kt p) f -> p kt f", p=128)[:, :, 768:1280]), writes=[B_wu])
    S.dma("sp", lambda e: e.dma_start(out=bubc[:, :], in_=b_in[768:1280].partition_broadcast(128)), writes=[B_bu])
    xs = [sb(f"s5xs{i}", [128, D], BF16) for i in range(2)]
    B_xs = [Buf(f"s5xs{i}") for i in range(2)]
    xsf = [sb(f"s5xsf{i}", [128, D], F32) for i in range(2)]
    B_xsf = [Buf(f"s5xsf{i}") for i in range(2)]
    xsT = [sb(f"s5xsT{i}", [128, KT, 128], BF16) for i in range(2)]
    B_xsT = [Buf(f"s5xsT{i}") for i in range(2)]
    Z = sb("s5Z", [128, 16, 2, 128], F32)
    G2 = sb("s5G2", [128, 16, 2, 129], F32)
    B_Z, B_G = Buf("s5Z"), Buf("s5G")
    ctmp = sb("s5ctmp", [128, 4, 16], F32)
    S.op("dve", lambda e: e.memset(G2[:, :, :, 0:1], 0.0), reads=[B_G], writes=[B_G])
    ev = 0
    Ud, UTd, BUd, BUTd = U, UT, B_U, B_UT
    for seg in range(4):
        seg_v = (xprev[seg * TOK:(seg + 1) * TOK, :] if seg < 3 else xloc[:, :]).rearrange("(c s) d -> s c d", s=16)
        for s_ in range(16):
            sl = s_ % 2
            S.dma("sp", lambda e, sl=sl, s_=s_, seg_v=seg_v: e.dma_start(out=xsf[sl][:, :], in_=seg_v[s_]), writes=[B_xsf[sl]])
            S.op("act", lambda e, sl=sl: e.copy(out=xs[sl][:, :], in_=xsf[sl][:, :]), reads=[B_xsf[sl]], writes=[B_xs[sl]])
            bank = 6 + sl
            S.op("pe", [lambda e, k=k, sl=sl, bank=bank: e.transpose(out=PS[bank][:, k * 128:(k + 1) * 128], in_=xs[sl][:, k * 128:(k + 1) * 128],
                                                                     identity=identb[:, :]) for k in range(KT)],
                 reads=[B_xs[sl], B_ident], writes=[PSB[bank]])
            eng = "dve" if s_ % 2 == 0 else "act"
            fn = (lambda e, sl=sl, bank=bank: e.tensor_copy(out=xsT[sl][:, :, :], in_=PS[bank][:, :].rearrange("p (k n) -> p k n", k=KT))) if eng == "dve" else \
                 (lambda e, sl=sl, bank=bank: e.copy(out=xsT[sl][:, :, :], in_=PS[bank][:, :].rearrange("p (k n) -> p k n", k=KT)))
            S.op(eng, fn, reads=[PSB[bank]], writes=[B_xsT[sl]])
            pb = s_ % 4
            S.op("pe", [lambda e, k=k, sl=sl, pb=pb: e.matmul(PS[pb][:, :], lhsT=xsT[sl][:, k, :], rhs=wu[:, k, :], start=(k == 0), stop=(k == KT - 1))
                        for k in range(KT)], reads=[B_xsT[sl], B_wu], writes=[PSB[pb]])
            S.op("dve", lambda e, s_=s_, pb=pb: e.tensor_tensor(out=Ud[:, :, s_, :], in0=PS[pb][:, :].rearrange("p (g h) -> p g h", g=32),
                                                                 in1=bubc[:, :].rearrange("p (g h) -> p g h", g=32), op=ALU.add),
                 reads=[PSB[pb], B_bu], writes=[BUd])
        for gq in range(8):
            bank = 6 + gq % 2
            S.op("pe", [lambda e, j=j, gq=gq, bank=bank, Ud=Ud: e.transpose(out=PS[bank][:, 128 * j:128 * j + 128],
                                                                          in_=Ud[:, 4 * gq + j // 2, 8 * (j % 2):8 * (j % 2) + 8, :].rearrange("p s h -> p (s h)"),
                                                                          identity=identb[:, :]) for j in range(8)],
                 reads=[BUd, B_ident], writes=[PSB[bank]])
            eng = "dve" if gq % 2 == 0 else "act"
            dst = UTd[:, 4 * gq:4 * gq + 4, :, :].rearrange("p g k c -> p (g k c)")
            fn = (lambda e, dst=dst, bank=bank: e.tensor_copy(out=dst, in_=PS[bank][:, :])) if eng == "dve" else (lambda e, dst=dst, bank=bank: e.copy(out=dst, in_=PS[bank][:, :]))
            S.op(eng, fn, reads=[PSB[bank]], writes=[BUTd])
        for gq in range(8):
            pb = gq % 4
            fns = []
            for j in range(4):
                g = 4 * gq + j
                for kh in range(2):
                    fns.append(lambda e, g=g, kh=kh, j=j, pb=pb, UTd=UTd: e.matmul(PS[pb][:, 128 * j:128 * j + 128], lhsT=LA[:, g, kh, :], rhs=UTd[:, g, kh, :],
                                                                                 start=(kh == 0), stop=(kh == 1)))
            S.op("pe", fns, reads=[BUTd, B_tb], writes=[PSB[pb]])
            pv = PS[pb][:, :].rearrange("p (j c) -> p j c", j=4)
            for q in range(2):
                for ri in range(2):
                    eng = "dve" if ev % 2 == 0 else "act"; ev += 1
                    dst = Z[64 * q:64 * q + 64, 2 * gq:2 * gq + 2, ri, :]
                    srcv = pv[64 * ri:64 * ri + 64, q:4:2, :]
                    fn = (lambda e, dst=dst, srcv=srcv: e.tensor_copy(out=dst, in_=srcv)) if eng == "dve" else (lambda e, dst=dst, srcv=srcv: e.copy(out=dst, in_=srcv))
                    S.op(eng, fn, reads=[PSB[pb], B_Z], writes=[B_Z])
        if stage == "s5z" and seg == 3:
            dz = dram_out("dbg_Z", [128, 16, 2, 128])
            S.dma("sp", lambda e: e.dma_start(out=dz.rearrange("p a b c -> p (a b c)"), in_=Z[:, :, :, :].rearrange("p a b c -> p (a b c)")), stream="out", reads=[B_Z], writes=[Buf("o")])
            S.finish(); close_phase(); close_phase(); es.close()
            return nc, ["dbg_Z"]
        Rr, Ri = R[:, 0, :, 1:129], R[:, 1, :, 1:129]
        zr_, zi_ = Z[:, :, 0, :], Z[:, :, 1, :]
        dz = dict(reads=[B_Z, B_tb, B_G, B_t4], writes=[B_G, B_t4])
        gi_r, gi_i = G2[:, :, 0, 1:129], G2[:, :, 1, 1:129]
        TT(gi_r, Rr, zr_, ALU.mult, **dz)
        TT(ztmp, Ri, zi_, ALU.mult, **dz)
        TT(gi_r, gi_r, ztmp, ALU.add, **dz)
        TT(gi_i, Rr, zi_, ALU.mult, **dz)
        TT(ztmp, Ri, zr_, ALU.mult, **dz)
        TT(gi_i, gi_i, ztmp, ALU.subtract, **dz)
        S.op("dve", [lambda e, gg=gg, ri=ri: e.tensor_tensor_scan(out=G2[:, gg, ri, 1:129], data0=rho_p[:, gg:gg + 1].to_broadcast([128, 128]),
                                                                    data1=G2[:, gg, ri, 1:129], initial=G2[:, gg, ri, 0:1], op0=ALU.mult, op1=ALU.add)
                     for gg in range(16) for ri in range(2)], reads=[B_G, B_tb], writes=[B_G])
        if seg < 3:
            c_ = lambda i: ctmp[:, i, :]
            TT(c_(0), R[:, 0, :, 128], G2[:, :, 0, 128], ALU.mult, **dz)
            TT(c_(1), R[:, 1, :, 128], G2[:, :, 1, 128], ALU.mult, **dz)
            TT(c_(0), c_(0), c_(1), ALU.subtract, **dz)
            TT(c_(2), R[:, 0, :, 128], G2[:, :, 1, 128], ALU.mult, **dz)
            TT(c_(3), R[:, 1, :, 128], G2[:, :, 0, 128], ALU.mult, **dz)
            TT(c_(2), c_(2), c_(3), ALU.add, **dz)
            S.op("dve", lambda e, seg=seg: e.tensor_scalar(out=G2[:, :, 0, 0], in0=c_(0), scalar1=segv[:, seg:seg + 1], scalar2=None, op0=ALU.mult), **dz)
            S.op("dve", lambda e, seg=seg: e.tensor_scalar(out=G2[:, :, 1, 0], in0=c_(2), scalar1=segv[:, seg:seg + 1], scalar2=None, op0=ALU.mult), **dz)
    Rr, Ri = R[:, 0, :, 0:128], R[:, 1, :, 0:128]
    gr, gi = G2[:, :, 0, 0:128], G2[:, :, 1, 0:128]
    dh = dict(reads=[B_G, B_tb, B_H], writes=[B_H])
    z0, z1 = Z[:, :, 0, :], Z[:, :, 1, :]
    dzz = dict(reads=[B_G, B_tb, B_Z, B_t4, B_H], writes=[B_Z, B_t4, B_H])
    TT(z0, Rr, gr, ALU.mult, **dzz)
    TT(ztmp, Ri, gi, ALU.mult, **dzz)
    for q in range(2):
        TT(Hst[0:64, q:32:2, :], z0[64 * q:64 * q + 64], ztmp[64 * q:64 * q + 64], ALU.subtract, **dzz)
    TT(z0, Rr, gi, ALU.mult, **dzz)
    TT(z1, Ri, gr, ALU.mult, **dzz)
    for q in range(2):
        TT(Hst[64:128, q:32:2, :], z0[64 * q:64 * q + 64], z1[64 * q:64 * q + 64], ALU.add, **dzz)
    if stage == "s5h":
        dhh = dram_out("dbg_H", [128, 32, 128])
        S.dma("pool", lambda e: e.dma_start(out=dhh.rearrange("p g c -> p (g c)"), in_=Hst[:, :, :].rearrange("p g c -> p (g c)")), stream="out", reads=[B_H], writes=[Buf("o")])
        S.finish(); close_phase(); close_phase(); es.close()
        return nc, ["dbg_H"]
    close_phase()

    open_phase()
    cmask = sb("s5cmask", [128, 2, 256], BF16)
    B_cm = Buf("s5cm")
    S.dma("pool", lambda e: e.dma_start(out=cmask[:, :, :], in_=cmask_d[:, :, :]), writes=[B_cm])
    QS2 = sb("s5QS2", [128, 8, 256], BF16)
    RB = sb("s5RB", [128, 8, 256], BF16)
    Mj = sb("s5Mj", [128, 8, 2, 256], BF16)
    dbc = sb("s5dbc", [128, 512], F32)
    wglu = sb("s5wglu", [128, 4, 512], BF16)
    bgr = sb("s5bgr", [4, 128], F32); bgfm = sb("s5bgfm", [128, 4], F32)
    yT = sb("s5yT", [128, 4, TOK], BF16)
    ygs = sb("s5ygs", [128, 16, 128], BF16)
    yf = [sb(f"s5yf{i}", [128, 2, 16, 16], F32) for i in range(2)]
    yw = [sb(f"s5yw{i}", [128, 2, 16, 16], F32) for i in range(2)]
    B_d, B_wg, B_bg, B_yT, B_ygs = Buf("s5d"), Buf("s5wg"), Buf("s5bg"), [Buf(f"s5yT{j}") for j in range(4)], Buf("s5ygs")
    B_yf = [Buf(f"s5yf{i}") for i in range(2)]
    S.dma("sp", lambda e: e.dma_start(out=dbc[:, :], in_=d_d.partition_broadcast(128)), writes=[B_d])
    S.dma("pool", lambda e: e.dma_start(out=wglu[:, :, :], in_=wglu_d.rearrange("(kt p) f -> p kt f", p=128)), writes=[B_wg])
    S.dma("sp", lambda e: e.dma_start(out=bgr[:, :], in_=bglu_d.rearrange("(t p) -> t p", p=128)), writes=[B_bg])
    S.op("pe", lambda e: e.transpose(out=PS[5][:, 0:4], in_=bgr[:, :], identity=identf[0:4, 0:4]), reads=[B_bg, B_identf], writes=[PSB[5]])
    S.op("dve", lambda e: e.tensor_copy(out=bgfm[:, :], in_=PS[5][:, 0:4]), reads=[PSB[5]], writes=[B_bg])

    def build_Q(dst, g0, Ctab, k0):
        bc_s = lambda ri: A_[:, ri, g0:g0 + 8, k0:k0 + 16].unsqueeze(3).to_broadcast([64, 8, 16, 16])
        bc_h = lambda ri: Ctab[:, ri, g0:g0 + 8, :].unsqueeze(2).to_broadcast([64, 8, 16, 16])
        o4 = lambda lo: dst[lo:lo + 64, :, :].rearrange("p g (s h) -> p g s h", s=16)
        tt(t4a, bc_s(0), bc_h(0), ALU.mult); tt(t4b, bc_s(1), bc_h(1), ALU.mult)
        tt(o4(0), t4a, t4b, ALU.subtract)
        tt(t4a, bc_s(0), bc_h(1), ALU.mult); tt(t4b, bc_s(1), bc_h(0), ALU.mult)
        tt(t4a, t4a, t4b, ALU.add)
        ts(o4(64), t4a, -1.0, ALU.mult)

    for j in range(4):
        g0 = 8 * j
        build_PSt(g0); build_Q(QS2, g0, C15, 0); build_Q(RB, g0, CT, 1)
        for g8 in range(8):
            pb = 4 + g8 % 2
            S.op("pe", [lambda e, g8=g8, kh=kh, pb=pb: e.matmul(PS[pb][:, 256 * kh:256 * kh + 256], lhsT=PSt[:, g8, 128 * kh:128 * kh + 128], rhs=QS2[:, g8, :],
                                                                start=True, stop=True) for kh in range(2)], reads=[B_tb], writes=[PSB[pb]])
            S.op("dve", lambda e, g8=g8, pb=pb: e.tensor_tensor(out=Mj[:, g8, :, :], in0=PS[pb][:, :].rearrange("p (k n) -> p k n", k=2), in1=cmask[:, :, :], op=ALU.mult),
                 reads=[PSB[pb], B_cm, B_tb], writes=[B_tb])
        for gp in range(4):
            pb = gp
            fns = []
            for q in range(2):
                g8 = 2 * gp + q; g = g0 + g8
                o = PS[pb][:, 256 * q:256 * q + 256]
                fns.append(lambda e, o=o, g=g, g8=g8: e.matmul(o, lhsT=Hst[:, g, :], rhs=RB[:, g8, :], start=True, stop=False))
                fns.append(lambda e, o=o, g=g, g8=g8: e.matmul(o, lhsT=UT[:, g, 0, :], rhs=Mj[:, g8, 0, :], start=False, stop=False))
                fns.append(lambda e, o=o, g=g, g8=g8: e.matmul(o, lhsT=UT[:, g, 1, :], rhs=Mj[:, g8, 1, :], start=False, stop=True))
            S.op("pe", fns, reads=[B_H, B_UT, B_tb], writes=[PSB[pb]])
            sl = gp % 2
            ga = g0 + 2 * gp
            pv4 = PS[pb][:, :].rearrange("p (g s h) -> p g s h", g=2, s=16)
            dv4 = dbc[:, 16 * ga:16 * ga + 32].rearrange("p (g h) -> p g h", g=2).unsqueeze(2).to_broadcast([128, 2, 16, 16])
            y_, w_ = yf[sl][:, :, :, :], yw[sl][:, :, :, :]
            S.op("dve", lambda e, y_=y_, ga=ga, dv4=dv4: e.tensor_tensor(out=y_, in0=U[:, ga:ga + 2, :, :], in1=dv4, op=ALU.mult), reads=[B_U, B_d, B_yf[sl]], writes=[B_yf[sl]])
            S.op("dve", lambda e, y_=y_, pv4=pv4: e.tensor_tensor(out=y_, in0=y_, in1=pv4, op=ALU.add), reads=[PSB[pb], B_yf[sl]], writes=[B_yf[sl]])
            if stage == "s5y" and j == 0 and gp == 0:
                dy = dram_out("dbg_y", [128, 2, 16, 16])
                S.dma("sp", lambda e: e.dma_start(out=dy.rearrange("p a b c -> p (a b c)"), in_=yf[0][:, :, :, :].rearrange("p a b c -> p (a b c)")), stream="out", reads=[B_yf[0]], writes=[Buf("o")])
                S.finish(); close_phase(); close_phase(); es.close()
                return nc, ["dbg_y"]
            S.op("dve", lambda e, y_=y_, w_=w_: e.tensor_tensor(out=w_, in0=y_, in1=y_, op=ALU.mult), reads=[B_yf[sl]], writes=[B_yf[sl]])
            S.op("dve", lambda e, w_=w_: e.tensor_scalar(out=w_, in0=w_, scalar1=0.044715, scalar2=1.0, op0=ALU.mult, op1=ALU.add), reads=[B_yf[sl]], writes=[B_yf[sl]])
            S.op("dve", lambda e, y_=y_, w_=w_: e.tensor_tensor(out=w_, in0=w_, in1=y_, op=ALU.mult), reads=[B_yf[sl]], writes=[B_yf[sl]])
            S.op("act", lambda e, w_=w_: e.activation(out=w_, in_=w_, func=AF.Sigmoid, scale=1.5957691216057308), reads=[B_yf[sl]], writes=[B_yf[sl]])
            dst = ygs[:, :, 32 * gp:32 * gp + 32].rearrange("p s (g h) -> p g s h", g=2)
            S.op("dve", lambda e, y_=y_, w_=w_, dst=dst: e.tensor_tensor(out=dst, in0=y_, in1=w_, op=ALU.mult), reads=[B_yf[sl], B_ygs], writes=[B_ygs])
        for hb in range(2):
            bank = 6 + hb
            S.op("pe", [lambda e, i=i, hb=hb, bank=bank: e.transpose(out=PS[bank][:, 128 * i:128 * i + 128], in_=ygs[:, 8 * hb + i, :], identity=identb[:, :]) for i in range(8)],
                 reads=[B_ygs, B_ident], writes=[PSB[bank]])
            dst = yT[:, j, :].rearrange("p (c s) -> p s c", s=16)[:, 8 * hb:8 * hb + 8, :]
            S.op("dve", lambda e, dst=dst, bank=bank: e.tensor_copy(out=dst, in_=PS[bank][:, :].rearrange("p (s c) -> p s c", s=8)), reads=[PSB[bank]], writes=[B_yT[j]])
    gt = [sb(f"s5gt{i}", [128, 512], BF16) for i in range(2)]
    B_gt = [Buf(f"s5gt{i}") for i in range(2)]
    ig = 0
    for co in range(4):
        for blk in range(4):
            pb = ig % 4; sl = ig % 2; ig += 1
            S.op("pe", [lambda e, k=k, co=co, blk=blk, pb=pb: e.matmul(PS[pb][:, :], lhsT=wglu[:, k, 128 * co:128 * co + 128], rhs=yT[:, k, 512 * blk:512 * blk + 512],
                                                                       start=(k == 0), stop=(k == 3)) for k in range(4)], reads=B_yT + [B_wg], writes=[PSB[pb]])
            S.op("act", lambda e, co=co, pb=pb, sl=sl: e.activation(out=gt[sl][:, :], in_=PS[pb][:, :], func=AF.Sigmoid, bias=bgfm[:, co:co + 1]), reads=[PSB[pb], B_bg], writes=[B_gt[sl]])
            S.op("dve", lambda e, co=co, blk=blk, sl=sl: e.tensor_tensor(out=catT[:, 4 + co, 512 * blk:512 * blk + 512], in0=yT[:, co, 512 * blk:512 * blk + 512], in1=gt[sl][:, :], op=ALU.mult),
                 reads=[B_gt[sl], B_yT[co]], writes=[B_cat[4 * blk + i] for i in range(4)])
    if stage == "s5":
        dss = dram_out("dbg_ssm", [512, TOK])
        S.dma("pool", lambda e: e.dma_start(out=dss.rearrange("(c p) t -> p c t", p=128), in_=catT[:, 4:8, :]), stream="out", reads=B_cat, writes=[Buf("o")])
        S.finish(); close_phase(); close_phase(); es.close()
        return nc, ["dbg_ssm"]
    close_phase()
    close_phase()
    ALPHA = 2.0 ** 0.25
    CAP = 384
    xloc = nc_ap(nc, "xloc")
    mem_d = dram_in("mem", [256, D])
    wout_d = dram_in("w_out", [D, D]); bout_d = dram_in("b_out", [D])
    ln_d = {k: dram_in(k, [D]) for k in ("ln1_g", "ln1_b", "ln2_g", "ln2_b", "ln3_g", "ln3_b")}
    wxq_d = dram_in("w_xq", [D, D]); wxkv_d = dram_in("w_xkv", [D, 2 * D]); wxo_d = dram_in("w_xo", [D, D])
    wr_d = dram_in("w_router", [D, 32]); br_d = dram_in("b_router", [32])
    tri_d = dram_in("tri", [128, 128]); ecap_d = dram_in("ecap", [128, 32])
    x2f = nc.dram_tensor("x2f", [TOK, D], F32, kind="Internal").ap()
    xg = nc.dram_tensor("xg", [32 * CAP, D], BF16, kind="Internal").ap()
    yg = nc.dram_tensor("yg", [32 * CAP, D], F32, kind="Internal").ap()

    gates = sb("gates", [128, NT, 4], F32)
    dest = sb("dest", [128, NT, 4], I32)
    B_route = [Buf(f"route{t}") for t in range(NT)]
    B_xg, B_x2f = Buf("xg"), [Buf(f"x2f{t}") for t in range(NT)]

    open_phase()
    onesb = sb("onesb", [128, 128], BF16)
    trib = sb("tri_sb", [128, 128], BF16)
    ecap = sb("ecap_sb", [128, 32], F32)
    B_c3 = Buf("c3")
    S.op("pool", lambda e: e.memset(onesb[:, :], 1.0), writes=[B_c3])
    B_tri, B_ecap = Buf("tri"), Buf("ecap")
    S.dma("pool", lambda e: e.dma_start(out=trib[:, :], in_=tri_d[:, :]), writes=[B_tri])
    S.dma("sp", lambda e: e.dma_start(out=ecap[:, :], in_=ecap_d[:, :]), writes=[B_ecap])
    bc = {}
    B_bc = {}
    for k, src in [("b_out", bout_d)] + [(k, ln_d[k]) for k in ("ln1_g", "ln1_b", "ln2_g", "ln2_b")]:
        bc[k] = sb("bc_" + k, [128, D], F32)
        B_bc[k] = Buf("bc_" + k)
        S.dma("sp", lambda e, k=k, src=src: e.dma_start(out=bc[k][:, :], in_=src.partition_broadcast(128)), writes=[B_bc[k]])
    brbc = sb("brbc", [128, 32], F32); wr = sb("wr", [128, KT, 32], F32)
    B_br, B_wr = Buf("brbc"), Buf("wr")
    S.dma("sp", lambda e: e.dma_start(out=brbc[:, :], in_=br_d.partition_broadcast(128)), writes=[B_br])
    S.dma("sp", lambda e: e.dma_start(out=wr[:, :, :], in_=wr_d.rearrange("(kt p) f -> p kt f", p=128)), writes=[B_wr])
    wout = sb("wout", [128, KT, D], BF16); wxq = sb("wxq", [128, KT, D], BF16); wxo = sb("wxo", [128, KT, D], BF16)
    B_wout, B_wxq, B_wxo = Buf("wout"), Buf("wxq"), Buf("wxo")
    wst3 = [sb(f"wst3_{i}", [128, D], F32) for i in range(4)]
    B_wst3 = [Buf(f"wst3_{i}") for i in range(4)]
    w3step = [0]

    def load_w3(dst, src, ncol, bf):
        for k in range(KT):
            for c0 in range(0, ncol, D):
                i = w3step[0] % 4; w3step[0] += 1
                S.dma("sp", lambda e, i=i, k=k, c0=c0, src=src: e.dma_start(out=wst3[i][:, :], in_=src[k * 128:(k + 1) * 128, c0:c0 + D]), writes=[B_wst3[i]])
                S.op("act", lambda e, i=i, k=k, c0=c0, dst=dst: e.copy(out=dst[:, k, c0:c0 + D], in_=wst3[i][:, :]), reads=[B_wst3[i], bf], writes=[bf])
    kmemT = sb("kmemT", [128, KT, 256], BF16)
    vmem = sb("vmem", [128, 2, D], BF16)
    B_kv = Buf("kv")
    open_phase()
    wxkv = sb("wxkv", [128, KT, 2 * D], BF16)
    B_wxkv = Buf("wxkv")
    load_w3(wxkv, wxkv_d, 2 * D, B_wxkv)
    load_w3(wout, wout_d, D, B_wout); load_w3(wxq, wxq_d, D, B_wxq); load_w3(wxo, wxo_d, D, B_wxo)
    memb = sb("memb", [128, 2, D], BF16)
    memT = sb("memT", [128, KT, 256], BF16)
    B_memb, B_memT = Buf("memb"), Buf("memT")
    S.dma("pool", lambda e: e.dma_start(out=memb[:, :, :], in_=mem_d.rearrange("(t p) d -> p t d", p=128)), writes=[B_memb])
    for mt in range(2):
        bank = 6 + mt
        S.op("pe", [lambda e, k=k, mt=mt, bank=bank: e.transpose(out=PS[bank][:, k * 128:(k + 1) * 128], in_=memb[:, mt, k * 128:(k + 1) * 128], identity=identb[:, :])
                    for k in range(KT)], reads=[B_memb, B_ident], writes=[PSB[bank]])
        S.op("dve", lambda e, mt=mt, bank=bank: e.tensor_copy(out=memT[:, :, mt * 128:(mt + 1) * 128], in_=PS[bank][:, :].rearrange("p (k n) -> p k n", k=KT)),
             reads=[PSB[bank]], writes=[B_memT])
    for ct in range(KT):
        pb = ct % 4
        S.op("pe", [lambda e, k=k, ct=ct, pb=pb: e.matmul(PS[pb][:, 0:256], lhsT=wxkv[:, k, ct * 128:(ct + 1) * 128], rhs=memT[:, k, :], start=(k == 0), stop=(k == KT - 1))
                    for k in range(KT)], reads=[B_wxkv, B_memT], writes=[PSB[pb]])
        S.op("dve", lambda e, ct=ct, pb=pb: e.tensor_copy(out=kmemT[:, ct, :], in_=PS[pb][:, 0:256]), reads=[PSB[pb]], writes=[B_kv])
    for mt in range(2):
        for hf in range(2):
            pb = 2 * mt + hf
            S.op("pe", [lambda e, k=k, mt=mt, hf=hf, pb=pb: e.matmul(PS[pb][:, :], lhsT=memT[:, k, mt * 128:(mt + 1) * 128], rhs=wxkv[:, k, D + 512 * hf:D + 512 * hf + 512],
                                                                    start=(k == 0), stop=(k == KT - 1)) for k in range(KT)], reads=[B_wxkv, B_memT], writes=[PSB[pb]])
            S.op("act", lambda e, mt=mt, hf=hf, pb=pb: e.copy(out=vmem[:, mt, 512 * hf:512 * hf + 512], in_=PS[pb][:, :]), reads=[PSB[pb]], writes=[B_kv])
    close_phase()

    x1 = sb("x1", [128, 4, D], F32)
    x1T = sb("x1T", [128, KT, 512], BF16)
    qxT = sb("qxT", [128, KT, 512], BF16)
    oT = sb("oT", [128, KT, 512], BF16)
    pX = [sb(f"pX{i}", [128, 2, 512], BF16) for i in range(2)]
    rl = sb("rl", [128, 512], F32)
    xin = [sb(f"xin{i}", [128, D], F32) for i in range(2)]
    hh = [sb(f"hh{i}", [128, D], F32) for i in range(2)]
    hb = [sb(f"hb{i}", [128, D], BF16) for i in range(2)]
    x2t = [sb(f"x2t{i}", [128, D], F32) for i in range(2)]
    x2T = sb("x2T", [128, KT, 128], F32)
    st6_ = [sb(f"st6_{i}", [128, 2, 6], F32) for i in range(2)]; mv_ = [sb(f"mv_{i}", [128, 2], F32) for i in range(2)]; rstd_ = [sb(f"rstd_{i}", [128, 1], F32) for i in range(2)]
    B_ln = [Buf(f"lnscr{i}") for i in range(2)]; ln_i = [0]
    lg = sb("lg", [128, 32], F32); v8 = sb("v8", [128, 8], F32); e4 = sb("e4", [128, 4], F32); ssum = sb("ssum", [128, 1], F32)
    nv1 = sb("nv1", [128, 1], F32)
    oh = sb("oh", [128, 4, 32], F32); maskall = sb("maskall", [128, NT, 32], BF16); slt = sb("slt", [128, 32], F32)
    destf = sb("destf", [128, 4], F32)
    B_x1 = [Buf(f"x1_{i}") for i in range(4)]; B_x1T, B_qxT, B_oT = Buf("x1T"), Buf("qxT"), Buf("oT")
    B_pX = [Buf(f"pX{i}") for i in range(2)]; B_rl = Buf("rl")
    B_xin = [Buf(f"xin{i}") for i in range(2)]; B_hh = [Buf(f"hh{i}") for i in range(2)]; B_hb = [Buf(f"hb{i}") for i in range(2)]
    B_x2t = [Buf(f"x2t{i}") for i in range(2)]; B_x2T = Buf("x2T"); B_sm = Buf("sm3"); B_mask = Buf("maskall")

    def layer_norm(src, dst, gk, bk, bsrc, bdst):
        i = ln_i[0] % 2; ln_i[0] += 1
        st6, mv, rstd, bl = st6_[i], mv_[i], rstd_[i], B_ln[i]
        S.op("dve", [lambda e, c=c: e.bn_stats(out=st6[:, c, :], in_=src[:, 512 * c:512 * c + 512]) for c in range(2)], reads=[bsrc, bl], writes=[bl])
        S.op("dve", lambda e: e.bn_aggr(out=mv[:, :], in_=st6[:, :, :].rearrange("p a b -> p (a b)")), reads=[bl], writes=[bl])
        S.op("dve", lambda e: e.tensor_scalar(out=rstd[:, :], in0=mv[:, 1:2], scalar1=1e-5, scalar2=None, op0=ALU.add), reads=[bl], writes=[bl])
        S.op("act", lambda e: e.activation(out=rstd[:, :], in_=rstd[:, :], func=AF.Sqrt), reads=[bl], writes=[bl])
        S.op("dve", lambda e: e.reciprocal(out=rstd[:, :], in_=rstd[:, :]), reads=[bl], writes=[bl])
        S.op("dve", lambda e: e.tensor_scalar(out=src, in0=src, scalar1=mv[:, 0:1], scalar2=rstd[:, 0:1], op0=ALU.subtract, op1=ALU.mult), reads=[bsrc, bl], writes=[bsrc])
        S.op("dve", lambda e: e.tensor_tensor(out=src, in0=src, in1=bc[gk][:, :], op=ALU.mult), reads=[bsrc, B_bc[gk]], writes=[bsrc])
        S.op("dve", lambda e: e.tensor_tensor(out=dst, in0=src, in1=bc[bk][:, :], op=ALU.add), reads=[bsrc, B_bc[bk]], writes=[bdst])

    for st in range(4):
        def A1(ti):
            t = 4 * st + ti; sl = t % 2
            S.dma("sp", lambda e, t=t, sl=sl: e.dma_start(out=xin[sl][:, :], in_=xloc[t * 128:(t + 1) * 128, :]), writes=[B_xin[sl]])
            for hf in range(2):
                pb = 2 * sl + hf
                S.op("pe", [lambda e, k=k, t=t, hf=hf, pb=pb: e.matmul(PS[pb][:, :], lhsT=catT[:, k, t * 128:(t + 1) * 128], rhs=wout[:, k, 512 * hf:512 * hf + 512],
                                                                      start=(k == 0), stop=(k == KT - 1)) for k in range(KT)], reads=[B_cat[t], B_wout], writes=[PSB[pb]])
                S.op("dve", lambda e, sl=sl, hf=hf, pb=pb: e.scalar_tensor_tensor(out=hh[sl][:, 512 * hf:512 * hf + 512], in0=xin[sl][:, 512 * hf:512 * hf + 512], scalar=ALPHA,
                                                                                  in1=PS[pb][:, :], op0=ALU.mult, op1=ALU.add), reads=[PSB[pb], B_xin[sl], B_hh[sl]], writes=[B_hh[sl]])
            S.op("dve", lambda e, sl=sl: e.tensor_tensor(out=hh[sl][:, :], in0=hh[sl][:, :], in1=bc["b_out"][:, :], op=ALU.add), reads=[B_hh[sl], B_bc["b_out"]], writes=[B_hh[sl]])
            layer_norm(hh[sl][:, :], x1[:, ti, :], "ln1_g", "ln1_b", B_hh[sl], B_x1[ti])
            S.op("act", lambda e, ti=ti, sl=sl: e.copy(out=hb[sl][:, :], in_=x1[:, ti, :]), reads=[B_x1[ti]], writes=[B_hb[sl]])
        def B1(ti):
            t = 4 * st + ti; sl = t % 2
            bank = 6 + sl
            S.op("pe", [lambda e, k=k, sl=sl, bank=bank: e.transpose(out=PS[bank][:, k * 128:(k + 1) * 128], in_=hb[sl][:, k * 128:(k + 1) * 128], identity=identb[:, :])
                        for k in range(KT)], reads=[B_hb[sl], B_ident], writes=[PSB[bank]])
            S.op("dve", lambda e, ti=ti, bank=bank: e.tensor_copy(out=x1T[:, :, ti * 128:(ti + 1) * 128], in_=PS[bank][:, :].rearrange("p (k n) -> p k n", k=KT)),
                 reads=[PSB[bank]], writes=[B_x1T])
        for ti in range(4):
            A1(ti)
            if ti > 0:
                B1(ti - 1)
        B1(3)
        if stage == "x1" and st == 0:
            dx1 = dram_out("dbg_x1", [512, D])
            S.dma("sp", lambda e: e.dma_start(out=dx1.rearrange("(t p) d -> p t d", p=128), in_=x1[:, :, :]), stream="out", reads=B_x1, writes=[Buf("o")])
            S.finish(); close_phase(); es.close()
            return nc, ["dbg_x1"]
        for ct in range(KT):
            pb = ct % 4
            S.op("pe", [lambda e, k=k, ct=ct, pb=pb: e.matmul(PS[pb][:, :], lhsT=wxq[:, k, ct * 128:(ct + 1) * 128], rhs=x1T[:, k, :], start=(k == 0), stop=(k == KT - 1))
                        for k in range(KT)], reads=[B_x1T, B_wxq], writes=[PSB[pb]])
            eng = "act" if ct % 2 else "dve"
            fn = (lambda e, ct=ct, pb=pb: e.copy(out=qxT[:, ct, :], in_=PS[pb][:, :])) if eng == "act" else (lambda e, ct=ct, pb=pb: e.tensor_copy(out=qxT[:, ct, :], in_=PS[pb][:, :]))
            S.op(eng, fn, reads=[PSB[pb]], writes=[B_qxT])
        for h in range(4):
            sl = h % 2
            for mt in range(2):
                pb = mt
                S.op("pe", [lambda e, cc=cc, h=h, mt=mt, pb=pb: e.matmul(PS[pb][:, :], lhsT=kmemT[:, 2 * h + cc, mt * 128:(mt + 1) * 128], rhs=qxT[:, 2 * h + cc, :],
                                                                        start=(cc == 0), stop=(cc == 1)) for cc in range(2)], reads=[B_kv, B_qxT], writes=[PSB[pb]])
                S.op("act", lambda e, sl=sl, mt=mt, pb=pb: e.activation(out=pX[sl][:, mt, :], in_=PS[pb][:, :], func=AF.Exp, scale=0.0625), reads=[PSB[pb]], writes=[B_pX[sl]])
            S.op("pe", [lambda e, mt=mt, sl=sl: e.matmul(PS[4][:, :], lhsT=onesb[:, :], rhs=pX[sl][:, mt, :], start=(mt == 0), stop=(mt == 1)) for mt in range(2)],
                 reads=[B_pX[sl], B_c3], writes=[PSB[4]])
            S.op("dve", lambda e: e.tensor_copy(out=rl[:, :], in_=PS[4][:, :]), reads=[PSB[4], B_rl], writes=[B_rl])
            S.op("dve", lambda e: e.reciprocal(out=rl[:, :], in_=rl[:, :]), reads=[B_rl], writes=[B_rl])
            for cc in range(2):
                pb = 2 + cc
                S.op("pe", [lambda e, mt=mt, cc=cc, h=h, sl=sl, pb=pb: e.matmul(PS[pb][:, :], lhsT=vmem[:, mt, (2 * h + cc) * 128:(2 * h + cc + 1) * 128], rhs=pX[sl][:, mt, :],
                                                                              start=(mt == 0), stop=(mt == 1)) for mt in range(2)], reads=[B_pX[sl], B_kv], writes=[PSB[pb]])
                S.op("dve", lambda e, cc=cc, h=h, pb=pb: e.tensor_tensor(out=oT[:, 2 * h + cc, :], in0=PS[pb][:, :], in1=rl[:, :], op=ALU.mult), reads=[PSB[pb], B_rl], writes=[B_oT])
        def A2(ti):
            t = 4 * st + ti; sl = t % 2
            for hf in range(2):
                pb = 2 * sl + hf
                S.op("pe", [lambda e, k=k, ti=ti, hf=hf, pb=pb: e.matmul(PS[pb][:, :], lhsT=oT[:, k, ti * 128:(ti + 1) * 128], rhs=wxo[:, k, 512 * hf:512 * hf + 512],
                                                                       start=(k == 0), stop=(k == KT - 1)) for k in range(KT)], reads=[B_oT, B_wxo], writes=[PSB[pb]])
                S.op("dve", lambda e, ti=ti, sl=sl, hf=hf, pb=pb: e.scalar_tensor_tensor(out=hh[sl][:, 512 * hf:512 * hf + 512], in0=x1[:, ti, 512 * hf:512 * hf + 512], scalar=ALPHA,
                                                                                         in1=PS[pb][:, :], op0=ALU.mult, op1=ALU.add), reads=[PSB[pb], B_x1[ti], B_hh[sl]], writes=[B_hh[sl]])
            layer_norm(hh[sl][:, :], x2t[sl][:, :], "ln2_g", "ln2_b", B_hh[sl], B_x2t[sl])
            if stage == "x2a":
                dxa = dram_out("dbg_x2a", [128, D])
                S.dma("sp", lambda e, sl=sl: e.dma_start(out=dxa[:, :], in_=x2t[sl][:, :]), stream="out", reads=[B_x2t[sl]], writes=[Buf("o")])
                S.finish(); close_phase(); es.close()
                return nc, ["dbg_x2a"]
            S.dma("sp", lambda e, t=t, sl=sl: e.dma_start(out=x2f[t * 128:(t + 1) * 128, :], in_=x2t[sl][:, :]), stream=f"x2f{sl}", reads=[B_x2t[sl]], writes=[B_x2f[t]])
            S.op("act", lambda e, sl=sl: e.copy(out=hb[sl][:, :], in_=x2t[sl][:, :]), reads=[B_x2t[sl]], writes=[B_hb[sl]])
        def B2(ti):
            t = 4 * st + ti; sl = t % 2
            for hf in range(2):
                S.op("pe", [lambda e, k=k, sl=sl, hf=hf: e.transpose(out=PS[4 + hf][:, k * 128:(k + 1) * 128], in_=x2t[sl][:, (4 * hf + k) * 128:(4 * hf + k + 1) * 128], identity=identf[:, :])
                            for k in range(4)], reads=[B_x2t[sl], B_identf], writes=[PSB[4 + hf]])
                eng = "act" if hf else "dve"
                fn = (lambda e, hf=hf: e.copy(out=x2T[:, 4 * hf:4 * hf + 4, :], in_=PS[4 + hf][:, :].rearrange("p (k n) -> p k n", k=4))) if eng == "act" else \
                     (lambda e, hf=hf: e.tensor_copy(out=x2T[:, 4 * hf:4 * hf + 4, :], in_=PS[4 + hf][:, :].rearrange("p (k n) -> p k n", k=4)))
                S.op(eng, fn, reads=[PSB[4 + hf]], writes=[B_x2T])
            S.op("pe", [lambda e, k=k: e.matmul(PS[4][:, 0:32], lhsT=x2T[:, k, :], rhs=wr[:, k, :], start=(k == 0), stop=(k == KT - 1)) for k in range(KT)],
                 reads=[B_x2T, B_wr], writes=[PSB[4]])
            dsm = dict(reads=[B_sm], writes=[B_sm])
            S.op("dve", lambda e: e.tensor_tensor(out=lg[:, :], in0=PS[4][:, 0:32], in1=brbc[:, :], op=ALU.add), reads=[PSB[4], B_br, B_sm], writes=[B_sm])
            S.op("dve", lambda e: e.max(out=v8[:, :], in_=lg[:, :]), **dsm)
            S.op("dve", lambda e: e.tensor_scalar(out=nv1[:, :], in0=v8[:, 0:1], scalar1=-1.0, scalar2=None, op0=ALU.mult), **dsm)
            S.op("act", lambda e: e.activation(out=e4[:, :], in_=v8[:, 0:4], func=AF.Exp, bias=nv1[:, 0:1]), **dsm)
            S.op("dve", lambda e: e.reduce_sum(out=ssum[:, :], in_=e4[:, :], axis=AX.X), **dsm)
            S.op("dve", lambda e: e.reciprocal(out=ssum[:, :], in_=ssum[:, :]), **dsm)
            S.op("dve", lambda e, t=t: e.tensor_scalar(out=gates[:, t, :], in0=e4[:, :], scalar1=ssum[:, 0:1], scalar2=None, op0=ALU.mult), reads=[B_sm], writes=[B_sm, B_route[t]])
            for k in range(4):
                S.op("dve", lambda e, k=k: e.tensor_scalar(out=oh[:, k, :], in0=lg[:, :], scalar1=v8[:, k:k + 1], scalar2=None, op0=ALU.is_equal), **dsm)
            S.op("dve", lambda e: e.tensor_tensor(out=slt[:, :], in0=oh[:, 0, :], in1=oh[:, 1, :], op=ALU.add), **dsm)
            S.op("dve", lambda e: e.tensor_tensor(out=slt[:, :], in0=slt[:, :], in1=oh[:, 2, :], op=ALU.add), **dsm)
            S.op("dve", lambda e, t=t: e.tensor_tensor(out=maskall[:, t, :], in0=slt[:, :], in1=oh[:, 3, :], op=ALU.add), reads=[B_sm, B_mask], writes=[B_mask])
            S.op("pe", [lambda e, t=t: e.matmul(PS[5][:, 0:32], lhsT=trib[:, :], rhs=maskall[:, t, :], start=True, stop=(t == 0))] +
                       [lambda e, t=t, tp=tp: e.matmul(PS[5][:, 0:32], lhsT=onesb[:, :], rhs=maskall[:, tp, :], start=False, stop=(tp == t - 1)) for tp in range(t)],
                 reads=[B_mask, B_tri, B_c3], writes=[PSB[5]])
            S.op("dve", lambda e: e.tensor_scalar(out=slt[:, :], in0=PS[5][:, 0:32], scalar1=float(CAP - 1), scalar2=None, op0=ALU.min), reads=[PSB[5], B_sm], writes=[B_sm])
            S.op("dve", lambda e: e.tensor_tensor(out=slt[:, :], in0=slt[:, :], in1=ecap[:, :], op=ALU.add), reads=[B_ecap, B_sm], writes=[B_sm])
            S.op("dve", lambda e: e.tensor_tensor(out=oh[:, :, :], in0=oh[:, :, :], in1=slt[:, :].unsqueeze(1).to_broadcast([128, 4, 32]), op=ALU.mult), **dsm)
            S.op("dve", lambda e: e.reduce_sum(out=destf[:, :], in_=oh[:, :, :], axis=AX.X), **dsm)
            S.op("dve", lambda e, t=t: e.tensor_copy(out=dest[:, t, :], in_=destf[:, :]), reads=[B_sm], writes=[B_sm, B_route[t]])
            for k in range(4):
                S.dma("pool", lambda e, t=t, k=k, sl=sl: e.indirect_dma_start(out=xg[:, :], out_offset=bass.IndirectOffsetOnAxis(dest[:, t, k:k + 1], 0),
                                                                              in_=hb[sl][:, :], in_offset=None), stream=f"xgsc{sl}", reads=[B_hb[sl], B_route[t]], writes=[B_xg])
        for ti in range(4):
            A2(ti)
            if ti > 0:
                B2(ti - 1)
        B2(3)
    if stage == "x2":
        dx2 = dram_out("dbg_x2", [TOK, D]); dg = dram_out("dbg_gates", [128, NT, 4]); dd = dram_out("dbg_dest", [128, NT, 4], I32)
        S.dma("sp", lambda e: e.dma_start(out=dx2[:, :], in_=x2f[:, :]), stream="out", reads=B_x2f, writes=[Buf("o")])
        S.dma("sp", lambda e: e.dma_start(out=dg.rearrange("p a b -> p (a b)"), in_=gates[:, :, :].rearrange("p a b -> p (a b)")), stream="out", reads=B_route, writes=[Buf("o1")])
        S.dma("sp", lambda e: e.dma_start(out=dd.rearrange("p a b -> p (a b)"), in_=dest[:, :, :].rearrange("p a b -> p (a b)")), stream="out", reads=B_route, writes=[Buf("o2")])
        dxg = dram_out("dbg_xg", [32 * CAP, D])
        S.dma("pool", lambda e: e.dma_start(out=dxg[:, :], in_=xg[:, :]), stream="outsw", reads=[B_xg], writes=[Buf("o3")])
        S.finish(); close_phase(); es.close()
        return nc, ["dbg_x2", "dbg_gates", "dbg_dest", "dbg_xg"]
    close_phase()
    we1_d = dram_in("w_e1", [32, D, 2 * D]); be1_d = dram_in("b_e1", [32, 2 * D])
    we2_d = dram_in("w_e2", [32, D, D]); be2_d = dram_in("b_e2", [32, D])
    n_exp = 32 if stage not in ("moe2",) else 2
    B_yg = Buf("yg")
    open_phase()
    b1all = sb("b1all", [32, 2 * D], F32)
    b1fm = sb("b1fm", [128, 8, 2, 32], F32)
    B_b1a, B_b1 = Buf("b1all"), Buf("b1fm")
    S.dma("sp", lambda e: e.dma_start(out=b1all[:, :], in_=be1_d[:, :]), writes=[B_b1a])
    S.op("pe", [lambda e, ft=ft, two=two: e.transpose(out=PS[5][:, 32 * (2 * ft + two):32 * (2 * ft + two) + 32], in_=b1all[:, 256 * ft + two:256 * (ft + 1):2],
                                                     identity=identf[0:32, 0:32]) for ft in range(8) for two in range(2)], reads=[B_b1a, B_identf], writes=[PSB[5]])
    S.op("dve", lambda e: e.tensor_copy(out=b1fm[:, :, :, :].rearrange("p a b c -> p (a b c)"), in_=PS[5][:, :]), reads=[PSB[5]], writes=[B_b1])
    w1 = [sb(f"w1_{i}", [128, KT, 2 * D], BF16) for i in range(2)]
    w2 = [sb(f"w2_{i}", [128, KT, D], BF16) for i in range(2)]
    b2bc = [sb(f"b2bc{i}", [128, D], F32) for i in range(2)]
    B_w1 = [Buf(f"w1_{i}") for i in range(2)]; B_w2 = [Buf(f"w2_{i}") for i in range(2)]; B_b2 = [Buf(f"b2bc{i}") for i in range(2)]
    NS = CAP // 128
    xgl = sb("xgl", [128, NS, D], BF16)
    xgT2 = [sb("xgT", [128, KT, CAP], BF16)] * 2; actT2 = [sb("actT", [128, 8, CAP], BF16)] * 2
    B_xgl = Buf("xgl"); B_xgT2 = [Buf("xgT")] * 2; B_actT2 = [Buf("actT")] * 2
    NWST = 6
    wst = [sb(f"wst{i}", [128, D], F32) for i in range(NWST)]
    B_wst = [Buf(f"wst{i}") for i in range(NWST)]
    gg_ = [sb(f"sg_g{i}", [128, CAP], F32) for i in range(2)]; ll_ = [sb(f"sg_l{i}", [128, CAP], F32) for i in range(2)]; ss_ = [sb(f"sg_s{i}", [128, CAP], F32) for i in range(2)]
    B_sg = [Buf(f"sg{i}") for i in range(2)]
    ysb = [sb(f"ysb{i}", [128, D], F32) for i in range(2)]
    B_ysb = [Buf(f"ysb{i}") for i in range(2)]

    wstep = [0]

    def step_issue(e_, j):
        i = wstep[0] % NWST; wstep[0] += 1
        if j < 16:
            k, hf = j // 2, j % 2
            S.dma("sp", lambda e, i=i, e_=e_, k=k, hf=hf: e.dma_start(out=wst[i][:, :], in_=we1_d[e_][k * 128:(k + 1) * 128, hf * D:(hf + 1) * D]), writes=[B_wst[i]])
        else:
            k = j - 16
            S.dma("sp", lambda e, i=i, e_=e_, k=k: e.dma_start(out=wst[i][:, :], in_=we2_d[e_][k * 128:(k + 1) * 128, :]), writes=[B_wst[i]])
        return i

    def step_cast(e_, j, i):
        sl = e_ % 2
        if j < 16:
            k, hf = j // 2, j % 2
            S.op("act", lambda e, i=i, sl=sl, k=k, hf=hf: e.copy(out=w1[sl][:, k, hf * D:(hf + 1) * D], in_=wst[i][:, :]), reads=[B_wst[i], B_w1[sl]], writes=[B_w1[sl]])
        else:
            k = j - 16
            S.op("act", lambda e, i=i, sl=sl, k=k: e.copy(out=w2[sl][:, k, :], in_=wst[i][:, :]), reads=[B_wst[i], B_w2[sl]], writes=[B_w2[sl]])

    def load_step(e_, j):
        step_cast(e_, j, step_issue(e_, j))

    def load_b2(e_):
        sl = e_ % 2
        S.dma("pool", lambda e, sl=sl, e_=e_: e.dma_start(out=b2bc[sl][:, :], in_=be2_d[e_].partition_broadcast(128)), writes=[B_b2[sl]])

    def load_expert(e_):
        for j in range(24):
            load_step(e_, j)
        load_b2(e_)

    load_expert(0)
    iy = 0
    for e_ in range(n_exp):
        sl = e_ % 2
        nxt = e_ + 1 if e_ + 1 < n_exp else None
        if nxt is not None:
            load_b2(nxt)
        xgT, actT, B_xgT, B_actT = xgT2[sl], actT2[sl], B_xgT2[sl], B_actT2[sl]
        S.dma("sp", lambda e, e_=e_: e.dma_start(out=xgl[:, :, :], in_=xg[e_ * CAP:(e_ + 1) * CAP, :].rearrange("(s p) d -> p s d", p=128)), reads=[B_xg], writes=[B_xgl])
        for st_ in range(NS):
            bank = 6 + st_ % 2
            S.op("pe", [lambda e, k=k, st_=st_, bank=bank: e.transpose(out=PS[bank][:, k * 128:(k + 1) * 128], in_=xgl[:, st_, k * 128:(k + 1) * 128], identity=identb[:, :])
                        for k in range(KT)], reads=[B_xgl, B_ident], writes=[PSB[bank]])
            eng = "act" if st_ % 2 else "dve"
            fn = (lambda e, st_=st_, bank=bank, xgT=xgT: e.copy(out=xgT[:, :, st_ * 128:(st_ + 1) * 128], in_=PS[bank][:, :].rearrange("p (k n) -> p k n", k=KT))) if eng == "act" else \
                 (lambda e, st_=st_, bank=bank, xgT=xgT: e.tensor_copy(out=xgT[:, :, st_ * 128:(st_ + 1) * 128], in_=PS[bank][:, :].rearrange("p (k n) -> p k n", k=KT)))
            S.op(eng, fn, reads=[PSB[bank]], writes=[B_xgT])
        pend = []
        for ft in range(8):
            if nxt is not None:
                for (jj, ii) in pend:
                    step_cast(nxt, jj, ii)
                pend = [(j, step_issue(nxt, j)) for j in range(3 * ft, 3 * ft + 3)]
            s2 = ft % 2
            pg, pl = 2 * s2, 2 * s2 + 1
            for two, pb in ((0, pg), (1, pl)):
                S.op("pe", [lambda e, k=k, ft=ft, two=two, pb=pb, sl=sl, xgT=xgT: e.matmul(PS[pb][:, 0:CAP], lhsT=w1[sl][:, k, 256 * ft + two:256 * (ft + 1):2], rhs=xgT[:, k, :],
                                                                                 start=(k == 0), stop=(k == KT - 1)) for k in range(KT)], reads=[B_w1[sl], B_xgT], writes=[PSB[pb]])
            g_, l_, s_ = gg_[s2][:, :], ll_[s2][:, :], ss_[s2][:, :]
            S.op("dve", lambda e, g_=g_, pg=pg, ft=ft, e_=e_: e.tensor_scalar(out=g_, in0=PS[pg][:, 0:CAP], scalar1=b1fm[:, ft, 0, e_:e_ + 1], scalar2=7.0, op0=ALU.add, op1=ALU.min),
                 reads=[PSB[pg], B_b1, B_sg[s2]], writes=[B_sg[s2]])
            S.op("act", lambda e, g_=g_, s_=s_: e.activation(out=s_, in_=g_, func=AF.Sigmoid, scale=1.702), reads=[B_sg[s2]], writes=[B_sg[s2]])
            S.op("dve", lambda e, l_=l_, pl=pl, ft=ft, e_=e_: e.tensor_scalar(out=l_, in0=PS[pl][:, 0:CAP], scalar1=b1fm[:, ft, 1, e_:e_ + 1], scalar2=7.0, op0=ALU.add, op1=ALU.min),
                 reads=[PSB[pl], B_b1, B_sg[s2]], writes=[B_sg[s2]])
            S.op("dve", lambda e, l_=l_: e.tensor_scalar(out=l_, in0=l_, scalar1=-7.0, scalar2=1.0, op0=ALU.max, op1=ALU.add), reads=[B_sg[s2]], writes=[B_sg[s2]])
            S.op("dve", lambda e, g_=g_, s_=s_: e.tensor_tensor(out=g_, in0=g_, in1=s_, op=ALU.mult), reads=[B_sg[s2]], writes=[B_sg[s2]])
            S.op("dve", lambda e, g_=g_, l_=l_, ft=ft, actT=actT: e.tensor_tensor(out=actT[:, ft, :], in0=g_, in1=l_, op=ALU.mult), reads=[B_sg[s2], B_actT], writes=[B_actT])
        for (jj, ii) in pend:
            step_cast(nxt, jj, ii)
        for st_ in range(NS):
            ys = iy % 2; iy += 1
            for hf in range(2):
                pb = 4 + hf
                S.op("pe", [lambda e, ft=ft, st_=st_, hf=hf, pb=pb, sl=sl, actT=actT: e.matmul(PS[pb][:, :], lhsT=actT[:, ft, st_ * 128:(st_ + 1) * 128], rhs=w2[sl][:, ft, 512 * hf:512 * hf + 512],
                                                                                   start=(ft == 0), stop=(ft == 7)) for ft in range(8)], reads=[B_actT, B_w2[sl]], writes=[PSB[pb]])
                S.op("dve", lambda e, ys=ys, hf=hf, pb=pb, sl=sl: e.tensor_tensor(out=ysb[ys][:, 512 * hf:512 * hf + 512], in0=PS[pb][:, :], in1=b2bc[sl][:, 512 * hf:512 * hf + 512], op=ALU.add),
                     reads=[PSB[pb], B_b2[sl], B_ysb[ys]], writes=[B_ysb[ys]])
            r0 = e_ * CAP + st_ * 128
            S.dma("pool", lambda e, ys=ys, r0=r0: e.dma_start(out=yg[r0:r0 + 128, :], in_=ysb[ys][:, :]), stream=f"ygst{ys}", reads=[B_ysb[ys]], writes=[B_yg])
    close_phase()

    out_d = dram_out("out", [TOK, D])
    open_phase()
    g3 = sb("ln3g", [128, D], F32); b3 = sb("ln3b", [128, D], F32)
    B_g3, B_b3 = Buf("ln3g"), Buf("ln3b")
    S.dma("sp", lambda e: e.dma_start(out=g3[:, :], in_=ln_d["ln3_g"].partition_broadcast(128)), writes=[B_g3])
    S.dma("sp", lambda e: e.dma_start(out=b3[:, :], in_=ln_d["ln3_b"].partition_broadcast(128)), writes=[B_b3])
    ygat = [[sb(f"ygat{i}_{k}", [128, D], F32) for k in range(4)] for i in range(2)]
    B_ygat = [[Buf(f"ygat{i}_{k}") for k in range(4)] for i in range(2)]
    x2r = [sb(f"x2r{i}", [128, D], F32) for i in range(2)]; B_x2r = [Buf(f"x2r{i}") for i in range(2)]
    acc = [sb(f"acc{i}", [128, D], F32) for i in range(2)]; B_acc = [Buf(f"acc{i}") for i in range(2)]
    oo = [sb(f"oo{i}", [128, D], F32) for i in range(2)]; B_oo = [Buf(f"oo{i}") for i in range(2)]
    st6b = sb("st6b", [128, 2, 6], F32); mvb = sb("mvb", [128, 2], F32); rstdb = sb("rstdb", [128, 1], F32)
    B_sm5 = Buf("sm5")
    for t in range(NT):
        sl = t % 2
        S.dma("sp", lambda e, t=t, sl=sl: e.dma_start(out=x2r[sl][:, :], in_=x2f[t * 128:(t + 1) * 128, :]), reads=[B_x2f[t]], writes=[B_x2r[sl]])
        for k in range(4):
            S.dma("pool", lambda e, t=t, k=k, sl=sl: e.indirect_dma_start(out=ygat[sl][k][:, :], out_offset=None, in_=yg[:, :],
                                                                          in_offset=bass.IndirectOffsetOnAxis(dest[:, t, k:k + 1], 0)), reads=[B_yg, B_route[t]], writes=[B_ygat[sl][k]])
        a_ = acc[sl][:, :]
        S.op("dve", lambda e, a_=a_, t=t, sl=sl: e.tensor_scalar(out=a_, in0=ygat[sl][0][:, :], scalar1=gates[:, t, 0:1], scalar2=None, op0=ALU.mult),
             reads=[B_ygat[sl][0], B_route[t], B_acc[sl]], writes=[B_acc[sl]])
        for k in range(1, 4):
            S.op("dve", lambda e, a_=a_, t=t, k=k, sl=sl: e.scalar_tensor_tensor(out=a_, in0=ygat[sl][k][:, :], scalar=gates[:, t, k:k + 1], in1=a_, op0=ALU.mult, op1=ALU.add),
                 reads=[B_ygat[sl][k], B_route[t], B_acc[sl]], writes=[B_acc[sl]])
        S.op("dve", lambda e, a_=a_, sl=sl: e.scalar_tensor_tensor(out=a_, in0=x2r[sl][:, :], scalar=ALPHA, in1=a_, op0=ALU.mult, op1=ALU.add),
             reads=[B_x2r[sl], B_acc[sl]], writes=[B_acc[sl]])
        S.op("dve", [lambda e, c=c, a_=a_: e.bn_stats(out=st6b[:, c, :], in_=a_[:, 512 * c:512 * c + 512]) for c in range(2)], reads=[B_acc[sl], B_sm5], writes=[B_sm5])
        S.op("dve", lambda e: e.bn_aggr(out=mvb[:, :], in_=st6b[:, :, :].rearrange("p a b -> p (a b)")), reads=[B_sm5], writes=[B_sm5])
        S.op("dve", lambda e: e.tensor_scalar(out=rstdb[:, :], in0=mvb[:, 1:2], scalar1=1e-5, scalar2=None, op0=ALU.add), reads=[B_sm5], writes=[B_sm5])
        S.op("act", lambda e: e.activation(out=rstdb[:, :], in_=rstdb[:, :], func=AF.Sqrt), reads=[B_sm5], writes=[B_sm5])
        S.op("dve", lambda e: e.reciprocal(out=rstdb[:, :], in_=rstdb[:, :]), reads=[B_sm5], writes=[B_sm5])
        S.op("dve", lambda e, a_=a_: e.tensor_scalar(out=a_, in0=a_, scalar1=mvb[:, 0:1], scalar2=rstdb[:, 0:1], op0=ALU.subtract, op1=ALU.mult), reads=[B_acc[sl], B_sm5], writes=[B_acc[sl]])
        S.op("dve", lambda e, a_=a_: e.tensor_tensor(out=a_, in0=a_, in1=g3[:, :], op=ALU.mult), reads=[B_acc[sl], B_g3], writes=[B_acc[sl]])
        S.op("dve", lambda e, a_=a_, sl=sl: e.tensor_tensor(out=oo[sl][:, :], in0=a_, in1=b3[:, :], op=ALU.add), reads=[B_acc[sl], B_b3, B_oo[sl]], writes=[B_oo[sl]])
        S.dma("sp", lambda e, t=t, sl=sl: e.dma_start(out=out_d[t * 128:(t + 1) * 128, :], in_=oo[sl][:, :]), stream=f"out{sl}", reads=[B_oo[sl]], writes=[Buf(f"o_{t}")])
    S.finish()
    close_phase()
    es.close()
    return nc, ["out"]


def nc_ap(nc, name):
    return _DRAM[name]


def _rope_tables(pos):
    half = 32
    inv = (10000.0 ** (-np.arange(half, dtype=np.float32) * np.float32(2.0 / 64))).astype(np.float32)
    ang = (pos.astype(np.float32)[:, None] * inv[None, :]).astype(np.float32)
    cos = np.cos(ang.astype(np.float64)).astype(np.float32).T
    sin = np.sin(ang.astype(np.float64)).astype(np.float32).T
    return np.ascontiguousarray(np.tile(cos, (4, 1))), np.ascontiguousarray(np.tile(sin, (4, 1)))


def prep_core_inputs(inputs, c, stage_needs_s5=True):
    b, j = c // 4, c % 4
    t0 = j * TOK
    x = inputs["x"]
    m = {}
    m["xloc"] = np.ascontiguousarray(x[b, t0:t0 + TOK])
    m["xhalo"] = np.ascontiguousarray(x[b, t0 - HALO:t0]) if j > 0 else np.zeros((HALO, D), np.float32)
    pos = np.arange(t0 - HALO, t0 + TOK)
    m["ropecos"], m["ropesin"] = _rope_tables(pos)
    kj = np.arange(128)[:, None]
    qi = np.arange(128)[None, :]
    prev = (kj > qi).astype(np.float32)
    own = (kj <= qi).astype(np.float32)
    first = prev if j > 0 else np.zeros_like(prev)
    m["masks"] = np.ascontiguousarray(np.stack([prev, own, first], axis=1))
    m["ident"] = np.eye(128, dtype=np.float32)
    m["w_in"] = np.ascontiguousarray(inputs["w_in"][0])
    m["b_in"] = np.ascontiguousarray(inputs["b_in"][0])
    m["attn_sinks"] = np.ascontiguousarray(inputs["attn_sinks"][0])
    if stage_needs_s5:
        xp = np.zeros((3 * TOK, D), np.float32)
        sv = np.zeros((128, 4), np.float32)
        for k in range(3):
            sj = j - 3 + k
            if sj >= 0:
                xp[k * TOK:(k + 1) * TOK] = x[b, sj * TOK:(sj + 1) * TOK]
                sv[:, k] = 1.0
        m["xprev"], m["segvalid"] = xp, sv
        for k in ("s5_lambda_re", "s5_lambda_im", "s5_log_dt", "s5_b_re", "s5_b_im", "s5_c_re", "s5_c_im", "s5_d", "s5_w_glu", "s5_b_glu"):
            m[k] = np.ascontiguousarray(inputs[k][0])
        sp = np.arange(128) // 16
        so = np.arange(256) // 16
        for k in ("w_out", "b_out", "ln1_g", "ln1_b", "ln2_g", "ln2_b", "ln3_g", "ln3_b", "w_xq", "w_xkv", "w_xo", "w_router", "b_router", "w_e1", "b_e1", "w_e2", "b_e2"):
            m[k] = np.ascontiguousarray(inputs[k][0])
        m["mem"] = np.ascontiguousarray(inputs["mem"][b])
        m["tri"] = np.triu(np.ones((128, 128), np.float32), 1)
        m["ecap"] = np.ascontiguousarray(np.tile((np.arange(32, dtype=np.float32) * 384.0)[None, :], (128, 1)))
        m["cmask"] = np.ascontiguousarray(np.stack([(so[None, :] >= (8 * kh + sp)[:, None]) for kh in range(2)], axis=1).astype(np.float32))
    return m


def kernel(**inputs):
    inputs = {k: np.asarray(v) for k, v in inputs.items()}
    nc, _ = build_nc("full")
    in_maps = [prep_core_inputs(inputs, c) for c in range(NCORES)]
    res = run_bass_kernel_spmd(nc, in_maps, core_ids=list(range(NCORES)))
    out = np.zeros((2, 8192, D), np.float32)
    for c in range(NCORES):
        out[c // 4, (c % 4) * TOK:(c % 4 + 1) * TOK] = res.results[c]["out"]
    return out
```

```python
from contextlib import ExitStack

import numpy as np
import concourse.bass as bass
import concourse.mybir as mybir
from concourse.bass_utils import run_bass_kernel_spmd

F32 = mybir.dt.float32
BF16 = mybir.dt.bfloat16
I32 = mybir.dt.int32
ALU = mybir.AluOpType
AF = mybir.ActivationFunctionType
AX = mybir.AxisListType

NCORES = 8
_DRAM = {}
TOK = 2048
NT = TOK // 128
HALO = 128
TH = TOK + HALO
D = 1024
KT = D // 128


class Buf:
    __slots__ = ("name", "lw", "rd")

    def __init__(self, name):
        self.name = name
        self.lw = None
        self.rd = {}


class Sched:
    ENG = ("pe", "act", "dve", "pool", "sp")

    def __init__(self, nc, es):
        self.nc = nc
        self.es = es
        self.sems = {e: es.enter_context(nc.semaphore("sem_" + e)) for e in self.ENG}
        self.cnt = {e: 0 for e in self.ENG}
        self.prog = {e: [] for e in self.ENG}
        self.seen = {e: {} for e in self.ENG}
        self.scnt = {}

    def _sem(self, key):
        if key not in self.sems:
            self.sems[key] = self.es.enter_context(self.nc.semaphore("sem_" + key))
            self.scnt[key] = 0
        return self.sems[key]

    def _waits(self, eng, reads, writes):
        deps = {}

        def add(tok):
            if tok is None:
                return
            key, val = tok
            if key == eng and eng == "pe":
                return
            if deps.get(key, 0) < val:
                deps[key] = val

        for b in reads:
            add(b.lw)
        for b in writes:
            add(b.lw)
            for t in b.rd.items():
                add(t)
        out = []
        for key, val in deps.items():
            if self.seen[eng].get(key, 0) >= val:
                continue
            self.seen[eng][key] = val
            out.append((self._sem(key), val))
        return out

    def _commit(self, tok, reads, writes):
        for b in writes:
            b.lw = tok
            b.rd = {}
        for b in reads:
            if b not in writes:
                if b.rd.get(tok[0], 0) < tok[1]:
                    b.rd[tok[0]] = tok[1]

    def op(self, eng, fns, reads=(), writes=()):
        if callable(fns):
            fns = [fns]
        waits = self._waits(eng, reads, writes)
        self.cnt[eng] += 1
        assert self.cnt[eng] < 60000, eng
        self.prog[eng].append((waits, fns, (self.sems[eng], 1)))
        self._commit((eng, self.cnt[eng]), reads, writes)

    def dma(self, eng, fn, stream=None, reads=(), writes=()):
        if stream is None:
            stream = "d_" + writes[0].name
        stream = stream + ("_sw" if eng == "pool" else "_hw")
        sem = self._sem(stream)
        waits = self._waits(eng, reads, writes)
        self.scnt[stream] += 16
        assert self.scnt[stream] < 60000, stream
        self.prog[eng].append((waits, [fn], (sem, 16)))
        self._commit((stream, self.scnt[stream]), reads, writes)

    def wait_all(self, eng, bufs):
        waits = self._waits(eng, bufs, ())
        self.prog[eng].append((waits, [], None))

    def barrier(self):
        toks = [(e, self.cnt[e]) for e in self.ENG if self.cnt[e]] + [(k, v) for k, v in self.scnt.items() if v]
        for eng in self.ENG:
            waits = []
            for key, val in toks:
                if key == eng or self.seen[eng].get(key, 0) >= val:
                    continue
                self.seen[eng][key] = val
                waits.append((self._sem(key), val))
            self.prog[eng].append((waits, [], None))

    def finish(self, eng="sp", prefix="out"):
        fins = []
        for key, val in self.scnt.items():
            if key.startswith(prefix) and val:
                b = Buf("fin_" + key)
                b.lw = (key, val)
                fins.append(b)
        self.wait_all(eng, fins)

    def emit(self):
        nc = self.nc
        with nc.Block() as block:
            table = (("pe", block.tensor), ("act", block.scalar), ("dve", block.vector),
                     ("pool", block.gpsimd), ("sp", block.sync))
            for eng, deco in table:
                def body(e, eng=eng):
                    for waits, fns, inc in self.prog[eng]:
                        for s, v in waits:
                            e.wait_ge(s, v)
                        for f in fns[:-1]:
                            f(e)
                        if fns:
                            fns[-1](e).then_inc(inc[0], inc[1])
                deco(body)
        self.prog = {e: [] for e in self.ENG}


def build_nc(stage="full"):
    nc = bass.Bass("TRN2", target_bir_lowering=False)
    es = ExitStack()
    S = Sched(nc, es)

    _DRAM.clear()

    def dram_in(name, shape, dt=F32):
        _DRAM[name] = nc.dram_tensor(name, list(shape), dt, kind="ExternalInput").ap()
        return _DRAM[name]

    def dram_out(name, shape, dt=F32):
        return nc.dram_tensor(name, list(shape), dt, kind="ExternalOutput").ap()

    scope = [es]

    def sb(name, shape, dt):
        return scope[-1].enter_context(nc.sbuf_tensor(name, list(shape), dt))

    def open_phase():
        ph = ExitStack()
        scope.append(ph)
        return ph

    def close_phase():
        S.barrier()
        S.emit()
        scope.pop().close()

    xloc = dram_in("xloc", [TOK, D])
    xhalo = dram_in("xhalo", [HALO, D])
    cos_d = dram_in("ropecos", [128, TH])
    sin_d = dram_in("ropesin", [128, TH])
    masks_d = dram_in("masks", [128, 3, 128])
    ident_d = dram_in("ident", [128, 128])
    w_in = dram_in("w_in", [D, 1280])
    b_in = dram_in("b_in", [1280])
    sinks_d = dram_in("attn_sinks", [8])

    PS = [es.enter_context(nc.psum_tensor(f"ps{i}", [128, 512], F32)) for i in range(6)]
    PS += [es.enter_context(nc.psum_tensor(f"ps{i}", [128, 1024], BF16)) for i in (6, 7)]
    PSB = [Buf(f"ps{i}") for i in range(8)]

    identb = sb("identb", [128, 128], BF16)
    identf = sb("identf", [128, 128], F32)
    catT = sb("catT", [128, 8, TOK], BF16)
    open_phase()
    cosT = sb("cosT", [128, TH], F32)
    sinT = sb("sinT", [128, TH], F32)
    maskb = sb("maskb", [128, 3, 128], BF16)
    B_ident, B_mask, B_cos, B_sin = Buf("ident"), Buf("mask"), Buf("cos"), Buf("sin")
    S.dma("pool", lambda e: e.dma_start(out=identb[:, :], in_=ident_d[:, :]), writes=[B_ident])
    B_identf = Buf("identf")
    S.dma("sp", lambda e: e.dma_start(out=identf[:, :], in_=ident_d[:, :]), writes=[B_identf])
    S.dma("pool", lambda e: e.dma_start(out=maskb[:, :, :], in_=masks_d[:, :, :]), writes=[B_mask])
    S.dma("sp", lambda e: e.dma_start(out=cosT[:, :], in_=cos_d[:, :]), writes=[B_cos])
    S.dma("sp", lambda e: e.dma_start(out=sinT[:, :], in_=sin_d[:, :]), writes=[B_sin])

    win = sb("win", [128, KT, 1280], BF16)
    B_win = Buf("win")
    w_in_v = w_in.rearrange("(kt p) f -> p kt f", p=128)
    for h in range(2):
        S.dma("pool", lambda e, h=h: e.dma_start(out=win[:, 4 * h:4 * h + 4, :], in_=w_in_v[:, 4 * h:4 * h + 4, :]),
              writes=[B_win])
    wkb = sb("wkb", [128, KT, 128], BF16)
    wrot = sb("wrot", [128, KT, 6, 128], BF16)
    B_wrot = Buf("wrot")

    def src_col(m, half):
        if m < 4:
            return 128 * m + 64 * half
        if m == 4:
            return 512 + 64 * half
        return 512 + 64 * (1 - half)

    for half in range(2):
        c0 = src_col(5, half)
        S.op("dve", lambda e, half=half, c0=c0: e.tensor_copy(out=wkb[:, :, 64 * half:64 * half + 64], in_=win[:, :, c0:c0 + 64]),
             reads=[B_win], writes=[B_wrot])
    for m in range(6):
        for half in range(2):
            c0 = src_col(m, half)
            d0 = 64 * half
            S.op("dve", lambda e, m=m, c0=c0, d0=d0: e.tensor_scalar(
                out=wrot[:, :, m, d0:d0 + 32], in0=win[:, :, c0 + 32:c0 + 64], scalar1=-1.0, scalar2=None, op0=ALU.mult),
                reads=[B_win], writes=[B_wrot])
            S.op("dve", lambda e, m=m, c0=c0, d0=d0: e.tensor_copy(out=wrot[:, :, m, d0 + 32:d0 + 64], in_=win[:, :, c0:c0 + 32]),
                 reads=[B_win], writes=[B_wrot])

    bfm = sb("bfm", [128, 10], F32)
    bqk = sb("bqk", [128, 6], F32)
    bqkr = sb("bqkr", [128, 6], F32)
    bvbc = sb("bvbc", [128, 128], F32)
    B_bias, B_bfm, B_bvbc = Buf("bias"), Buf("bfm"), Buf("bvbc")
    brow = sb("brow", [10, 128], F32)
    B_brow = Buf("brow")
    S.dma("sp", lambda e: e.dma_start(out=brow[:, :], in_=b_in.rearrange("(t p) -> t p", p=128)), writes=[B_brow])
    S.op("pe", lambda e: e.transpose(out=PS[5][:, 0:10], in_=brow[:, :], identity=identf[0:10, 0:10]),
         reads=[B_brow, B_identf], writes=[PSB[5]])
    S.op("dve", lambda e: e.tensor_copy(out=bfm[:, :], in_=PS[5][:, 0:10]), reads=[PSB[5]], writes=[B_bfm])
    S.dma("sp", lambda e: e.dma_start(out=bvbc[:, :], in_=b_in[640:768].partition_broadcast(128)), writes=[B_bvbc])
    S.op("dve", lambda e: e.tensor_copy(out=bqk[:, 0:5], in_=bfm[:, 0:5]), reads=[B_bfm], writes=[B_bias])
    S.op("dve", lambda e: e.tensor_copy(out=bqk[0:64, 5:6], in_=bfm[64:128, 4:5]), reads=[B_bfm, B_bias], writes=[B_bias])
    S.op("dve", lambda e: e.tensor_copy(out=bqk[64:128, 5:6], in_=bfm[0:64, 4:5]), reads=[B_bfm, B_bias], writes=[B_bias])
    for hb in (0, 64):
        S.op("dve", lambda e, hb=hb: e.tensor_scalar(out=bqkr[hb:hb + 32, :], in0=bqk[hb + 32:hb + 64, :], scalar1=-1.0,
                                                     scalar2=None, op0=ALU.mult), reads=[B_bias], writes=[B_bias])
        S.op("dve", lambda e, hb=hb: e.tensor_copy(out=bqkr[hb + 32:hb + 64, :], in_=bqk[hb:hb + 32, :]),
             reads=[B_bias], writes=[B_bias])

    es8 = sb("es8", [128, 8], F32)
    esfm = sb("esfm", [128, 4], F32)
    B_sink = Buf("sink")
    S.dma("sp", lambda e: e.dma_start(out=es8[:, :], in_=sinks_d.partition_broadcast(128)), writes=[B_sink])
    S.op("act", lambda e: e.activation(out=es8[:, :], in_=es8[:, :], func=AF.Exp), reads=[B_sink], writes=[B_sink])
    S.op("dve", lambda e: e.tensor_copy(out=esfm[0:64, :], in_=es8[0:64, 0:8:2]), reads=[B_sink], writes=[B_sink])
    S.op("dve", lambda e: e.tensor_copy(out=esfm[64:128, :], in_=es8[64:128, 1:8:2]), reads=[B_sink], writes=[B_sink])

    xT = sb("xT", [128, KT, TH], BF16)
    B_xT = [Buf(f"xT{t}") for t in range(NT + 1)]
    xtok = [sb(f"xtok{i}", [128, D], BF16) for i in range(2)]
    B_xtok = [Buf(f"xtok{i}") for i in range(2)]
    for t in range(NT + 1):
        sl = t % 2
        src = xhalo[:, :] if t == 0 else xloc[(t - 1) * 128:t * 128, :]
        S.dma("pool", lambda e, sl=sl, src=src: e.dma_start(out=xtok[sl][:, :], in_=src), writes=[B_xtok[sl]])
        pb = 6 + sl
        pv = PS[pb][:, :]
        S.op("pe", [lambda e, k=k, sl=sl, pv=pv: e.transpose(out=pv[:, k * 128:(k + 1) * 128], in_=xtok[sl][:, k * 128:(k + 1) * 128],
                                                             identity=identb[:, :]) for k in range(KT)],
             reads=[B_xtok[sl], B_ident], writes=[PSB[pb]])
        ev = "dve" if t % 2 == 0 else "act"
        if ev == "dve":
            S.op("dve", lambda e, t=t, pv=pv: e.tensor_copy(out=xT[:, :, t * 128:(t + 1) * 128],
                                                           in_=pv.rearrange("p (k n) -> p k n", k=KT)),
                 reads=[PSB[pb]], writes=[B_xT[t]])
        else:
            S.op("act", lambda e, t=t, pv=pv: e.copy(out=xT[:, :, t * 128:(t + 1) * 128],
                                                    in_=pv.rearrange("p (k n) -> p k n", k=KT)),
                 reads=[PSB[pb]], writes=[B_xT[t]])

    if stage == "xt":
        dx = dram_out("dbg_xT", [D, TH])
        S.dma("pool", lambda e: e.dma_start(out=dx.rearrange("(k p) t -> p k t", p=128)[:, :, :], in_=xT[:, :, :]), stream="out",
              reads=B_xT, writes=[Buf("o")])
        db = dram_out("dbg_bfm", [128, 10])
        S.dma("sp", lambda e: e.dma_start(out=db[:, :], in_=bfm[:, :]), stream="out", reads=[B_bfm], writes=[Buf("o1")])
        S.finish()
        close_phase(); es.close()
        return nc, ["dbg_xT", "dbg_bfm"]

    qT = sb("qT", [128, 4, TOK], BF16)
    kT = sb("kT", [128, 2, TH], BF16)
    vpad = sb("vpad", [128, NT + 1, 2, 192], BF16)
    opad = sb("opad", [128, 192], BF16)
    B_pad = Buf("pad")
    S.op("pool", lambda e: e.memset(vpad[:, :, :, :], 0.0), writes=[B_pad])
    S.op("pool", lambda e: e.memset(opad[:, :], 0.0), writes=[B_pad])
    S.op("pool", lambda e: e.memset(opad[:, 64:128], 1.0), reads=[B_pad], writes=[B_pad])
    B_q = [Buf(f"q{b}") for b in range(4)]
    B_k = [Buf(f"k{b}") for b in range(5)]
    B_v = [Buf(f"v{t}") for t in range(NT + 1)]
    rtmp = [sb(f"rtmp{i}", [128, 2, 512], F32) for i in range(2)]
    B_rtmp = [Buf(f"rtmp{i}") for i in range(2)]
    it = 0

    def wcols(m):
        if m < 4:
            return lambda k: win[:, k, 128 * m:128 * m + 128]
        if m == 4:
            return lambda k: win[:, k, 512:640]
        return lambda k: wkb[:, k, :]

    blocks = [(0, 128, 0)] + [(128 + 512 * b, 512, b + 1) for b in range(4)]
    for (c0, n, bi) in blocks:
        tiles = range((c0 // 128), (c0 + n) // 128)
        xdeps = [B_xT[t] for t in tiles]
        for m in (range(6) if bi > 0 else (4, 5)):
            pa, pb = 2 * (it % 2), 2 * (it % 2) + 1
            wc = wcols(m)
            S.op("pe", [lambda e, k=k, wc=wc, pa=pa, c0=c0, n=n: e.matmul(PS[pa][:, 0:n], lhsT=wc(k), rhs=xT[:, k, c0:c0 + n],
                                                                         start=(k == 0), stop=(k == KT - 1)) for k in range(KT)],
                 reads=xdeps + [B_win, B_wrot], writes=[PSB[pa]])
            S.op("pe", [lambda e, k=k, m=m, pb=pb, c0=c0, n=n: e.matmul(PS[pb][:, 0:n], lhsT=wrot[:, k, m, :], rhs=xT[:, k, c0:c0 + n],
                                                                       start=(k == 0), stop=(k == KT - 1)) for k in range(KT)],
                 reads=xdeps + [B_wrot], writes=[PSB[pb]])
            rt = it % 2
            S.op("dve", lambda e, pa=pa, m=m, c0=c0, n=n, rt=rt: e.scalar_tensor_tensor(
                out=rtmp[rt][:, 0, 0:n], in0=PS[pa][:, 0:n], scalar=bqk[:, m:m + 1], in1=cosT[:, c0:c0 + n], op0=ALU.add, op1=ALU.mult),
                reads=[PSB[pa], B_bias, B_cos], writes=[B_rtmp[rt]])
            S.op("dve", lambda e, pb=pb, m=m, c0=c0, n=n, rt=rt: e.scalar_tensor_tensor(
                out=rtmp[rt][:, 1, 0:n], in0=PS[pb][:, 0:n], scalar=bqkr[:, m:m + 1], in1=sinT[:, c0:c0 + n], op0=ALU.add, op1=ALU.mult),
                reads=[PSB[pb], B_bias, B_sin], writes=[B_rtmp[rt]])
            if m < 4:
                dst, dbuf = qT[:, m, c0 - 128:c0 - 128 + n], B_q[bi - 1]
            else:
                dst, dbuf = kT[:, m - 4, c0:c0 + n], B_k[bi]
            S.op("dve", lambda e, dst=dst, rt=rt, n=n: e.tensor_tensor(out=dst, in0=rtmp[rt][:, 0, 0:n], in1=rtmp[rt][:, 1, 0:n], op=ALU.add),
                 reads=[B_rtmp[rt]], writes=[dbuf])
            it += 1
    for t in range(NT + 1):
        pv_ = 4 + (t % 2)
        S.op("pe", [lambda e, k=k, t=t, pv_=pv_: e.matmul(PS[pv_][:, 0:128], lhsT=xT[:, k, t * 128:(t + 1) * 128], rhs=win[:, k, 640:768],
                                                          start=(k == 0), stop=(k == KT - 1)) for k in range(KT)],
             reads=[B_xT[t], B_win], writes=[PSB[pv_]])
        S.op("dve", lambda e, t=t, pv_=pv_: e.tensor_tensor(out=vpad[:, t, :, 64:128], in0=PS[pv_][:, 0:128].rearrange("p (g d) -> p g d", g=2),
                                                            in1=bvbc[:, :].rearrange("p (g d) -> p g d", g=2), op=ALU.add),
             reads=[PSB[pv_], B_bvbc, B_pad], writes=[B_v[t]])

    if stage == "qk":
        dq = dram_out("dbg_q", [512, TOK])
        S.dma("pool", lambda e: e.dma_start(out=dq.rearrange("(c p) t -> p c t", p=128)[:, :, :], in_=qT[:, :, :]), stream="out", reads=B_q, writes=[Buf("o2")])
        dk = dram_out("dbg_k", [256, TH])
        S.dma("pool", lambda e: e.dma_start(out=dk.rearrange("(c p) t -> p c t", p=128)[:, :, :], in_=kT[:, :, :]), stream="out", reads=B_k, writes=[Buf("o3")])
        dvv = dram_out("dbg_v", [TH, 128])
        for g in range(2):
            S.dma("pool", lambda e, g=g: e.dma_start(out=dvv[:, 64 * g:64 * g + 64].rearrange("(t p) d -> p t d", p=128)[:, :, :],
                                                     in_=vpad[:, :, g, 64:128]),
                  stream="out", reads=B_v, writes=[Buf(f"o4{g}")])
        S.finish()
        close_phase(); es.close()
        return nc, ["dbg_q", "dbg_k", "dbg_v"]

    kTz = sb("kTz", [128, 4, TH], BF16)
    B_kz0 = Buf("kz0")
    S.op("pool", lambda e: e.memset(kTz[:, :, :], 0.0), writes=[B_kz0])
    B_kz = [Buf(f"kz{b}") for b in range(5)]
    kz_map = ((0, 0, 0), (1, 1, 1), (2, 0, 1), (3, 1, 0))
    for (c0, n, bi) in blocks:
        for (vi, half, var) in kz_map:
            p0 = 64 * half
            S.op("dve", lambda e, vi=vi, p0=p0, var=var, c0=c0, n=n: e.tensor_copy(out=kTz[p0:p0 + 64, vi, c0:c0 + n], in_=kT[p0:p0 + 64, var, c0:c0 + n]),
                 reads=[B_k[bi], B_kz0], writes=[B_kz[bi]])

    B_cat = [Buf(f"cat{t}") for t in range(NT)]
    pT = [sb(f"pT{i}", [128, 2, 512], BF16) for i in range(2)]
    B_pT = [Buf(f"pT{i}") for i in range(2)]
    ltmp = [sb(f"ltmp{i}", [128, 128], F32) for i in range(2)]
    B_ltmp = [Buf(f"ltmp{i}") for i in range(2)]
    kblk = lambda t: B_k[0] if t == 0 else B_k[1 + (t - 1) // 4]
    kzblk = lambda t: B_kz[0] if t == 0 else B_kz[1 + (t - 1) // 4]
    n_qblk = 1 if stage == "attn1" else NT

    def swa_F(i, g, sl):
        for kk, ktile in enumerate((i, i + 1)):
            pbk = 2 * sl + kk
            fns = []
            for hh in range(4):
                h = 4 * g + hh
                half = h % 2
                fns.append(lambda e, pbk=pbk, hh=hh, h=h, half=half, ktile=ktile: e.matmul(
                    PS[pbk][:, hh * 128:(hh + 1) * 128],
                    lhsT=kTz[:, 2 * g + half, ktile * 128:(ktile + 1) * 128],
                    rhs=qT[:, h // 2, i * 128:(i + 1) * 128], start=True, stop=True))
            S.op("pe", fns, reads=[kzblk(ktile), B_q[i // 4]], writes=[PSB[pbk]])
            S.op("act", lambda e, pbk=pbk, kk=kk: e.activation(out=pT[sl][:, kk, :], in_=PS[pbk][:, :], func=AF.Exp, scale=0.125),
                 reads=[PSB[pbk]], writes=[B_pT[sl]])
            mi = 1 if kk == 1 else (2 if i == 0 else 0)
            S.op("dve", lambda e, kk=kk, mi=mi: e.tensor_tensor(
                out=pT[sl][:, kk, :].rearrange("p (h q) -> p h q", h=4), in0=pT[sl][:, kk, :].rearrange("p (h q) -> p h q", h=4),
                in1=maskb[:, mi, :].unsqueeze(1).to_broadcast([128, 4, 128]), op=ALU.mult),
                reads=[B_pT[sl], B_mask], writes=[B_pT[sl]])

    def swa_G(i, g, sl):
        for tt in range(2):
            ct = 2 * g + tt
            po = 4 + tt
            fo, fl = [], []
            n_mm = 0
            for hl in range(2):
                hh = 2 * tt + hl
                for kk, ktile in enumerate((i, i + 1)):
                    first, last = (n_mm == 0), (n_mm == 3)
                    vs = (64, 192) if hl == 0 else (0, 128)
                    fo.append(lambda e, po=po, vs=vs, ktile=ktile, kk=kk, hh=hh, first=first, last=last: e.matmul(
                        PS[po][:, 0:128], lhsT=vpad[:, ktile, g, vs[0]:vs[1]], rhs=pT[sl][:, kk, hh * 128:(hh + 1) * 128],
                        start=first, stop=last))
                    fl.append(lambda e, po=po, vs=vs, kk=kk, hh=hh, first=first, last=last: e.matmul(
                        PS[po][:, 128:256], lhsT=opad[:, vs[0]:vs[1]], rhs=pT[sl][:, kk, hh * 128:(hh + 1) * 128],
                        start=first, stop=last))
                    n_mm += 1
            S.op("pe", fo + fl, reads=[B_pT[sl], B_v[i], B_v[i + 1], B_pad], writes=[PSB[po]])
            ls = tt
            S.op("dve", lambda e, po=po, ct=ct, ls=ls: e.tensor_scalar(out=ltmp[ls][:, :], in0=PS[po][:, 128:256], scalar1=esfm[:, ct:ct + 1],
                                                                        scalar2=None, op0=ALU.add), reads=[PSB[po], B_sink], writes=[B_ltmp[ls]])
            S.op("dve", lambda e, ls=ls: e.reciprocal(out=ltmp[ls][:, :], in_=ltmp[ls][:, :]), reads=[B_ltmp[ls]], writes=[B_ltmp[ls]])
            S.op("dve", lambda e, po=po, ct=ct, ls=ls: e.tensor_tensor(out=catT[:, ct, i * 128:(i + 1) * 128], in0=PS[po][:, 0:128],
                                                                       in1=ltmp[ls][:, :], op=ALU.mult),
                 reads=[PSB[po], B_ltmp[ls]], writes=[B_cat[i]])

    its = [(i, g) for i in range(n_qblk) for g in range(2)]
    for n, (i, g) in enumerate(its):
        swa_F(i, g, n % 2)
        if n > 0:
            swa_G(its[n - 1][0], its[n - 1][1], (n - 1) % 2)
    swa_G(its[-1][0], its[-1][1], (len(its) - 1) % 2)

    outs = []
    if stage in ("attn", "attn1"):
        dbg = dram_out("dbg_attn", [512, TOK])
        dv = dbg.rearrange("(c p) t -> p c t", p=128)
        nqo = 128 * n_qblk
        S.dma("pool", lambda e: e.dma_start(out=dv[:, :, 0:nqo], in_=catT[:, 0:4, 0:nqo]), stream="out", reads=B_cat[:n_qblk], writes=[Buf("o")])
        dq = dram_out("dbg_q", [512, TOK])
        S.dma("pool", lambda e: e.dma_start(out=dq.rearrange("(c p) t -> p c t", p=128)[:, :, :], in_=qT[:, :, :]), stream="out", reads=B_q, writes=[Buf("o2")])
        dk = dram_out("dbg_k", [256, TH])
        S.dma("pool", lambda e: e.dma_start(out=dk.rearrange("(c p) t -> p c t", p=128)[:, :, :], in_=kT[:, :, :]), stream="out", reads=B_k, writes=[Buf("o3")])
        outs = ["dbg_attn", "dbg_q", "dbg_k"]
        S.finish()
        close_phase(); es.close()
        return nc, outs
    close_phase()
    print("[build] sbuf free after phase 1:", nc.sbuf_bytes_remaining)
    return build_rest(nc, es, S, sb, open_phase, close_phase, dram_in, dram_out, PS, PSB, identb, identf, catT, B_cat,
                      B_ident, B_identf, stage)


def build_rest(nc, es, S, sb, open_phase, close_phase, dram_in, dram_out, PS, PSB, identb, identf, catT, B_cat,
               B_ident, B_identf, stage):
    xloc = nc_ap(nc, "xloc")
    xprev = dram_in("xprev", [3 * TOK, D])
    segv_d = dram_in("segvalid", [128, 4])
    w_in = nc_ap(nc, "w_in")
    b_in = nc_ap(nc, "b_in")
    lam_re_d = dram_in("s5_lambda_re", [32, 64]); lam_im_d = dram_in("s5_lambda_im", [32, 64])
    logdt_d = dram_in("s5_log_dt", [32])
    bre_d = dram_in("s5_b_re", [32, 64, 16]); bim_d = dram_in("s5_b_im", [32, 64, 16])
    cre_d = dram_in("s5_c_re", [32, 16, 64]); cim_d = dram_in("s5_c_im", [32, 16, 64])
    d_d = dram_in("s5_d", [512])
    wglu_d = dram_in("s5_w_glu", [512, 512]); bglu_d = dram_in("s5_b_glu", [512])
    cmask_d = dram_in("cmask", [128, 2, 256])

    open_phase()
    B_tb = Buf("tb")
    B_t4 = Buf("t4")

    def tt(out, a, b, op, eng="dve"):
        S.op(eng, lambda e: e.tensor_tensor(out=out, in0=a, in1=b, op=op), reads=[B_tb, B_t4], writes=[B_tb, B_t4])

    def ts(out, a, s1, op0, s2=None, op1=None):
        if op1 is None:
            S.op("dve", lambda e: e.tensor_scalar(out=out, in0=a, scalar1=s1, scalar2=None, op0=op0), reads=[B_tb, B_t4], writes=[B_tb, B_t4])
        else:
            S.op("dve", lambda e: e.tensor_scalar(out=out, in0=a, scalar1=s1, scalar2=s2, op0=op0, op1=op1), reads=[B_tb, B_t4], writes=[B_tb, B_t4])

    def TT(out, a, b, op, reads, writes, eng="dve"):
        S.op(eng, lambda e: e.tensor_tensor(out=out, in0=a, in1=b, op=op), reads=reads, writes=writes)

    def cp(out, a):
        S.op("dve", lambda e: e.tensor_copy(out=out, in_=a), reads=[B_tb, B_t4], writes=[B_tb, B_t4])

    def act(out, a, func, scale=1.0):
        S.op("act", lambda e: e.activation(out=out, in_=a, func=func, scale=scale), reads=[B_tb, B_t4], writes=[B_tb, B_t4])

    def cmul(outr, outi, ar, ai, br, bi, t1, t2, neg_i=False):
        tt(t1, ar, br, ALU.mult); tt(t2, ai, bi, ALU.mult); tt(outr, t1, t2, ALU.subtract)
        tt(t1, ar, bi, ALU.mult); tt(t2, ai, br, ALU.mult)
        if neg_i:
            tt(t1, t1, t2, ALU.add); ts(outi, t1, -1.0, ALU.mult)
        else:
            tt(outi, t1, t2, ALU.add)

    U = sb("s5U", [128, 32, 16, 16], BF16)
    UT = sb("s5UT", [128, 32, 2, 128], BF16)
    Hst = sb("s5H", [128, 32, 128], BF16)
    A_ = sb("s5A", [64, 2, 32, 17], F32)
    AI = sb("s5AI", [64, 2, 32, 16], F32)
    B15 = sb("s5B15", [64, 2, 32, 16], F32)
    CT = sb("s5CT", [64, 2, 32, 16], F32)
    C15 = sb("s5C15", [64, 2, 32, 16], F32)
    t4 = sb("s5t4", [128, 2, 2048], F32)
    t4a = t4[0:64, 0, :].rearrange("p (g s h) -> p g s h", g=8, s=16)
    t4b = t4[0:64, 1, :].rearrange("p (g s h) -> p g s h", g=8, s=16)
    ztmp = t4[:, 0, :].rearrange("p (g c) -> p g c", g=16)
    PSt = sb("s5PSt", [128, 8, 256], BF16)
    B_U, B_UT, B_H = Buf("s5U"), Buf("s5UT"), Buf("s5H")

    def build_PSt(g0):
        bc_s = lambda t, ri: t[:, ri, g0:g0 + 8, :].unsqueeze(3).to_broadcast([64, 8, 16, 16])
        bc_h = lambda t, ri: t[:, ri, g0:g0 + 8, :].unsqueeze(2).to_broadcast([64, 8, 16, 16])
        o4 = lambda lo: PSt[lo:lo + 64, :, :].rearrange("p g (s h) -> p g s h", s=16)
        tt(t4a, bc_s(AI, 0), bc_h(B15, 0), ALU.mult); tt(t4b, bc_s(AI, 1), bc_h(B15, 1), ALU.mult)
        tt(o4(0), t4a, t4b, ALU.subtract)
        tt(t4a, bc_s(AI, 0), bc_h(B15, 1), ALU.mult); tt(t4b, bc_s(AI, 1), bc_h(B15, 0), ALU.mult)
        tt(o4(64), t4a, t4b, ALU.add)

    open_phase()
    LA = sb("s5LA", [128, 32, 2, 128], BF16)
    R = sb("s5R", [128, 2, 16, 129], F32)
    rho_p = sb("s5rho", [128, 16], F32)
    segv = sb("s5segv", [128, 4], F32)
    open_phase()
    sm = sb("s5sm", [64, 56, 32], F32)
    smi = iter(range(56))
    T = lambda: sm[:, next(smi), :]
    lamn = sb("s5lamn", [32, 2, 64], F32)
    Bnat = sb("s5Bnat", [128, 2, 16, 16], F32)
    Cnat = sb("s5Cnat", [128, 2, 4, 64], F32)
    Bq = sb("s5Bq", [64, 2, 32, 16], F32)
    Bbar = sb("s5Bbar", [64, 2, 32, 16], F32)
    t3a = sb("s5t3a", [64, 32, 16], F32)
    t3b = sb("s5t3b", [64, 32, 16], F32)
    S.dma("sp", lambda e: e.dma_start(out=lamn[:, 0, :], in_=lam_re_d[:, :]), stream="tbl", writes=[B_tb])
    S.dma("sp", lambda e: e.dma_start(out=lamn[:, 1, :], in_=lam_im_d[:, :]), stream="tbl", writes=[B_tb])
    dtb = T()
    S.dma("sp", lambda e: e.dma_start(out=dtb, in_=logdt_d.partition_broadcast(64)), stream="tbl", writes=[B_tb])
    for ri, src in enumerate((bre_d, bim_d)):
        S.dma("sp", lambda e, ri=ri, src=src: e.dma_start(out=Bnat[:, ri, :, :], in_=src.rearrange("g p h -> (g p) h").rearrange("(gg r) h -> r gg h", r=128)),
              stream="tbl", writes=[B_tb])
    for ri, src in enumerate((cre_d, cim_d)):
        S.dma("sp", lambda e, ri=ri, src=src: e.dma_start(out=Cnat[:, ri, :, :], in_=src.rearrange("g h p -> (g h) p").rearrange("(t r) p -> r t p", r=128)),
              stream="tbl", writes=[B_tb])
    S.dma("sp", lambda e: e.dma_start(out=segv[:, :], in_=segv_d[:, :]), stream="tbl", writes=[B_tb])
    S.op("pe", [lambda e, ri=ri: e.transpose(out=PS[5][0:64, 32 * ri:32 * ri + 32], in_=lamn[:, ri, :], identity=identf[0:32, 0:32]) for ri in range(2)],
         reads=[B_tb, B_identf], writes=[PSB[5]])
    lam_r, lam_i = T(), T()
    S.op("dve", lambda e: e.tensor_copy(out=lam_r, in_=PS[5][0:64, 0:32]), reads=[PSB[5], B_tb], writes=[B_tb])
    S.op("dve", lambda e: e.tensor_copy(out=lam_i, in_=PS[5][0:64, 32:64]), reads=[PSB[5], B_tb], writes=[B_tb])
    for ri in range(2):
        S.op("pe", [lambda e, ri=ri, t=t: e.transpose(out=PS[4][0:64, 128 * t:128 * t + 128], in_=Cnat[:, ri, t, :], identity=identf[:, :]) for t in range(4)],
             reads=[B_tb, B_identf], writes=[PSB[4]])
        S.op("dve", lambda e, ri=ri: e.tensor_copy(out=CT[:, ri, :, :].rearrange("p g h -> p (g h)"), in_=PS[4][0:64, :]), reads=[PSB[4], B_tb], writes=[B_tb])
    for ri in range(2):
        cp(Bq[:, ri, 0:32:2, :], Bnat[0:64, ri, :, :])
        cp(Bq[:, ri, 1:32:2, :], Bnat[64:128, ri, :, :])
    act(dtb, dtb, AF.Exp)
    lr = T(); ts(lr, lam_r, -1e-4, ALU.min)
    mu, th = T(), T(); tt(mu, lr, dtb, ALU.mult); tt(th, lam_i, dtb, ALU.mult)
    sn, sh, cs = T(), T(), T()
    act(sn, th, AF.Sin, scale=0.125); act(sh, th, AF.Sin, scale=0.0625)
    tt(sh, sh, sh, ALU.mult); ts(cs, sh, -2.0, ALU.mult, 1.0, ALU.add)
    u1, u2 = T(), T()
    for _ in range(3):
        cn, s2 = T(), T()
        tt(u1, cs, cs, ALU.mult); tt(u2, sn, sn, ALU.mult); tt(cn, u1, u2, ALU.subtract)
        tt(u1, cs, sn, ALU.mult); ts(s2, u1, 2.0, ALU.mult)
        cs, sn = cn, s2
    mag, ar, ai = T(), T(), T()
    act(mag, mu, AF.Exp); tt(ar, mag, cs, ALU.mult); tt(ai, mag, sn, ALU.mult)
    den, am1, zr, zi = T(), T(), T(), T()
    tt(u1, lr, lr, ALU.mult); tt(u2, lam_i, lam_i, ALU.mult); tt(den, u1, u2, ALU.add)
    S.op("dve", lambda e: e.reciprocal(out=den, in_=den), reads=[B_tb, B_t4], writes=[B_tb, B_t4])
    ts(am1, ar, -1.0, ALU.add)
    tt(u1, am1, lr, ALU.mult); tt(u2, ai, lam_i, ALU.mult); tt(u1, u1, u2, ALU.add); tt(zr, u1, den, ALU.mult)
    tt(u1, ai, lr, ALU.mult); tt(u2, am1, lam_i, ALU.mult); tt(u1, u1, u2, ALU.subtract); tt(zi, u1, den, ALU.mult)
    bz = lambda t: t.unsqueeze(2).to_broadcast([64, 32, 16])
    cmul(Bbar[:, 0, :, :], Bbar[:, 1, :, :], bz(zr), bz(zi), Bq[:, 0, :, :], Bq[:, 1, :, :], t3a[:, :, :], t3b[:, :, :])
    air, aii = T(), T()
    tt(u1, ar, ar, ALU.mult); tt(u2, ai, ai, ALU.mult); tt(u1, u1, u2, ALU.add)
    S.op("dve", lambda e: e.reciprocal(out=u1, in_=u1), reads=[B_tb, B_t4], writes=[B_tb, B_t4])
    tt(air, ar, u1, ALU.mult); tt(aii, ai, u1, ALU.mult); ts(aii, aii, -1.0, ALU.mult)

    def cpow(tab, n, br, bi, npart, shp, tmpa, tmpb, bm_bufs):
        nd = len(shp)
        S.op("dve", lambda e: e.memset(tab_slice(tab, 0, nd, 0, 1), 1.0), reads=[B_tb, B_t4], writes=[B_tb, B_t4])
        S.op("dve", lambda e: e.memset(tab_slice(tab, 1, nd, 0, 1), 0.0), reads=[B_tb, B_t4], writes=[B_tb, B_t4])
        filled, bmr, bmi, bi_ = 1, br, bi, 0
        while filled < n:
            cnt = min(filled, n - filled)
            bb = lambda t: t.unsqueeze(nd + 1).to_broadcast([npart] + shp + [cnt])
            cmul(tab_slice(tab, 0, nd, filled, filled + cnt), tab_slice(tab, 1, nd, filled, filled + cnt),
                 tab_slice(tab, 0, nd, 0, cnt), tab_slice(tab, 1, nd, 0, cnt), bb(bmr), bb(bmi),
                 tab_slice(tmpa, None, nd, 0, cnt), tab_slice(tmpb, None, nd, 0, cnt))
            filled += cnt
            if filled < n:
                nr, ni = bm_bufs[bi_]; bi_ += 1
                q1, q2 = bm_bufs[-1]
                tt(q1, bmr, bmr, ALU.mult); tt(q2, bmi, bmi, ALU.mult); tt(nr, q1, q2, ALU.subtract)
                tt(q1, bmr, bmi, ALU.mult); ts(ni, q1, 2.0, ALU.mult)
                bmr, bmi = nr, ni

    def tab_slice(tab, ri, nd, k0, k1):
        if ri is None:
            return tab[:, :, k0:k1] if nd == 1 else tab[:, :, :, k0:k1]
        return tab[:, ri, :, k0:k1] if nd == 1 else tab[:, ri, :, :, k0:k1]

    t17a = sb("s5t17a", [64, 32, 8], F32); t17b = sb("s5t17b", [64, 32, 8], F32)
    bmA = [(T(), T()) for _ in range(5)]
    cpow(A_, 17, ar, ai, 64, [32], t17a, t17b, bmA)
    bmI = [(T(), T()) for _ in range(5)]
    cpow(AI, 16, air, aii, 64, [32], t17a, t17b, bmI)
    b15 = lambda t, ri, k: t[:, ri, :, k:k + 1].to_broadcast([64, 32, 16])
    cmul(B15[:, 0, :, :], B15[:, 1, :, :], b15(A_, 0, 15), b15(A_, 1, 15), Bbar[:, 0, :, :], Bbar[:, 1, :, :], t3a[:, :, :], t3b[:, :, :])
    cmul(C15[:, 0, :, :], C15[:, 1, :, :], b15(AI, 0, 15), b15(AI, 1, 15), CT[:, 0, :, :], CT[:, 1, :, :], t3a[:, :, :], t3b[:, :, :])
    rho = T(); act(rho, mu, AF.Exp, scale=16.0)
    rr = T()
    S.op("dve", lambda e: e.reciprocal(out=rr, in_=rho), reads=[B_tb, B_t4], writes=[B_tb, B_t4])
    rot_r, rot_i = T(), T()
    tt(rot_r, A_[:, 0, :, 16], rr, ALU.mult); tt(rot_i, A_[:, 1, :, 16], rr, ALU.mult)
    rotp = sb("s5rotp", [128, 2, 16], F32)
    for src, dst in ((rot_r, rotp[:, 0, :]), (rot_i, rotp[:, 1, :]), (rho, rho_p[:, :])):
        cp(dst[0:64], src[:, 0:32:2]); cp(dst[64:128], src[:, 1:32:2])
    tRa = sb("s5tRa", [128, 16, 64], F32); tRb = sb("s5tRb", [128, 16, 64], F32)
    smp = sb("s5smp", [128, 16, 16], F32)
    bmR = [(smp[:, 2 * i, :], smp[:, 2 * i + 1, :]) for i in range(8)]
    cpow(R, 129, rotp[:, 0, :], rotp[:, 1, :], 128, [16], tRa, tRb, bmR)
    for gb in range(4):
        build_PSt(8 * gb)
        for hb in range(2):
            bank = 6 + hb
            S.op("pe", [lambda e, j=j, hb=hb, bank=bank: e.transpose(out=PS[bank][:, 128 * j:128 * j + 128],
                                                                     in_=PSt[:, 4 * hb + j // 2, 128 * (j % 2):128 * (j % 2) + 128], identity=identb[:, :])
                        for j in range(8)], reads=[B_tb, B_ident], writes=[PSB[bank]])
            S.op("act", lambda e, gb=gb, hb=hb, bank=bank: e.copy(out=LA[:, 8 * gb + 4 * hb:8 * gb + 4 * hb + 4, :, :].rearrange("p g k c -> p (g k c)"),
                                                                  in_=PS[bank][:, :]), reads=[PSB[bank], B_tb], writes=[B_tb])

    cmul(B15[:, 0, :, :], B15[:, 1, :, :], b15(AI, 0, 1), b15(AI, 1, 1), Bbar[:, 0, :, :], Bbar[:, 1, :, :], t3a[:, :, :], t3b[:, :, :])
    close_phase()
    print("[build] sbuf free before S5 segment buffers:", nc.sbuf_bytes_remaining)

    wu = sb("s5wu", [128, KT, 512], BF16)
    bubc = sb("s5bubc", [128, 512], F32)
    B_wu, B_bu = Buf("s5wu"), Buf("s5bu")
    S.dma("pool", lambda e: e.dma_start(out=wu[:, :, :], in_=w_in.rearrange("(kt p) f -> p kt f", p=128)[:, :, 768:1280]), writes=[B_wu])
    S.dma("sp", lambda e: e.dma_start(out=bubc[:, :], in_=b_in[768:1280].partition_broadcast(128)), writes=[B_bu])
    xs = [sb(f"s5xs{i}", [128, D], BF16) for i in range(2)]
    B_xs = [Buf(f"s5xs{i}") for i in range(2)]
    xsf = [sb(f"s5xsf{i}", [128, D], F32) for i in range(2)]
    B_xsf = [Buf(f"s5xsf{i}") for i in range(2)]
    xsT = [sb(f"s5xsT{i}", [128, KT, 128], BF16) for i in range(2)]
    B_xsT = [Buf(f"s5xsT{i}") for i in range(2)]
    Z = sb("s5Z", [128, 16, 2, 128], F32)
    G2 = sb("s5G2", [128, 16, 2, 129], F32)
    B_Z, B_G = Buf("s5Z"), Buf("s5G")
    ctmp = sb("s5ctmp", [128, 4, 16], F32)
    S.op("dve", lambda e: e.memset(G2[:, :, :, 0:1], 0.0), reads=[B_G], writes=[B_G])
    ev = 0
    Ud, UTd, BUd, BUTd = U, UT, B_U, B_UT
    for seg in range(4):
        seg_v = (xprev[seg * TOK:(seg + 1) * TOK, :] if seg < 3 else xloc[:, :]).rearrange("(c s) d -> s c d", s=16)
        def seg_front(s_):
            sl = s_ % 2
            S.dma("sp", lambda e, sl=sl, s_=s_, seg_v=seg_v: e.dma_start(out=xsf[sl][:, :], in_=seg_v[s_]), writes=[B_xsf[sl]])
            S.op("act", lambda e, sl=sl: e.copy(out=xs[sl][:, :], in_=xsf[sl][:, :]), reads=[B_xsf[sl]], writes=[B_xs[sl]])
            bank = 6 + sl
            S.op("pe", [lambda e, k=k, sl=sl, bank=bank: e.transpose(out=PS[bank][:, k * 128:(k + 1) * 128], in_=xs[sl][:, k * 128:(k + 1) * 128],
                                                                     identity=identb[:, :]) for k in range(KT)],
                 reads=[B_xs[sl], B_ident], writes=[PSB[bank]])
            eng = "dve" if s_ % 2 == 0 else "act"
            fn = (lambda e, sl=sl, bank=bank: e.tensor_copy(out=xsT[sl][:, :, :], in_=PS[bank][:, :].rearrange("p (k n) -> p k n", k=KT))) if eng == "dve" else \
                 (lambda e, sl=sl, bank=bank: e.copy(out=xsT[sl][:, :, :], in_=PS[bank][:, :].rearrange("p (k n) -> p k n", k=KT)))
            S.op(eng, fn, reads=[PSB[bank]], writes=[B_xsT[sl]])
        def seg_back(s_):
            sl = s_ % 2
            pb = s_ % 4
            S.op("pe", [lambda e, k=k, sl=sl, pb=pb: e.matmul(PS[pb][:, :], lhsT=xsT[sl][:, k, :], rhs=wu[:, k, :], start=(k == 0), stop=(k == KT - 1))
                        for k in range(KT)], reads=[B_xsT[sl], B_wu], writes=[PSB[pb]])
            S.op("dve", lambda e, s_=s_, pb=pb: e.tensor_tensor(out=Ud[:, :, s_, :], in0=PS[pb][:, :].rearrange("p (g h) -> p g h", g=32),
                                                                 in1=bubc[:, :].rearrange("p (g h) -> p g h", g=32), op=ALU.add),
                 reads=[PSB[pb], B_bu], writes=[BUd])
        for s_ in range(16):
            seg_front(s_)
            if s_ > 0:
                seg_back(s_ - 1)
        seg_back(15)
        for gq in range(8):
            bank = 6 + gq % 2
            S.op("pe", [lambda e, j=j, gq=gq, bank=bank, Ud=Ud: e.transpose(out=PS[bank][:, 128 * j:128 * j + 128],
                                                                          in_=Ud[:, 4 * gq + j // 2, 8 * (j % 2):8 * (j % 2) + 8, :].rearrange("p s h -> p (s h)"),
                                                                          identity=identb[:, :]) for j in range(8)],
                 reads=[BUd, B_ident], writes=[PSB[bank]])
            eng = "dve" if gq % 2 == 0 else "act"
            dst = UTd[:, 4 * gq:4 * gq + 4, :, :].rearrange("p g k c -> p (g k c)")
            fn = (lambda e, dst=dst, bank=bank: e.tensor_copy(out=dst, in_=PS[bank][:, :])) if eng == "dve" else (lambda e, dst=dst, bank=bank: e.copy(out=dst, in_=PS[bank][:, :]))
            S.op(eng, fn, reads=[PSB[bank]], writes=[BUTd])
        for gq in range(8):
            pb = gq % 4
            fns = []
            for j in range(4):
                g = 4 * gq + j
                for kh in range(2):
                    fns.append(lambda e, g=g, kh=kh, j=j, pb=pb, UTd=UTd: e.matmul(PS[pb][:, 128 * j:128 * j + 128], lhsT=LA[:, g, kh, :], rhs=UTd[:, g, kh, :],
                                                                                 start=(kh == 0), stop=(kh == 1)))
            S.op("pe", fns, reads=[BUTd, B_tb], writes=[PSB[pb]])
            pv = PS[pb][:, :].rearrange("p (j c) -> p j c", j=4)
            for q in range(2):
                for ri in range(2):
                    eng = "dve" if ev % 2 == 0 else "act"; ev += 1
                    dst = Z[64 * q:64 * q + 64, 2 * gq:2 * gq + 2, ri, :]
                    srcv = pv[64 * ri:64 * ri + 64, q:4:2, :]
                    fn = (lambda e, dst=dst, srcv=srcv: e.tensor_copy(out=dst, in_=srcv)) if eng == "dve" else (lambda e, dst=dst, srcv=srcv: e.copy(out=dst, in_=srcv))
                    S.op(eng, fn, reads=[PSB[pb], B_Z], writes=[B_Z])
        if stage == "s5z" and seg == 3:
            dz = dram_out("dbg_Z", [128, 16, 2, 128])
            S.dma("sp", lambda e: e.dma_start(out=dz.rearrange("p a b c -> p (a b c)"), in_=Z[:, :, :, :].rearrange("p a b c -> p (a b c)")), stream="out", reads=[B_Z], writes=[Buf("o")])
            S.finish(); close_phase(); close_phase(); es.close()
            return nc, ["dbg_Z"]
        Rr, Ri = R[:, 0, :, 1:129], R[:, 1, :, 1:129]
        zr_, zi_ = Z[:, :, 0, :], Z[:, :, 1, :]
        dz = dict(reads=[B_Z, B_tb, B_G, B_t4], writes=[B_G, B_t4])
        gi_r, gi_i = G2[:, :, 0, 1:129], G2[:, :, 1, 1:129]
        TT(gi_r, Rr, zr_, ALU.mult, **dz)
        TT(ztmp, Ri, zi_, ALU.mult, **dz)
        TT(gi_r, gi_r, ztmp, ALU.add, **dz)
        TT(gi_i, Rr, zi_, ALU.mult, **dz)
        TT(ztmp, Ri, zr_, ALU.mult, **dz)
        TT(gi_i, gi_i, ztmp, ALU.subtract, **dz)
        S.op("dve", [lambda e, gg=gg, ri=ri: e.tensor_tensor_scan(out=G2[:, gg, ri, 1:129], data0=rho_p[:, gg:gg + 1].to_broadcast([128, 128]),
                                                                    data1=G2[:, gg, ri, 1:129], initial=G2[:, gg, ri, 0:1], op0=ALU.mult, op1=ALU.add)
                     for gg in range(16) for ri in range(2)], reads=[B_G, B_tb], writes=[B_G])
        if seg < 3:
            c_ = lambda i: ctmp[:, i, :]
            TT(c_(0), R[:, 0, :, 128], G2[:, :, 0, 128], ALU.mult, **dz)
            TT(c_(1), R[:, 1, :, 128], G2[:, :, 1, 128], ALU.mult, **dz)
            TT(c_(0), c_(0), c_(1), ALU.subtract, **dz)
            TT(c_(2), R[:, 0, :, 128], G2[:, :, 1, 128], ALU.mult, **dz)
            TT(c_(3), R[:, 1, :, 128], G2[:, :, 0, 128], ALU.mult, **dz)
            TT(c_(2), c_(2), c_(3), ALU.add, **dz)
            S.op("dve", lambda e, seg=seg: e.tensor_scalar(out=G2[:, :, 0, 0], in0=c_(0), scalar1=segv[:, seg:seg + 1], scalar2=None, op0=ALU.mult), **dz)
            S.op("dve", lambda e, seg=seg: e.tensor_scalar(out=G2[:, :, 1, 0], in0=c_(2), scalar1=segv[:, seg:seg + 1], scalar2=None, op0=ALU.mult), **dz)
    Rr, Ri = R[:, 0, :, 0:128], R[:, 1, :, 0:128]
    gr, gi = G2[:, :, 0, 0:128], G2[:, :, 1, 0:128]
    dh = dict(reads=[B_G, B_tb, B_H], writes=[B_H])
    z0, z1 = Z[:, :, 0, :], Z[:, :, 1, :]
    dzz = dict(reads=[B_G, B_tb, B_Z, B_t4, B_H], writes=[B_Z, B_t4, B_H])
    TT(z0, Rr, gr, ALU.mult, **dzz)
    TT(ztmp, Ri, gi, ALU.mult, **dzz)
    for q in range(2):
        TT(Hst[0:64, q:32:2, :], z0[64 * q:64 * q + 64], ztmp[64 * q:64 * q + 64], ALU.subtract, **dzz)
    TT(z0, Rr, gi, ALU.mult, **dzz)
    TT(z1, Ri, gr, ALU.mult, **dzz)
    for q in range(2):
        TT(Hst[64:128, q:32:2, :], z0[64 * q:64 * q + 64], z1[64 * q:64 * q + 64], ALU.add, **dzz)
    if stage == "s5h":
        dhh = dram_out("dbg_H", [128, 32, 128])
        S.dma("pool", lambda e: e.dma_start(out=dhh.rearrange("p g c -> p (g c)"), in_=Hst[:, :, :].rearrange("p g c -> p (g c)")), stream="out", reads=[B_H], writes=[Buf("o")])
        S.finish(); close_phase(); close_phase(); es.close()
        return nc, ["dbg_H"]
    close_phase()

    open_phase()
    cmask = sb("s5cmask", [128, 2, 256], BF16)
    B_cm = Buf("s5cm")
    S.dma("pool", lambda e: e.dma_start(out=cmask[:, :, :], in_=cmask_d[:, :, :]), writes=[B_cm])
    RB = sb("s5RB", [128, 8, 256], BF16)
    Mj = sb("s5Mj", [128, 8, 2, 256], BF16)
    dbc = sb("s5dbc", [128, 512], F32)
    wglu = sb("s5wglu", [128, 4, 512], BF16)
    bgr = sb("s5bgr", [4, 128], F32); bgfm = sb("s5bgfm", [128, 4], F32)
    yT = sb("s5yT", [128, 4, TOK], BF16)
    ygs = sb("s5ygs", [128, 16, 128], BF16)
    yf = [sb(f"s5yf{i}", [128, 2, 16, 16], F32) for i in range(2)]
    yw = [sb(f"s5yw{i}", [128, 2, 16, 16], F32) for i in range(2)]
    B_d, B_wg, B_bg, B_yT, B_ygs = Buf("s5d"), Buf("s5wg"), Buf("s5bg"), [Buf(f"s5yT{j}") for j in range(4)], Buf("s5ygs")
    B_yf = [Buf(f"s5yf{i}") for i in range(2)]
    S.dma("sp", lambda e: e.dma_start(out=dbc[:, :], in_=d_d.partition_broadcast(128)), writes=[B_d])
    S.dma("pool", lambda e: e.dma_start(out=wglu[:, :, :], in_=wglu_d.rearrange("(kt p) f -> p kt f", p=128)), writes=[B_wg])
    S.dma("sp", lambda e: e.dma_start(out=bgr[:, :], in_=bglu_d.rearrange("(t p) -> t p", p=128)), writes=[B_bg])
    S.op("pe", lambda e: e.transpose(out=PS[5][:, 0:4], in_=bgr[:, :], identity=identf[0:4, 0:4]), reads=[B_bg, B_identf], writes=[PSB[5]])
    S.op("dve", lambda e: e.tensor_copy(out=bgfm[:, :], in_=PS[5][:, 0:4]), reads=[PSB[5]], writes=[B_bg])

    def build_Q(dst, g0, Ctab, k0):
        bc_s = lambda ri: A_[:, ri, g0:g0 + 8, k0:k0 + 16].unsqueeze(3).to_broadcast([64, 8, 16, 16])
        bc_h = lambda ri: Ctab[:, ri, g0:g0 + 8, :].unsqueeze(2).to_broadcast([64, 8, 16, 16])
        o4 = lambda lo: dst[lo:lo + 64, :, :].rearrange("p g (s h) -> p g s h", s=16)
        tt(t4a, bc_s(0), bc_h(0), ALU.mult); tt(t4b, bc_s(1), bc_h(1), ALU.mult)
        tt(o4(0), t4a, t4b, ALU.subtract)
        tt(t4a, bc_s(0), bc_h(1), ALU.mult); tt(t4b, bc_s(1), bc_h(0), ALU.mult)
        tt(t4a, t4a, t4b, ALU.add)
        ts(o4(64), t4a, -1.0, ALU.mult)

    for j in range(4):
        g0 = 8 * j
        build_PSt(g0); build_Q(RB, g0, CT, 1)
        for g8 in range(8):
            pb = 4 + g8 % 2
            S.op("pe", [lambda e, g8=g8, kh=kh, pb=pb: e.matmul(PS[pb][:, 256 * kh:256 * kh + 256], lhsT=PSt[:, g8, 128 * kh:128 * kh + 128], rhs=RB[:, g8, :],
                                                                start=True, stop=True) for kh in range(2)], reads=[B_tb], writes=[PSB[pb]])
            S.op("dve", lambda e, g8=g8, pb=pb: e.tensor_tensor(out=Mj[:, g8, :, :], in0=PS[pb][:, :].rearrange("p (k n) -> p k n", k=2), in1=cmask[:, :, :], op=ALU.mult),
                 reads=[PSB[pb], B_cm, B_tb], writes=[B_tb])
        for gp in range(4):
            pb = gp
            fns = []
            for q in range(2):
                g8 = 2 * gp + q; g = g0 + g8
                o = PS[pb][:, 256 * q:256 * q + 256]
                fns.append(lambda e, o=o, g=g, g8=g8: e.matmul(o, lhsT=Hst[:, g, :], rhs=RB[:, g8, :], start=True, stop=False))
                fns.append(lambda e, o=o, g=g, g8=g8: e.matmul(o, lhsT=UT[:, g, 0, :], rhs=Mj[:, g8, 0, :], start=False, stop=False))
                fns.append(lambda e, o=o, g=g, g8=g8: e.matmul(o, lhsT=UT[:, g, 1, :], rhs=Mj[:, g8, 1, :], start=False, stop=True))
            S.op("pe", fns, reads=[B_H, B_UT, B_tb], writes=[PSB[pb]])
            sl = gp % 2
            ga = g0 + 2 * gp
            pv4 = PS[pb][:, :].rearrange("p (g s h) -> p g s h", g=2, s=16)
            dv4 = dbc[:, 16 * ga:16 * ga + 32].rearrange("p (g h) -> p g h", g=2).unsqueeze(2).to_broadcast([128, 2, 16, 16])
            y_, w_ = yf[sl][:, :, :, :], yw[sl][:, :, :, :]
            S.op("dve", lambda e, y_=y_, ga=ga, dv4=dv4: e.tensor_tensor(out=y_, in0=U[:, ga:ga + 2, :, :], in1=dv4, op=ALU.mult), reads=[B_U, B_d, B_yf[sl]], writes=[B_yf[sl]])
            S.op("dve", lambda e, y_=y_, pv4=pv4: e.tensor_tensor(out=y_, in0=y_, in1=pv4, op=ALU.add), reads=[PSB[pb], B_yf[sl]], writes=[B_yf[sl]])
            if stage == "s5y" and j == 0 and gp == 0:
                dy = dram_out("dbg_y", [128, 2, 16, 16])
                S.dma("sp", lambda e: e.dma_start(out=dy.rearrange("p a b c -> p (a b c)"), in_=yf[0][:, :, :, :].rearrange("p a b c -> p (a b c)")), stream="out", reads=[B_yf[0]], writes=[Buf("o")])
                S.finish(); close_phase(); close_phase(); es.close()
                return nc, ["dbg_y"]
            S.op("dve", lambda e, y_=y_, w_=w_: e.tensor_tensor(out=w_, in0=y_, in1=y_, op=ALU.mult), reads=[B_yf[sl]], writes=[B_yf[sl]])
            S.op("dve", lambda e, w_=w_: e.tensor_scalar(out=w_, in0=w_, scalar1=0.044715, scalar2=1.0, op0=ALU.mult, op1=ALU.add), reads=[B_yf[sl]], writes=[B_yf[sl]])
            S.op("dve", lambda e, y_=y_, w_=w_: e.tensor_tensor(out=w_, in0=w_, in1=y_, op=ALU.mult), reads=[B_yf[sl]], writes=[B_yf[sl]])
            S.op("act", lambda e, w_=w_: e.activation(out=w_, in_=w_, func=AF.Sigmoid, scale=1.5957691216057308), reads=[B_yf[sl]], writes=[B_yf[sl]])
            dst = ygs[:, :, 32 * gp:32 * gp + 32].rearrange("p s (g h) -> p g s h", g=2)
            S.op("dve", lambda e, y_=y_, w_=w_, dst=dst: e.tensor_tensor(out=dst, in0=y_, in1=w_, op=ALU.mult), reads=[B_yf[sl], B_ygs], writes=[B_ygs])
        for hb in range(2):
            bank = 6 + hb
            S.op("pe", [lambda e, i=i, hb=hb, bank=bank: e.transpose(out=PS[bank][:, 128 * i:128 * i + 128], in_=ygs[:, 8 * hb + i, :], identity=identb[:, :]) for i in range(8)],
                 reads=[B_ygs, B_ident], writes=[PSB[bank]])
            dst = yT[:, j, :].rearrange("p (c s) -> p s c", s=16)[:, 8 * hb:8 * hb + 8, :]
            S.op("dve", lambda e, dst=dst, bank=bank: e.tensor_copy(out=dst, in_=PS[bank][:, :].rearrange("p (s c) -> p s c", s=8)), reads=[PSB[bank]], writes=[B_yT[j]])
    gt = [sb(f"s5gt{i}", [128, 512], BF16) for i in range(2)]
    B_gt = [Buf(f"s5gt{i}") for i in range(2)]
    ig = 0
    for co in range(4):
        for blk in range(4):
            pb = ig % 4; sl = ig % 2; ig += 1
            S.op("pe", [lambda e, k=k, co=co, blk=blk, pb=pb: e.matmul(PS[pb][:, :], lhsT=wglu[:, k, 128 * co:128 * co + 128], rhs=yT[:, k, 512 * blk:512 * blk + 512],
                                                                       start=(k == 0), stop=(k == 3)) for k in range(4)], reads=B_yT + [B_wg], writes=[PSB[pb]])
            S.op("act", lambda e, co=co, pb=pb, sl=sl: e.activation(out=gt[sl][:, :], in_=PS[pb][:, :], func=AF.Sigmoid, bias=bgfm[:, co:co + 1]), reads=[PSB[pb], B_bg], writes=[B_gt[sl]])
            S.op("dve", lambda e, co=co, blk=blk, sl=sl: e.tensor_tensor(out=catT[:, 4 + co, 512 * blk:512 * blk + 512], in0=yT[:, co, 512 * blk:512 * blk + 512], in1=gt[sl][:, :], op=ALU.mult),
                 reads=[B_gt[sl], B_yT[co]], writes=[B_cat[4 * blk + i] for i in range(4)])
    if stage == "s5":
        dss = dram_out("dbg_ssm", [512, TOK])
        S.dma("pool", lambda e: e.dma_start(out=dss.rearrange("(c p) t -> p c t", p=128), in_=catT[:, 4:8, :]), stream="out", reads=B_cat, writes=[Buf("o")])
        S.finish(); close_phase(); close_phase(); es.close()
        return nc, ["dbg_ssm"]
    close_phase()
    close_phase()
    ALPHA = 2.0 ** 0.25
    CAP = 384
    xloc = nc_ap(nc, "xloc")
    mem_d = dram_in("mem", [256, D])
    wout_d = dram_in("w_out", [D, D]); bout_d = dram_in("b_out", [D])
    ln_d = {k: dram_in(k, [D]) for k in ("ln1_g", "ln1_b", "ln2_g", "ln2_b", "ln3_g", "ln3_b")}
    wxq_d = dram_in("w_xq", [D, D]); wxkv_d = dram_in("w_xkv", [D, 2 * D]); wxo_d = dram_in("w_xo", [D, D])
    wr_d = dram_in("w_router", [D, 32]); br_d = dram_in("b_router", [32])
    tri_d = dram_in("tri", [128, 128]); ecap_d = dram_in("ecap", [128, 32])
    x2f = nc.dram_tensor("x2f", [TOK, D], F32, kind="Internal").ap()
    xg = nc.dram_tensor("xg", [32 * CAP, D], BF16, kind="Internal").ap()
    yg = nc.dram_tensor("yg", [32 * CAP, D], F32, kind="Internal").ap()

    gates = sb("gates", [128, NT, 4], F32)
    dest = sb("dest", [128, NT, 4], I32)
    B_route = [Buf(f"route{t}") for t in range(NT)]
    B_xg, B_x2f = Buf("xg"), [Buf(f"x2f{t}") for t in range(NT)]

    open_phase()
    onesb = sb("onesb", [128, 128], BF16)
    trib = sb("tri_sb", [128, 128], BF16)
    ecap = sb("ecap_sb", [128, 32], F32)
    B_c3 = Buf("c3")
    S.op("pool", lambda e: e.memset(onesb[:, :], 1.0), writes=[B_c3])
    B_tri, B_ecap = Buf("tri"), Buf("ecap")
    S.dma("pool", lambda e: e.dma_start(out=trib[:, :], in_=tri_d[:, :]), writes=[B_tri])
    S.dma("sp", lambda e: e.dma_start(out=ecap[:, :], in_=ecap_d[:, :]), writes=[B_ecap])
    bc = {}
    B_bc = {}
    for k, src in [("b_out", bout_d)] + [(k, ln_d[k]) for k in ("ln1_g", "ln1_b", "ln2_g", "ln2_b")]:
        bc[k] = sb("bc_" + k, [128, D], F32)
        B_bc[k] = Buf("bc_" + k)
        S.dma("sp", lambda e, k=k, src=src: e.dma_start(out=bc[k][:, :], in_=src.partition_broadcast(128)), writes=[B_bc[k]])
    brbc = sb("brbc", [128, 32], F32); wr = sb("wr", [128, KT, 32], F32)
    B_br, B_wr = Buf("brbc"), Buf("wr")
    S.dma("sp", lambda e: e.dma_start(out=brbc[:, :], in_=br_d.partition_broadcast(128)), writes=[B_br])
    S.dma("sp", lambda e: e.dma_start(out=wr[:, :, :], in_=wr_d.rearrange("(kt p) f -> p kt f", p=128)), writes=[B_wr])
    wout = sb("wout", [128, KT, D], BF16); wxq = sb("wxq", [128, KT, D], BF16); wxo = sb("wxo", [128, KT, D], BF16)
    B_wout, B_wxq, B_wxo = Buf("wout"), Buf("wxq"), Buf("wxo")
    wst3 = [sb(f"wst3_{i}", [128, D], F32) for i in range(4)]
    B_wst3 = [Buf(f"wst3_{i}") for i in range(4)]
    w3step = [0]

    def load_w3(dst, src, ncol, bf):
        for k in range(KT):
            for c0 in range(0, ncol, D):
                i = w3step[0] % 4; w3step[0] += 1
                S.dma("sp", lambda e, i=i, k=k, c0=c0, src=src: e.dma_start(out=wst3[i][:, :], in_=src[k * 128:(k + 1) * 128, c0:c0 + D]), writes=[B_wst3[i]])
                S.op("act", lambda e, i=i, k=k, c0=c0, dst=dst: e.copy(out=dst[:, k, c0:c0 + D], in_=wst3[i][:, :]), reads=[B_wst3[i], bf], writes=[bf])
    kmemT = sb("kmemT", [128, KT, 256], BF16)
    vmem = sb("vmem", [128, 2, D], BF16)
    B_kv = Buf("kv")
    open_phase()
    wxkv = sb("wxkv", [128, KT, 2 * D], BF16)
    B_wxkv = Buf("wxkv")
    load_w3(wxkv, wxkv_d, 2 * D, B_wxkv)
    load_w3(wout, wout_d, D, B_wout); load_w3(wxq, wxq_d, D, B_wxq); load_w3(wxo, wxo_d, D, B_wxo)
    memb = sb("memb", [128, 2, D], BF16)
    memT = sb("memT", [128, KT, 256], BF16)
    B_memb, B_memT = Buf("memb"), Buf("memT")
    S.dma("pool", lambda e: e.dma_start(out=memb[:, :, :], in_=mem_d.rearrange("(t p) d -> p t d", p=128)), writes=[B_memb])
    for mt in range(2):
        bank = 6 + mt
        S.op("pe", [lambda e, k=k, mt=mt, bank=bank: e.transpose(out=PS[bank][:, k * 128:(k + 1) * 128], in_=memb[:, mt, k * 128:(k + 1) * 128], identity=identb[:, :])
                    for k in range(KT)], reads=[B_memb, B_ident], writes=[PSB[bank]])
        S.op("dve", lambda e, mt=mt, bank=bank: e.tensor_copy(out=memT[:, :, mt * 128:(mt + 1) * 128], in_=PS[bank][:, :].rearrange("p (k n) -> p k n", k=KT)),
             reads=[PSB[bank]], writes=[B_memT])
    for ct in range(KT):
        pb = ct % 4
        S.op("pe", [lambda e, k=k, ct=ct, pb=pb: e.matmul(PS[pb][:, 0:256], lhsT=wxkv[:, k, ct * 128:(ct + 1) * 128], rhs=memT[:, k, :], start=(k == 0), stop=(k == KT - 1))
                    for k in range(KT)], reads=[B_wxkv, B_memT], writes=[PSB[pb]])
        S.op("dve", lambda e, ct=ct, pb=pb: e.tensor_copy(out=kmemT[:, ct, :], in_=PS[pb][:, 0:256]), reads=[PSB[pb]], writes=[B_kv])
    for mt in range(2):
        for hf in range(2):
            pb = 2 * mt + hf
            S.op("pe", [lambda e, k=k, mt=mt, hf=hf, pb=pb: e.matmul(PS[pb][:, :], lhsT=memT[:, k, mt * 128:(mt + 1) * 128], rhs=wxkv[:, k, D + 512 * hf:D + 512 * hf + 512],
                                                                    start=(k == 0), stop=(k == KT - 1)) for k in range(KT)], reads=[B_wxkv, B_memT], writes=[PSB[pb]])
            S.op("act", lambda e, mt=mt, hf=hf, pb=pb: e.copy(out=vmem[:, mt, 512 * hf:512 * hf + 512], in_=PS[pb][:, :]), reads=[PSB[pb]], writes=[B_kv])
    close_phase()

    x1 = sb("x1", [128, 4, D], F32)
    x1T = sb("x1T", [128, KT, 512], BF16)
    qxT = sb("qxT", [128, KT, 512], BF16)
    oT = sb("oT", [128, KT, 512], BF16)
    pX = [sb(f"pX{i}", [128, 2, 512], BF16) for i in range(2)]
    rl = sb("rl", [128, 512], F32)
    xin = [sb(f"xin{i}", [128, D], F32) for i in range(2)]
    hh = [sb(f"hh{i}", [128, D], F32) for i in range(2)]
    hb = [sb(f"hb{i}", [128, D], BF16) for i in range(2)]
    x2t = [sb(f"x2t{i}", [128, D], F32) for i in range(2)]
    x2T = sb("x2T", [128, KT, 128], F32)
    st6_ = [sb(f"st6_{i}", [128, 2, 6], F32) for i in range(2)]; mv_ = [sb(f"mv_{i}", [128, 2], F32) for i in range(2)]; rstd_ = [sb(f"rstd_{i}", [128, 1], F32) for i in range(2)]
    B_ln = [Buf(f"lnscr{i}") for i in range(2)]; ln_i = [0]
    lg = sb("lg", [128, 32], F32); v8 = sb("v8", [128, 8], F32); e4 = sb("e4", [128, 4], F32); ssum = sb("ssum", [128, 1], F32)
    nv1 = sb("nv1", [128, 1], F32)
    oh = sb("oh", [128, 4, 32], F32); maskall = sb("maskall", [128, NT, 32], BF16); slt = sb("slt", [128, 32], F32)
    destf = sb("destf", [128, 4], F32)
    B_x1 = [Buf(f"x1_{i}") for i in range(4)]; B_x1T, B_qxT, B_oT = Buf("x1T"), Buf("qxT"), Buf("oT")
    B_pX = [Buf(f"pX{i}") for i in range(2)]; B_rl = Buf("rl")
    B_xin = [Buf(f"xin{i}") for i in range(2)]; B_hh = [Buf(f"hh{i}") for i in range(2)]; B_hb = [Buf(f"hb{i}") for i in range(2)]
    B_x2t = [Buf(f"x2t{i}") for i in range(2)]; B_x2T = Buf("x2T"); B_sm = Buf("sm3"); B_mask = Buf("maskall")

    def layer_norm(src, dst, gk, bk, bsrc, bdst):
        i = ln_i[0] % 2; ln_i[0] += 1
        st6, mv, rstd, bl = st6_[i], mv_[i], rstd_[i], B_ln[i]
        S.op("dve", [lambda e, c=c: e.bn_stats(out=st6[:, c, :], in_=src[:, 512 * c:512 * c + 512]) for c in range(2)], reads=[bsrc, bl], writes=[bl])
        S.op("dve", lambda e: e.bn_aggr(out=mv[:, :], in_=st6[:, :, :].rearrange("p a b -> p (a b)")), reads=[bl], writes=[bl])
        S.op("dve", lambda e: e.tensor_scalar(out=rstd[:, :], in0=mv[:, 1:2], scalar1=1e-5, scalar2=None, op0=ALU.add), reads=[bl], writes=[bl])
        S.op("act", lambda e: e.activation(out=rstd[:, :], in_=rstd[:, :], func=AF.Sqrt), reads=[bl], writes=[bl])
        S.op("dve", lambda e: e.reciprocal(out=rstd[:, :], in_=rstd[:, :]), reads=[bl], writes=[bl])
        S.op("dve", lambda e: e.tensor_scalar(out=src, in0=src, scalar1=mv[:, 0:1], scalar2=rstd[:, 0:1], op0=ALU.subtract, op1=ALU.mult), reads=[bsrc, bl], writes=[bsrc])
        S.op("dve", lambda e: e.tensor_tensor(out=src, in0=src, in1=bc[gk][:, :], op=ALU.mult), reads=[bsrc, B_bc[gk]], writes=[bsrc])
        S.op("dve", lambda e: e.tensor_tensor(out=dst, in0=src, in1=bc[bk][:, :], op=ALU.add), reads=[bsrc, B_bc[bk]], writes=[bdst])

    for st in range(4):
        def A1(ti):
            t = 4 * st + ti; sl = t % 2
            S.dma("sp", lambda e, t=t, sl=sl: e.dma_start(out=xin[sl][:, :], in_=xloc[t * 128:(t + 1) * 128, :]), writes=[B_xin[sl]])
            for hf in range(2):
                pb = 2 * sl + hf
                S.op("pe", [lambda e, k=k, t=t, hf=hf, pb=pb: e.matmul(PS[pb][:, :], lhsT=catT[:, k, t * 128:(t + 1) * 128], rhs=wout[:, k, 512 * hf:512 * hf + 512],
                                                                      start=(k == 0), stop=(k == KT - 1)) for k in range(KT)], reads=[B_cat[t], B_wout], writes=[PSB[pb]])
                S.op("dve", lambda e, sl=sl, hf=hf, pb=pb: e.scalar_tensor_tensor(out=hh[sl][:, 512 * hf:512 * hf + 512], in0=xin[sl][:, 512 * hf:512 * hf + 512], scalar=ALPHA,
                                                                                  in1=PS[pb][:, :], op0=ALU.mult, op1=ALU.add), reads=[PSB[pb], B_xin[sl], B_hh[sl]], writes=[B_hh[sl]])
            S.op("dve", lambda e, sl=sl: e.tensor_tensor(out=hh[sl][:, :], in0=hh[sl][:, :], in1=bc["b_out"][:, :], op=ALU.add), reads=[B_hh[sl], B_bc["b_out"]], writes=[B_hh[sl]])
            layer_norm(hh[sl][:, :], x1[:, ti, :], "ln1_g", "ln1_b", B_hh[sl], B_x1[ti])
            S.op("act", lambda e, ti=ti, sl=sl: e.copy(out=hb[sl][:, :], in_=x1[:, ti, :]), reads=[B_x1[ti]], writes=[B_hb[sl]])
        def B1(ti):
            t = 4 * st + ti; sl = t % 2
            bank = 6 + sl
            S.op("pe", [lambda e, k=k, sl=sl, bank=bank: e.transpose(out=PS[bank][:, k * 128:(k + 1) * 128], in_=hb[sl][:, k * 128:(k + 1) * 128], identity=identb[:, :])
                        for k in range(KT)], reads=[B_hb[sl], B_ident], writes=[PSB[bank]])
            S.op("dve", lambda e, ti=ti, bank=bank: e.tensor_copy(out=x1T[:, :, ti * 128:(ti + 1) * 128], in_=PS[bank][:, :].rearrange("p (k n) -> p k n", k=KT)),
                 reads=[PSB[bank]], writes=[B_x1T])
        for ti in range(4):
            A1(ti)
            if ti > 0:
                B1(ti - 1)
        B1(3)
        if stage == "x1" and st == 0:
            dx1 = dram_out("dbg_x1", [512, D])
            S.dma("sp", lambda e: e.dma_start(out=dx1.rearrange("(t p) d -> p t d", p=128), in_=x1[:, :, :]), stream="out", reads=B_x1, writes=[Buf("o")])
            S.finish(); close_phase(); es.close()
            return nc, ["dbg_x1"]
        for ct in range(KT):
            pb = ct % 4
            S.op("pe", [lambda e, k=k, ct=ct, pb=pb: e.matmul(PS[pb][:, :], lhsT=wxq[:, k, ct * 128:(ct + 1) * 128], rhs=x1T[:, k, :], start=(k == 0), stop=(k == KT - 1))
                        for k in range(KT)], reads=[B_x1T, B_wxq], writes=[PSB[pb]])
            eng = "act" if ct % 2 else "dve"
            fn = (lambda e, ct=ct, pb=pb: e.copy(out=qxT[:, ct, :], in_=PS[pb][:, :])) if eng == "act" else (lambda e, ct=ct, pb=pb: e.tensor_copy(out=qxT[:, ct, :], in_=PS[pb][:, :]))
            S.op(eng, fn, reads=[PSB[pb]], writes=[B_qxT])
        for h in range(4):
            sl = h % 2
            for mt in range(2):
                pb = mt
                S.op("pe", [lambda e, cc=cc, h=h, mt=mt, pb=pb: e.matmul(PS[pb][:, :], lhsT=kmemT[:, 2 * h + cc, mt * 128:(mt + 1) * 128], rhs=qxT[:, 2 * h + cc, :],
                                                                        start=(cc == 0), stop=(cc == 1)) for cc in range(2)], reads=[B_kv, B_qxT], writes=[PSB[pb]])
                S.op("act", lambda e, sl=sl, mt=mt, pb=pb: e.activation(out=pX[sl][:, mt, :], in_=PS[pb][:, :], func=AF.Exp, scale=0.0625), reads=[PSB[pb]], writes=[B_pX[sl]])
            S.op("pe", [lambda e, mt=mt, sl=sl: e.matmul(PS[4][:, :], lhsT=onesb[:, :], rhs=pX[sl][:, mt, :], start=(mt == 0), stop=(mt == 1)) for mt in range(2)],
                 reads=[B_pX[sl], B_c3], writes=[PSB[4]])
            S.op("dve", lambda e: e.tensor_copy(out=rl[:, :], in_=PS[4][:, :]), reads=[PSB[4], B_rl], writes=[B_rl])
            S.op("dve", lambda e: e.reciprocal(out=rl[:, :], in_=rl[:, :]), reads=[B_rl], writes=[B_rl])
            for cc in range(2):
                pb = 2 + cc
                S.op("pe", [lambda e, mt=mt, cc=cc, h=h, sl=sl, pb=pb: e.matmul(PS[pb][:, :], lhsT=vmem[:, mt, (2 * h + cc) * 128:(2 * h + cc + 1) * 128], rhs=pX[sl][:, mt, :],
                                                                              start=(mt == 0), stop=(mt == 1)) for mt in range(2)], reads=[B_pX[sl], B_kv], writes=[PSB[pb]])
                S.op("dve", lambda e, cc=cc, h=h, pb=pb: e.tensor_tensor(out=oT[:, 2 * h + cc, :], in0=PS[pb][:, :], in1=rl[:, :], op=ALU.mult), reads=[PSB[pb], B_rl], writes=[B_oT])
        def A2(ti):
            t = 4 * st + ti; sl = t % 2
            for hf in range(2):
                pb = 2 * sl + hf
                S.op("pe", [lambda e, k=k, ti=ti, hf=hf, pb=pb: e.matmul(PS[pb][:, :], lhsT=oT[:, k, ti * 128:(ti + 1) * 128], rhs=wxo[:, k, 512 * hf:512 * hf + 512],
                                                                       start=(k == 0), stop=(k == KT - 1)) for k in range(KT)], reads=[B_oT, B_wxo], writes=[PSB[pb]])
                S.op("dve", lambda e, ti=ti, sl=sl, hf=hf, pb=pb: e.scalar_tensor_tensor(out=hh[sl][:, 512 * hf:512 * hf + 512], in0=x1[:, ti, 512 * hf:512 * hf + 512], scalar=ALPHA,
                                                                                         in1=PS[pb][:, :], op0=ALU.mult, op1=ALU.add), reads=[PSB[pb], B_x1[ti], B_hh[sl]], writes=[B_hh[sl]])
            layer_norm(hh[sl][:, :], x2t[sl][:, :], "ln2_g", "ln2_b", B_hh[sl], B_x2t[sl])
            if stage == "x2a":
                dxa = dram_out("dbg_x2a", [128, D])
                S.dma("sp", lambda e, sl=sl: e.dma_start(out=dxa[:, :], in_=x2t[sl][:, :]), stream="out", reads=[B_x2t[sl]], writes=[Buf("o")])
                S.finish(); close_phase(); es.close()
                return nc, ["dbg_x2a"]
            S.dma("sp", lambda e, t=t, sl=sl: e.dma_start(out=x2f[t * 128:(t + 1) * 128, :], in_=x2t[sl][:, :]), stream=f"x2f{sl}", reads=[B_x2t[sl]], writes=[B_x2f[t]])
            S.op("act", lambda e, sl=sl: e.copy(out=hb[sl][:, :], in_=x2t[sl][:, :]), reads=[B_x2t[sl]], writes=[B_hb[sl]])
        def B2(ti):
            t = 4 * st + ti; sl = t % 2
            for hf in range(2):
                S.op("pe", [lambda e, k=k, sl=sl, hf=hf: e.transpose(out=PS[4 + hf][:, k * 128:(k + 1) * 128], in_=x2t[sl][:, (4 * hf + k) * 128:(4 * hf + k + 1) * 128], identity=identf[:, :])
                            for k in range(4)], reads=[B_x2t[sl], B_identf], writes=[PSB[4 + hf]])
                eng = "act" if hf else "dve"
                fn = (lambda e, hf=hf: e.copy(out=x2T[:, 4 * hf:4 * hf + 4, :], in_=PS[4 + hf][:, :].rearrange("p (k n) -> p k n", k=4))) if eng == "act" else \
                     (lambda e, hf=hf: e.tensor_copy(out=x2T[:, 4 * hf:4 * hf + 4, :], in_=PS[4 + hf][:, :].rearrange("p (k n) -> p k n", k=4)))
                S.op(eng, fn, reads=[PSB[4 + hf]], writes=[B_x2T])
            S.op("pe", [lambda e, k=k: e.matmul(PS[4][:, 0:32], lhsT=x2T[:, k, :], rhs=wr[:, k, :], start=(k == 0), stop=(k == KT - 1)) for k in range(KT)],
                 reads=[B_x2T, B_wr], writes=[PSB[4]])
            dsm = dict(reads=[B_sm], writes=[B_sm])
            S.op("dve", lambda e: e.tensor_tensor(out=lg[:, :], in0=PS[4][:, 0:32], in1=brbc[:, :], op=ALU.add), reads=[PSB[4], B_br, B_sm], writes=[B_sm])
            S.op("dve", lambda e: e.max(out=v8[:, :], in_=lg[:, :]), **dsm)
            S.op("dve", lambda e: e.tensor_scalar(out=nv1[:, :], in0=v8[:, 0:1], scalar1=-1.0, scalar2=None, op0=ALU.mult), **dsm)
            S.op("act", lambda e: e.activation(out=e4[:, :], in_=v8[:, 0:4], func=AF.Exp, bias=nv1[:, 0:1]), **dsm)
            S.op("dve", lambda e: e.reduce_sum(out=ssum[:, :], in_=e4[:, :], axis=AX.X), **dsm)
            S.op("dve", lambda e: e.reciprocal(out=ssum[:, :], in_=ssum[:, :]), **dsm)
            S.op("dve", lambda e, t=t: e.tensor_scalar(out=gates[:, t, :], in0=e4[:, :], scalar1=ssum[:, 0:1], scalar2=None, op0=ALU.mult), reads=[B_sm], writes=[B_sm, B_route[t]])
            for k in range(4):
                S.op("dve", lambda e, k=k: e.tensor_scalar(out=oh[:, k, :], in0=lg[:, :], scalar1=v8[:, k:k + 1], scalar2=None, op0=ALU.is_equal), **dsm)
            S.op("dve", lambda e: e.tensor_tensor(out=slt[:, :], in0=oh[:, 0, :], in1=oh[:, 1, :], op=ALU.add), **dsm)
            S.op("dve", lambda e: e.tensor_tensor(out=slt[:, :], in0=slt[:, :], in1=oh[:, 2, :], op=ALU.add), **dsm)
            S.op("dve", lambda e, t=t: e.tensor_tensor(out=maskall[:, t, :], in0=slt[:, :], in1=oh[:, 3, :], op=ALU.add), reads=[B_sm, B_mask], writes=[B_mask])
            S.op("pe", [lambda e, t=t: e.matmul(PS[5][:, 0:32], lhsT=trib[:, :], rhs=maskall[:, t, :], start=True, stop=(t == 0))] +
                       [lambda e, t=t, tp=tp: e.matmul(PS[5][:, 0:32], lhsT=onesb[:, :], rhs=maskall[:, tp, :], start=False, stop=(tp == t - 1)) for tp in range(t)],
                 reads=[B_mask, B_tri, B_c3], writes=[PSB[5]])
            S.op("dve", lambda e: e.tensor_scalar(out=slt[:, :], in0=PS[5][:, 0:32], scalar1=float(CAP - 1), scalar2=None, op0=ALU.min), reads=[PSB[5], B_sm], writes=[B_sm])
            S.op("dve", lambda e: e.tensor_tensor(out=slt[:, :], in0=slt[:, :], in1=ecap[:, :], op=ALU.add), reads=[B_ecap, B_sm], writes=[B_sm])
            S.op("dve", lambda e: e.tensor_tensor(out=oh[:, :, :], in0=oh[:, :, :], in1=slt[:, :].unsqueeze(1).to_broadcast([128, 4, 32]), op=ALU.mult), **dsm)
            S.op("dve", lambda e: e.reduce_sum(out=destf[:, :], in_=oh[:, :, :], axis=AX.X), **dsm)
            S.op("dve", lambda e, t=t: e.tensor_copy(out=dest[:, t, :], in_=destf[:, :]), reads=[B_sm], writes=[B_sm, B_route[t]])
            for k in range(4):
                S.dma("pool", lambda e, t=t, k=k, sl=sl: e.indirect_dma_start(out=xg[:, :], out_offset=bass.IndirectOffsetOnAxis(dest[:, t, k:k + 1], 0),
                                                                              in_=hb[sl][:, :], in_offset=None), stream=f"xgsc{sl}", reads=[B_hb[sl], B_route[t]], writes=[B_xg])
        for ti in range(4):
            A2(ti)
            if ti > 0:
                B2(ti - 1)
        B2(3)
    if stage == "x2":
        dx2 = dram_out("dbg_x2", [TOK, D]); dg = dram_out("dbg_gates", [128, NT, 4]); dd = dram_out("dbg_dest", [128, NT, 4], I32)
        S.dma("sp", lambda e: e.dma_start(out=dx2[:, :], in_=x2f[:, :]), stream="out", reads=B_x2f, writes=[Buf("o")])
        S.dma("sp", lambda e: e.dma_start(out=dg.rearrange("p a b -> p (a b)"), in_=gates[:, :, :].rearrange("p a b -> p (a b)")), stream="out", reads=B_route, writes=[Buf("o1")])
        S.dma("sp", lambda e: e.dma_start(out=dd.rearrange("p a b -> p (a b)"), in_=dest[:, :, :].rearrange("p a b -> p (a b)")), stream="out", reads=B_route, writes=[Buf("o2")])
        dxg = dram_out("dbg_xg", [32 * CAP, D])
        S.dma("pool", lambda e: e.dma_start(out=dxg[:, :], in_=xg[:, :]), stream="outsw", reads=[B_xg], writes=[Buf("o3")])
        S.finish(); close_phase(); es.close()
        return nc, ["dbg_x2", "dbg_gates", "dbg_dest", "dbg_xg"]
    close_phase()
    we1_d = dram_in("w_e1", [32, D, 2 * D]); be1_d = dram_in("b_e1", [32, 2 * D])
    we2_d = dram_in("w_e2", [32, D, D]); be2_d = dram_in("b_e2", [32, D])
    n_exp = 32 if stage not in ("moe2",) else 2
    B_yg = Buf("yg")
    open_phase()
    b1all = sb("b1all", [32, 2 * D], F32)
    b1fm = sb("b1fm", [128, 8, 2, 32], F32)
    B_b1a, B_b1 = Buf("b1all"), Buf("b1fm")
    S.dma("sp", lambda e: e.dma_start(out=b1all[:, :], in_=be1_d[:, :]), writes=[B_b1a])
    S.op("pe", [lambda e, ft=ft, two=two: e.transpose(out=PS[5][:, 32 * (2 * ft + two):32 * (2 * ft + two) + 32], in_=b1all[:, 256 * ft + two:256 * (ft + 1):2],
                                                     identity=identf[0:32, 0:32]) for ft in range(8) for two in range(2)], reads=[B_b1a, B_identf], writes=[PSB[5]])
    S.op("dve", lambda e: e.tensor_copy(out=b1fm[:, :, :, :].rearrange("p a b c -> p (a b c)"), in_=PS[5][:, :]), reads=[PSB[5]], writes=[B_b1])
    w1 = [sb(f"w1_{i}", [128, KT, 2 * D], BF16) for i in range(2)]
    w2 = [sb(f"w2_{i}", [128, KT, D], BF16) for i in range(2)]
    b2bc = [sb(f"b2bc{i}", [128, D], F32) for i in range(2)]
    B_w1 = [Buf(f"w1_{i}") for i in range(2)]; B_w2 = [Buf(f"w2_{i}") for i in range(2)]; B_b2 = [Buf(f"b2bc{i}") for i in range(2)]
    NS = CAP // 128
    xgl = sb("xgl", [128, NS, D], BF16)
    xgT2 = [sb("xgT", [128, KT, CAP], BF16)] * 2; actT2 = [sb("actT", [128, 8, CAP], BF16)] * 2
    B_xgl = Buf("xgl"); B_xgT2 = [Buf("xgT")] * 2; B_actT2 = [Buf("actT")] * 2
    NWST = 6
    wst = [sb(f"wst{i}", [128, D], F32) for i in range(NWST)]
    B_wst = [Buf(f"wst{i}") for i in range(NWST)]
    gg_ = [sb(f"sg_g{i}", [128, CAP], F32) for i in range(2)]; ll_ = [sb(f"sg_l{i}", [128, CAP], F32) for i in range(2)]; ss_ = [sb(f"sg_s{i}", [128, CAP], F32) for i in range(2)]
    B_sg = [Buf(f"sg{i}") for i in range(2)]
    ysb = [sb(f"ysb{i}", [128, D], F32) for i in range(2)]
    B_ysb = [Buf(f"ysb{i}") for i in range(2)]

    wstep = [0]

    def step_issue(e_, j):
        i = wstep[0] % NWST; wstep[0] += 1
        if j < 16:
            k, hf = j // 2, j % 2
            S.dma("sp", lambda e, i=i, e_=e_, k=k, hf=hf: e.dma_start(out=wst[i][:, :], in_=we1_d[e_][k * 128:(k + 1) * 128, hf * D:(hf + 1) * D]), writes=[B_wst[i]])
        else:
            k = j - 16
            S.dma("sp", lambda e, i=i, e_=e_, k=k: e.dma_start(out=wst[i][:, :], in_=we2_d[e_][k * 128:(k + 1) * 128, :]), writes=[B_wst[i]])
        return i

    def step_cast(e_, j, i):
        sl = e_ % 2
        if j < 16:
            k, hf = j // 2, j % 2
            S.op("act", lambda e, i=i, sl=sl, k=k, hf=hf: e.copy(out=w1[sl][:, k, hf * D:(hf + 1) * D], in_=wst[i][:, :]), reads=[B_wst[i], B_w1[sl]], writes=[B_w1[sl]])
        else:
            k = j - 16
            S.op("act", lambda e, i=i, sl=sl, k=k: e.copy(out=w2[sl][:, k, :], in_=wst[i][:, :]), reads=[B_wst[i], B_w2[sl]], writes=[B_w2[sl]])

    def load_step(e_, j):
        step_cast(e_, j, step_issue(e_, j))

    def load_b2(e_):
        sl = e_ % 2
        S.dma("pool", lambda e, sl=sl, e_=e_: e.dma_start(out=b2bc[sl][:, :], in_=be2_d[e_].partition_broadcast(128)), writes=[B_b2[sl]])

    def load_expert(e_):
        for j in range(24):
            load_step(e_, j)
        load_b2(e_)

    load_expert(0)
    iy = 0
    for e_ in range(n_exp):
        sl = e_ % 2
        nxt = e_ + 1 if e_ + 1 < n_exp else None
        if nxt is not None:
            load_b2(nxt)
        xgT, actT, B_xgT, B_actT = xgT2[sl], actT2[sl], B_xgT2[sl], B_actT2[sl]
        S.dma("sp", lambda e, e_=e_: e.dma_start(out=xgl[:, :, :], in_=xg[e_ * CAP:(e_ + 1) * CAP, :].rearrange("(s p) d -> p s d", p=128)), reads=[B_xg], writes=[B_xgl])
        for st_ in range(NS):
            bank = 6 + st_ % 2
            S.op("pe", [lambda e, k=k, st_=st_, bank=bank: e.transpose(out=PS[bank][:, k * 128:(k + 1) * 128], in_=xgl[:, st_, k * 128:(k + 1) * 128], identity=identb[:, :])
                        for k in range(KT)], reads=[B_xgl, B_ident], writes=[PSB[bank]])
            eng = "act" if st_ % 2 else "dve"
            fn = (lambda e, st_=st_, bank=bank, xgT=xgT: e.copy(out=xgT[:, :, st_ * 128:(st_ + 1) * 128], in_=PS[bank][:, :].rearrange("p (k n) -> p k n", k=KT))) if eng == "act" else \
                 (lambda e, st_=st_, bank=bank, xgT=xgT: e.tensor_copy(out=xgT[:, :, st_ * 128:(st_ + 1) * 128], in_=PS[bank][:, :].rearrange("p (k n) -> p k n", k=KT)))
            S.op(eng, fn, reads=[PSB[bank]], writes=[B_xgT])
        pend = []
        for ft in range(8):
            if nxt is not None:
                for (jj, ii) in pend:
                    step_cast(nxt, jj, ii)
                pend = [(j, step_issue(nxt, j)) for j in range(3 * ft, 3 * ft + 3)]
            s2 = ft % 2
            pg, pl = 2 * s2, 2 * s2 + 1
            for two, pb in ((0, pg), (1, pl)):
                S.op("pe", [lambda e, k=k, ft=ft, two=two, pb=pb, sl=sl, xgT=xgT: e.matmul(PS[pb][:, 0:CAP], lhsT=w1[sl][:, k, 256 * ft + two:256 * (ft + 1):2], rhs=xgT[:, k, :],
                                                                                 start=(k == 0), stop=(k == KT - 1)) for k in range(KT)], reads=[B_w1[sl], B_xgT], writes=[PSB[pb]])
            g_, l_, s_ = gg_[s2][:, :], ll_[s2][:, :], ss_[s2][:, :]
            S.op("dve", lambda e, g_=g_, pg=pg, ft=ft, e_=e_: e.tensor_scalar(out=g_, in0=PS[pg][:, 0:CAP], scalar1=b1fm[:, ft, 0, e_:e_ + 1], scalar2=7.0, op0=ALU.add, op1=ALU.min),
                 reads=[PSB[pg], B_b1, B_sg[s2]], writes=[B_sg[s2]])
            S.op("act", lambda e, g_=g_, s_=s_: e.activation(out=s_, in_=g_, func=AF.Sigmoid, scale=1.702), reads=[B_sg[s2]], writes=[B_sg[s2]])
            S.op("dve", lambda e, l_=l_, pl=pl, ft=ft, e_=e_: e.tensor_scalar(out=l_, in0=PS[pl][:, 0:CAP], scalar1=b1fm[:, ft, 1, e_:e_ + 1], scalar2=7.0, op0=ALU.add, op1=ALU.min),
                 reads=[PSB[pl], B_b1, B_sg[s2]], writes=[B_sg[s2]])
            S.op("dve", lambda e, l_=l_: e.tensor_scalar(out=l_, in0=l_, scalar1=-7.0, scalar2=1.0, op0=ALU.max, op1=ALU.add), reads=[B_sg[s2]], writes=[B_sg[s2]])
            S.op("dve", lambda e, g_=g_, s_=s_: e.tensor_tensor(out=g_, in0=g_, in1=s_, op=ALU.mult), reads=[B_sg[s2]], writes=[B_sg[s2]])
            S.op("dve", lambda e, g_=g_, l_=l_, ft=ft, actT=actT: e.tensor_tensor(out=actT[:, ft, :], in0=g_, in1=l_, op=ALU.mult), reads=[B_sg[s2], B_actT], writes=[B_actT])
        for (jj, ii) in pend:
            step_cast(nxt, jj, ii)
        for st_ in range(NS):
            ys = iy % 2; iy += 1
            for hf in range(2):
                pb = 4 + hf
                S.op("pe", [lambda e, ft=ft, st_=st_, hf=hf, pb=pb, sl=sl, actT=actT: e.matmul(PS[pb][:, :], lhsT=actT[:, ft, st_ * 128:(st_ + 1) * 128], rhs=w2[sl][:, ft, 512 * hf:512 * hf + 512],
                                                                                   start=(ft == 0), stop=(ft == 7)) for ft in range(8)], reads=[B_actT, B_w2[sl]], writes=[PSB[pb]])
                S.op("dve", lambda e, ys=ys, hf=hf, pb=pb, sl=sl: e.tensor_tensor(out=ysb[ys][:, 512 * hf:512 * hf + 512], in0=PS[pb][:, :], in1=b2bc[sl][:, 512 * hf:512 * hf + 512], op=ALU.add),
                     reads=[PSB[pb], B_b2[sl], B_ysb[ys]], writes=[B_ysb[ys]])
            r0 = e_ * CAP + st_ * 128
            S.dma("pool", lambda e, ys=ys, r0=r0: e.dma_start(out=yg[r0:r0 + 128, :], in_=ysb[ys][:, :]), stream=f"ygst{ys}", reads=[B_ysb[ys]], writes=[B_yg])
    close_phase()

    out_d = dram_out("out", [TOK, D])
    open_phase()
    g3 = sb("ln3g", [128, D], F32); b3 = sb("ln3b", [128, D], F32)
    B_g3, B_b3 = Buf("ln3g"), Buf("ln3b")
    S.dma("sp", lambda e: e.dma_start(out=g3[:, :], in_=ln_d["ln3_g"].partition_broadcast(128)), writes=[B_g3])
    S.dma("sp", lambda e: e.dma_start(out=b3[:, :], in_=ln_d["ln3_b"].partition_broadcast(128)), writes=[B_b3])
    ygat = [[sb(f"ygat{i}_{k}", [128, D], F32) for k in range(4)] for i in range(2)]
    B_ygat = [[Buf(f"ygat{i}_{k}") for k in range(4)] for i in range(2)]
    x2r = [sb(f"x2r{i}", [128, D], F32) for i in range(2)]; B_x2r = [Buf(f"x2r{i}") for i in range(2)]
    acc = [sb(f"acc{i}", [128, D], F32) for i in range(2)]; B_acc = [Buf(f"acc{i}") for i in range(2)]
    oo = [sb(f"oo{i}", [128, D], F32) for i in range(2)]; B_oo = [Buf(f"oo{i}") for i in range(2)]
    st6b = sb("st6b", [128, 2, 6], F32); mvb = sb("mvb", [128, 2], F32); rstdb = sb("rstdb", [128, 1], F32)
    B_sm5 = Buf("sm5")
    for t in range(NT):
        sl = t % 2
        S.dma("sp", lambda e, t=t, sl=sl: e.dma_start(out=x2r[sl][:, :], in_=x2f[t * 128:(t + 1) * 128, :]), reads=[B_x2f[t]], writes=[B_x2r[sl]])
        for k in range(4):
            S.dma("pool", lambda e, t=t, k=k, sl=sl: e.indirect_dma_start(out=ygat[sl][k][:, :], out_offset=None, in_=yg[:, :],
                                                                          in_offset=bass.IndirectOffsetOnAxis(dest[:, t, k:k + 1], 0)), reads=[B_yg, B_route[t]], writes=[B_ygat[sl][k]])
        a_ = acc[sl][:, :]
        S.op("dve", lambda e, a_=a_, t=t, sl=sl: e.tensor_scalar(out=a_, in0=ygat[sl][0][:, :], scalar1=gates[:, t, 0:1], scalar2=None, op0=ALU.mult),
             reads=[B_ygat[sl][0], B_route[t], B_acc[sl]], writes=[B_acc[sl]])
        for k in range(1, 4):
            S.op("dve", lambda e, a_=a_, t=t, k=k, sl=sl: e.scalar_tensor_tensor(out=a_, in0=ygat[sl][k][:, :], scalar=gates[:, t, k:k + 1], in1=a_, op0=ALU.mult, op1=ALU.add),
                 reads=[B_ygat[sl][k], B_route[t], B_acc[sl]], writes=[B_acc[sl]])
        S.op("dve", lambda e, a_=a_, sl=sl: e.scalar_tensor_tensor(out=a_, in0=x2r[sl][:, :], scalar=ALPHA, in1=a_, op0=ALU.mult, op1=ALU.add),
             reads=[B_x2r[sl], B_acc[sl]], writes=[B_acc[sl]])
        S.op("dve", [lambda e, c=c, a_=a_: e.bn_stats(out=st6b[:, c, :], in_=a_[:, 512 * c:512 * c + 512]) for c in range(2)], reads=[B_acc[sl], B_sm5], writes=[B_sm5])
        S.op("dve", lambda e: e.bn_aggr(out=mvb[:, :], in_=st6b[:, :, :].rearrange("p a b -> p (a b)")), reads=[B_sm5], writes=[B_sm5])
        S.op("dve", lambda e: e.tensor_scalar(out=rstdb[:, :], in0=mvb[:, 1:2], scalar1=1e-5, scalar2=None, op0=ALU.add), reads=[B_sm5], writes=[B_sm5])
        S.op("act", lambda e: e.activation(out=rstdb[:, :], in_=rstdb[:, :], func=AF.Sqrt), reads=[B_sm5], writes=[B_sm5])
        S.op("dve", lambda e: e.reciprocal(out=rstdb[:, :], in_=rstdb[:, :]), reads=[B_sm5], writes=[B_sm5])
        S.op("dve", lambda e, a_=a_: e.tensor_scalar(out=a_, in0=a_, scalar1=mvb[:, 0:1], scalar2=rstdb[:, 0:1], op0=ALU.subtract, op1=ALU.mult), reads=[B_acc[sl], B_sm5], writes=[B_acc[sl]])
        S.op("dve", lambda e, a_=a_: e.tensor_tensor(out=a_, in0=a_, in1=g3[:, :], op=ALU.mult), reads=[B_acc[sl], B_g3], writes=[B_acc[sl]])
        S.op("dve", lambda e, a_=a_, sl=sl: e.tensor_tensor(out=oo[sl][:, :], in0=a_, in1=b3[:, :], op=ALU.add), reads=[B_acc[sl], B_b3, B_oo[sl]], writes=[B_oo[sl]])
        S.dma("sp", lambda e, t=t, sl=sl: e.dma_start(out=out_d[t * 128:(t + 1) * 128, :], in_=oo[sl][:, :]), stream=f"out{sl}", reads=[B_oo[sl]], writes=[Buf(f"o_{t}")])
    S.finish()
    close_phase()
    es.close()
    return nc, ["out"]


def nc_ap(nc, name):
    return _DRAM[name]


def _rope_tables(pos):
    half = 32
    inv = (10000.0 ** (-np.arange(half, dtype=np.float32) * np.float32(2.0 / 64))).astype(np.float32)
    ang = (pos.astype(np.float32)[:, None] * inv[None, :]).astype(np.float32)
    cos = np.cos(ang.astype(np.float64)).astype(np.float32).T
    sin = np.sin(ang.astype(np.float64)).astype(np.float32).T
    return np.ascontiguousarray(np.tile(cos, (4, 1))), np.ascontiguousarray(np.tile(sin, (4, 1)))


def prep_core_inputs(inputs, c, stage_needs_s5=True):
    b, j = c // 4, c % 4
    t0 = j * TOK
    x = inputs["x"]
    m = {}
    m["xloc"] = np.ascontiguousarray(x[b, t0:t0 + TOK])
    m["xhalo"] = np.ascontiguousarray(x[b, t0 - HALO:t0]) if j > 0 else np.zeros((HALO, D), np.float32)
    pos = np.arange(t0 - HALO, t0 + TOK)
    m["ropecos"], m["ropesin"] = _rope_tables(pos)
    kj = np.arange(128)[:, None]
    qi = np.arange(128)[None, :]
    prev = (kj > qi).astype(np.float32)
    own = (kj <= qi).astype(np.float32)
    first = prev if j > 0 else np.zeros_like(prev)
    m["masks"] = np.ascontiguousarray(np.stack([prev, own, first], axis=1))
    m["ident"] = np.eye(128, dtype=np.float32)
    m["w_in"] = np.ascontiguousarray(inputs["w_in"][0])
    m["b_in"] = np.ascontiguousarray(inputs["b_in"][0])
    m["attn_sinks"] = np.ascontiguousarray(inputs["attn_sinks"][0])
    if stage_needs_s5:
        xp = np.zeros((3 * TOK, D), np.float32)
        sv = np.zeros((128, 4), np.float32)
        for k in range(3):
            sj = j - 3 + k
            if sj >= 0:
                xp[k * TOK:(k + 1) * TOK] = x[b, sj * TOK:(sj + 1) * TOK]
                sv[:, k] = 1.0
        m["xprev"], m["segvalid"] = xp, sv
        for k in ("s5_lambda_re", "s5_lambda_im", "s5_log_dt", "s5_b_re", "s5_b_im", "s5_c_re", "s5_c_im", "s5_d", "s5_w_glu", "s5_b_glu"):
            m[k] = np.ascontiguousarray(inputs[k][0])
        sp = np.arange(128) // 16
        so = np.arange(256) // 16
        for k in ("w_out", "b_out", "ln1_g", "ln1_b", "ln2_g", "ln2_b", "ln3_g", "ln3_b", "w_xq", "w_xkv", "w_xo", "w_router", "b_router", "w_e1", "b_e1", "w_e2", "b_e2"):
            m[k] = np.ascontiguousarray(inputs[k][0])
        m["mem"] = np.ascontiguousarray(inputs["mem"][b])
        m["tri"] = np.triu(np.ones((128, 128), np.float32), 1)
        m["ecap"] = np.ascontiguousarray(np.tile((np.arange(32, dtype=np.float32) * 384.0)[None, :], (128, 1)))
        m["cmask"] = np.ascontiguousarray(np.stack([(so[None, :] >= (8 * kh + sp)[:, None]) for kh in range(2)], axis=1).astype(np.float32))
    return m


def kernel(**inputs):
    inputs = {k: np.asarray(v) for k, v in inputs.items()}
    nc, _ = build_nc("full")
    in_maps = [prep_core_inputs(inputs, c) for c in range(NCORES)]
    res = run_bass_kernel_spmd(nc, in_maps, core_ids=list(range(NCORES)))
    out = np.zeros((2, 8192, D), np.float32)
    for c in range(NCORES):
        out[c // 4, (c % 4) * TOK:(c % 4 + 1) * TOK] = res.results[c]["out"]
    return out
```
